# Optimizing a Trainium2 kernel written in Bass

```python
import jax
import jax.numpy as jnp
from jax import lax
import numpy as np

D_MODEL = 1024
BATCH = 8
SEQ = 4096
DEPTH = 2

EPS = 1e-6
MEM_LEN = 256
GDN_HEADS = 6
GDN_DK = 128
GDN_DV = 128
GDN_WIDTH = GDN_HEADS * GDN_DV
CONV_K = 4
CHUNK = 64
SB_HEADS = 12
SB_DH = 64
SB_WIDTH = SB_HEADS * SB_DH
SB_BLOCK = 128
MEM_HEADS = 4
MEM_DH = 64
MEM_WIDTH = MEM_HEADS * MEM_DH
MIX_WIDTH = GDN_WIDTH + MEM_WIDTH
A_IN = 4 * GDN_WIDTH + 2 * GDN_HEADS + MEM_WIDTH
B_IN = SB_WIDTH + MEM_WIDTH
N_GROUPS = 4
EXPERTS_PER_GROUP = 4
N_EXPERTS = N_GROUPS * EXPERTS_PER_GROUP
TOP_K = 2
EXPERT_FF = 256

kernel_name = 'yoco_gdn_stickbreak_hmoe'


def rmsnorm(x, g):
    xf = x.astype(jnp.float32)
    y = xf * lax.rsqrt(jnp.mean(xf * xf, axis=-1, keepdims=True) + EPS)
    return (y * g.astype(jnp.float32)).astype(x.dtype)


def l2norm(x):
    return x * lax.rsqrt(jnp.sum(x * x, axis=-1, keepdims=True) + EPS)


def causal_dwconv(x, w):
    c = x.shape[-1]
    return lax.conv_general_dilated(
        x, w[:, None, :].astype(x.dtype), window_strides=(1,),
        padding=((CONV_K - 1, 0),), dimension_numbers=('NWC', 'WIO', 'NWC'),
        feature_group_count=c)


def gated_delta_rule(q, k, v, beta, g):
    bsz, s, h, _ = q.shape
    n = s // CHUNK

    def chunks(t):
        return t.reshape(bsz, n, CHUNK, h, -1).transpose(0, 3, 1, 2, 4)

    q, k, v = chunks(q), chunks(k), chunks(v)
    beta = beta.reshape(bsz, n, CHUNK, h).transpose(0, 3, 1, 2)
    gc = jnp.cumsum(g.reshape(bsz, n, CHUNK, h).transpose(0, 3, 1, 2), axis=-1)
    idx = jnp.arange(CHUNK)
    incl = idx[:, None] >= idx[None, :]
    strict = idx[:, None] > idx[None, :]
    decay = jnp.exp(jnp.where(incl, gc[..., :, None] - gc[..., None, :], -jnp.inf))
    kb = k * beta[..., None]
    a_mat = jnp.where(strict, jnp.einsum('bhnid,bhnjd->bhnij', kb, k) * decay, 0.0)
    eye = jnp.eye(CHUNK, dtype=q.dtype)
    rhs = jnp.concatenate([v * beta[..., None], kb * jnp.exp(gc)[..., None]], axis=-1)
    sol = lax.linalg.triangular_solve(eye + a_mat, rhs, left_side=True, lower=True)
    u, w = sol[..., :GDN_DV], sol[..., GDN_DV:]
    qg = q * jnp.exp(gc)[..., None]
    attn_intra = jnp.where(incl, jnp.einsum('bhnid,bhnjd->bhnij', q, k) * decay, 0.0)
    g_last = gc[..., -1]
    k_tail = k * jnp.exp(g_last[..., None] - gc)[..., None]

    def step(state, xs):
        qg_c, w_c, u_c, att_c, kt_c, gl_c = xs
        v_new = u_c - jnp.einsum('bhck,bhkv->bhcv', w_c, state)
        o = jnp.einsum('bhck,bhkv->bhcv', qg_c, state) + jnp.einsum('bhij,bhjv->bhiv', att_c, v_new)
        state = state * jnp.exp(gl_c)[..., None, None] + jnp.einsum('bhck,bhcv->bhkv', kt_c, v_new)
        return state, o

    xs = tuple(jnp.moveaxis(t, 2, 0) for t in (qg, w, u, attn_intra, k_tail, g_last))
    s0 = jnp.zeros((bsz, h, GDN_DK, GDN_DV), q.dtype)
    _, o = lax.scan(step, s0, xs)
    return o.transpose(1, 0, 3, 2, 4).reshape(bsz, s, h, GDN_DV)


def memory_attend(mq, mem_k, mem_v):
    bsz, s, _ = mq.shape
    q = mq.reshape(bsz, s, MEM_HEADS, MEM_DH)
    sc = jnp.einsum('bshd,bmhd->bhsm', q, mem_k).astype(jnp.float32) * (MEM_DH ** -0.5)
    p = jax.nn.softmax(sc, axis=-1).astype(mem_v.dtype)
    return jnp.einsum('bhsm,bmhd->bshd', p, mem_v).reshape(bsz, s, MEM_WIDTH)


def stick_breaking(q, k, v):
    s_len = q.shape[2]
    outs = []
    for i in range(s_len // SB_BLOCK):
        hi = (i + 1) * SB_BLOCK
        qb = q[:, :, i * SB_BLOCK:hi]
        kb = k[:, :, :hi]
        vb = v[:, :, :hi]
        z = jnp.einsum('bhtd,bhsd->bhts', qb, kb).astype(jnp.float32) * (SB_DH ** -0.5)
        t_pos = i * SB_BLOCK + jnp.arange(SB_BLOCK)
        causal = jnp.arange(hi)[None, :] < t_pos[:, None]
        log_1mb = jnp.where(causal, jax.nn.log_sigmoid(-z), 0.0)
        after = lax.cumsum(log_1mb, axis=3, reverse=True) - log_1mb
        a = jnp.where(causal, jnp.exp(jax.nn.log_sigmoid(z) + after), 0.0)
        outs.append(jnp.einsum('bhts,bhsd->bhtd', a.astype(vb.dtype), vb))
    return jnp.concatenate(outs, axis=2)


def mixer_a(h, norm_g, w_in, w_conv, a_log, dt_bias, o_gain, w_out, mem_k, mem_v):
    bsz, s, _ = h.shape
    f32 = jnp.float32
    p = rmsnorm(h, norm_g) @ w_in
    qkv = jax.nn.silu(causal_dwconv(p[..., :3 * GDN_WIDTH], w_conv)).astype(f32)
    gate = p[..., 3 * GDN_WIDTH:4 * GDN_WIDTH].astype(f32).reshape(bsz, s, GDN_HEADS, GDN_DV)
    b_raw = p[..., 4 * GDN_WIDTH:4 * GDN_WIDTH + GDN_HEADS].astype(f32)
    a_raw = p[..., 4 * GDN_WIDTH + GDN_HEADS:4 * GDN_WIDTH + 2 * GDN_HEADS].astype(f32)
    mq = p[..., 4 * GDN_WIDTH + 2 * GDN_HEADS:]
    q = l2norm(qkv[..., :GDN_WIDTH].reshape(bsz, s, GDN_HEADS, GDN_DK)) * (GDN_DK ** -0.5)
    k = l2norm(qkv[..., GDN_WIDTH:2 * GDN_WIDTH].reshape(bsz, s, GDN_HEADS, GDN_DK))
    v = qkv[..., 2 * GDN_WIDTH:].reshape(bsz, s, GDN_HEADS, GDN_DV)
    beta = jax.nn.sigmoid(b_raw)
    g = -jnp.exp(a_log.astype(f32)) * jax.nn.softplus(a_raw + dt_bias.astype(f32))
    o = gated_delta_rule(q, k, v, beta, g)
    o = o * lax.rsqrt(jnp.mean(o * o, axis=-1, keepdims=True) + EPS) * o_gain.astype(f32) * jax.nn.silu(gate)
    o = o.reshape(bsz, s, GDN_WIDTH).astype(h.dtype)
    m = memory_attend(mq, mem_k, mem_v)
    return jnp.concatenate([o, m], axis=-1) @ w_out


def mixer_b(h, norm_g, w_in, w_out, k_sh, v_sh, mem_k, mem_v):
    bsz, s, _ = h.shape
    p = rmsnorm(h, norm_g) @ w_in
    q = p[..., :SB_WIDTH].reshape(bsz, s, SB_HEADS, SB_DH).transpose(0, 2, 1, 3)
    o = stick_breaking(q, k_sh, v_sh).transpose(0, 2, 1, 3).reshape(bsz, s, SB_WIDTH)
    m = memory_attend(p[..., SB_WIDTH:], mem_k, mem_v)
    return jnp.concatenate([o.astype(h.dtype), m], axis=-1) @ w_out


def hier_moe(xn, w_group, b_group, w_router, b_router, w1, w3, w2):
    bsz, s, d = xn.shape
    t = xn.reshape(-1, d)
    n_tok = t.shape[0]
    gl = (t @ w_group).astype(jnp.float32) + b_group.astype(jnp.float32)
    gsel = jnp.argmax(gl, axis=-1)
    p_group = jnp.max(jax.nn.softmax(gl, axis=-1), axis=-1, keepdims=True)
    el = ((t @ w_router).astype(jnp.float32) + b_router.astype(jnp.float32)).reshape(n_tok, N_GROUPS, EXPERTS_PER_GROUP)
    el = el[jnp.arange(n_tok), gsel]
    top_p, top_i = lax.top_k(jax.nn.softmax(el, axis=-1), TOP_K)
    top_p = top_p / jnp.sum(top_p, axis=-1, keepdims=True)
    eid = gsel[:, None] * EXPERTS_PER_GROUP + top_i
    combine = jnp.sum(jax.nn.one_hot(eid, N_EXPERTS, dtype=jnp.float32) * (p_group * top_p)[..., None], axis=1)
    combine = combine.astype(t.dtype)
    y = jnp.zeros_like(t)
    for e in range(N_EXPERTS):
        hid = jax.nn.silu(t @ w1[e]) * (t @ w3[e])
        y = y + combine[:, e:e + 1] * (hid @ w2[e])
    return y.reshape(bsz, s, d)


def setup_inputs(seed: int = 0) -> dict:
    key = jax.random.key(seed)
    ks = iter(jax.random.split(key, 32))
    n_a = DEPTH // 2
    n_b = DEPTH - n_a

    def nrm(shape, scale):
        return jax.random.normal(next(ks), shape, jnp.float32) * scale

    def gain(shape):
        return 1.0 + nrm(shape, 0.02)

    x = nrm((BATCH, SEQ, D_MODEL), 1.0)
    mem = nrm((BATCH, MEM_LEN, D_MODEL), 1.0)
    a_norm = gain((n_a, D_MODEL))
    a_w_in = nrm((n_a, D_MODEL, A_IN), D_MODEL ** -0.5)
    a_conv = nrm((n_a, CONV_K, 3 * GDN_WIDTH), CONV_K ** -0.5)
    a_log = jnp.log(jax.random.uniform(next(ks), (n_a, GDN_HEADS), jnp.float32, 1.0, 16.0))
    dt = jnp.exp(jax.random.uniform(next(ks), (n_a, GDN_HEADS), jnp.float32, np.log(1e-3), np.log(1e-1)))
    a_dt_bias = dt + jnp.log(-jnp.expm1(-dt))
    a_out_gain = gain((n_a, GDN_DV))
    a_w_out = nrm((n_a, MIX_WIDTH, D_MODEL), MIX_WIDTH ** -0.5)
    kv_norm = gain((D_MODEL,))
    w_kv = nrm((D_MODEL, 2 * SB_WIDTH), D_MODEL ** -0.5)
    b_norm = gain((n_b, D_MODEL))
    b_w_in = nrm((n_b, D_MODEL, B_IN), D_MODEL ** -0.5)
    b_w_out = nrm((n_b, MIX_WIDTH, D_MODEL), MIX_WIDTH ** -0.5)
    mem_norm = gain((DEPTH, D_MODEL))
    w_mem_kv = nrm((DEPTH, D_MODEL, 2 * MEM_WIDTH), D_MODEL ** -0.5)
    ffn_norm = gain((DEPTH, D_MODEL))
    w_group = nrm((DEPTH, D_MODEL, N_GROUPS), D_MODEL ** -0.5)
    b_group = nrm((DEPTH, N_GROUPS), 0.01)
    w_router = nrm((DEPTH, D_MODEL, N_EXPERTS), D_MODEL ** -0.5)
    b_router = nrm((DEPTH, N_EXPERTS), 0.01)
    w1 = nrm((DEPTH, N_EXPERTS, D_MODEL, EXPERT_FF), D_MODEL ** -0.5)
    w3 = nrm((DEPTH, N_EXPERTS, D_MODEL, EXPERT_FF), D_MODEL ** -0.5)
    w2 = nrm((DEPTH, N_EXPERTS, EXPERT_FF, D_MODEL), EXPERT_FF ** -0.5)
    final_norm = gain((D_MODEL,))
    return {'x': x, 'mem': mem, 'a_norm': a_norm, 'a_w_in': a_w_in, 'a_conv': a_conv,
            'a_log': a_log, 'a_dt_bias': a_dt_bias, 'a_out_gain': a_out_gain, 'a_w_out': a_w_out,
            'kv_norm': kv_norm, 'w_kv': w_kv, 'b_norm': b_norm, 'b_w_in': b_w_in, 'b_w_out': b_w_out,
            'mem_norm': mem_norm, 'w_mem_kv': w_mem_kv, 'ffn_norm': ffn_norm,
            'w_group': w_group, 'b_group': b_group, 'w_router': w_router, 'b_router': b_router,
            'w1': w1, 'w3': w3, 'w2': w2, 'final_norm': final_norm}


def reference(x, mem, a_norm, a_w_in, a_conv, a_log, a_dt_bias, a_out_gain, a_w_out,
              kv_norm, w_kv, b_norm, b_w_in, b_w_out, mem_norm, w_mem_kv, ffn_norm,
              w_group, b_group, w_router, b_router, w1, w3, w2, final_norm):
    bsz, s, _ = x.shape
    m_len = mem.shape[1]
    n_a = DEPTH // 2
    h = x
    k_sh = None
    v_sh = None
    for l in range(DEPTH):
        mkv = rmsnorm(mem, mem_norm[l]) @ w_mem_kv[l]
        mem_k = mkv[..., :MEM_WIDTH].reshape(bsz, m_len, MEM_HEADS, MEM_DH)
        mem_v = mkv[..., MEM_WIDTH:].reshape(bsz, m_len, MEM_HEADS, MEM_DH)
        if l < n_a:
            h = h + mixer_a(h, a_norm[l], a_w_in[l], a_conv[l], a_log[l], a_dt_bias[l],
                            a_out_gain[l], a_w_out[l], mem_k, mem_v)
        else:
            if l == n_a:
                kv = rmsnorm(h, kv_norm) @ w_kv
                k_sh = kv[..., :SB_WIDTH].reshape(bsz, s, SB_HEADS, SB_DH).transpose(0, 2, 1, 3)
                v_sh = kv[..., SB_WIDTH:].reshape(bsz, s, SB_HEADS, SB_DH).transpose(0, 2, 1, 3)
            lb = l - n_a
            h = h + mixer_b(h, b_norm[lb], b_w_in[lb], b_w_out[lb], k_sh, v_sh, mem_k, mem_v)
        h = h + hier_moe(rmsnorm(h, ffn_norm[l]), w_group[l], b_group[l], w_router[l], b_router[l],
                         w1[l], w3[l], w2[l])
    return rmsnorm(h, final_norm)
```

```python
from contextlib import ExitStack
import os
import numpy as np
import concourse.bass as bass
import concourse.mybir as mybir
from concourse.bass_utils import run_bass_kernel_spmd

F32 = mybir.dt.float32
BF16 = mybir.dt.bfloat16
AF = mybir.ActivationFunctionType
ALU = mybir.AluOpType
AX = mybir.AxisListType

S = 4096
D = 1024
NT = S // 128
EPS = 1e-6
NCORES = 8


class Buf:
    __slots__ = ("ap", "name", "_lw", "_rd", "_excl")
    lw = property(lambda self: self._lw, lambda self, v: setattr(self, "_lw", v))
    rd = property(lambda self: self._rd, lambda self, v: setattr(self, "_rd", v))
    excl = property(lambda self: self._excl, lambda self, v: setattr(self, "_excl", v))

    def __init__(self, ap, name="", excl=False):
        self.ap = ap
        self.name = name
        self.excl = excl
        self.lw = None
        self.rd = []

    def __getitem__(self, idx):
        return self.ap[idx]


class View(Buf):
    __slots__ = ("parent",)

    def __init__(self, parent, ap):
        self.parent = parent
        self.ap = ap
        self.name = parent.name

    lw = property(lambda self: self.parent.lw, lambda self, v: setattr(self.parent, "lw", v))
    rd = property(lambda self: self.parent.rd, lambda self, v: setattr(self.parent, "rd", v))
    excl = property(lambda self: self.parent.excl, lambda self, v: None)


class K:
    NDMA = 48

    def __init__(self, nc):
        self.nc = nc
        self.eng = {"pe": nc.tensor, "act": nc.scalar, "dve": nc.vector,
                    "pool": nc.gpsimd, "sp": nc.sync}
        self.sem = {e: nc.alloc_semaphore("s_" + e) for e in ("pe", "act", "dve", "pool")}
        self.cnt = {e: 0 for e in self.sem}
        self.waited = {}
        self.dsem = [nc.alloc_semaphore("d%d" % i) for i in range(self.NDMA)]
        self.dcnt = [0] * self.NDMA
        self.dnext = 0
        self.nins = 0

    def _semh(self, key):
        return self.sem[key] if isinstance(key, str) else self.dsem[key]

    def _wait(self, e, dep):
        key, val = dep
        if key == e and e == "pe":
            return
        w = self.waited.get((e, key), 0)
        if w >= val:
            return
        self.eng[e].wait_ge(self._semh(key), val)
        self.nins += 1
        self.waited[(e, key)] = val

    def _deps(self, e, reads, writes):
        best = {}
        for r in reads:
            if r.lw is not None:
                if best.get(r.lw[0], 0) < r.lw[1]:
                    best[r.lw[0]] = r.lw[1]
            if r.excl:
                for key, val in r.rd:
                    if key != e and best.get(key, 0) < val:
                        best[key] = val
        for w in writes:
            if w.lw is not None:
                if best.get(w.lw[0], 0) < w.lw[1]:
                    best[w.lw[0]] = w.lw[1]
            for key, val in w.rd:
                if best.get(key, 0) < val:
                    best[key] = val
        for key, val in best.items():
            self._wait(e, (key, val))

    def _mark(self, tag, reads, writes):
        for r in reads:
            r.rd.append(tag)
            if len(r.rd) > 64:
                best = {}
                for key, val in r.rd:
                    if best.get(key, 0) < val:
                        best[key] = val
                r.rd = list(best.items())
        for w in writes:
            w.lw = tag
            w.rd = []

    def op(self, e, fn, reads=(), writes=(), inc=True):
        self._deps(e, reads, writes)
        ins = fn(self.eng[e])
        self.nins += 1
        if inc:
            ins.then_inc(self.sem[e], 1)
            self.cnt[e] += 1
            tag = (e, self.cnt[e])
        else:
            tag = (e, self.cnt[e] + 1)
        self._mark(tag, reads, writes)
        return ins

    def dma(self, out, in_, reads=(), writes=(), q="sp", **kw):
        slot = self.dnext
        self.dnext = (self.dnext + 1) % self.NDMA
        if self.dcnt[slot] > 0:
            self._wait(q, (slot, 16 * self.dcnt[slot]))
        self._deps(q, reads, writes)
        ins = self.eng[q].dma_start(out=out, in_=in_, **kw)
        self.nins += 1
        ins.then_inc(self.dsem[slot], 16)
        self.dcnt[slot] += 1
        tag = (slot, 16 * self.dcnt[slot])
        self._mark(tag, reads, writes)
        return tag

    def barrier(self):
        for e in ("pe", "act", "dve", "pool", "sp"):
            for e2 in ("pe", "act", "dve", "pool"):
                if e2 != e and self.cnt[e2] > 0:
                    self._wait(e, (e2, self.cnt[e2]))
            for slot in range(self.NDMA):
                if self.dcnt[slot] > 0:
                    self._wait(e, (slot, 16 * self.dcnt[slot]))


class MK:
    def __init__(self, phases, h0_from_input=True):
        self.nc = nc = bass.Bass("TRN2", target_bir_lowering=False)
        self.k = K(nc)
        self.uid = 0
        self.ins = {}
        self.ps = [Buf(nc.alloc_psum_tensor("psb%d" % i, [128, 512], F32).ap(), "ps%d" % i, excl=True)
                   for i in range(8)]

    def din(self, name, shape):
        ap = self.nc.dram_tensor(name, list(shape), F32, kind="ExternalInput").ap()
        self.ins[name] = ap
        return ap

    def dscratch(self, name, shape, dt=F32):
        return self.nc.dram_tensor(name, list(shape), dt, kind="Internal").ap()

    def sbuf(self, st, name, shape, dt):
        self.uid += 1
        h = st.enter_context(self.nc.sbuf_tensor("%s_%d" % (name, self.uid), list(shape), dt))
        return Buf(h.ap(), name)

    def load_consts(self, st):
        k = self.k
        c = self.din("consts", [128, 5, 128])
        self.cst = self.sbuf(st, "cst", [128, 5, 128], F32)
        k.dma(self.cst[:], c, writes=[self.cst])
        self.ident = self.cst[:, 0, :]
        self.U = self.cst[:, 1, :]
        self.SL = self.cst[:, 2, :]
        self.strictT = self.cst[:, 3, :]
        self.ones = self.cst[:, 4, :]
        self.cstb = self.sbuf(st, "cstb", [128, 5, 128], BF16)
        k.dma(self.cstb[:], c, writes=[self.cstb], q="pool")
        self.identb = self.cstb[:, 0, :]
        self.onesb = self.cstb[:, 4, :]

    def rmsnorm(self, h, gainb, xn, junk, st2):
        k = self.k
        k.op("act", lambda e: e.activation(junk[:], h[:], AF.Square, accum_out=st2[:, 0:1]),
             reads=[h], writes=[junk, st2])
        k.op("act", lambda e: e.activation(st2[:, 1:2], st2[:, 0:1], AF.Sqrt, bias=self.epsb[:, 0:1], scale=1.0 / D),
             reads=[st2, self.epsbuf], writes=[st2])
        k.op("dve", lambda e: e.reciprocal(st2[:, 2:3], st2[:, 1:2]), reads=[st2], writes=[st2])
        k.op("dve", lambda e: e.scalar_tensor_tensor(xn[:], h[:], st2[:, 2:3], gainb[:], ALU.mult, ALU.mult),
             reads=[h, st2, gainb], writes=[xn])

    def transpose8(self, src, dsts, psa, psb, evac=("act", "dve"), second="pool"):
        k = self.k
        for half, ps in enumerate((psa, psb)):
            for j in range(4):
                c = half * 4 + j
                k.op("pe", lambda e: e.transpose(ps[:, j * 128:(j + 1) * 128], src[:, c * 128:(c + 1) * 128], self.ident),
                     reads=[src, self.cst], writes=[ps], inc=(j == 3))
            dbuf, fn = dsts[0]
            eng = evac[half % len(evac)]
            pv = ps[:].rearrange("p (c t) -> p c t", c=4)
            if eng == "act":
                k.op("act", lambda e: e.copy(fn(half), pv), reads=[ps], writes=[dbuf])
            else:
                k.op(eng, lambda e: e.tensor_copy(fn(half), pv), reads=[ps], writes=[dbuf])
            for dbuf2, fn2 in dsts[1:]:
                k.op(second, lambda e: e.tensor_copy(fn2(half), fn(half)), reads=[dbuf], writes=[dbuf2])

    def moe_phase(self, l, hin, hout, final=False, out_ap=None):
        nc, k = self.nc, self.k
        G = 2048
        NTG = G // 128
        P = self.P
        with ExitStack() as st:
            sb = lambda n, s, d: self.sbuf(st, n, s, d)
            gain = sb("gain", [128, D], F32)
            k.dma(gain[:], P["ffn_norm"][l].partition_broadcast(128), writes=[gain])
            if final:
                fgain = sb("fgain", [128, D], F32)
                k.dma(fgain[:], P["final_norm"].partition_broadcast(128), writes=[fgain])
            wgr = sb("wgr", [128, 8, 20], F32)
            k.dma(wgr[:], P["wgr"][l], writes=[wgr])
            rb = sb("rbias", [128, 20], F32)
            k.dma(rb[:], P["rbias"][l].partition_broadcast(128), writes=[rb])
            xnT = sb("xnT", [128, 8, G], BF16)
            yacc = [sb("yacc%d" % i, [128, D], F32) for i in range(NTG)]
            comb = [sb("comb%d" % i, [128, 16], F32) for i in range(NTG)]
            w1b = [sb("w1b%d" % i, [128, 8, 256], BF16) for i in range(2)]
            w3b = [sb("w3b%d" % i, [128, 8, 256], BF16) for i in range(2)]
            w2b = [sb("w2b%d" % i, [128, 2, D], BF16) for i in range(2)]
            ht = [sb("ht%d" % i, [128, D], F32) for i in range(2)]
            xn = [sb("xn%d" % i, [128, D], F32) for i in range(2)]
            junk = sb("junk", [128, D], F32)
            xnT32 = [sb("xnT32_%d" % i, [128, 8, 128], F32) for i in range(2)]
            stt = [sb("stt%d" % i, [128, 4], F32) for i in range(2)]
            rt = [sb("rt%d" % i, [128, 96], F32) for i in range(2)]
            hid = [sb("hid%d" % i, [128, 2, 512], BF16) for i in range(2)]
            sil = [sb("sil%d" % i, [128, 512], F32) for i in range(2)]
            ho = [sb("ho%d" % i, [128, D], F32) for i in range(2)]
            ps = self.ps
            w1d, w3d, w2d = P["w1"], P["w3"], P["w2"]

            def load_w(e, slot):
                k.dma(w1b[slot][:], w1d[l, e].rearrange("(c p) f -> p c f", p=128), writes=[w1b[slot]], q="pool")
                k.dma(w3b[slot][:], w3d[l, e].rearrange("(c p) f -> p c f", p=128), writes=[w3b[slot]], q="pool")
                k.dma(w2b[slot][:], w2d[l, e].rearrange("(c p) n -> p c n", p=128), writes=[w2b[slot]], q="pool")

            for g in range(S // G):
                for ti in range(NTG):
                    t = g * NTG + ti
                    b = ti % 2
                    h = ht[b]
                    k.dma(h[:], hin[0][t * 128:(t + 1) * 128, :], reads=[hin[1][t]], writes=[h])
                    DBG = int(os.environ.get("MK_DBG", "9"))
                    if DBG < 2:
                        continue
                    self.rmsnorm(h, gain, xn[b], junk, stt[b])
                    if DBG < 3:
                        continue
                    x32 = xnT32[b]
                    self.transpose8(
                        xn[b],
                        [(x32, lambda half: x32[:, half * 4:(half + 1) * 4, :]),
                         (xnT, lambda half: xnT[:, half * 4:(half + 1) * 4, ti * 128:(ti + 1) * 128])],
                        ps[6], ps[7])
                    if DBG < 4:
                        continue
                    pr = ps[6 + (ti % 2)]
                    for dc in range(8):
                        k.op("pe", lambda e: e.matmul(pr[:, 0:20], x32[:, dc, :], wgr[:, dc, :], start=(dc == 0), stop=(dc == 7)),
                             reads=[x32, wgr], writes=[pr], inc=(dc == 7))
                    if DBG < 5:
                        continue
                    r = rt[b]
                    R = lambda a, n: r[:, a:a + n]
                    lg, gmax, ngmax, oh, ge, gsum, pg = R(0, 20), R(20, 1), R(21, 1), R(22, 4), R(26, 4), R(30, 1), R(31, 1)
                    tmp, elsel, m1, nm1, ee, mask1, ee2 = R(32, 16), R(48, 4), R(52, 1), R(53, 1), R(54, 4), R(58, 4), R(62, 4)
                    v2, mask2, den, rden, wl, scl = R(66, 1), R(67, 4), R(71, 1), R(72, 1), R(73, 4), R(77, 1)
                    dv = lambda fn, rd=(), wr=(): k.op("dve", fn, reads=[r] + list(rd), writes=[r] + list(wr))
                    dv(lambda e: e.tensor_tensor(lg, pr[:, 0:20], rb[:], ALU.add), rd=[pr, rb])
                    dv(lambda e: e.tensor_reduce(gmax, lg[:, 0:4], AX.X, ALU.max))
                    dv(lambda e: e.tensor_single_scalar(ngmax, gmax, -1.0, ALU.mult))
                    dv(lambda e: e.tensor_scalar(oh, lg[:, 0:4], gmax, None, ALU.is_equal))
                    k.op("act", lambda e: e.activation(ge, lg[:, 0:4], AF.Exp, bias=ngmax, accum_out=gsum), reads=[r], writes=[r])
                    dv(lambda e: e.reciprocal(pg, gsum))
                    dv(lambda e: e.tensor_tensor(tmp.rearrange("p (g j) -> p g j", g=4),
                                                 lg[:, 4:20].rearrange("p (g j) -> p g j", g=4),
                                                 oh.unsqueeze(2).to_broadcast([128, 4, 4]), ALU.mult))
                    dv(lambda e: e.tensor_reduce(elsel, tmp.rearrange("p (g j) -> p j g", g=4), AX.X, ALU.add))
                    dv(lambda e: e.tensor_reduce(m1, elsel, AX.X, ALU.max))
                    dv(lambda e: e.tensor_single_scalar(nm1, m1, -1.0, ALU.mult))
                    k.op("act", lambda e: e.activation(ee, elsel, AF.Exp, bias=nm1), reads=[r], writes=[r])
                    dv(lambda e: e.tensor_scalar(mask1, elsel, m1, None, ALU.is_equal))
                    dv(lambda e: e.scalar_tensor_tensor(ee2, mask1, -2.0, ee, ALU.mult, ALU.add))
                    dv(lambda e: e.tensor_reduce(v2, ee2, AX.X, ALU.max))
                    dv(lambda e: e.tensor_scalar(mask2, ee2, v2, None, ALU.is_equal))
                    dv(lambda e: e.tensor_single_scalar(den, v2, 1.0, ALU.add))
                    dv(lambda e: e.reciprocal(rden, den))
                    dv(lambda e: e.scalar_tensor_tensor(wl, mask2, v2, mask1, ALU.mult, ALU.add))
                    dv(lambda e: e.tensor_tensor(scl, pg, rden, ALU.mult))
                    dv(lambda e: e.tensor_scalar(wl, wl, scl, None, ALU.mult))
                    cb = comb[ti]
                    dv(lambda e: e.tensor_tensor(cb[:].rearrange("p (g j) -> p g j", g=4),
                                                 oh.unsqueeze(2).to_broadcast([128, 4, 4]),
                                                 wl.unsqueeze(1).to_broadcast([128, 4, 4]), ALU.mult), wr=[cb])
                NEX = int(os.environ.get("MK_NEX", "16"))
                if NEX:
                    load_w(0, 0)
                for ex in range(NEX):
                    slot = ex % 2
                    if ex + 1 < NEX:
                        load_w(ex + 1, 1 - slot)
                    w1, w3, w2 = w1b[slot], w3b[slot], w2b[slot]
                    for tb in range(G // 512):
                        hd = hid[tb % 2]
                        for fc in range(2):
                            p1 = ps[fc]
                            p3 = ps[2 + fc]
                            for dc in range(8):
                                k.op("pe", lambda e: e.matmul(p1[:], w1[:, dc, fc * 128:(fc + 1) * 128], xnT[:, dc, tb * 512:(tb + 1) * 512],
                                                              start=(dc == 0), stop=(dc == 7)),
                                     reads=[w1, xnT], writes=[p1], inc=(dc == 7))
                            for dc in range(8):
                                k.op("pe", lambda e: e.matmul(p3[:], w3[:, dc, fc * 128:(fc + 1) * 128], xnT[:, dc, tb * 512:(tb + 1) * 512],
                                                              start=(dc == 0), stop=(dc == 7)),
                                     reads=[w3, xnT], writes=[p3], inc=(dc == 7))
                            sl = sil[fc]
                            k.op("act", lambda e: e.activation(sl[:], p1[:], AF.Silu), reads=[p1], writes=[sl])
                            k.op("dve", lambda e: e.tensor_tensor(hd[:, fc, :], sl[:], p3[:], ALU.mult), reads=[sl, p3], writes=[hd])
                        for tt in range(4):
                            ti = tb * 4 + tt
                            for half in range(2):
                                py = ps[4 + half]
                                for fc in range(2):
                                    k.op("pe", lambda e: e.matmul(py[:], hd[:, fc, tt * 128:(tt + 1) * 128], w2[:, fc, half * 512:(half + 1) * 512],
                                                                  start=(fc == 0), stop=(fc == 1)),
                                         reads=[hd, w2], writes=[py], inc=(fc == 1))
                                ya = yacc[ti]
                                cs = comb[ti][:, ex:ex + 1]
                                if ex == 0:
                                    k.op("dve", lambda e: e.tensor_scalar(ya[:, half * 512:(half + 1) * 512], py[:], cs, None, ALU.mult),
                                         reads=[py, comb[ti]], writes=[ya])
                                else:
                                    k.op("dve", lambda e: e.scalar_tensor_tensor(ya[:, half * 512:(half + 1) * 512], py[:], cs,
                                                                                 ya[:, half * 512:(half + 1) * 512], ALU.mult, ALU.add),
                                         reads=[py, comb[ti], ya], writes=[ya])
                for ti in range(NTG):
                    t = g * NTG + ti
                    b = ti % 2
                    h = ht[b]
                    k.dma(h[:], hin[0][t * 128:(t + 1) * 128, :], reads=[hin[1][t]], writes=[h])
                    o = ho[b]
                    k.op("dve", lambda e: e.tensor_tensor(o[:], h[:], yacc[ti][:], ALU.add), reads=[h, yacc[ti]], writes=[o])
                    if final:
                        o2 = xn[b]
                        self.rmsnorm(o, fgain, o2, junk, stt[b])
                        k.dma(hout[0][t * 128:(t + 1) * 128, :], o2[:], reads=[o2], writes=[hout[1][t]])
                    else:
                        k.dma(hout[0][t * 128:(t + 1) * 128, :], o[:], reads=[o], writes=[hout[1][t]])
            k.barrier()

    def nextbank(self):
        self.pbi = (getattr(self, "pbi", -1) + 1) % 8
        return self.ps[self.pbi]

    def mem_kv(self, st, l):
        k, P = self.k, self.P
        sb = lambda n, s, d: self.sbuf(st, n, s, d)
        memkT = sb("memkT", [128, 2, 256], BF16)
        memv = sb("memv", [128, 2, 256], BF16)
        with ExitStack() as st2:
            sb2 = lambda n, s, d: self.sbuf(st2, n, s, d)
            g = sb2("mg", [128, D], F32)
            k.dma(g[:], P["mem_norm"][l].partition_broadcast(128), writes=[g])
            w = sb2("wmkv", [128, 8, 512], BF16)
            k.dma(w[:], P["w_mem_kv"][l].rearrange("(c p) n -> p c n", p=128), writes=[w], q="pool")
            mT = sb2("memnT", [128, 8, 256], BF16)
            junk = sb2("mjunk", [128, D], F32)
            for mt in range(2):
                h = sb2("mh%d" % mt, [128, D], F32)
                xn = sb2("mxn%d" % mt, [128, D], F32)
                stt = sb2("mst%d" % mt, [128, 4], F32)
                k.dma(h[:], P["mem"][mt * 128:(mt + 1) * 128, :], writes=[h])
                self.rmsnorm(h, g, xn, junk, stt)
                self.transpose8(xn, [(mT, lambda half: mT[:, half * 4:(half + 1) * 4, mt * 128:(mt + 1) * 128])],
                                self.nextbank(), self.nextbank())
            for j in range(2):
                pb = self.nextbank()
                for dc in range(8):
                    k.op("pe", lambda e: e.matmul(pb[:, 0:256], w[:, dc, j * 128:(j + 1) * 128], mT[:, dc, :],
                                                  start=(dc == 0), stop=(dc == 7)),
                         reads=[w, mT], writes=[pb], inc=(dc == 7))
                k.op("act", lambda e: e.copy(memkT[:, j, :], pb[:, 0:256]), reads=[pb], writes=[memkT])
            for mt in range(2):
                pb = self.nextbank()
                for dc in range(8):
                    k.op("pe", lambda e: e.matmul(pb[:, 0:256], mT[:, dc, mt * 128:(mt + 1) * 128], w[:, dc, 256:512],
                                                  start=(dc == 0), stop=(dc == 7)),
                         reads=[w, mT], writes=[pb], inc=(dc == 7))
                k.op("dve", lambda e: e.tensor_copy(memv[:, mt, :], pb[:, 0:256]), reads=[pb], writes=[memv])
            k.barrier()
        return memkT, memv

    def mem_attend(self, W, mqT, qoff, memkT, memv, mix):
        k = self.k
        pe_ = W["pexp"]; ms = W["mstat"]; pT = W["pT"]
        banks = [self.nextbank(), self.nextbank()]
        for hh in range(4):
            pair, s = hh // 2, hh % 2
            pb = banks[s]
            k.op("pe", lambda e: e.matmul(pb[:, pair * 256:(pair + 1) * 256], mqT[s * 64:(s + 1) * 64, pair, qoff:qoff + 128],
                                          memkT[s * 64:(s + 1) * 64, pair, :], start=True, stop=True),
                 reads=[mqT, memkT], writes=[pb])
        for s in range(2):
            pb = banks[s]
            k.op("dve", lambda e: e.tensor_reduce(ms[:, s:s + 3:2], pb[:].rearrange("p (h m) -> p h m", h=2), AX.X, ALU.max),
                 reads=[pb], writes=[ms])
        k.op("dve", lambda e: e.tensor_single_scalar(ms[:, 4:8], ms[:, 0:4], -0.125, ALU.mult), reads=[ms], writes=[ms])
        for hh in range(4):
            pair, s = hh // 2, hh % 2
            pb = banks[s]
            k.op("act", lambda e: e.activation(pe_[:, hh, :], pb[:, pair * 256:(pair + 1) * 256], AF.Exp, bias=ms[:, 4 + hh:5 + hh],
                                               scale=0.125, accum_out=ms[:, 8 + hh:9 + hh]),
                 reads=[pb, ms], writes=[pe_, ms])
        k.op("dve", lambda e: e.reciprocal(ms[:, 12:16], ms[:, 8:12]), reads=[ms], writes=[ms])
        for half in range(2):
            pb = self.nextbank()
            for j in range(4):
                idx = half * 4 + j
                hh, mc = idx // 2, idx % 2
                k.op("pe", lambda e: e.transpose(pb[:, j * 128:(j + 1) * 128], pe_[:, hh, mc * 128:(mc + 1) * 128], self.ident),
                     reads=[pe_, self.cst], writes=[pb], inc=(j == 3))
            if half == 0:
                k.op("act", lambda e: e.copy(pT[:, 0:4, :], pb[:].rearrange("p (c t) -> p c t", c=4)), reads=[pb], writes=[pT])
            else:
                k.op("dve", lambda e: e.tensor_copy(pT[:, 4:8, :], pb[:].rearrange("p (c t) -> p c t", c=4)), reads=[pb], writes=[pT])
        pb = self.nextbank()
        for hh in range(4):
            for mc in range(2):
                k.op("pe", lambda e: e.matmul(pb[:, hh * 64:(hh + 1) * 64], pT[:, hh * 2 + mc, :], memv[:, mc, hh * 64:(hh + 1) * 64],
                                              start=(mc == 0), stop=(mc == 1)),
                     reads=[pT, memv], writes=[pb], inc=(mc == 1))
        k.op("dve", lambda e: e.tensor_tensor(mix[:, 768:1024].rearrange("p (h d) -> p h d", h=4),
                                              pb[:, 0:256].rearrange("p (h d) -> p h d", h=4),
                                              ms[:, 12:16].unsqueeze(2).to_broadcast([128, 4, 64]), ALU.mult),
             reads=[pb, ms], writes=[mix])

    def mem_work(self, st):
        sb = lambda n, s, d: self.sbuf(st, n, s, d)
        return {"pexp": sb("pexp", [128, 4, 256], F32), "mstat": sb("mstat", [128, 16], F32),
                "pT": sb("pT", [128, 8, 128], BF16)}

    def out_proj(self, mix, mixT, w_out, h, hn, dst_ap, dst_buf):
        k = self.k
        self.transpose8(mix, [(mixT, lambda half: mixT[:, half * 4:(half + 1) * 4, :])], self.nextbank(), self.nextbank())
        for half in range(2):
            pb = self.nextbank()
            for fc in range(8):
                k.op("pe", lambda e: e.matmul(pb[:], mixT[:, fc, :], w_out[:, fc, half * 512:(half + 1) * 512],
                                              start=(fc == 0), stop=(fc == 7)),
                     reads=[mixT, w_out], writes=[pb], inc=(fc == 7))
            k.op("dve", lambda e: e.tensor_tensor(hn[:, half * 512:(half + 1) * 512], h[:, half * 512:(half + 1) * 512], pb[:], ALU.add),
                 reads=[h, pb], writes=[hn])
        k.dma(dst_ap, hn[:], reads=[hn], writes=[dst_buf])

    def mixer_a_phase(self, hin, hout):
        nc, k, P = self.nc, self.k, self.P
        NTA = int(os.environ.get("MK_NTA", str(NT)))
        with ExitStack() as st:
            sb = lambda n, s, d: self.sbuf(st, n, s, d)
            memkT, memv = self.mem_kv(st, 0)
            gain = sb("gainA", [128, D], F32)
            k.dma(gain[:], P["a_norm"][0].partition_broadcast(128), writes=[gain])
            w_in = sb("w_inA", [128, 8, 3340], BF16)
            for c in range(8):
                k.dma(w_in[:, c, :], P["a_w_in"][0, c * 128:(c + 1) * 128, :], writes=[w_in], q="pool")
            w_out = sb("w_outA", [128, 8, D], BF16)
            k.dma(w_out[:], P["a_w_out"][0].rearrange("(c p) n -> p c n", p=128), writes=[w_out], q="pool")
            convw = sb("convw", [128, 18, 4], F32)
            k.dma(convw[:], P["convw"], writes=[convw])
            sc6 = sb("sc6", [128, 32], F32)
            k.dma(sc6[:, 0:6], P["a_log"][0].partition_broadcast(128), writes=[sc6])
            k.dma(sc6[:, 6:12], P["a_dt_bias"][0].partition_broadcast(128), writes=[sc6])
            k.op("act", lambda e: e.activation(sc6[:, 12:18], sc6[:, 0:6], AF.Exp), reads=[sc6], writes=[sc6])
            k.op("dve", lambda e: e.tensor_single_scalar(sc6[:, 12:18], sc6[:, 12:18], -1.0, ALU.mult), reads=[sc6], writes=[sc6])
            ogain = sb("ogain", [128, 128], F32)
            k.dma(ogain[:], P["a_out_gain"][0].partition_broadcast(128), writes=[ogain])
            MW = self.mem_work(st)
            pc = sb("pc", [128, 18, 131], F32)
            k.op("dve", lambda e: e.memset(pc[:], 0.0), writes=[pc])
            Sf = [sb("Sf%d" % h, [128, 128], F32) for h in range(6)]
            Sb = [sb("Sb%d" % h, [128, 128], BF16) for h in range(6)]
            for h in range(6):
                k.op("dve", lambda e: e.memset(Sf[h][:], 0.0), writes=[Sf[h]])
                k.op("pool", lambda e: e.memset(Sb[h][:], 0.0), writes=[Sb[h]])
            ht = [sb("htA%d" % i, [128, D], F32) for i in range(2)]
            hn = [sb("hnA", [128, D], F32)] * 2
            xn = sb("xnA", [128, D], F32)
            junk = sb("junkA", [128, D], F32)
            stt = [sb("sttA%d" % i, [128, 4], F32) for i in range(2)]
            xnT = [sb("xnTA%d" % i, [128, 8, 128], BF16) for i in range(2)]
            cv = sb("cv", [128, 18, 128], F32)
            ctmp = sb("ctmp", [128, 128], F32)
            qkv = sb("qkv", [128, 18, 128], F32)
            sq = sb("sq", [128, 12, 128], BF16)
            rs = sb("rs", [128, 12, 128], F32)
            qkT = sb("qkT", [128, 6, 2, 128], BF16)
            kn32 = sb("kn32", [128, 6, 128], F32)
            mqT = sb("mqTA", [128, 2, 128], BF16)
            gg = sb("gg", [128, 768], F32)
            sc = [sb("scA%d" % i, [128, 96], F32) for i in range(2)]
            vtok = sb("vtok", [128, 6, 128], F32)
            kt = sb("kt", [128, 6, 128], BF16)
            SLg = [sb("SLg%d" % i, [128, 128], F32) for i in range(2)]
            decT = sb("decT", [128, 6, 128], F32)
            dm = decT
            dmi = sb("dmi", [128, 6, 128], F32)
            attnT = [sb("attnT%d" % h, [128, 128], BF16) for h in range(6)]
            Wq = [[sb("W%d_%d" % (h, i), [128, 3, 128], F32) for i in range(2)] for h in range(6)]
            Rb = [sb("R%d" % i, [128, 128], F32) for i in range(2)]
            vnew = [sb("vnew%d" % i, [128, 128], BF16) for i in range(2)]
            o1 = [sb("o1_%d" % i, [128, 128], F32) for i in range(2)]
            ob = [sb("ob%d" % i, [128, 128], F32) for i in range(2)]
            ost = [sb("ost%d" % i, [128, 4], F32) for i in range(2)]
            mix = sb("mixA", [128, D], F32)
            mixT = sb("mixTA", [128, 8, 128], BF16)
            ident, U, SL, strictT, ones = self.ident, self.U, self.SL, self.strictT, self.ones
            cst = self.cst
            QS = float(128 ** -0.5)

            for t in range(NTA):
                b = t % 2
                h = ht[b]
                k.dma(h[:], hin[0][t * 128:(t + 1) * 128, :], reads=[hin[1][t]], writes=[h])
                self.rmsnorm(h, gain, xn, junk, stt[b])
                xT = xnT[b]
                self.transpose8(xn, [(xT, lambda half: xT[:, half * 4:(half + 1) * 4, :])], self.nextbank(), self.nextbank())
                for g4 in range(5):
                    nf = 4 if g4 < 4 else 2
                    pb = self.nextbank()
                    for j in range(nf):
                        fc = g4 * 4 + j
                        for dc in range(8):
                            k.op("pe", lambda e: e.matmul(pb[:, j * 128:(j + 1) * 128], w_in[:, dc, fc * 128:(fc + 1) * 128], xT[:, dc, :],
                                                          start=(dc == 0), stop=(dc == 7)),
                                 reads=[w_in, xT], writes=[pb], inc=(dc == 7 and j == nf - 1))
                    eng = "act" if g4 % 2 == 0 else "dve"
                    dstv = pc[:, g4 * 4:g4 * 4 + nf, 3:131]
                    srcv = pb[:, 0:nf * 128].rearrange("p (c t) -> p c t", c=nf)
                    if eng == "act":
                        k.op("act", lambda e: e.copy(dstv, srcv), reads=[pb], writes=[pc])
                    else:
                        k.op("dve", lambda e: e.tensor_copy(dstv, srcv), reads=[pb], writes=[pc])
                pb = self.nextbank()
                for j in range(2):
                    for dc in range(8):
                        k.op("pe", lambda e: e.matmul(pb[:, j * 128:(j + 1) * 128], w_in[:, dc, 3084 + j * 128:3084 + (j + 1) * 128], xT[:, dc, :],
                                                      start=(dc == 0), stop=(dc == 7)),
                             reads=[w_in, xT], writes=[pb], inc=(dc == 7 and j == 1))
                k.op("act", lambda e: e.copy(mqT[:], pb[:, 0:256].rearrange("p (c t) -> p c t", c=2)), reads=[pb], writes=[mqT])
                pg1 = self.nextbank()
                for dc in range(8):
                    k.op("pe", lambda e: e.matmul(pg1[:], xT[:, dc, :], w_in[:, dc, 2304:2816], start=(dc == 0), stop=(dc == 7)),
                         reads=[w_in, xT], writes=[pg1], inc=(dc == 7))
                pg2 = self.nextbank()
                for dc in range(8):
                    k.op("pe", lambda e: e.matmul(pg2[:, 0:268], xT[:, dc, :], w_in[:, dc, 2816:3084], start=(dc == 0), stop=(dc == 7)),
                         reads=[w_in, xT], writes=[pg2], inc=(dc == 7))
                k.op("act", lambda e: e.activation(gg[:, 0:512], pg1[:], AF.Silu), reads=[pg1], writes=[gg])
                k.op("act", lambda e: e.activation(gg[:, 512:768], pg2[:, 0:256], AF.Silu), reads=[pg2], writes=[gg])
                s_ = sc[b]
                C = lambda a, n=6: s_[:, a:a + n]
                beta, tt_, ex_, sp_, g_, gcl, egc, negc, etl, egl, dd = (C(0), C(6), C(12), C(18), C(24), C(32, 16), C(48), C(54), C(60), C(66), C(72))
                k.op("act", lambda e: e.activation(beta, pg2[:, 256:262], AF.Sigmoid), reads=[pg2], writes=[s_])
                k.op("dve", lambda e: e.tensor_tensor(tt_, pg2[:, 262:268], sc6[:, 6:12], ALU.add), reads=[pg2, sc6], writes=[s_])
                k.op("act", lambda e: e.activation(ex_, tt_, AF.Exp), reads=[s_], writes=[s_])
                k.op("act", lambda e: e.activation(sp_, ex_, AF.Ln, bias=1.0), reads=[s_], writes=[s_])
                k.op("dve", lambda e: e.tensor_tensor(g_, sp_, sc6[:, 12:18], ALU.mult), reads=[s_, sc6], writes=[s_])
                k.op("pool", lambda e: e.tensor_tensor(gg[:].rearrange("p (h d) -> p h d", h=6), gg[:].rearrange("p (h d) -> p h d", h=6),
                                                       ogain[:].unsqueeze(1).to_broadcast([128, 6, 128]), ALU.mult),
                     reads=[gg, ogain], writes=[gg])
                pgc = self.nextbank()
                k.op("pe", lambda e: e.matmul(pgc[:, 0:6], U, g_, start=True, stop=True), reads=[cst, s_], writes=[pgc])
                k.op("pe", lambda e: e.matmul(pgc[:, 8:14], ones, g_, start=True, stop=True), reads=[cst, s_], writes=[pgc])
                k.op("dve", lambda e: e.tensor_copy(gcl, pgc[:, 0:16]), reads=[pgc], writes=[s_])
                k.op("act", lambda e: e.activation(egc, s_[:, 32:38], AF.Exp), reads=[s_], writes=[s_])
                k.op("dve", lambda e: e.tensor_single_scalar(negc, egc, -1.0, ALU.mult), reads=[s_], writes=[s_])
                k.op("dve", lambda e: e.tensor_tensor(dd, s_[:, 40:46], s_[:, 32:38], ALU.subtract), reads=[s_], writes=[s_])
                k.op("act", lambda e: e.activation(etl, dd, AF.Exp), reads=[s_], writes=[s_])
                k.op("act", lambda e: e.activation(egl, s_[:, 40:46], AF.Exp), reads=[s_], writes=[s_])
                for fc in range(18):
                    o_ = cv[:, fc, :]
                    if fc % 3 != 2:
                        k.op("dve", lambda e: e.tensor_scalar(o_, pc[:, fc, 0:128], convw[:, fc, 0:1], None, ALU.mult),
                             reads=[pc, convw], writes=[cv])
                        for j in range(1, 4):
                            k.op("dve", lambda e: e.scalar_tensor_tensor(o_, pc[:, fc, j:j + 128], convw[:, fc, j:j + 1], o_, ALU.mult, ALU.add),
                                 reads=[pc, convw, cv], writes=[cv])
                    else:
                        k.op("pool", lambda e: e.tensor_scalar(o_, pc[:, fc, 0:128], convw[:, fc, 0:1], None, ALU.mult),
                             reads=[pc, convw], writes=[cv])
                        for j in range(1, 4):
                            k.op("pool", lambda e: e.tensor_scalar(ctmp[:], pc[:, fc, j:j + 128], convw[:, fc, j:j + 1], None, ALU.mult),
                                 reads=[pc, convw], writes=[ctmp])
                            k.op("pool", lambda e: e.tensor_tensor(o_, o_, ctmp[:], ALU.add), reads=[cv, ctmp], writes=[cv])
                k.op("pool", lambda e: e.tensor_copy(pc[:, :, 0:3], pc[:, :, 128:131]), reads=[pc], writes=[pc])
                k.op("act", lambda e: e.activation(qkv[:], cv[:], AF.Silu), reads=[cv], writes=[qkv])
                k.op("act", lambda e: e.activation(sq[:], qkv[:, 0:12, :], AF.Square), reads=[qkv], writes=[sq])
                for g3 in range(3):
                    pb = self.nextbank()
                    k.op("pe", lambda e: e.matmul(pb[:], self.onesb, sq[:, g3 * 4:(g3 + 1) * 4, :], start=True, stop=True),
                         reads=[self.cstb, sq], writes=[pb])
                    k.op("act", lambda e: e.activation(rs[:, g3 * 4:(g3 + 1) * 4, :], pb[:].rearrange("p (c t) -> p c t", c=4), AF.Sqrt,
                                                       bias=self.epsb[:, 0:1]), reads=[pb, self.epsbuf], writes=[rs])
                k.op("dve", lambda e: e.reciprocal(rs[:], rs[:]), reads=[rs], writes=[rs])
                k.op("dve", lambda e: e.scalar_tensor_tensor(qkT[:, :, 1, :], qkv[:, 0:6, :], QS, rs[:, 0:6, :], ALU.mult, ALU.mult),
                     reads=[qkv, rs], writes=[qkT])
                k.op("dve", lambda e: e.tensor_tensor(kn32[:], qkv[:, 6:12, :], rs[:, 6:12, :], ALU.mult), reads=[qkv, rs], writes=[kn32])
                k.op("pool", lambda e: e.tensor_copy(qkT[:, :, 0, :], kn32[:]), reads=[kn32], writes=[qkT])
                for grp in range(3):
                    pb = self.nextbank()
                    for j in range(4):
                        idx = grp * 4 + j
                        src = qkv[:, 12 + idx, :] if idx < 6 else kn32[:, idx - 6, :]
                        srcb = qkv if idx < 6 else kn32
                        k.op("pe", lambda e: e.transpose(pb[:, j * 128:(j + 1) * 128], src, ident), reads=[srcb, cst], writes=[pb], inc=(j == 3))
                    for j in range(4):
                        idx = grp * 4 + j
                        if idx < 6:
                            k.op("act", lambda e: e.copy(vtok[:, idx, :], pb[:, j * 128:(j + 1) * 128]), reads=[pb], writes=[vtok])
                        else:
                            hh = idx - 6
                            k.op("dve", lambda e: e.tensor_scalar(kt[:, hh, :], pb[:, j * 128:(j + 1) * 128], etl[:, hh:hh + 1], None, ALU.mult),
                                 reads=[pb, s_], writes=[kt])
                for hp in range(3):
                    pb = self.nextbank()
                    for j in range(2):
                        hh = hp * 2 + j
                        sg = SLg[hh % 2]
                        k.op("pool", lambda e: e.tensor_scalar(sg[:], SL, g_[:, hh:hh + 1], None, ALU.mult), reads=[cst, s_], writes=[sg])
                        k.op("pe", lambda e: e.matmul(pb[:, j * 128:(j + 1) * 128], sg[:], U, start=True, stop=True),
                             reads=[sg, cst], writes=[pb])
                    k.op("act", lambda e: e.activation(decT[:, hp * 2:hp * 2 + 2, :], pb[:, 0:256].rearrange("p (c t) -> p c t", c=2), AF.Exp),
                         reads=[pb], writes=[decT])
                k.op("pool", lambda e: e.tensor_tensor(dm[:], decT[:], strictT.unsqueeze(1).to_broadcast([128, 6, 128]), ALU.mult),
                     reads=[decT, cst], writes=[dm])
                k.op("pool", lambda e: e.tensor_tensor(dmi[:], dm[:], ident.unsqueeze(1).to_broadcast([128, 6, 128]), ALU.add),
                     reads=[dm, cst], writes=[dmi])
                for hh in range(6):
                    pb = self.nextbank()
                    W0 = Wq[hh][0]
                    k.op("pe", lambda e: e.matmul(pb[:, 0:256], qkT[:, hh, 0, :], qkT[:, hh, :, :].rearrange("p a t -> p (a t)"),
                                                  start=True, stop=True), reads=[qkT], writes=[pb])
                    k.op("dve", lambda e: e.scalar_tensor_tensor(W0[:, 0, :], pb[:, 0:128], beta[:, hh:hh + 1], dm[:, hh, :], ALU.mult, ALU.mult),
                         reads=[pb, s_, dm], writes=[W0])
                    k.op("dve", lambda e: e.tensor_tensor(attnT[hh][:], pb[:, 128:256], dmi[:, hh, :], ALU.mult),
                         reads=[pb, dmi], writes=[attnT[hh]])
                    k.op("pool", lambda e: e.tensor_tensor(W0[:, 1, :], ident, W0[:, 0, :], ALU.subtract), reads=[cst, W0], writes=[W0])
                    pb2 = self.nextbank()
                    k.op("pe", lambda e: e.transpose(pb2[:, 0:128], W0[:, 0, :], ident), reads=[W0, cst], writes=[pb2])
                    k.op("act", lambda e: e.copy(W0[:, 2, :], pb2[:, 0:128]), reads=[pb2], writes=[W0])
                for lvl in range(7):
                    for hh in range(6):
                        Wc = Wq[hh][lvl % 2]
                        Wn = Wq[hh][(lvl + 1) % 2]
                        pb = self.nextbank()
                        Qk, Xk, Pk = Wc[:, 0, :], Wc[:, 1, :], Wc[:, 2, :]
                        mm = lambda out, l_, r_, st_, sp_2, inc_: k.op(
                            "pe", lambda e: e.matmul(out, l_, r_, start=st_, stop=sp_2), reads=[Wc, cst], writes=[pb], inc=inc_)
                        if lvl == 0:
                            mm(pb[:, 0:128], Pk, Qk, True, True, False)
                            mm(pb[:, 256:384], Qk, Pk, True, True, True)
                            k.op("act", lambda e: e.copy(Wn[:, 0, :], pb[:, 0:128]), reads=[pb], writes=[Wn])
                            k.op("act", lambda e: e.copy(Wn[:, 2, :], pb[:, 256:384]), reads=[pb], writes=[Wn])
                            k.op("pool", lambda e: e.tensor_copy(Wn[:, 1, :], Xk), reads=[Wc], writes=[Wn])
                        elif lvl < 6:
                            mm(pb[:, 0:128], Pk, Qk, True, True, False)
                            mm(pb[:, 128:256], Pk, Xk, True, False, False)
                            mm(pb[:, 128:256], ident, Xk, False, True, False)
                            mm(pb[:, 256:384], Qk, Pk, True, True, True)
                            if hh % 2 == 0:
                                k.op("act", lambda e: e.copy(Wn[:], pb[:, 0:384].rearrange("p (c t) -> p c t", c=3)), reads=[pb], writes=[Wn])
                            else:
                                k.op("dve", lambda e: e.tensor_copy(Wn[:], pb[:, 0:384].rearrange("p (c t) -> p c t", c=3)), reads=[pb], writes=[Wn])
                        else:
                            mm(pb[:, 128:256], Pk, Xk, True, False, False)
                            mm(pb[:, 128:256], ident, Xk, False, True, True)
                            k.op("act", lambda e: e.copy(Wn[:, 1, :], pb[:, 128:256]), reads=[pb], writes=[Wn])
                for hh in range(6):
                    TT = Wq[hh][1]
                    r2 = hh % 2
                    pb = self.nextbank()
                    k.op("pe", lambda e: e.matmul(pb[:, 0:128], qkT[:, hh, 0, :], Sb[hh][:], start=True, stop=True), reads=[qkT, Sb[hh]], writes=[pb], inc=False)
                    k.op("pe", lambda e: e.matmul(pb[:, 128:256], qkT[:, hh, 1, :], Sb[hh][:], start=True, stop=True), reads=[qkT, Sb[hh]], writes=[pb])
                    R_ = Rb[r2]
                    k.op("dve", lambda e: e.scalar_tensor_tensor(R_[:], pb[:, 0:128], negc[:, hh:hh + 1], vtok[:, hh, :], ALU.mult, ALU.add),
                         reads=[pb, s_, vtok], writes=[R_])
                    k.op("act", lambda e: e.mul(o1[r2][:], pb[:, 128:256], egc[:, hh:hh + 1]), reads=[pb, s_], writes=[o1[r2]])
                    pb2 = self.nextbank()
                    k.op("pe", lambda e: e.matmul(pb2[:, 0:128], TT[:, 1, :], R_[:], start=True, stop=True), reads=[TT, R_], writes=[pb2])
                    vn = vnew[r2]
                    k.op("dve", lambda e: e.tensor_scalar(vn[:], pb2[:, 0:128], beta[:, hh:hh + 1], None, ALU.mult), reads=[pb2, s_], writes=[vn])
                    pb3 = self.nextbank()
                    k.op("pe", lambda e: e.matmul(pb3[:, 0:128], attnT[hh][:], vn[:], start=True, stop=True), reads=[attnT[hh], vn], writes=[pb3], inc=False)
                    k.op("pe", lambda e: e.matmul(pb3[:, 128:256], kt[:, hh, :], vn[:], start=True, stop=True), reads=[kt, vn], writes=[pb3])
                    o_ = ob[r2]
                    k.op("dve", lambda e: e.tensor_tensor(o_[:], o1[r2][:], pb3[:, 0:128], ALU.add), reads=[o1[r2], pb3], writes=[o_])
                    k.op("dve", lambda e: e.scalar_tensor_tensor(Sf[hh][:], Sf[hh][:], egl[:, hh:hh + 1], pb3[:, 128:256], ALU.mult, ALU.add),
                         reads=[Sf[hh], s_, pb3], writes=[Sf[hh]])
                    k.op("pool", lambda e: e.tensor_copy(Sb[hh][:], Sf[hh][:]), reads=[Sf[hh]], writes=[Sb[hh]])
                    os_ = ost[r2]
                    k.op("act", lambda e: e.activation(junk[:, 0:128], o_[:], AF.Square, accum_out=os_[:, 0:1]), reads=[o_], writes=[junk, os_])
                    k.op("act", lambda e: e.activation(os_[:, 1:2], os_[:, 0:1], AF.Sqrt, bias=self.epsb[:, 0:1], scale=1.0 / 128),
                         reads=[os_, self.epsbuf], writes=[os_])
                    k.op("dve", lambda e: e.reciprocal(os_[:, 2:3], os_[:, 1:2]), reads=[os_], writes=[os_])
                    k.op("dve", lambda e: e.scalar_tensor_tensor(mix[:, hh * 128:(hh + 1) * 128], o_[:], os_[:, 2:3], gg[:, hh * 128:(hh + 1) * 128],
                                                                 ALU.mult, ALU.mult), reads=[o_, os_, gg], writes=[mix])
                self.mem_attend(MW, mqT, 0, memkT, memv, mix)
                self.out_proj(mix, mixT, w_out, h, hn[b], hout[0][t * 128:(t + 1) * 128, :], hout[1][t])
            k.barrier()

    def mixer_b_phase(self, hin, hout):
        nc, k, P = self.nc, self.k, self.P
        NGB = int(os.environ.get("MK_NGB", "8"))
        NHB = int(os.environ.get("MK_NHB", "12"))
        with ExitStack() as st:
            sb = lambda n, s, d: self.sbuf(st, n, s, d)
            memkT, memv = self.mem_kv(st, 1)
            win_d = self.dscratch("b_w_in_bf", [D, D], BF16)
            wout_d = self.dscratch("b_w_out_bf", [D, D], BF16)
            wdb = [Buf(None, "win_d"), Buf(None, "wout_d")]
            k.dma(win_d, P["b_w_in"][0], writes=[wdb[0]], q="pool")
            k.dma(wout_d, P["b_w_out"][0], writes=[wdb[1]], q="pool")
            KT = sb("KT", [128, 6, S], BF16)
            Vt = sb("Vt", [128, NT, 768], BF16)
            ht = [sb("htB%d" % i, [128, D], F32) for i in range(2)]
            xn = sb("xnB", [128, D], F32)
            stt = [sb("sttB%d" % i, [128, 4], F32) for i in range(2)]
            with ExitStack() as st1:
                sb1 = lambda n, s, d: self.sbuf(st1, n, s, d)
                kvg = sb1("kvg", [128, D], F32)
                k.dma(kvg[:], P["kv_norm"].partition_broadcast(128), writes=[kvg])
                w_kv = sb1("w_kv", [128, 8, 1536], BF16)
                for c in range(8):
                    k.dma(w_kv[:, c, :], P["w_kv"][c * 128:(c + 1) * 128, :], writes=[w_kv], q="pool")
                xT1 = [sb1("xT1_%d" % i, [128, 8, 128], BF16) for i in range(2)]
                for t in range(NT):
                    b = t % 2
                    h = ht[b]
                    k.dma(h[:], hin[0][t * 128:(t + 1) * 128, :], reads=[hin[1][t]], writes=[h])
                    self.rmsnorm(h, kvg, xn, xn, stt[b])
                    xT = xT1[b]
                    self.transpose8(xn, [(xT, lambda half: xT[:, half * 4:(half + 1) * 4, :])], self.nextbank(), self.nextbank())
                    for g4 in range(2):
                        nf = 4 if g4 == 0 else 2
                        pb = self.nextbank()
                        for j in range(nf):
                            fc = g4 * 4 + j
                            for dc in range(8):
                                k.op("pe", lambda e: e.matmul(pb[:, j * 128:(j + 1) * 128], w_kv[:, dc, fc * 128:(fc + 1) * 128], xT[:, dc, :],
                                                              start=(dc == 0), stop=(dc == 7)),
                                     reads=[w_kv, xT], writes=[pb], inc=(dc == 7 and j == nf - 1))
                        dstv = KT[:, g4 * 4:g4 * 4 + nf, t * 128:(t + 1) * 128]
                        srcv = pb[:, 0:nf * 128].rearrange("p (c t) -> p c t", c=nf)
                        k.op("act", lambda e: e.copy(dstv, srcv), reads=[pb], writes=[KT])
                    for half, (c0, c1) in enumerate(((0, 512), (512, 768))):
                        pb = self.nextbank()
                        for dc in range(8):
                            k.op("pe", lambda e: e.matmul(pb[:, 0:c1 - c0], xT[:, dc, :], w_kv[:, dc, 768 + c0:768 + c1],
                                                          start=(dc == 0), stop=(dc == 7)),
                                 reads=[w_kv, xT], writes=[pb], inc=(dc == 7))
                        k.op("dve", lambda e: e.tensor_copy(Vt[:, t, c0:c1], pb[:, 0:c1 - c0]), reads=[pb], writes=[Vt])
                k.barrier()
            bg = sb("bgain", [128, D], F32)
            k.dma(bg[:], P["b_norm"][0].partition_broadcast(128), writes=[bg])
            wB = sb("wB", [128, 8, D], BF16)
            xTg = sb("xTg", [128, 8, 512], BF16)
            qT = sb("qT", [128, 6, 512], BF16)
            mqT = sb("mqTB", [128, 2, 512], BF16)
            mixg = sb("mixg", [128, 4, D], F32)
            bsb = [sb("bsb%d" % i, [128, 512], F32) for i in range(2)]
            L32 = sb("L32", [128, 512], F32)
            Lb = [sb("Lb%d" % i, [128, 512], BF16) for i in range(2)]
            tmp = [sb("tmpB%d" % i, [128, 512], F32) for i in range(2)]
            ea = [sb("ea%d" % i, [128, 512], F32) for i in range(2)]
            ab = [sb("ab%d" % i, [128, 512], BF16) for i in range(2)]
            carry = sb("carry", [128, 512], F32)
            MW = self.mem_work(st)
            mixT = sb("mixTB", [128, 8, 128], BF16)
            hn = sb("hnB", [128, D], F32)
            SLb = self.cstb[:, 2, :]
            onesb = self.onesb
            strictT = self.strictT
            PA = self.ps[7]
            rot = [0]

            def nb():
                rot[0] = (rot[0] + 1) % 7
                return self.ps[rot[0]]
            self.nextbank = nb
            it = 0
            for g in range(NGB):
                k.dma(wB[:], win_d.rearrange("(c p) n -> p c n", p=128), reads=[wdb[0]], writes=[wB])
                for tt in range(4):
                    t = g * 4 + tt
                    b = t % 2
                    h = ht[b]
                    k.dma(h[:], hin[0][t * 128:(t + 1) * 128, :], reads=[hin[1][t]], writes=[h])
                    self.rmsnorm(h, bg, xn, xn, stt[b])
                    self.transpose8(xn, [(xTg, lambda half: xTg[:, half * 4:(half + 1) * 4, tt * 128:(tt + 1) * 128])], nb(), nb())
                for fc in range(8):
                    pb = nb()
                    for dc in range(8):
                        k.op("pe", lambda e: e.matmul(pb[:], wB[:, dc, fc * 128:(fc + 1) * 128], xTg[:, dc, :], start=(dc == 0), stop=(dc == 7)),
                             reads=[wB, xTg], writes=[pb], inc=(dc == 7))
                    if fc < 6:
                        k.op("act", lambda e: e.copy(qT[:, fc, :], pb[:]), reads=[pb], writes=[qT])
                    else:
                        k.op("dve", lambda e: e.tensor_copy(mqT[:, fc - 6, :], pb[:]), reads=[pb], writes=[mqT])
                for hh in range(NHB):
                    fc, s = hh // 2, hh % 2
                    ps_ = slice(s * 64, (s + 1) * 64)
                    k.op("dve", lambda e: e.memset(PA[:, 0:256], 0.0), writes=[PA])
                    k.op("pool", lambda e: e.memset(carry[:], 0.0), writes=[carry])
                    for kb in range(4 * g + 3, -1, -1):
                        it += 1
                        i2 = it % 2
                        r = max(kb - 4 * g, 0)
                        diag = kb >= 4 * g
                        c0 = r * 128
                        cs = slice(c0, 512)
                        pz = nb()
                        k.op("pe", lambda e: e.matmul(pz[:, cs], KT[ps_, fc, kb * 128:(kb + 1) * 128], qT[ps_, fc, cs], start=True, stop=True),
                             reads=[KT, qT], writes=[pz])
                        bb = bsb[i2]
                        k.op("act", lambda e: e.activation(bb[:, cs], pz[:, cs], AF.Sigmoid, scale=0.125), reads=[pz], writes=[bb])
                        lb = Lb[i2]
                        if diag:
                            k.op("act", lambda e: e.activation(L32[:, cs], bb[:, cs], AF.Ln, bias=1.0, scale=-1.0), reads=[bb], writes=[L32])
                            k.op("pool", lambda e: e.tensor_tensor(L32[:, c0:c0 + 128], L32[:, c0:c0 + 128], strictT, ALU.mult),
                                 reads=[L32, self.cst], writes=[L32])
                            k.op("pool", lambda e: e.tensor_copy(lb[:, cs], L32[:, cs]), reads=[L32], writes=[lb])
                            k.op("pool", lambda e: e.tensor_tensor(bb[:, c0:c0 + 128], bb[:, c0:c0 + 128], strictT, ALU.mult),
                                 reads=[bb, self.cst], writes=[bb])
                        else:
                            k.op("act", lambda e: e.activation(lb[:, cs], bb[:, cs], AF.Ln, bias=1.0, scale=-1.0), reads=[bb], writes=[lb])
                        pa = nb()
                        k.op("pe", lambda e: e.matmul(pa[:, cs], SLb, lb[:, cs], start=True, stop=True), reads=[self.cstb, lb], writes=[pa])
                        if kb > 0:
                            pt = nb()
                            k.op("pe", lambda e: e.matmul(pt[:, cs], onesb, lb[:, cs], start=True, stop=True), reads=[self.cstb, lb], writes=[pt])
                        e_ = ea[i2]
                        t_ = tmp[i2]
                        k.op("dve", lambda e: e.tensor_tensor(t_[:, cs], pa[:, cs], carry[:, cs], ALU.add), reads=[pa, carry], writes=[t_])
                        k.op("act", lambda e: e.activation(e_[:, cs], t_[:, cs], AF.Exp), reads=[t_], writes=[e_])
                        if kb > 0:
                            k.op("dve", lambda e: e.tensor_tensor(carry[:, cs], carry[:, cs], pt[:, cs], ALU.add), reads=[pt, carry], writes=[carry])
                        a_ = ab[i2]
                        k.op("pool", lambda e: e.tensor_tensor(a_[:, cs], bb[:, cs], e_[:, cs], ALU.mult), reads=[bb, e_], writes=[a_])
                        for c in range(r, 4):
                            k.op("pe", lambda e: e.matmul(PA[:, c * 64:(c + 1) * 64], a_[:, c * 128:(c + 1) * 128], Vt[:, kb, hh * 64:(hh + 1) * 64],
                                                          start=False, stop=False, skip_group_check=True),
                                 reads=[a_, Vt], writes=[PA], inc=(c == 3))
                    k.op("act", lambda e: e.copy(mixg[:, :, hh * 64:(hh + 1) * 64], PA[:, 0:256].rearrange("p (c d) -> p c d", c=4)),
                         reads=[PA], writes=[mixg])
                k.dma(wB[:], wout_d.rearrange("(c p) n -> p c n", p=128), reads=[wdb[1]], writes=[wB])
                for tt in range(4):
                    t = g * 4 + tt
                    b = t % 2
                    h = ht[b]
                    k.dma(h[:], hin[0][t * 128:(t + 1) * 128, :], reads=[hin[1][t]], writes=[h])
                    mv_ = View(mixg, mixg[:, tt, :])
                    self.mem_attend(MW, mqT, tt * 128, memkT, memv, mv_)
                    self.out_proj(mv_, mixT, wB, h, hn, hout[0][t * 128:(t + 1) * 128, :], hout[1][t])
            k.barrier()
            del self.nextbank

    def build(self, phases):
        nc, k = self.nc, self.k
        P = self.P = {}
        shapes = dict(
            x=[S, D], mem=[256, D], a_norm=[1, D], a_w_in=[1, D, 3340], a_conv=[1, 4, 2304],
            a_log=[1, 6], a_dt_bias=[1, 6], a_out_gain=[1, 128], a_w_out=[1, D, D],
            kv_norm=[D], w_kv=[D, 1536], b_norm=[1, D], b_w_in=[1, D, D], b_w_out=[1, D, D],
            mem_norm=[2, D], w_mem_kv=[2, D, 512], ffn_norm=[2, D], w_group=[2, D, 4], b_group=[2, 4],
            w_router=[2, D, 16], b_router=[2, 16], w1=[2, 16, D, 256], w3=[2, 16, D, 256],
            w2=[2, 16, 256, D], final_norm=[D], wgr=[2, 128, 8, 20], rbias=[2, 20], convw=[128, 18, 4])
        for n, s in shapes.items():
            P[n] = self.din(n, s)
        out = nc.dram_tensor("out", [S, D], F32, kind="ExternalOutput").ap()
        mkbufs = lambda nm: [Buf(None, "%s%d" % (nm, i)) for i in range(NT)]
        hx = (P["x"], mkbufs("x"))
        hA = (self.dscratch("hA", [S, D]), mkbufs("hA"))
        hB = (self.dscratch("hB", [S, D]), mkbufs("hB"))
        ho = (out, mkbufs("out"))
        with ExitStack() as gst:
            self.load_consts(gst)
            self.epsbuf = self.sbuf(gst, "epsb", [128, 1], F32)
            self.epsb = self.epsbuf
            k.op("dve", lambda e: e.memset(self.epsbuf[:], EPS), writes=[self.epsbuf])
            cur = hx
            seq = {"A": hA, "M0": hB, "B": hA, "M1": ho}
            for ph in phases:
                dst = seq[ph] if ph != phases[-1] else ho
                if ph == "M0":
                    self.moe_phase(0, cur, dst, final=False)
                elif ph == "M1":
                    self.moe_phase(1, cur, dst, final=True)
                elif ph == "A":
                    self.mixer_a_phase(cur, dst)
                elif ph == "B":
                    self.mixer_b_phase(cur, dst)
                cur = dst
            for b in ho[1]:
                if b.lw is not None:
                    k._wait("sp", b.lw)
        return nc


def make_consts():
    c = np.zeros((128, 5, 128), np.float32)
    i = np.arange(128)
    c[:, 0, :] = np.eye(128)
    c[:, 1, :] = (i[:, None] <= i[None, :])
    c[:, 2, :] = (i[:, None] > i[None, :])
    c[:, 3, :] = (i[:, None] < i[None, :])
    c[:, 4, :] = 1.0
    return c


_CACHE = {}


def run(inputs, phases=("A", "M0", "B", "M1"), ncores=NCORES, trace=False):
    key = tuple(phases)
    if key not in _CACHE:
        mk = MK(phases)
        _CACHE[key] = mk.build(list(phases))
    nc = _CACHE[key]
    consts = make_consts()
    inputs = dict(inputs)
    wg = np.concatenate([np.asarray(inputs["w_group"]), np.asarray(inputs["w_router"])], axis=2)
    inputs["wgr"] = np.ascontiguousarray(wg.reshape(2, 8, 128, 20).transpose(0, 2, 1, 3))
    inputs["rbias"] = np.concatenate([np.asarray(inputs["b_group"]), np.asarray(inputs["b_router"])], axis=1)
    cw = np.asarray(inputs["a_conv"])[0]
    inputs["convw"] = np.ascontiguousarray(cw.reshape(4, 18, 128).transpose(2, 1, 0))
    in_maps = []
    for c in range(ncores):
        m = {"consts": consts}
        for n, v in inputs.items():
            v = np.asarray(v)
            if n in ("x", "mem"):
                m[n] = np.ascontiguousarray(v[c])
            else:
                m[n] = np.ascontiguousarray(v, dtype=np.float32)
        in_maps.append(m)
    res = run_bass_kernel_spmd(nc, in_maps, core_ids=list(range(ncores)), trace=trace)
    outs = np.stack([r["out"] for r in res.results], axis=0)
    return outs, res


def kernel(**inputs):
    outs, _ = run(inputs)
    return outs.astype(np.float32)
```

```python
from contextlib import ExitStack
import os
import numpy as np
import concourse.bass as bass
import concourse.mybir as mybir
from concourse.bass_utils import run_bass_kernel_spmd

F32 = mybir.dt.float32
BF16 = mybir.dt.bfloat16
AF = mybir.ActivationFunctionType
ALU = mybir.AluOpType
AX = mybir.AxisListType

S = 4096
D = 1024
NT = S // 128
EPS = 1e-6
NCORES = 8


class Buf:
    __slots__ = ("ap", "name", "_lw", "_rd", "_excl")
    lw = property(lambda self: self._lw, lambda self, v: setattr(self, "_lw", v))
    rd = property(lambda self: self._rd, lambda self, v: setattr(self, "_rd", v))
    excl = property(lambda self: self._excl, lambda self, v: setattr(self, "_excl", v))

    def __init__(self, ap, name="", excl=False):
        self.ap = ap
        self.name = name
        self.excl = excl
        self.lw = None
        self.rd = []

    def __getitem__(self, idx):
        return self.ap[idx]


class View(Buf):
    __slots__ = ("parent",)

    def __init__(self, parent, ap):
        self.parent = parent
        self.ap = ap
        self.name = parent.name

    lw = property(lambda self: self.parent.lw, lambda self, v: setattr(self.parent, "lw", v))
    rd = property(lambda self: self.parent.rd, lambda self, v: setattr(self.parent, "rd", v))
    excl = property(lambda self: self.parent.excl, lambda self, v: None)


class K:
    NDMA = 48

    def __init__(self, nc):
        self.nc = nc
        self.eng = {"pe": nc.tensor, "act": nc.scalar, "dve": nc.vector,
                    "pool": nc.gpsimd, "sp": nc.sync}
        self.sem = {e: nc.alloc_semaphore("s_" + e) for e in ("pe", "act", "dve", "pool")}
        self.cnt = {e: 0 for e in self.sem}
        self.waited = {}
        self.dsem = [nc.alloc_semaphore("d%d" % i) for i in range(self.NDMA)]
        self.dcnt = [0] * self.NDMA
        self.dnext = 0
        self.nins = 0

    def _semh(self, key):
        return self.sem[key] if isinstance(key, str) else self.dsem[key]

    def _wait(self, e, dep):
        key, val = dep
        if key == e and e == "pe":
            return
        w = self.waited.get((e, key), 0)
        if w >= val:
            return
        self.eng[e].wait_ge(self._semh(key), val)
        self.nins += 1
        self.waited[(e, key)] = val

    def _deps(self, e, reads, writes):
        best = {}
        for r in reads:
            if r.lw is not None:
                if best.get(r.lw[0], 0) < r.lw[1]:
                    best[r.lw[0]] = r.lw[1]
            if r.excl:
                for key, val in r.rd:
                    if key != e and best.get(key, 0) < val:
                        best[key] = val
        for w in writes:
            if w.lw is not None:
                if best.get(w.lw[0], 0) < w.lw[1]:
                    best[w.lw[0]] = w.lw[1]
            for key, val in w.rd:
                if best.get(key, 0) < val:
                    best[key] = val
        for key, val in best.items():
            self._wait(e, (key, val))

    def _mark(self, tag, reads, writes):
        for r in reads:
            r.rd.append(tag)
            if len(r.rd) > 64:
                best = {}
                for key, val in r.rd:
                    if best.get(key, 0) < val:
                        best[key] = val
                r.rd = list(best.items())
        for w in writes:
            w.lw = tag
            w.rd = []

    def op(self, e, fn, reads=(), writes=(), inc=True):
        self._deps(e, reads, writes)
        ins = fn(self.eng[e])
        self.nins += 1
        if inc:
            ins.then_inc(self.sem[e], 1)
            self.cnt[e] += 1
            tag = (e, self.cnt[e])
        else:
            tag = (e, self.cnt[e] + 1)
        self._mark(tag, reads, writes)
        return ins

    def dma(self, out, in_, reads=(), writes=(), q="sp", **kw):
        slot = self.dnext
        self.dnext = (self.dnext + 1) % self.NDMA
        if self.dcnt[slot] > 0:
            self._wait(q, (slot, 16 * self.dcnt[slot]))
        self._deps(q, reads, writes)
        ins = self.eng[q].dma_start(out=out, in_=in_, **kw)
        self.nins += 1
        ins.then_inc(self.dsem[slot], 16)
        self.dcnt[slot] += 1
        tag = (slot, 16 * self.dcnt[slot])
        self._mark(tag, reads, writes)
        return tag

    def barrier(self):
        for e in ("pe", "act", "dve", "pool", "sp"):
            for e2 in ("pe", "act", "dve", "pool"):
                if e2 != e and self.cnt[e2] > 0:
                    self._wait(e, (e2, self.cnt[e2]))
            for slot in range(self.NDMA):
                if self.dcnt[slot] > 0:
                    self._wait(e, (slot, 16 * self.dcnt[slot]))


class MK:
    def __init__(self, phases, h0_from_input=True):
        self.nc = nc = bass.Bass("TRN2", target_bir_lowering=False)
        self.k = K(nc)
        self.uid = 0
        self.ins = {}
        self.ps = [Buf(nc.alloc_psum_tensor("psb%d" % i, [128, 512], F32).ap(), "ps%d" % i, excl=True)
                   for i in range(8)]

    def din(self, name, shape):
        ap = self.nc.dram_tensor(name, list(shape), F32, kind="ExternalInput").ap()
        self.ins[name] = ap
        return ap

    def dscratch(self, name, shape, dt=F32):
        return self.nc.dram_tensor(name, list(shape), dt, kind="Internal").ap()

    def sbuf(self, st, name, shape, dt):
        self.uid += 1
        h = st.enter_context(self.nc.sbuf_tensor("%s_%d" % (name, self.uid), list(shape), dt))
        return Buf(h.ap(), name)

    def load_consts(self, st):
        k = self.k
        c = self.din("consts", [128, 7, 128])
        self.cst = self.sbuf(st, "cst", [128, 7, 128], F32)
        k.dma(self.cst[:], c, writes=[self.cst])
        self.ident = self.cst[:, 0, :]
        self.U = self.cst[:, 1, :]
        self.SL = self.cst[:, 2, :]
        self.strictT = self.cst[:, 3, :]
        self.ones = self.cst[:, 4, :]
        self.cstb = self.sbuf(st, "cstb", [128, 7, 128], BF16)
        k.dma(self.cstb[:], c, writes=[self.cstb], q="pool")
        self.identb = self.cstb[:, 0, :]
        self.onesb = self.cstb[:, 4, :]

    def rmsnorm(self, h, gainb, xn, junk, st2):
        k = self.k
        k.op("act", lambda e: e.activation(junk[:], h[:], AF.Square, accum_out=st2[:, 0:1]),
             reads=[h], writes=[junk, st2])
        k.op("act", lambda e: e.activation(st2[:, 1:2], st2[:, 0:1], AF.Sqrt, bias=self.epsb[:, 0:1], scale=1.0 / D),
             reads=[st2, self.epsbuf], writes=[st2])
        k.op("dve", lambda e: e.reciprocal(st2[:, 2:3], st2[:, 1:2]), reads=[st2], writes=[st2])
        k.op("dve", lambda e: e.scalar_tensor_tensor(xn[:], h[:], st2[:, 2:3], gainb[:], ALU.mult, ALU.mult),
             reads=[h, st2, gainb], writes=[xn])

    def transpose8(self, src, dsts, psa, psb, evac=("act", "dve"), second="pool"):
        k = self.k
        for half, ps in enumerate((psa, psb)):
            for j in range(4):
                c = half * 4 + j
                k.op("pe", lambda e: e.transpose(ps[:, j * 128:(j + 1) * 128], src[:, c * 128:(c + 1) * 128], self.ident),
                     reads=[src, self.cst], writes=[ps], inc=(j == 3))
            dbuf, fn = dsts[0]
            eng = evac[half % len(evac)]
            pv = ps[:].rearrange("p (c t) -> p c t", c=4)
            if eng == "act":
                k.op("act", lambda e: e.copy(fn(half), pv), reads=[ps], writes=[dbuf])
            else:
                k.op(eng, lambda e: e.tensor_copy(fn(half), pv), reads=[ps], writes=[dbuf])
            for dbuf2, fn2 in dsts[1:]:
                k.op(second, lambda e: e.tensor_copy(fn2(half), fn(half)), reads=[dbuf], writes=[dbuf2])

    def moe_phase(self, l, hin, hout, final=False, out_ap=None):
        nc, k = self.nc, self.k
        G = 2048
        NTG = G // 128
        P = self.P
        with ExitStack() as st:
            sb = lambda n, s, d: self.sbuf(st, n, s, d)
            gain = sb("gain", [128, D], F32)
            k.dma(gain[:], P["ffn_norm"][l].partition_broadcast(128), writes=[gain])
            if final:
                fgain = sb("fgain", [128, D], F32)
                k.dma(fgain[:], P["final_norm"].partition_broadcast(128), writes=[fgain])
            wgr = sb("wgr", [128, 8, 20], F32)
            k.dma(wgr[:], P["wgr"][l], writes=[wgr])
            rb = sb("rbias", [128, 20], F32)
            k.dma(rb[:], P["rbias"][l].partition_broadcast(128), writes=[rb])
            xnT = sb("xnT", [128, 8, G], BF16)
            yacc = [sb("yacc%d" % i, [128, D], F32) for i in range(NTG)]
            comb = [sb("comb%d" % i, [128, 16], F32) for i in range(NTG)]
            w1b = [sb("w1b%d" % i, [128, 8, 256], BF16) for i in range(2)]
            w3b = [sb("w3b%d" % i, [128, 8, 256], BF16) for i in range(2)]
            w2b = [sb("w2b%d" % i, [128, 2, D], BF16) for i in range(2)]
            ht = [sb("ht%d" % i, [128, D], F32) for i in range(2)]
            xn = [sb("xn%d" % i, [128, D], F32) for i in range(2)]
            junk = sb("junk", [128, D], F32)
            xnT32 = [sb("xnT32_%d" % i, [128, 8, 128], F32) for i in range(2)]
            stt = [sb("stt%d" % i, [128, 4], F32) for i in range(2)]
            rt = [sb("rt%d" % i, [128, 96], F32) for i in range(2)]
            hid = [sb("hid%d" % i, [128, 2, 512], BF16) for i in range(2)]
            sil = [sb("sil%d" % i, [128, 512], F32) for i in range(2)]
            ho = [sb("ho%d" % i, [128, D], F32) for i in range(2)]
            ps = self.ps
            w1d, w3d, w2d = P["w1"], P["w3"], P["w2"]

            def load_w(e, slot):
                k.dma(w1b[slot][:], w1d[l, e].rearrange("(c p) f -> p c f", p=128), writes=[w1b[slot]], q="pool")
                k.dma(w3b[slot][:], w3d[l, e].rearrange("(c p) f -> p c f", p=128), writes=[w3b[slot]], q="pool")
                k.dma(w2b[slot][:], w2d[l, e].rearrange("(c p) n -> p c n", p=128), writes=[w2b[slot]], q="pool")

            for g in range(S // G):
                for ti in range(NTG):
                    t = g * NTG + ti
                    b = ti % 2
                    h = ht[b]
                    k.dma(h[:], hin[0][t * 128:(t + 1) * 128, :], reads=[hin[1][t]], writes=[h])
                    DBG = int(os.environ.get("MK_DBG", "9"))
                    if DBG < 2:
                        continue
                    self.rmsnorm(h, gain, xn[b], junk, stt[b])
                    if DBG < 3:
                        continue
                    x32 = xnT32[b]
                    self.transpose8(
                        xn[b],
                        [(x32, lambda half: x32[:, half * 4:(half + 1) * 4, :]),
                         (xnT, lambda half: xnT[:, half * 4:(half + 1) * 4, ti * 128:(ti + 1) * 128])],
                        ps[6], ps[7])
                    if DBG < 4:
                        continue
                    pr = ps[6 + (ti % 2)]
                    for dc in range(8):
                        k.op("pe", lambda e: e.matmul(pr[:, 0:20], x32[:, dc, :], wgr[:, dc, :], start=(dc == 0), stop=(dc == 7)),
                             reads=[x32, wgr], writes=[pr], inc=(dc == 7))
                    if DBG < 5:
                        continue
                    r = rt[b]
                    R = lambda a, n: r[:, a:a + n]
                    lg, gmax, ngmax, oh, ge, gsum, pg = R(0, 20), R(20, 1), R(21, 1), R(22, 4), R(26, 4), R(30, 1), R(31, 1)
                    tmp, elsel, m1, nm1, ee, mask1, ee2 = R(32, 16), R(48, 4), R(52, 1), R(53, 1), R(54, 4), R(58, 4), R(62, 4)
                    v2, mask2, den, rden, wl, scl = R(66, 1), R(67, 4), R(71, 1), R(72, 1), R(73, 4), R(77, 1)
                    dv = lambda fn, rd=(), wr=(): k.op("dve", fn, reads=[r] + list(rd), writes=[r] + list(wr))
                    dv(lambda e: e.tensor_tensor(lg, pr[:, 0:20], rb[:], ALU.add), rd=[pr, rb])
                    dv(lambda e: e.tensor_reduce(gmax, lg[:, 0:4], AX.X, ALU.max))
                    dv(lambda e: e.tensor_single_scalar(ngmax, gmax, -1.0, ALU.mult))
                    dv(lambda e: e.tensor_scalar(oh, lg[:, 0:4], gmax, None, ALU.is_equal))
                    k.op("act", lambda e: e.activation(ge, lg[:, 0:4], AF.Exp, bias=ngmax, accum_out=gsum), reads=[r], writes=[r])
                    dv(lambda e: e.reciprocal(pg, gsum))
                    dv(lambda e: e.tensor_tensor(tmp.rearrange("p (g j) -> p g j", g=4),
                                                 lg[:, 4:20].rearrange("p (g j) -> p g j", g=4),
                                                 oh.unsqueeze(2).to_broadcast([128, 4, 4]), ALU.mult))
                    dv(lambda e: e.tensor_reduce(elsel, tmp.rearrange("p (g j) -> p j g", g=4), AX.X, ALU.add))
                    dv(lambda e: e.tensor_reduce(m1, elsel, AX.X, ALU.max))
                    dv(lambda e: e.tensor_single_scalar(nm1, m1, -1.0, ALU.mult))
                    k.op("act", lambda e: e.activation(ee, elsel, AF.Exp, bias=nm1), reads=[r], writes=[r])
                    dv(lambda e: e.tensor_scalar(mask1, elsel, m1, None, ALU.is_equal))
                    dv(lambda e: e.scalar_tensor_tensor(ee2, mask1, -2.0, ee, ALU.mult, ALU.add))
                    dv(lambda e: e.tensor_reduce(v2, ee2, AX.X, ALU.max))
                    dv(lambda e: e.tensor_scalar(mask2, ee2, v2, None, ALU.is_equal))
                    dv(lambda e: e.tensor_single_scalar(den, v2, 1.0, ALU.add))
                    dv(lambda e: e.reciprocal(rden, den))
                    dv(lambda e: e.scalar_tensor_tensor(wl, mask2, v2, mask1, ALU.mult, ALU.add))
                    dv(lambda e: e.tensor_tensor(scl, pg, rden, ALU.mult))
                    dv(lambda e: e.tensor_scalar(wl, wl, scl, None, ALU.mult))
                    cb = comb[ti]
                    dv(lambda e: e.tensor_tensor(cb[:].rearrange("p (g j) -> p g j", g=4),
                                                 oh.unsqueeze(2).to_broadcast([128, 4, 4]),
                                                 wl.unsqueeze(1).to_broadcast([128, 4, 4]), ALU.mult), wr=[cb])
                NEX = int(os.environ.get("MK_NEX", "16"))
                if NEX:
                    load_w(0, 0)
                for ex in range(NEX):
                    slot = ex % 2
                    if ex + 1 < NEX:
                        load_w(ex + 1, 1 - slot)
                    w1, w3, w2 = w1b[slot], w3b[slot], w2b[slot]
                    for tb in range(G // 512):
                        hd = hid[tb % 2]
                        for fc in range(2):
                            p1 = ps[fc]
                            p3 = ps[2 + fc]
                            for dc in range(8):
                                k.op("pe", lambda e: e.matmul(p1[:], w1[:, dc, fc * 128:(fc + 1) * 128], xnT[:, dc, tb * 512:(tb + 1) * 512],
                                                              start=(dc == 0), stop=(dc == 7)),
                                     reads=[w1, xnT], writes=[p1], inc=(dc == 7))
                            for dc in range(8):
                                k.op("pe", lambda e: e.matmul(p3[:], w3[:, dc, fc * 128:(fc + 1) * 128], xnT[:, dc, tb * 512:(tb + 1) * 512],
                                                              start=(dc == 0), stop=(dc == 7)),
                                     reads=[w3, xnT], writes=[p3], inc=(dc == 7))
                            sl = sil[fc]
                            k.op("act", lambda e: e.activation(sl[:], p1[:], AF.Silu), reads=[p1], writes=[sl])
                            k.op("dve", lambda e: e.tensor_tensor(hd[:, fc, :], sl[:], p3[:], ALU.mult), reads=[sl, p3], writes=[hd])
                        for tt in range(4):
                            ti = tb * 4 + tt
                            for half in range(2):
                                py = ps[4 + half]
                                for fc in range(2):
                                    k.op("pe", lambda e: e.matmul(py[:], hd[:, fc, tt * 128:(tt + 1) * 128], w2[:, fc, half * 512:(half + 1) * 512],
                                                                  start=(fc == 0), stop=(fc == 1)),
                                         reads=[hd, w2], writes=[py], inc=(fc == 1))
                                ya = yacc[ti]
                                cs = comb[ti][:, ex:ex + 1]
                                if ex == 0:
                                    k.op("dve", lambda e: e.tensor_scalar(ya[:, half * 512:(half + 1) * 512], py[:], cs, None, ALU.mult),
                                         reads=[py, comb[ti]], writes=[ya])
                                else:
                                    k.op("dve", lambda e: e.scalar_tensor_tensor(ya[:, half * 512:(half + 1) * 512], py[:], cs,
                                                                                 ya[:, half * 512:(half + 1) * 512], ALU.mult, ALU.add),
                                         reads=[py, comb[ti], ya], writes=[ya])
                for ti in range(NTG):
                    t = g * NTG + ti
                    b = ti % 2
                    h = ht[b]
                    k.dma(h[:], hin[0][t * 128:(t + 1) * 128, :], reads=[hin[1][t]], writes=[h])
                    o = ho[b]
                    k.op("dve", lambda e: e.tensor_tensor(o[:], h[:], yacc[ti][:], ALU.add), reads=[h, yacc[ti]], writes=[o])
                    if final:
                        o2 = xn[b]
                        self.rmsnorm(o, fgain, o2, junk, stt[b])
                        k.dma(hout[0][t * 128:(t + 1) * 128, :], o2[:], reads=[o2], writes=[hout[1][t]])
                    else:
                        k.dma(hout[0][t * 128:(t + 1) * 128, :], o[:], reads=[o], writes=[hout[1][t]])
            k.barrier()

    def nextbank(self):
        self.pbi = (getattr(self, "pbi", -1) + 1) % 8
        return self.ps[self.pbi]

    def mem_kv(self, st, l):
        k, P = self.k, self.P
        sb = lambda n, s, d: self.sbuf(st, n, s, d)
        memkT = sb("memkT", [128, 2, 256], BF16)
        memv = sb("memv", [128, 2, 256], BF16)
        with ExitStack() as st2:
            sb2 = lambda n, s, d: self.sbuf(st2, n, s, d)
            g = sb2("mg", [128, D], F32)
            k.dma(g[:], P["mem_norm"][l].partition_broadcast(128), writes=[g])
            w = sb2("wmkv", [128, 8, 512], BF16)
            k.dma(w[:], P["w_mem_kv"][l].rearrange("(c p) n -> p c n", p=128), writes=[w], q="pool")
            mT = sb2("memnT", [128, 8, 256], BF16)
            junk = sb2("mjunk", [128, D], F32)
            for mt in range(2):
                h = sb2("mh%d" % mt, [128, D], F32)
                xn = sb2("mxn%d" % mt, [128, D], F32)
                stt = sb2("mst%d" % mt, [128, 4], F32)
                k.dma(h[:], P["mem"][mt * 128:(mt + 1) * 128, :], writes=[h])
                self.rmsnorm(h, g, xn, junk, stt)
                self.transpose8(xn, [(mT, lambda half: mT[:, half * 4:(half + 1) * 4, mt * 128:(mt + 1) * 128])],
                                self.nextbank(), self.nextbank())
            for j in range(2):
                pb = self.nextbank()
                for dc in range(8):
                    k.op("pe", lambda e: e.matmul(pb[:, 0:256], w[:, dc, j * 128:(j + 1) * 128], mT[:, dc, :],
                                                  start=(dc == 0), stop=(dc == 7)),
                         reads=[w, mT], writes=[pb], inc=(dc == 7))
                k.op("act", lambda e: e.copy(memkT[:, j, :], pb[:, 0:256]), reads=[pb], writes=[memkT])
            for mt in range(2):
                pb = self.nextbank()
                for dc in range(8):
                    k.op("pe", lambda e: e.matmul(pb[:, 0:256], mT[:, dc, mt * 128:(mt + 1) * 128], w[:, dc, 256:512],
                                                  start=(dc == 0), stop=(dc == 7)),
                         reads=[w, mT], writes=[pb], inc=(dc == 7))
                k.op("dve", lambda e: e.tensor_copy(memv[:, mt, :], pb[:, 0:256]), reads=[pb], writes=[memv])
            k.barrier()
        return memkT, memv

    def mem_attend(self, W, mqT, qoff, memkT, memv, mix, col0=768):
        k = self.k
        pe_ = W["pexp"]; ms = W["mstat"]; pT = W["pT"]
        banks = [self.nextbank(), self.nextbank()]
        for hh in range(4):
            pair, s = hh // 2, hh % 2
            pb = banks[s]
            k.op("pe", lambda e: e.matmul(pb[:, pair * 256:(pair + 1) * 256], mqT[s * 64:(s + 1) * 64, pair, qoff:qoff + 128],
                                          memkT[s * 64:(s + 1) * 64, pair, :], start=True, stop=True),
                 reads=[mqT, memkT], writes=[pb])
        for s in range(2):
            pb = banks[s]
            k.op("dve", lambda e: e.tensor_reduce(ms[:, s:s + 3:2], pb[:].rearrange("p (h m) -> p h m", h=2), AX.X, ALU.max),
                 reads=[pb], writes=[ms])
        k.op("dve", lambda e: e.tensor_single_scalar(ms[:, 4:8], ms[:, 0:4], -0.125, ALU.mult), reads=[ms], writes=[ms])
        for hh in range(4):
            pair, s = hh // 2, hh % 2
            pb = banks[s]
            k.op("act", lambda e: e.activation(pe_[:, hh, :], pb[:, pair * 256:(pair + 1) * 256], AF.Exp, bias=ms[:, 4 + hh:5 + hh],
                                               scale=0.125, accum_out=ms[:, 8 + hh:9 + hh]),
                 reads=[pb, ms], writes=[pe_, ms])
        k.op("dve", lambda e: e.reciprocal(ms[:, 12:16], ms[:, 8:12]), reads=[ms], writes=[ms])
        for half in range(2):
            pb = self.nextbank()
            for j in range(4):
                idx = half * 4 + j
                hh, mc = idx // 2, idx % 2
                k.op("pe", lambda e: e.transpose(pb[:, j * 128:(j + 1) * 128], pe_[:, hh, mc * 128:(mc + 1) * 128], self.ident),
                     reads=[pe_, self.cst], writes=[pb], inc=(j == 3))
            if half == 0:
                k.op("act", lambda e: e.copy(pT[:, 0:4, :], pb[:].rearrange("p (c t) -> p c t", c=4)), reads=[pb], writes=[pT])
            else:
                k.op("dve", lambda e: e.tensor_copy(pT[:, 4:8, :], pb[:].rearrange("p (c t) -> p c t", c=4)), reads=[pb], writes=[pT])
        pb = self.nextbank()
        for hh in range(4):
            for mc in range(2):
                k.op("pe", lambda e: e.matmul(pb[:, hh * 64:(hh + 1) * 64], pT[:, hh * 2 + mc, :], memv[:, mc, hh * 64:(hh + 1) * 64],
                                              start=(mc == 0), stop=(mc == 1)),
                     reads=[pT, memv], writes=[pb], inc=(mc == 1))
        k.op("dve", lambda e: e.tensor_tensor(mix[:, col0:col0 + 256].rearrange("p (h d) -> p h d", h=4),
                                              pb[:, 0:256].rearrange("p (h d) -> p h d", h=4),
                                              ms[:, 12:16].unsqueeze(2).to_broadcast([128, 4, 64]), ALU.mult),
             reads=[pb, ms], writes=[mix])

    def mem_work(self, st):
        sb = lambda n, s, d: self.sbuf(st, n, s, d)
        return {"pexp": sb("pexp", [128, 4, 256], F32), "mstat": sb("mstat", [128, 16], F32),
                "pT": sb("pT", [128, 8, 128], BF16)}

    def out_proj(self, mix, mixT, w_out, h, hn, dst_ap, dst_buf):
        k = self.k
        self.transpose8(mix, [(mixT, lambda half: mixT[:, half * 4:(half + 1) * 4, :])], self.nextbank(), self.nextbank())
        for half in range(2):
            pb = self.nextbank()
            for fc in range(8):
                k.op("pe", lambda e: e.matmul(pb[:], mixT[:, fc, :], w_out[:, fc, half * 512:(half + 1) * 512],
                                              start=(fc == 0), stop=(fc == 7)),
                     reads=[mixT, w_out], writes=[pb], inc=(fc == 7))
            k.op("dve", lambda e: e.tensor_tensor(hn[:, half * 512:(half + 1) * 512], h[:, half * 512:(half + 1) * 512], pb[:], ALU.add),
                 reads=[h, pb], writes=[hn])
        k.dma(dst_ap, hn[:], reads=[hn], writes=[dst_buf])

    def mixer_a_phase(self, hin, hout):
        nc, k, P = self.nc, self.k, self.P
        NTA = int(os.environ.get("MK_NTA", str(NT)))
        with ExitStack() as st:
            sb = lambda n, s, d: self.sbuf(st, n, s, d)
            memkT, memv = self.mem_kv(st, 0)
            gain = sb("gainA", [128, D], F32)
            k.dma(gain[:], P["a_norm"][0].partition_broadcast(128), writes=[gain])
            w_in = sb("w_inA", [128, 8, 3340], BF16)
            for c in range(8):
                k.dma(w_in[:, c, :], P["a_w_in"][0, c * 128:(c + 1) * 128, :], writes=[w_in], q="pool")
            w_out = sb("w_outA", [128, 8, D], BF16)
            k.dma(w_out[:], P["a_w_out"][0].rearrange("(c p) n -> p c n", p=128), writes=[w_out], q="pool")
            convw = sb("convw", [128, 18, 4], F32)
            k.dma(convw[:], P["convw"], writes=[convw])
            sc6 = sb("sc6", [128, 32], F32)
            k.dma(sc6[:, 0:6], P["a_log"][0].partition_broadcast(128), writes=[sc6])
            k.dma(sc6[:, 6:12], P["a_dt_bias"][0].partition_broadcast(128), writes=[sc6])
            k.op("act", lambda e: e.activation(sc6[:, 12:18], sc6[:, 0:6], AF.Exp), reads=[sc6], writes=[sc6])
            k.op("dve", lambda e: e.tensor_single_scalar(sc6[:, 12:18], sc6[:, 12:18], -1.0, ALU.mult), reads=[sc6], writes=[sc6])
            ogain = sb("ogain", [128, 128], F32)
            k.dma(ogain[:], P["a_out_gain"][0].partition_broadcast(128), writes=[ogain])
            MW = self.mem_work(st)
            pc = sb("pc", [128, 18, 131], F32)
            k.op("dve", lambda e: e.memset(pc[:], 0.0), writes=[pc])
            Sf = [sb("Sf%d" % h, [128, 128], F32) for h in range(6)]
            Sb = [sb("Sb%d" % h, [128, 128], BF16) for h in range(6)]
            for h in range(6):
                k.op("dve", lambda e: e.memset(Sf[h][:], 0.0), writes=[Sf[h]])
                k.op("pool", lambda e: e.memset(Sb[h][:], 0.0), writes=[Sb[h]])
            ht = [sb("htA%d" % i, [128, D], F32) for i in range(2)]
            hn = [sb("hnA", [128, D], F32)] * 2
            xn = sb("xnA", [128, D], F32)
            junk = sb("junkA", [128, D], F32)
            stt = [sb("sttA%d" % i, [128, 4], F32) for i in range(2)]
            xnT = [sb("xnTA%d" % i, [128, 8, 128], BF16) for i in range(2)]
            cv = sb("cv", [128, 18, 128], F32)
            ctmp = sb("ctmp", [128, 128], F32)
            qkv = sb("qkv", [128, 18, 128], F32)
            sq = sb("sq", [128, 12, 128], BF16)
            rs = sb("rs", [128, 12, 128], F32)
            qkT = sb("qkT", [128, 6, 2, 128], BF16)
            kn32 = sb("kn32", [128, 6, 128], F32)
            mqT = sb("mqTA", [128, 2, 128], BF16)
            gg = sb("gg", [128, 768], F32)
            sc = [sb("scA%d" % i, [128, 96], F32) for i in range(2)]
            vtok = sb("vtok", [128, 6, 128], F32)
            kt = sb("kt", [128, 6, 128], BF16)
            SLg = [sb("SLg%d" % i, [128, 128], F32) for i in range(2)]
            decT = sb("decT", [128, 6, 128], F32)
            dm = decT
            dmi = sb("dmi", [128, 6, 128], F32)
            attnT = [sb("attnT%d" % h, [128, 128], BF16) for h in range(6)]
            Wq = [[sb("W%d_%d" % (h, i), [128, 3, 128], F32) for i in range(2)] for h in range(6)]
            Rb = [sb("R%d" % i, [128, 128], F32) for i in range(2)]
            vnew = [sb("vnew%d" % i, [128, 128], BF16) for i in range(2)]
            o1 = [sb("o1_%d" % i, [128, 128], F32) for i in range(2)]
            ob = [sb("ob%d" % i, [128, 128], F32) for i in range(2)]
            ost = [sb("ost%d" % i, [128, 4], F32) for i in range(2)]
            mix = sb("mixA", [128, D], F32)
            mixT = sb("mixTA", [128, 8, 128], BF16)
            ident, U, SL, strictT, ones = self.ident, self.U, self.SL, self.strictT, self.ones
            cst = self.cst
            QS = float(128 ** -0.5)

            for t in range(NTA):
                b = t % 2
                h = ht[b]
                k.dma(h[:], hin[0][t * 128:(t + 1) * 128, :], reads=[hin[1][t]], writes=[h])
                self.rmsnorm(h, gain, xn, junk, stt[b])
                xT = xnT[b]
                self.transpose8(xn, [(xT, lambda half: xT[:, half * 4:(half + 1) * 4, :])], self.nextbank(), self.nextbank())
                for g4 in range(5):
                    nf = 4 if g4 < 4 else 2
                    pb = self.nextbank()
                    for j in range(nf):
                        fc = g4 * 4 + j
                        for dc in range(8):
                            k.op("pe", lambda e: e.matmul(pb[:, j * 128:(j + 1) * 128], w_in[:, dc, fc * 128:(fc + 1) * 128], xT[:, dc, :],
                                                          start=(dc == 0), stop=(dc == 7)),
                                 reads=[w_in, xT], writes=[pb], inc=(dc == 7 and j == nf - 1))
                    eng = "act" if g4 % 2 == 0 else "dve"
                    dstv = pc[:, g4 * 4:g4 * 4 + nf, 3:131]
                    srcv = pb[:, 0:nf * 128].rearrange("p (c t) -> p c t", c=nf)
                    if eng == "act":
                        k.op("act", lambda e: e.copy(dstv, srcv), reads=[pb], writes=[pc])
                    else:
                        k.op("dve", lambda e: e.tensor_copy(dstv, srcv), reads=[pb], writes=[pc])
                pb = self.nextbank()
                for j in range(2):
                    for dc in range(8):
                        k.op("pe", lambda e: e.matmul(pb[:, j * 128:(j + 1) * 128], w_in[:, dc, 3084 + j * 128:3084 + (j + 1) * 128], xT[:, dc, :],
                                                      start=(dc == 0), stop=(dc == 7)),
                             reads=[w_in, xT], writes=[pb], inc=(dc == 7 and j == 1))
                k.op("act", lambda e: e.copy(mqT[:], pb[:, 0:256].rearrange("p (c t) -> p c t", c=2)), reads=[pb], writes=[mqT])
                pg1 = self.nextbank()
                for dc in range(8):
                    k.op("pe", lambda e: e.matmul(pg1[:], xT[:, dc, :], w_in[:, dc, 2304:2816], start=(dc == 0), stop=(dc == 7)),
                         reads=[w_in, xT], writes=[pg1], inc=(dc == 7))
                pg2 = self.nextbank()
                for dc in range(8):
                    k.op("pe", lambda e: e.matmul(pg2[:, 0:268], xT[:, dc, :], w_in[:, dc, 2816:3084], start=(dc == 0), stop=(dc == 7)),
                         reads=[w_in, xT], writes=[pg2], inc=(dc == 7))
                k.op("act", lambda e: e.activation(gg[:, 0:512], pg1[:], AF.Silu), reads=[pg1], writes=[gg])
                k.op("act", lambda e: e.activation(gg[:, 512:768], pg2[:, 0:256], AF.Silu), reads=[pg2], writes=[gg])
                s_ = sc[b]
                C = lambda a, n=6: s_[:, a:a + n]
                beta, tt_, ex_, sp_, g_, gcl, egc, negc, etl, egl, dd = (C(0), C(6), C(12), C(18), C(24), C(32, 16), C(48), C(54), C(60), C(66), C(72))
                k.op("act", lambda e: e.activation(beta, pg2[:, 256:262], AF.Sigmoid), reads=[pg2], writes=[s_])
                k.op("dve", lambda e: e.tensor_tensor(tt_, pg2[:, 262:268], sc6[:, 6:12], ALU.add), reads=[pg2, sc6], writes=[s_])
                k.op("act", lambda e: e.activation(ex_, tt_, AF.Exp), reads=[s_], writes=[s_])
                k.op("act", lambda e: e.activation(sp_, ex_, AF.Ln, bias=1.0), reads=[s_], writes=[s_])
                k.op("dve", lambda e: e.tensor_tensor(g_, sp_, sc6[:, 12:18], ALU.mult), reads=[s_, sc6], writes=[s_])
                k.op("pool", lambda e: e.tensor_tensor(gg[:].rearrange("p (h d) -> p h d", h=6), gg[:].rearrange("p (h d) -> p h d", h=6),
                                                       ogain[:].unsqueeze(1).to_broadcast([128, 6, 128]), ALU.mult),
                     reads=[gg, ogain], writes=[gg])
                pgc = self.nextbank()
                k.op("pe", lambda e: e.matmul(pgc[:, 0:6], U, g_, start=True, stop=True), reads=[cst, s_], writes=[pgc])
                k.op("pe", lambda e: e.matmul(pgc[:, 8:14], ones, g_, start=True, stop=True), reads=[cst, s_], writes=[pgc])
                k.op("dve", lambda e: e.tensor_copy(gcl, pgc[:, 0:16]), reads=[pgc], writes=[s_])
                k.op("act", lambda e: e.activation(egc, s_[:, 32:38], AF.Exp), reads=[s_], writes=[s_])
                k.op("dve", lambda e: e.tensor_single_scalar(negc, egc, -1.0, ALU.mult), reads=[s_], writes=[s_])
                k.op("dve", lambda e: e.tensor_tensor(dd, s_[:, 40:46], s_[:, 32:38], ALU.subtract), reads=[s_], writes=[s_])
                k.op("act", lambda e: e.activation(etl, dd, AF.Exp), reads=[s_], writes=[s_])
                k.op("act", lambda e: e.activation(egl, s_[:, 40:46], AF.Exp), reads=[s_], writes=[s_])
                for fc in range(18):
                    o_ = cv[:, fc, :]
                    if fc % 3 != 2:
                        k.op("dve", lambda e: e.tensor_scalar(o_, pc[:, fc, 0:128], convw[:, fc, 0:1], None, ALU.mult),
                             reads=[pc, convw], writes=[cv])
                        for j in range(1, 4):
                            k.op("dve", lambda e: e.scalar_tensor_tensor(o_, pc[:, fc, j:j + 128], convw[:, fc, j:j + 1], o_, ALU.mult, ALU.add),
                                 reads=[pc, convw, cv], writes=[cv])
                    else:
                        k.op("pool", lambda e: e.tensor_scalar(o_, pc[:, fc, 0:128], convw[:, fc, 0:1], None, ALU.mult),
                             reads=[pc, convw], writes=[cv])
                        for j in range(1, 4):
                            k.op("pool", lambda e: e.tensor_scalar(ctmp[:], pc[:, fc, j:j + 128], convw[:, fc, j:j + 1], None, ALU.mult),
                                 reads=[pc, convw], writes=[ctmp])
                            k.op("pool", lambda e: e.tensor_tensor(o_, o_, ctmp[:], ALU.add), reads=[cv, ctmp], writes=[cv])
                k.op("pool", lambda e: e.tensor_copy(pc[:, :, 0:3], pc[:, :, 128:131]), reads=[pc], writes=[pc])
                k.op("act", lambda e: e.activation(qkv[:], cv[:], AF.Silu), reads=[cv], writes=[qkv])
                k.op("act", lambda e: e.activation(sq[:], qkv[:, 0:12, :], AF.Square), reads=[qkv], writes=[sq])
                for g3 in range(3):
                    pb = self.nextbank()
                    k.op("pe", lambda e: e.matmul(pb[:], self.onesb, sq[:, g3 * 4:(g3 + 1) * 4, :], start=True, stop=True),
                         reads=[self.cstb, sq], writes=[pb])
                    k.op("act", lambda e: e.activation(rs[:, g3 * 4:(g3 + 1) * 4, :], pb[:].rearrange("p (c t) -> p c t", c=4), AF.Sqrt,
                                                       bias=self.epsb[:, 0:1]), reads=[pb, self.epsbuf], writes=[rs])
                k.op("dve", lambda e: e.reciprocal(rs[:], rs[:]), reads=[rs], writes=[rs])
                k.op("dve", lambda e: e.scalar_tensor_tensor(qkT[:, :, 1, :], qkv[:, 0:6, :], QS, rs[:, 0:6, :], ALU.mult, ALU.mult),
                     reads=[qkv, rs], writes=[qkT])
                k.op("dve", lambda e: e.tensor_tensor(kn32[:], qkv[:, 6:12, :], rs[:, 6:12, :], ALU.mult), reads=[qkv, rs], writes=[kn32])
                k.op("pool", lambda e: e.tensor_copy(qkT[:, :, 0, :], kn32[:]), reads=[kn32], writes=[qkT])
                for grp in range(3):
                    pb = self.nextbank()
                    for j in range(4):
                        idx = grp * 4 + j
                        src = qkv[:, 12 + idx, :] if idx < 6 else kn32[:, idx - 6, :]
                        srcb = qkv if idx < 6 else kn32
                        k.op("pe", lambda e: e.transpose(pb[:, j * 128:(j + 1) * 128], src, ident), reads=[srcb, cst], writes=[pb], inc=(j == 3))
                    for j in range(4):
                        idx = grp * 4 + j
                        if idx < 6:
                            k.op("act", lambda e: e.copy(vtok[:, idx, :], pb[:, j * 128:(j + 1) * 128]), reads=[pb], writes=[vtok])
                        else:
                            hh = idx - 6
                            k.op("dve", lambda e: e.tensor_scalar(kt[:, hh, :], pb[:, j * 128:(j + 1) * 128], etl[:, hh:hh + 1], None, ALU.mult),
                                 reads=[pb, s_], writes=[kt])
                for hp in range(3):
                    pb = self.nextbank()
                    for j in range(2):
                        hh = hp * 2 + j
                        sg = SLg[hh % 2]
                        k.op("pool", lambda e: e.tensor_scalar(sg[:], SL, g_[:, hh:hh + 1], None, ALU.mult), reads=[cst, s_], writes=[sg])
                        k.op("pe", lambda e: e.matmul(pb[:, j * 128:(j + 1) * 128], sg[:], U, start=True, stop=True),
                             reads=[sg, cst], writes=[pb])
                    k.op("act", lambda e: e.activation(decT[:, hp * 2:hp * 2 + 2, :], pb[:, 0:256].rearrange("p (c t) -> p c t", c=2), AF.Exp),
                         reads=[pb], writes=[decT])
                k.op("pool", lambda e: e.tensor_tensor(dm[:], decT[:], strictT.unsqueeze(1).to_broadcast([128, 6, 128]), ALU.mult),
                     reads=[decT, cst], writes=[dm])
                k.op("pool", lambda e: e.tensor_tensor(dmi[:], dm[:], ident.unsqueeze(1).to_broadcast([128, 6, 128]), ALU.add),
                     reads=[dm, cst], writes=[dmi])
                for hh in range(6):
                    pb = self.nextbank()
                    W0 = Wq[hh][0]
                    k.op("pe", lambda e: e.matmul(pb[:, 0:256], qkT[:, hh, 0, :], qkT[:, hh, :, :].rearrange("p a t -> p (a t)"),
                                                  start=True, stop=True), reads=[qkT], writes=[pb])
                    k.op("dve", lambda e: e.scalar_tensor_tensor(W0[:, 0, :], pb[:, 0:128], beta[:, hh:hh + 1], dm[:, hh, :], ALU.mult, ALU.mult),
                         reads=[pb, s_, dm], writes=[W0])
                    k.op("dve", lambda e: e.tensor_tensor(attnT[hh][:], pb[:, 128:256], dmi[:, hh, :], ALU.mult),
                         reads=[pb, dmi], writes=[attnT[hh]])
                    k.op("pool", lambda e: e.tensor_tensor(W0[:, 1, :], ident, W0[:, 0, :], ALU.subtract), reads=[cst, W0], writes=[W0])
                    pb2 = self.nextbank()
                    k.op("pe", lambda e: e.transpose(pb2[:, 0:128], W0[:, 0, :], ident), reads=[W0, cst], writes=[pb2])
                    k.op("act", lambda e: e.copy(W0[:, 2, :], pb2[:, 0:128]), reads=[pb2], writes=[W0])
                for lvl in range(7):
                    for hh in range(6):
                        Wc = Wq[hh][lvl % 2]
                        Wn = Wq[hh][(lvl + 1) % 2]
                        pb = self.nextbank()
                        Qk, Xk, Pk = Wc[:, 0, :], Wc[:, 1, :], Wc[:, 2, :]
                        mm = lambda out, l_, r_, st_, sp_2, inc_: k.op(
                            "pe", lambda e: e.matmul(out, l_, r_, start=st_, stop=sp_2), reads=[Wc, cst], writes=[pb], inc=inc_)
                        if lvl == 0:
                            mm(pb[:, 0:128], Pk, Qk, True, True, False)
                            mm(pb[:, 256:384], Qk, Pk, True, True, True)
                            k.op("act", lambda e: e.copy(Wn[:, 0, :], pb[:, 0:128]), reads=[pb], writes=[Wn])
                            k.op("act", lambda e: e.copy(Wn[:, 2, :], pb[:, 256:384]), reads=[pb], writes=[Wn])
                            k.op("pool", lambda e: e.tensor_copy(Wn[:, 1, :], Xk), reads=[Wc], writes=[Wn])
                        elif lvl < 6:
                            mm(pb[:, 0:128], Pk, Qk, True, True, False)
                            mm(pb[:, 128:256], Pk, Xk, True, False, False)
                            mm(pb[:, 128:256], ident, Xk, False, True, False)
                            mm(pb[:, 256:384], Qk, Pk, True, True, True)
                            if hh % 2 == 0:
                                k.op("act", lambda e: e.copy(Wn[:], pb[:, 0:384].rearrange("p (c t) -> p c t", c=3)), reads=[pb], writes=[Wn])
                            else:
                                k.op("dve", lambda e: e.tensor_copy(Wn[:], pb[:, 0:384].rearrange("p (c t) -> p c t", c=3)), reads=[pb], writes=[Wn])
                        else:
                            mm(pb[:, 128:256], Pk, Xk, True, False, False)
                            mm(pb[:, 128:256], ident, Xk, False, True, True)
                            k.op("act", lambda e: e.copy(Wn[:, 1, :], pb[:, 128:256]), reads=[pb], writes=[Wn])
                for hh in range(6):
                    TT = Wq[hh][1]
                    r2 = hh % 2
                    pb = self.nextbank()
                    k.op("pe", lambda e: e.matmul(pb[:, 0:128], qkT[:, hh, 0, :], Sb[hh][:], start=True, stop=True), reads=[qkT, Sb[hh]], writes=[pb], inc=False)
                    k.op("pe", lambda e: e.matmul(pb[:, 128:256], qkT[:, hh, 1, :], Sb[hh][:], start=True, stop=True), reads=[qkT, Sb[hh]], writes=[pb])
                    R_ = Rb[r2]
                    k.op("dve", lambda e: e.scalar_tensor_tensor(R_[:], pb[:, 0:128], negc[:, hh:hh + 1], vtok[:, hh, :], ALU.mult, ALU.add),
                         reads=[pb, s_, vtok], writes=[R_])
                    k.op("act", lambda e: e.mul(o1[r2][:], pb[:, 128:256], egc[:, hh:hh + 1]), reads=[pb, s_], writes=[o1[r2]])
                    pb2 = self.nextbank()
                    k.op("pe", lambda e: e.matmul(pb2[:, 0:128], TT[:, 1, :], R_[:], start=True, stop=True), reads=[TT, R_], writes=[pb2])
                    vn = vnew[r2]
                    k.op("dve", lambda e: e.tensor_scalar(vn[:], pb2[:, 0:128], beta[:, hh:hh + 1], None, ALU.mult), reads=[pb2, s_], writes=[vn])
                    pb3 = self.nextbank()
                    k.op("pe", lambda e: e.matmul(pb3[:, 0:128], attnT[hh][:], vn[:], start=True, stop=True), reads=[attnT[hh], vn], writes=[pb3], inc=False)
                    k.op("pe", lambda e: e.matmul(pb3[:, 128:256], kt[:, hh, :], vn[:], start=True, stop=True), reads=[kt, vn], writes=[pb3])
                    o_ = ob[r2]
                    k.op("dve", lambda e: e.tensor_tensor(o_[:], o1[r2][:], pb3[:, 0:128], ALU.add), reads=[o1[r2], pb3], writes=[o_])
                    k.op("dve", lambda e: e.scalar_tensor_tensor(Sf[hh][:], Sf[hh][:], egl[:, hh:hh + 1], pb3[:, 128:256], ALU.mult, ALU.add),
                         reads=[Sf[hh], s_, pb3], writes=[Sf[hh]])
                    k.op("pool", lambda e: e.tensor_copy(Sb[hh][:], Sf[hh][:]), reads=[Sf[hh]], writes=[Sb[hh]])
                    os_ = ost[r2]
                    k.op("act", lambda e: e.activation(junk[:, 0:128], o_[:], AF.Square, accum_out=os_[:, 0:1]), reads=[o_], writes=[junk, os_])
                    k.op("act", lambda e: e.activation(os_[:, 1:2], os_[:, 0:1], AF.Sqrt, bias=self.epsb[:, 0:1], scale=1.0 / 128),
                         reads=[os_, self.epsbuf], writes=[os_])
                    k.op("dve", lambda e: e.reciprocal(os_[:, 2:3], os_[:, 1:2]), reads=[os_], writes=[os_])
                    k.op("dve", lambda e: e.scalar_tensor_tensor(mix[:, hh * 128:(hh + 1) * 128], o_[:], os_[:, 2:3], gg[:, hh * 128:(hh + 1) * 128],
                                                                 ALU.mult, ALU.mult), reads=[o_, os_, gg], writes=[mix])
                self.mem_attend(MW, mqT, 0, memkT, memv, mix)
                self.out_proj(mix, mixT, w_out, h, hn[b], hout[0][t * 128:(t + 1) * 128, :], hout[1][t])
            k.barrier()

    def mixer_b_phase(self, hin, hout):
        nc, k, P = self.nc, self.k, self.P
        NGB = int(os.environ.get("MK_NGB", "8"))
        NHB = int(os.environ.get("MK_NHB", "12"))
        NDUM = int(os.environ.get("MK_NDUM", "0"))
        with ExitStack() as st:
            sb = lambda n, s, d: self.sbuf(st, n, s, d)
            memkT, memv = self.mem_kv(st, 1)
            win_d = self.dscratch("b_w_in_bf", [D, D], BF16)
            wout_d = self.dscratch("b_w_out_bf", [D, D], BF16)
            wdb = [Buf(None, "win_d"), Buf(None, "wout_d")]
            k.dma(win_d, P["b_w_in"][0], writes=[wdb[0]], q="pool")
            k.dma(wout_d, P["b_w_out"][0], writes=[wdb[1]], q="pool")
            KT = sb("KT", [128, 6, S], BF16)
            Vt = sb("Vt", [128, NT, 768], BF16)
            ht = [sb("htB%d" % i, [128, D], F32) for i in range(2)]
            xn = sb("xnB", [128, D], F32)
            stt = [sb("sttB%d" % i, [128, 4], F32) for i in range(2)]
            with ExitStack() as st1:
                sb1 = lambda n, s, d: self.sbuf(st1, n, s, d)
                kvg = sb1("kvg", [128, D], F32)
                k.dma(kvg[:], P["kv_norm"].partition_broadcast(128), writes=[kvg])
                w_kv = sb1("w_kv", [128, 8, 1536], BF16)
                for c in range(8):
                    k.dma(w_kv[:, c, :], P["w_kv"][c * 128:(c + 1) * 128, :], writes=[w_kv], q="pool")
                xT1 = [sb1("xT1_%d" % i, [128, 8, 128], BF16) for i in range(2)]
                for t in range(NT):
                    b = t % 2
                    h = ht[b]
                    k.dma(h[:], hin[0][t * 128:(t + 1) * 128, :], reads=[hin[1][t]], writes=[h])
                    self.rmsnorm(h, kvg, xn, xn, stt[b])
                    xT = xT1[b]
                    self.transpose8(xn, [(xT, lambda half: xT[:, half * 4:(half + 1) * 4, :])], self.nextbank(), self.nextbank())
                    for g4 in range(2):
                        nf = 4 if g4 == 0 else 2
                        pb = self.nextbank()
                        for j in range(nf):
                            fc = g4 * 4 + j
                            for dc in range(8):
                                k.op("pe", lambda e: e.matmul(pb[:, j * 128:(j + 1) * 128], w_kv[:, dc, fc * 128:(fc + 1) * 128], xT[:, dc, :],
                                                              start=(dc == 0), stop=(dc == 7)),
                                     reads=[w_kv, xT], writes=[pb], inc=(dc == 7 and j == nf - 1))
                        dstv = KT[:, g4 * 4:g4 * 4 + nf, t * 128:(t + 1) * 128]
                        srcv = pb[:, 0:nf * 128].rearrange("p (c t) -> p c t", c=nf)
                        k.op("act", lambda e: e.copy(dstv, srcv), reads=[pb], writes=[KT])
                    for half, (c0, c1) in enumerate(((0, 512), (512, 768))):
                        pb = self.nextbank()
                        for dc in range(8):
                            k.op("pe", lambda e: e.matmul(pb[:, 0:c1 - c0], xT[:, dc, :], w_kv[:, dc, 768 + c0:768 + c1],
                                                          start=(dc == 0), stop=(dc == 7)),
                                 reads=[w_kv, xT], writes=[pb], inc=(dc == 7))
                        k.op("dve", lambda e: e.tensor_copy(Vt[:, t, c0:c1], pb[:, 0:c1 - c0]), reads=[pb], writes=[Vt])
                k.barrier()
            bg = sb("bgain", [128, D], F32)
            k.dma(bg[:], P["b_norm"][0].partition_broadcast(128), writes=[bg])
            wB = sb("wB", [128, 8, D], BF16)
            xTg = sb("xTg", [128, 8, 512], BF16)
            qT = sb("qT", [128, 6, 512], BF16)
            mqT = sb("mqTB", [128, 2, 512], BF16)
            mixTg = sb("mixTg", [128, 8, 512], BF16)
            Eb = [sb("Eb%d" % i, [128, 512], F32) for i in range(3)]
            spb = [sb("spb%d" % i, [128, 512], BF16) for i in range(3)]
            eab = [sb("eab%d" % i, [128, 512], F32) for i in range(2)]
            ab = [sb("ab%d" % i, [128, 512], BF16) for i in range(2)]
            MW = self.mem_work(st)
            mmB = sb("mmB", [128, 256], F32)
            hn = sb("hnB", [128, D], F32)
            NGEb = self.cstb[:, 5, :]
            NLTb = self.cstb[:, 6, :]
            strictTb = self.cstb[:, 3, :]
            cstb = self.cstb
            PZ = [self.ps[0], self.ps[1]]
            PC = [self.ps[2], self.ps[3]]
            PO = [self.ps[4], self.ps[5]]
            rot = [0]

            def nb():
                rot[0] = (rot[0] + 1) % 8
                return self.ps[rot[0]]
            self.nextbank = nb
            for g in range(NGB):
                k.dma(wB[:], win_d.rearrange("(c p) n -> p c n", p=128), reads=[wdb[0]], writes=[wB])
                for tt in range(4):
                    t = g * 4 + tt
                    b = t % 2
                    h = ht[b]
                    k.dma(h[:], hin[0][t * 128:(t + 1) * 128, :], reads=[hin[1][t]], writes=[h])
                    self.rmsnorm(h, bg, xn, xn, stt[b])
                    self.transpose8(xn, [(xTg, lambda half: xTg[:, half * 4:(half + 1) * 4, tt * 128:(tt + 1) * 128])], nb(), nb())
                for fc in range(8):
                    pb = nb()
                    for dc in range(8):
                        k.op("pe", lambda e: e.matmul(pb[:], wB[:, dc, fc * 128:(fc + 1) * 128], xTg[:, dc, :], start=(dc == 0), stop=(dc == 7)),
                             reads=[wB, xTg], writes=[pb], inc=(dc == 7))
                    if fc < 6:
                        k.op("act", lambda e: e.mul(qT[:, fc, :], pb[:], 0.125), reads=[pb], writes=[qT])
                    else:
                        k.op("dve", lambda e: e.tensor_copy(mqT[:, fc - 6, :], pb[:]), reads=[pb], writes=[mqT])
                items = [(2 * p + s, kb) for p in range(NHB // 2) for kb in range(4 * g + 3, -1, -1) for s in range(2)]

                def geom(i):
                    hh, kb = items[i]
                    r = max(kb - 4 * g, 0)
                    return hh, kb, hh // 2, hh % 2, r * 128, kb >= 4 * g

                def s1_pe(i):
                    hh, kb, fc, s, c0, diag = geom(i)
                    ps_ = slice(s * 64, (s + 1) * 64)
                    cs = slice(c0, 512)
                    pz = PZ[i % 2]
                    k.op("pe", lambda e: e.matmul(pz[:, cs], KT[ps_, fc, kb * 128:(kb + 1) * 128], qT[ps_, fc, cs], start=True, stop=True),
                         reads=[KT, qT], writes=[pz])

                def s1_act(i):
                    hh, kb, fc, s, c0, diag = geom(i)
                    cs = slice(c0, 512)
                    pz, E, sp = PZ[i % 2], Eb[i % 3], spb[i % 3]
                    k.op("act", lambda e: e.activation(E[:, cs], pz[:, cs], AF.Exp), reads=[pz], writes=[E])
                    k.op("act", lambda e: e.activation(sp[:, cs], E[:, cs], AF.Ln, bias=1.0), reads=[E], writes=[sp])
                    if diag:
                        k.op("dve", lambda e: e.tensor_tensor(sp[:, c0:c0 + 128], sp[:, c0:c0 + 128], strictTb, ALU.mult),
                             reads=[sp, cstb], writes=[sp])

                def s2_peA(i):
                    hh, kb, fc, s, c0, diag = geom(i)
                    cs = slice(c0, 512)
                    C, sp = PC[s], spb[i % 3]
                    if kb == 4 * g + 3:
                        k.op("dve", lambda e: e.memset(C[:], 0.0), writes=[C])
                    k.op("pe", lambda e: e.matmul(C[:, cs], NGEb, sp[:, cs], start=False, stop=False, skip_group_check=True),
                         reads=[cstb, sp], writes=[C])

                def s2_act(i):
                    hh, kb, fc, s, c0, diag = geom(i)
                    cs = slice(c0, 512)
                    C, ea_ = PC[s], eab[i % 2]
                    k.op("act", lambda e: e.activation(ea_[:, cs], C[:, cs], AF.Exp), reads=[C], writes=[ea_])

                def s2_peB(i):
                    hh, kb, fc, s, c0, diag = geom(i)
                    cs = slice(c0, 512)
                    C, sp = PC[s], spb[i % 3]
                    if kb > 0:
                        k.op("pe", lambda e: e.matmul(C[:, cs], NLTb, sp[:, cs], start=False, stop=False, skip_group_check=True),
                             reads=[cstb, sp], writes=[C])

                def s3_pool(i):
                    hh, kb, fc, s, c0, diag = geom(i)
                    cs = slice(c0, 512)
                    E, ea_, a_ = Eb[i % 3], eab[i % 2], ab[i % 2]
                    k.op("pool", lambda e: e.tensor_tensor(a_[:, cs], E[:, cs], ea_[:, cs], ALU.mult), reads=[E, ea_], writes=[a_])
                    if diag:
                        k.op("dve", lambda e: e.tensor_tensor(a_[:, c0:c0 + 128], a_[:, c0:c0 + 128], strictTb, ALU.mult),
                             reads=[a_, cstb], writes=[a_])

                def s3_pe(i):
                    hh, kb, fc, s, c0, diag = geom(i)
                    cs = slice(c0, 512)
                    a_ = ab[i % 2]
                    po = PO[fc % 2]
                    if kb == 4 * g + 3 and s == 0:
                        k.op("dve", lambda e: e.memset(po[:], 0.0), writes=[po])
                    vblk = Vt[:, kb, hh * 64:(hh + 1) * 64]
                    if s == 0:
                        k.op("pe", lambda e: e.matmul(po[0:64, cs], vblk, a_[:, cs], start=False, stop=False, skip_group_check=True),
                             reads=[Vt, a_], writes=[po])
                    else:
                        k.op("pe", lambda e: e.matmul(po[64:128, cs], vblk, a_[:, cs], start=False, stop=False, skip_group_check=True,
                                                      tile_position=(0, 64)), reads=[Vt, a_], writes=[po])
                    if kb == 0 and s == 1:
                        k.op("act", lambda e: e.copy(mixTg[:, fc, :], po[:]), reads=[po], writes=[mixTg])

                n_it = len(items)
                for step in range(-2, n_it):
                    i1, i2_, i3 = step + 2, step + 1, step
                    if 0 <= i3:
                        s3_pool(i3)
                    if i1 < n_it:
                        s1_pe(i1)
                    if 0 <= i2_ < n_it:
                        s2_peA(i2_)
                    if i1 < n_it:
                        s1_act(i1)
                    if 0 <= i2_ < n_it:
                        s2_act(i2_)
                    if 0 <= i3:
                        s2_peB(i3)
                        s3_pe(i3)
                    for _d in range(NDUM):
                        k.op("pe", lambda e: e.matmul(self.ps[6][:], NGEb, spb[0][:], start=True, stop=True), reads=[], writes=[], inc=False)
                k.dma(wB[:], wout_d.rearrange("(c p) n -> p c n", p=128), reads=[wdb[1]], writes=[wB])
                for tt in range(4):
                    t = g * 4 + tt
                    b = t % 2
                    h = ht[b]
                    k.dma(h[:], hin[0][t * 128:(t + 1) * 128, :], reads=[hin[1][t]], writes=[h])
                    self.mem_attend(MW, mqT, tt * 128, memkT, memv, mmB, col0=0)
                    pb = nb()
                    for j in range(2):
                        k.op("pe", lambda e: e.transpose(pb[:, j * 128:(j + 1) * 128], mmB[:, j * 128:(j + 1) * 128], self.ident),
                             reads=[mmB, self.cst], writes=[pb], inc=(j == 1))
                    k.op("act", lambda e: e.copy(mixTg[:, 6:8, tt * 128:(tt + 1) * 128], pb[:, 0:256].rearrange("p (c t) -> p c t", c=2)),
                         reads=[pb], writes=[mixTg])
                    for half in range(2):
                        pb = nb()
                        for fc in range(8):
                            k.op("pe", lambda e: e.matmul(pb[:], mixTg[:, fc, tt * 128:(tt + 1) * 128], wB[:, fc, half * 512:(half + 1) * 512],
                                                          start=(fc == 0), stop=(fc == 7)),
                                 reads=[mixTg, wB], writes=[pb], inc=(fc == 7))
                        k.op("dve", lambda e: e.tensor_tensor(hn[:, half * 512:(half + 1) * 512], h[:, half * 512:(half + 1) * 512], pb[:], ALU.add),
                             reads=[h, pb], writes=[hn])
                    k.dma(hout[0][t * 128:(t + 1) * 128, :], hn[:], reads=[hn], writes=[hout[1][t]])
            k.barrier()
            del self.nextbank

    def build(self, phases):
        nc, k = self.nc, self.k
        P = self.P = {}
        shapes = dict(
            x=[S, D], mem=[256, D], a_norm=[1, D], a_w_in=[1, D, 3340], a_conv=[1, 4, 2304],
            a_log=[1, 6], a_dt_bias=[1, 6], a_out_gain=[1, 128], a_w_out=[1, D, D],
            kv_norm=[D], w_kv=[D, 1536], b_norm=[1, D], b_w_in=[1, D, D], b_w_out=[1, D, D],
            mem_norm=[2, D], w_mem_kv=[2, D, 512], ffn_norm=[2, D], w_group=[2, D, 4], b_group=[2, 4],
            w_router=[2, D, 16], b_router=[2, 16], w1=[2, 16, D, 256], w3=[2, 16, D, 256],
            w2=[2, 16, 256, D], final_norm=[D], wgr=[2, 128, 8, 20], rbias=[2, 20], convw=[128, 18, 4])
        for n, s in shapes.items():
            P[n] = self.din(n, s)
        out = nc.dram_tensor("out", [S, D], F32, kind="ExternalOutput").ap()
        mkbufs = lambda nm: [Buf(None, "%s%d" % (nm, i)) for i in range(NT)]
        hx = (P["x"], mkbufs("x"))
        hA = (self.dscratch("hA", [S, D]), mkbufs("hA"))
        hB = (self.dscratch("hB", [S, D]), mkbufs("hB"))
        ho = (out, mkbufs("out"))
        with ExitStack() as gst:
            self.load_consts(gst)
            self.epsbuf = self.sbuf(gst, "epsb", [128, 1], F32)
            self.epsb = self.epsbuf
            k.op("dve", lambda e: e.memset(self.epsbuf[:], EPS), writes=[self.epsbuf])
            cur = hx
            seq = {"A": hA, "M0": hB, "B": hA, "M1": ho}
            for ph in phases:
                dst = seq[ph] if ph != phases[-1] else ho
                if ph == "M0":
                    self.moe_phase(0, cur, dst, final=False)
                elif ph == "M1":
                    self.moe_phase(1, cur, dst, final=True)
                elif ph == "A":
                    self.mixer_a_phase(cur, dst)
                elif ph == "B":
                    self.mixer_b_phase(cur, dst)
                cur = dst
            for b in ho[1]:
                if b.lw is not None:
                    k._wait("sp", b.lw)
        return nc


def make_consts():
    c = np.zeros((128, 7, 128), np.float32)
    i = np.arange(128)
    c[:, 0, :] = np.eye(128)
    c[:, 1, :] = (i[:, None] <= i[None, :])
    c[:, 2, :] = (i[:, None] > i[None, :])
    c[:, 3, :] = (i[:, None] < i[None, :])
    c[:, 4, :] = 1.0
    c[:, 5, :] = -(i[:, None] >= i[None, :]).astype(np.float32)
    c[:, 6, :] = -(i[:, None] < i[None, :]).astype(np.float32)
    return c


_CACHE = {}


def run(inputs, phases=("A", "M0", "B", "M1"), ncores=NCORES, trace=False):
    key = tuple(phases)
    if key not in _CACHE:
        mk = MK(phases)
        _CACHE[key] = mk.build(list(phases))
    nc = _CACHE[key]
    consts = make_consts()
    inputs = dict(inputs)
    wg = np.concatenate([np.asarray(inputs["w_group"]), np.asarray(inputs["w_router"])], axis=2)
    inputs["wgr"] = np.ascontiguousarray(wg.reshape(2, 8, 128, 20).transpose(0, 2, 1, 3))
    inputs["rbias"] = np.concatenate([np.asarray(inputs["b_group"]), np.asarray(inputs["b_router"])], axis=1)
    cw = np.asarray(inputs["a_conv"])[0]
    inputs["convw"] = np.ascontiguousarray(cw.reshape(4, 18, 128).transpose(2, 1, 0))
    in_maps = []
    for c in range(ncores):
        m = {"consts": consts}
        for n, v in inputs.items():
            v = np.asarray(v)
            if n in ("x", "mem"):
                m[n] = np.ascontiguousarray(v[c])
            else:
                m[n] = np.ascontiguousarray(v, dtype=np.float32)
        in_maps.append(m)
    res = run_bass_kernel_spmd(nc, in_maps, core_ids=list(range(ncores)), trace=trace)
    outs = np.stack([r["out"] for r in res.results], axis=0)
    return outs, res


def kernel(**inputs):
    outs, _ = run(inputs)
    return outs.astype(np.float32)
```

```python
from contextlib import ExitStack
import os
import numpy as np
import concourse.bass as bass
import concourse.mybir as mybir
from concourse.bass_utils import run_bass_kernel_spmd

F32 = mybir.dt.float32
BF16 = mybir.dt.bfloat16
AF = mybir.ActivationFunctionType
ALU = mybir.AluOpType
AX = mybir.AxisListType

S = 4096
D = 1024
NT = S // 128
EPS = 1e-6
NCORES = 8


class Buf:
    __slots__ = ("ap", "name", "_lw", "_rd", "_excl")
    lw = property(lambda self: self._lw, lambda self, v: setattr(self, "_lw", v))
    rd = property(lambda self: self._rd, lambda self, v: setattr(self, "_rd", v))
    excl = property(lambda self: self._excl, lambda self, v: setattr(self, "_excl", v))

    def __init__(self, ap, name="", excl=False):
        self.ap = ap
        self.name = name
        self.excl = excl
        self.lw = None
        self.rd = []

    def __getitem__(self, idx):
        return self.ap[idx]


class View(Buf):
    __slots__ = ("parent",)

    def __init__(self, parent, ap):
        self.parent = parent
        self.ap = ap
        self.name = parent.name

    lw = property(lambda self: self.parent.lw, lambda self, v: setattr(self.parent, "lw", v))
    rd = property(lambda self: self.parent.rd, lambda self, v: setattr(self.parent, "rd", v))
    excl = property(lambda self: self.parent.excl, lambda self, v: None)


class K:
    NDMA = 48

    def __init__(self, nc):
        self.nc = nc
        self.eng = {"pe": nc.tensor, "act": nc.scalar, "dve": nc.vector,
                    "pool": nc.gpsimd, "sp": nc.sync}
        self.sem = {e: nc.alloc_semaphore("s_" + e) for e in ("pe", "act", "dve", "pool")}
        self.cnt = {e: 0 for e in self.sem}
        self.waited = {}
        self.dsem = [nc.alloc_semaphore("d%d" % i) for i in range(self.NDMA)]
        self.dcnt = [0] * self.NDMA
        self.dnext = 0
        self.nins = 0

    def _semh(self, key):
        return self.sem[key] if isinstance(key, str) else self.dsem[key]

    def _wait(self, e, dep):
        key, val = dep
        if key == e and e == "pe":
            return
        w = self.waited.get((e, key), 0)
        if w >= val:
            return
        self.eng[e].wait_ge(self._semh(key), val)
        self.nins += 1
        self.waited[(e, key)] = val

    def _deps(self, e, reads, writes):
        best = {}
        for r in reads:
            if r.lw is not None:
                if best.get(r.lw[0], 0) < r.lw[1]:
                    best[r.lw[0]] = r.lw[1]
            if r.excl:
                for key, val in r.rd:
                    if key != e and best.get(key, 0) < val:
                        best[key] = val
        for w in writes:
            if w.lw is not None:
                if best.get(w.lw[0], 0) < w.lw[1]:
                    best[w.lw[0]] = w.lw[1]
            for key, val in w.rd:
                if best.get(key, 0) < val:
                    best[key] = val
        for key, val in best.items():
            self._wait(e, (key, val))

    def _mark(self, tag, reads, writes):
        for r in reads:
            r.rd.append(tag)
            if len(r.rd) > 64:
                best = {}
                for key, val in r.rd:
                    if best.get(key, 0) < val:
                        best[key] = val
                r.rd = list(best.items())
        for w in writes:
            w.lw = tag
            w.rd = []

    def op(self, e, fn, reads=(), writes=(), inc=True):
        self._deps(e, reads, writes)
        ins = fn(self.eng[e])
        self.nins += 1
        if inc:
            ins.then_inc(self.sem[e], 1)
            self.cnt[e] += 1
            tag = (e, self.cnt[e])
        else:
            tag = (e, self.cnt[e] + 1)
        self._mark(tag, reads, writes)
        return ins

    def dma(self, out, in_, reads=(), writes=(), q="sp", **kw):
        slot = self.dnext
        self.dnext = (self.dnext + 1) % self.NDMA
        if self.dcnt[slot] > 0:
            self._wait(q, (slot, 16 * self.dcnt[slot]))
        self._deps(q, reads, writes)
        ins = self.eng[q].dma_start(out=out, in_=in_, **kw)
        self.nins += 1
        ins.then_inc(self.dsem[slot], 16)
        self.dcnt[slot] += 1
        tag = (slot, 16 * self.dcnt[slot])
        self._mark(tag, reads, writes)
        return tag

    def barrier(self):
        for e in ("pe", "act", "dve", "pool", "sp"):
            for e2 in ("pe", "act", "dve", "pool"):
                if e2 != e and self.cnt[e2] > 0:
                    self._wait(e, (e2, self.cnt[e2]))
            for slot in range(self.NDMA):
                if self.dcnt[slot] > 0:
                    self._wait(e, (slot, 16 * self.dcnt[slot]))


class MK:
    def __init__(self, phases, h0_from_input=True):
        self.nc = nc = bass.Bass("TRN2", target_bir_lowering=False)
        self.k = K(nc)
        self.uid = 0
        self.ins = {}
        self.ps = [Buf(nc.alloc_psum_tensor("psb%d" % i, [128, 512], F32).ap(), "ps%d" % i, excl=True)
                   for i in range(8)]

    def din(self, name, shape):
        ap = self.nc.dram_tensor(name, list(shape), F32, kind="ExternalInput").ap()
        self.ins[name] = ap
        return ap

    def dscratch(self, name, shape, dt=F32):
        return self.nc.dram_tensor(name, list(shape), dt, kind="Internal").ap()

    def sbuf(self, st, name, shape, dt):
        self.uid += 1
        h = st.enter_context(self.nc.sbuf_tensor("%s_%d" % (name, self.uid), list(shape), dt))
        return Buf(h.ap(), name)

    def load_consts(self, st):
        k = self.k
        c = self.din("consts", [128, 8, 128])
        self.cst = self.sbuf(st, "cst", [128, 8, 128], F32)
        k.dma(self.cst[:], c, writes=[self.cst])
        self.ident = self.cst[:, 0, :]
        self.U = self.cst[:, 1, :]
        self.SL = self.cst[:, 2, :]
        self.strictT = self.cst[:, 3, :]
        self.ones = self.cst[:, 4, :]
        self.cstb = self.sbuf(st, "cstb", [128, 8, 128], BF16)
        k.dma(self.cstb[:], c, writes=[self.cstb], q="pool")
        self.identb = self.cstb[:, 0, :]
        self.onesb = self.cstb[:, 4, :]

    def rmsnorm(self, h, gainb, xn, junk, st2):
        k = self.k
        k.op("act", lambda e: e.activation(junk[:], h[:], AF.Square, accum_out=st2[:, 0:1]),
             reads=[h], writes=[junk, st2])
        k.op("act", lambda e: e.activation(st2[:, 1:2], st2[:, 0:1], AF.Sqrt, bias=self.epsb[:, 0:1], scale=1.0 / D),
             reads=[st2, self.epsbuf], writes=[st2])
        k.op("dve", lambda e: e.reciprocal(st2[:, 2:3], st2[:, 1:2]), reads=[st2], writes=[st2])
        k.op("dve", lambda e: e.scalar_tensor_tensor(xn[:], h[:], st2[:, 2:3], gainb[:], ALU.mult, ALU.mult),
             reads=[h, st2, gainb], writes=[xn])

    def transpose8(self, src, dsts, psa, psb, evac=("act", "dve"), second="pool"):
        k = self.k
        for half, ps in enumerate((psa, psb)):
            for j in range(4):
                c = half * 4 + j
                k.op("pe", lambda e: e.transpose(ps[:, j * 128:(j + 1) * 128], src[:, c * 128:(c + 1) * 128], self.ident),
                     reads=[src, self.cst], writes=[ps], inc=(j == 3))
            dbuf, fn = dsts[0]
            eng = evac[half % len(evac)]
            pv = ps[:].rearrange("p (c t) -> p c t", c=4)
            if eng == "act":
                k.op("act", lambda e: e.copy(fn(half), pv), reads=[ps], writes=[dbuf])
            else:
                k.op(eng, lambda e: e.tensor_copy(fn(half), pv), reads=[ps], writes=[dbuf])
            for dbuf2, fn2 in dsts[1:]:
                k.op(second, lambda e: e.tensor_copy(fn2(half), fn(half)), reads=[dbuf], writes=[dbuf2])

    def moe_phase(self, l, hin, hout, final=False, out_ap=None):
        nc, k = self.nc, self.k
        G = 2048
        NTG = G // 128
        P = self.P
        with ExitStack() as st:
            sb = lambda n, s, d: self.sbuf(st, n, s, d)
            gain = sb("gain", [128, D], F32)
            k.dma(gain[:], P["ffn_norm"][l].partition_broadcast(128), writes=[gain])
            if final:
                fgain = sb("fgain", [128, D], F32)
                k.dma(fgain[:], P["final_norm"].partition_broadcast(128), writes=[fgain])
            wgr = sb("wgr", [128, 8, 20], F32)
            k.dma(wgr[:], P["wgr"][l], writes=[wgr])
            rb = sb("rbias", [128, 20], F32)
            k.dma(rb[:], P["rbias"][l].partition_broadcast(128), writes=[rb])
            xnT = sb("xnT", [128, 8, G], BF16)
            yacc = [sb("yacc%d" % i, [128, D], F32) for i in range(NTG)]
            comb = [sb("comb%d" % i, [128, 16], F32) for i in range(NTG)]
            w1b = [sb("w1b%d" % i, [128, 8, 256], BF16) for i in range(2)]
            w3b = [sb("w3b%d" % i, [128, 8, 256], BF16) for i in range(2)]
            w2b = [sb("w2b%d" % i, [128, 2, D], BF16) for i in range(2)]
            ht = [sb("ht%d" % i, [128, D], F32) for i in range(2)]
            xn = [sb("xn%d" % i, [128, D], F32) for i in range(2)]
            junk = sb("junk", [128, D], F32)
            xnT32 = [sb("xnT32_%d" % i, [128, 8, 128], F32) for i in range(2)]
            stt = [sb("stt%d" % i, [128, 4], F32) for i in range(2)]
            rt = [sb("rt%d" % i, [128, 96], F32) for i in range(2)]
            hid = [sb("hid%d" % i, [128, 2, 512], BF16) for i in range(2)]
            sil = [sb("sil%d" % i, [128, 512], F32) for i in range(2)]
            ho = [sb("ho%d" % i, [128, D], F32) for i in range(2)]
            ps = self.ps
            w1d, w3d, w2d = P["w1"], P["w3"], P["w2"]

            def load_w(e, slot):
                k.dma(w1b[slot][:], w1d[l, e].rearrange("(c p) f -> p c f", p=128), writes=[w1b[slot]], q="pool")
                k.dma(w3b[slot][:], w3d[l, e].rearrange("(c p) f -> p c f", p=128), writes=[w3b[slot]], q="pool")
                k.dma(w2b[slot][:], w2d[l, e].rearrange("(c p) n -> p c n", p=128), writes=[w2b[slot]], q="pool")

            for g in range(S // G):
                for ti in range(NTG):
                    t = g * NTG + ti
                    b = ti % 2
                    h = ht[b]
                    k.dma(h[:], hin[0][t * 128:(t + 1) * 128, :], reads=[hin[1][t]], writes=[h])
                    DBG = int(os.environ.get("MK_DBG", "9"))
                    if DBG < 2:
                        continue
                    self.rmsnorm(h, gain, xn[b], junk, stt[b])
                    if DBG < 3:
                        continue
                    x32 = xnT32[b]
                    self.transpose8(
                        xn[b],
                        [(x32, lambda half: x32[:, half * 4:(half + 1) * 4, :]),
                         (xnT, lambda half: xnT[:, half * 4:(half + 1) * 4, ti * 128:(ti + 1) * 128])],
                        ps[6], ps[7])
                    if DBG < 4:
                        continue
                    pr = ps[6 + (ti % 2)]
                    for dc in range(8):
                        k.op("pe", lambda e: e.matmul(pr[:, 0:20], x32[:, dc, :], wgr[:, dc, :], start=(dc == 0), stop=(dc == 7)),
                             reads=[x32, wgr], writes=[pr], inc=(dc == 7))
                    if DBG < 5:
                        continue
                    r = rt[b]
                    R = lambda a, n: r[:, a:a + n]
                    lg, gmax, ngmax, oh, ge, gsum, pg = R(0, 20), R(20, 1), R(21, 1), R(22, 4), R(26, 4), R(30, 1), R(31, 1)
                    tmp, elsel, m1, nm1, ee, mask1, ee2 = R(32, 16), R(48, 4), R(52, 1), R(53, 1), R(54, 4), R(58, 4), R(62, 4)
                    v2, mask2, den, rden, wl, scl = R(66, 1), R(67, 4), R(71, 1), R(72, 1), R(73, 4), R(77, 1)
                    dv = lambda fn, rd=(), wr=(): k.op("dve", fn, reads=[r] + list(rd), writes=[r] + list(wr))
                    dv(lambda e: e.tensor_tensor(lg, pr[:, 0:20], rb[:], ALU.add), rd=[pr, rb])
                    dv(lambda e: e.tensor_reduce(gmax, lg[:, 0:4], AX.X, ALU.max))
                    dv(lambda e: e.tensor_single_scalar(ngmax, gmax, -1.0, ALU.mult))
                    dv(lambda e: e.tensor_scalar(oh, lg[:, 0:4], gmax, None, ALU.is_equal))
                    k.op("act", lambda e: e.activation(ge, lg[:, 0:4], AF.Exp, bias=ngmax, accum_out=gsum), reads=[r], writes=[r])
                    dv(lambda e: e.reciprocal(pg, gsum))
                    dv(lambda e: e.tensor_tensor(tmp.rearrange("p (g j) -> p g j", g=4),
                                                 lg[:, 4:20].rearrange("p (g j) -> p g j", g=4),
                                                 oh.unsqueeze(2).to_broadcast([128, 4, 4]), ALU.mult))
                    dv(lambda e: e.tensor_reduce(elsel, tmp.rearrange("p (g j) -> p j g", g=4), AX.X, ALU.add))
                    dv(lambda e: e.tensor_reduce(m1, elsel, AX.X, ALU.max))
                    dv(lambda e: e.tensor_single_scalar(nm1, m1, -1.0, ALU.mult))
                    k.op("act", lambda e: e.activation(ee, elsel, AF.Exp, bias=nm1), reads=[r], writes=[r])
                    dv(lambda e: e.tensor_scalar(mask1, elsel, m1, None, ALU.is_equal))
                    dv(lambda e: e.scalar_tensor_tensor(ee2, mask1, -2.0, ee, ALU.mult, ALU.add))
                    dv(lambda e: e.tensor_reduce(v2, ee2, AX.X, ALU.max))
                    dv(lambda e: e.tensor_scalar(mask2, ee2, v2, None, ALU.is_equal))
                    dv(lambda e: e.tensor_single_scalar(den, v2, 1.0, ALU.add))
                    dv(lambda e: e.reciprocal(rden, den))
                    dv(lambda e: e.scalar_tensor_tensor(wl, mask2, v2, mask1, ALU.mult, ALU.add))
                    dv(lambda e: e.tensor_tensor(scl, pg, rden, ALU.mult))
                    dv(lambda e: e.tensor_scalar(wl, wl, scl, None, ALU.mult))
                    cb = comb[ti]
                    dv(lambda e: e.tensor_tensor(cb[:].rearrange("p (g j) -> p g j", g=4),
                                                 oh.unsqueeze(2).to_broadcast([128, 4, 4]),
                                                 wl.unsqueeze(1).to_broadcast([128, 4, 4]), ALU.mult), wr=[cb])
                NEX = int(os.environ.get("MK_NEX", "16"))
                if NEX:
                    load_w(0, 0)
                for ex in range(NEX):
                    slot = ex % 2
                    if ex + 1 < NEX:
                        load_w(ex + 1, 1 - slot)
                    w1, w3, w2 = w1b[slot], w3b[slot], w2b[slot]
                    for tb in range(G // 512):
                        hd = hid[tb % 2]
                        for fc in range(2):
                            p1 = ps[fc]
                            p3 = ps[2 + fc]
                            for dc in range(8):
                                k.op("pe", lambda e: e.matmul(p1[:], w1[:, dc, fc * 128:(fc + 1) * 128], xnT[:, dc, tb * 512:(tb + 1) * 512],
                                                              start=(dc == 0), stop=(dc == 7)),
                                     reads=[w1, xnT], writes=[p1], inc=(dc == 7))
                            for dc in range(8):
                                k.op("pe", lambda e: e.matmul(p3[:], w3[:, dc, fc * 128:(fc + 1) * 128], xnT[:, dc, tb * 512:(tb + 1) * 512],
                                                              start=(dc == 0), stop=(dc == 7)),
                                     reads=[w3, xnT], writes=[p3], inc=(dc == 7))
                            sl = sil[fc]
                            k.op("act", lambda e: e.activation(sl[:], p1[:], AF.Silu), reads=[p1], writes=[sl])
                            k.op("dve", lambda e: e.tensor_tensor(hd[:, fc, :], sl[:], p3[:], ALU.mult), reads=[sl, p3], writes=[hd])
                        for tt in range(4):
                            ti = tb * 4 + tt
                            for half in range(2):
                                py = ps[4 + half]
                                for fc in range(2):
                                    k.op("pe", lambda e: e.matmul(py[:], hd[:, fc, tt * 128:(tt + 1) * 128], w2[:, fc, half * 512:(half + 1) * 512],
                                                                  start=(fc == 0), stop=(fc == 1)),
                                         reads=[hd, w2], writes=[py], inc=(fc == 1))
                                ya = yacc[ti]
                                cs = comb[ti][:, ex:ex + 1]
                                if ex == 0:
                                    k.op("dve", lambda e: e.tensor_scalar(ya[:, half * 512:(half + 1) * 512], py[:], cs, None, ALU.mult),
                                         reads=[py, comb[ti]], writes=[ya])
                                else:
                                    k.op("dve", lambda e: e.scalar_tensor_tensor(ya[:, half * 512:(half + 1) * 512], py[:], cs,
                                                                                 ya[:, half * 512:(half + 1) * 512], ALU.mult, ALU.add),
                                         reads=[py, comb[ti], ya], writes=[ya])
                for ti in range(NTG):
                    t = g * NTG + ti
                    b = ti % 2
                    h = ht[b]
                    k.dma(h[:], hin[0][t * 128:(t + 1) * 128, :], reads=[hin[1][t]], writes=[h])
                    o = ho[b]
                    k.op("dve", lambda e: e.tensor_tensor(o[:], h[:], yacc[ti][:], ALU.add), reads=[h, yacc[ti]], writes=[o])
                    if final:
                        o2 = xn[b]
                        self.rmsnorm(o, fgain, o2, junk, stt[b])
                        k.dma(hout[0][t * 128:(t + 1) * 128, :], o2[:], reads=[o2], writes=[hout[1][t]])
                    else:
                        k.dma(hout[0][t * 128:(t + 1) * 128, :], o[:], reads=[o], writes=[hout[1][t]])
            k.barrier()

    def nextbank(self):
        self.pbi = (getattr(self, "pbi", -1) + 1) % 8
        return self.ps[self.pbi]

    def mem_kv(self, st, l):
        k, P = self.k, self.P
        sb = lambda n, s, d: self.sbuf(st, n, s, d)
        memkT = sb("memkT", [128, 2, 256], BF16)
        memv = sb("memv", [128, 2, 256], BF16)
        with ExitStack() as st2:
            sb2 = lambda n, s, d: self.sbuf(st2, n, s, d)
            g = sb2("mg", [128, D], F32)
            k.dma(g[:], P["mem_norm"][l].partition_broadcast(128), writes=[g])
            w = sb2("wmkv", [128, 8, 512], BF16)
            k.dma(w[:], P["w_mem_kv"][l].rearrange("(c p) n -> p c n", p=128), writes=[w], q="pool")
            mT = sb2("memnT", [128, 8, 256], BF16)
            junk = sb2("mjunk", [128, D], F32)
            for mt in range(2):
                h = sb2("mh%d" % mt, [128, D], F32)
                xn = sb2("mxn%d" % mt, [128, D], F32)
                stt = sb2("mst%d" % mt, [128, 4], F32)
                k.dma(h[:], P["mem"][mt * 128:(mt + 1) * 128, :], writes=[h])
                self.rmsnorm(h, g, xn, junk, stt)
                self.transpose8(xn, [(mT, lambda half: mT[:, half * 4:(half + 1) * 4, mt * 128:(mt + 1) * 128])],
                                self.nextbank(), self.nextbank())
            for j in range(2):
                pb = self.nextbank()
                for dc in range(8):
                    k.op("pe", lambda e: e.matmul(pb[:, 0:256], w[:, dc, j * 128:(j + 1) * 128], mT[:, dc, :],
                                                  start=(dc == 0), stop=(dc == 7)),
                         reads=[w, mT], writes=[pb], inc=(dc == 7))
                k.op("act", lambda e: e.copy(memkT[:, j, :], pb[:, 0:256]), reads=[pb], writes=[memkT])
            for mt in range(2):
                pb = self.nextbank()
                for dc in range(8):
                    k.op("pe", lambda e: e.matmul(pb[:, 0:256], mT[:, dc, mt * 128:(mt + 1) * 128], w[:, dc, 256:512],
                                                  start=(dc == 0), stop=(dc == 7)),
                         reads=[w, mT], writes=[pb], inc=(dc == 7))
                k.op("dve", lambda e: e.tensor_copy(memv[:, mt, :], pb[:, 0:256]), reads=[pb], writes=[memv])
            k.barrier()
        return memkT, memv

    def mem_attend(self, W, mqT, qoff, memkT, memv, mix, col0=768):
        k = self.k
        pe_ = W["pexp"]; ms = W["mstat"]; pT = W["pT"]
        banks = [self.nextbank(), self.nextbank()]
        for hh in range(4):
            pair, s = hh // 2, hh % 2
            pb = banks[s]
            k.op("pe", lambda e: e.matmul(pb[:, pair * 256:(pair + 1) * 256], mqT[s * 64:(s + 1) * 64, pair, qoff:qoff + 128],
                                          memkT[s * 64:(s + 1) * 64, pair, :], start=True, stop=True),
                 reads=[mqT, memkT], writes=[pb])
        for s in range(2):
            pb = banks[s]
            k.op("dve", lambda e: e.tensor_reduce(ms[:, s:s + 3:2], pb[:].rearrange("p (h m) -> p h m", h=2), AX.X, ALU.max),
                 reads=[pb], writes=[ms])
        k.op("dve", lambda e: e.tensor_single_scalar(ms[:, 4:8], ms[:, 0:4], -0.125, ALU.mult), reads=[ms], writes=[ms])
        for hh in range(4):
            pair, s = hh // 2, hh % 2
            pb = banks[s]
            k.op("act", lambda e: e.activation(pe_[:, hh, :], pb[:, pair * 256:(pair + 1) * 256], AF.Exp, bias=ms[:, 4 + hh:5 + hh],
                                               scale=0.125, accum_out=ms[:, 8 + hh:9 + hh]),
                 reads=[pb, ms], writes=[pe_, ms])
        k.op("dve", lambda e: e.reciprocal(ms[:, 12:16], ms[:, 8:12]), reads=[ms], writes=[ms])
        for half in range(2):
            pb = self.nextbank()
            for j in range(4):
                idx = half * 4 + j
                hh, mc = idx // 2, idx % 2
                k.op("pe", lambda e: e.transpose(pb[:, j * 128:(j + 1) * 128], pe_[:, hh, mc * 128:(mc + 1) * 128], self.ident),
                     reads=[pe_, self.cst], writes=[pb], inc=(j == 3))
            if half == 0:
                k.op("act", lambda e: e.copy(pT[:, 0:4, :], pb[:].rearrange("p (c t) -> p c t", c=4)), reads=[pb], writes=[pT])
            else:
                k.op("dve", lambda e: e.tensor_copy(pT[:, 4:8, :], pb[:].rearrange("p (c t) -> p c t", c=4)), reads=[pb], writes=[pT])
        pb = self.nextbank()
        for hh in range(4):
            for mc in range(2):
                k.op("pe", lambda e: e.matmul(pb[:, hh * 64:(hh + 1) * 64], pT[:, hh * 2 + mc, :], memv[:, mc, hh * 64:(hh + 1) * 64],
                                              start=(mc == 0), stop=(mc == 1)),
                     reads=[pT, memv], writes=[pb], inc=(mc == 1))
        k.op("dve", lambda e: e.tensor_tensor(mix[:, col0:col0 + 256].rearrange("p (h d) -> p h d", h=4),
                                              pb[:, 0:256].rearrange("p (h d) -> p h d", h=4),
                                              ms[:, 12:16].unsqueeze(2).to_broadcast([128, 4, 64]), ALU.mult),
             reads=[pb, ms], writes=[mix])

    def mem_work(self, st):
        sb = lambda n, s, d: self.sbuf(st, n, s, d)
        return {"pexp": sb("pexp", [128, 4, 256], F32), "mstat": sb("mstat", [128, 16], F32),
                "pT": sb("pT", [128, 8, 128], BF16)}

    def out_proj(self, mix, mixT, w_out, h, hn, dst_ap, dst_buf):
        k = self.k
        self.transpose8(mix, [(mixT, lambda half: mixT[:, half * 4:(half + 1) * 4, :])], self.nextbank(), self.nextbank())
        for half in range(2):
            pb = self.nextbank()
            for fc in range(8):
                k.op("pe", lambda e: e.matmul(pb[:], mixT[:, fc, :], w_out[:, fc, half * 512:(half + 1) * 512],
                                              start=(fc == 0), stop=(fc == 7)),
                     reads=[mixT, w_out], writes=[pb], inc=(fc == 7))
            k.op("dve", lambda e: e.tensor_tensor(hn[:, half * 512:(half + 1) * 512], h[:, half * 512:(half + 1) * 512], pb[:], ALU.add),
                 reads=[h, pb], writes=[hn])
        k.dma(dst_ap, hn[:], reads=[hn], writes=[dst_buf])

    def mixer_a_phase(self, hin, hout):
        nc, k, P = self.nc, self.k, self.P
        NTA = int(os.environ.get("MK_NTA", str(NT)))
        with ExitStack() as st:
            sb = lambda n, s, d: self.sbuf(st, n, s, d)
            memkT, memv = self.mem_kv(st, 0)
            gain = sb("gainA", [128, D], F32)
            k.dma(gain[:], P["a_norm"][0].partition_broadcast(128), writes=[gain])
            w_in = sb("w_inA", [128, 8, 3340], BF16)
            for c in range(8):
                k.dma(w_in[:, c, :], P["a_w_in"][0, c * 128:(c + 1) * 128, :], writes=[w_in], q="pool")
            w_out = sb("w_outA", [128, 8, D], BF16)
            k.dma(w_out[:], P["a_w_out"][0].rearrange("(c p) n -> p c n", p=128), writes=[w_out], q="pool")
            convw = sb("convw", [128, 18, 4], F32)
            k.dma(convw[:], P["convw"], writes=[convw])
            sc6 = sb("sc6", [128, 32], F32)
            k.dma(sc6[:, 0:6], P["a_log"][0].partition_broadcast(128), writes=[sc6])
            k.dma(sc6[:, 6:12], P["a_dt_bias"][0].partition_broadcast(128), writes=[sc6])
            k.op("act", lambda e: e.activation(sc6[:, 12:18], sc6[:, 0:6], AF.Exp), reads=[sc6], writes=[sc6])
            k.op("dve", lambda e: e.tensor_single_scalar(sc6[:, 12:18], sc6[:, 12:18], -1.0, ALU.mult), reads=[sc6], writes=[sc6])
            ogain = sb("ogain", [128, 128], F32)
            k.dma(ogain[:], P["a_out_gain"][0].partition_broadcast(128), writes=[ogain])
            MW = self.mem_work(st)
            pc = sb("pc", [128, 18, 131], F32)
            k.op("dve", lambda e: e.memset(pc[:], 0.0), writes=[pc])
            Sf = [sb("Sf%d" % h, [128, 128], F32) for h in range(6)]
            Sb = [sb("Sb%d" % h, [128, 128], BF16) for h in range(6)]
            for h in range(6):
                k.op("dve", lambda e: e.memset(Sf[h][:], 0.0), writes=[Sf[h]])
                k.op("pool", lambda e: e.memset(Sb[h][:], 0.0), writes=[Sb[h]])
            xn = sb("xnA", [128, D], F32)
            xT = sb("xnTA", [128, 8, 128], BF16)
            cv = sb("cv", [128, 18, 128], F32)
            sq = sb("sq", [128, 12, 128], BF16)
            rs = sb("rs", [128, 12, 128], F32)
            kn32 = sb("kn32", [128, 6, 128], F32)
            SLg = [sb("SLg%d" % i, [128, 128], F32) for i in range(2)]
            dm = sb("dm", [128, 6, 128], F32)
            dmi = sb("dmi", [128, 6, 128], F32)
            Wq = [[sb("W%d_%d" % (h, i), [128, 3, 128], BF16) for i in range(2)] for h in range(6)]
            Q0f = [sb("Q0f%d" % i, [128, 128], F32) for i in range(2)]
            stt = [sb("sttA%d" % i, [128, 4], F32) for i in range(2)]
            ht = [sb("htA%d" % i, [128, D], F32) for i in range(2)]
            qkT = [sb("qkT%d" % i, [128, 6, 2, 128], BF16) for i in range(2)]
            kt = [sb("kt%d" % i, [128, 6, 128], BF16) for i in range(2)]
            vtok = [sb("vtok%d" % i, [128, 6, 128], F32) for i in range(2)]
            attnT = [sb("attnT%d" % i, [128, 6, 128], BF16) for i in range(2)]
            TT = [sb("TT%d" % i, [128, 6, 128], BF16) for i in range(2)]
            sc = [sb("scA%d" % i, [128, 96], F32) for i in range(2)]
            gg = [sb("gg%d" % i, [128, 768], F32) for i in range(2)]
            mqT = [sb("mqTA%d" % i, [128, 2, 128], BF16) for i in range(2)]
            Rb = [sb("R%d" % i, [128, 128], BF16) for i in range(2)]
            vnew = [sb("vnew%d" % i, [128, 128], BF16) for i in range(2)]
            o1 = [sb("o1_%d" % i, [128, 128], F32) for i in range(2)]
            ob = [sb("ob%d" % i, [128, 128], F32) for i in range(2)]
            ost = [sb("ost%d" % i, [128, 4], F32) for i in range(2)]
            ojunk = sb("ojunk", [128, 128], F32)
            mix = sb("mixA", [128, D], F32)
            mixT = sb("mixTA", [128, 8, 128], BF16)
            hn = sb("hnA", [128, D], F32)
            ident, U, SL, ones = self.ident, self.U, self.SL, self.ones
            NEGs = self.cst[:, 7, :]
            identb = self.identb
            cst, cstb = self.cst, self.cstb
            QS = float(128 ** -0.5)

            def front(t):
                b = t % 2
                h = ht[b]
                k.dma(h[:], hin[0][t * 128:(t + 1) * 128, :], reads=[hin[1][t]], writes=[h])
                self.rmsnorm(h, gain, xn, xn, stt[b])
                self.transpose8(xn, [(xT, lambda half: xT[:, half * 4:(half + 1) * 4, :])], self.nextbank(), self.nextbank())
                for g4 in range(5):
                    nf = 4 if g4 < 4 else 2
                    pb = self.nextbank()
                    for j in range(nf):
                        fc = g4 * 4 + j
                        for dc in range(8):
                            k.op("pe", lambda e: e.matmul(pb[:, j * 128:(j + 1) * 128], w_in[:, dc, fc * 128:(fc + 1) * 128], xT[:, dc, :],
                                                          start=(dc == 0), stop=(dc == 7)),
                                 reads=[w_in, xT], writes=[pb], inc=(dc == 7 and j == nf - 1))
                    dstv = pc[:, g4 * 4:g4 * 4 + nf, 3:131]
                    srcv = pb[:, 0:nf * 128].rearrange("p (c t) -> p c t", c=nf)
                    k.op("act", lambda e: e.copy(dstv, srcv), reads=[pb], writes=[pc])
                pb = self.nextbank()
                for j in range(2):
                    for dc in range(8):
                        k.op("pe", lambda e: e.matmul(pb[:, j * 128:(j + 1) * 128], w_in[:, dc, 3084 + j * 128:3084 + (j + 1) * 128], xT[:, dc, :],
                                                      start=(dc == 0), stop=(dc == 7)),
                             reads=[w_in, xT], writes=[pb], inc=(dc == 7 and j == 1))
                k.op("act", lambda e: e.copy(mqT[b][:], pb[:, 0:256].rearrange("p (c t) -> p c t", c=2)), reads=[pb], writes=[mqT[b]])
                pg1 = self.nextbank()
                for dc in range(8):
                    k.op("pe", lambda e: e.matmul(pg1[:], xT[:, dc, :], w_in[:, dc, 2304:2816], start=(dc == 0), stop=(dc == 7)),
                         reads=[w_in, xT], writes=[pg1], inc=(dc == 7))
                pg2 = self.nextbank()
                for dc in range(8):
                    k.op("pe", lambda e: e.matmul(pg2[:, 0:268], xT[:, dc, :], w_in[:, dc, 2816:3084], start=(dc == 0), stop=(dc == 7)),
                         reads=[w_in, xT], writes=[pg2], inc=(dc == 7))
                g_g = gg[b]
                k.op("act", lambda e: e.activation(g_g[:, 0:512], pg1[:], AF.Silu), reads=[pg1], writes=[g_g])
                k.op("act", lambda e: e.activation(g_g[:, 512:768], pg2[:, 0:256], AF.Silu), reads=[pg2], writes=[g_g])
                s_ = sc[b]
                C = lambda a_, n=6: s_[:, a_:a_ + n]
                beta, tt_, ex_, sp_, g_, gcl, egc, negc, etl, egl, dd = (C(0), C(6), C(12), C(18), C(24), C(32, 16), C(48), C(54), C(60), C(66), C(72))
                k.op("act", lambda e: e.activation(beta, pg2[:, 256:262], AF.Sigmoid), reads=[pg2], writes=[s_])
                k.op("dve", lambda e: e.tensor_tensor(tt_, pg2[:, 262:268], sc6[:, 6:12], ALU.add), reads=[pg2, sc6], writes=[s_])
                k.op("act", lambda e: e.activation(ex_, tt_, AF.Exp), reads=[s_], writes=[s_])
                k.op("act", lambda e: e.activation(sp_, ex_, AF.Ln, bias=1.0), reads=[s_], writes=[s_])
                k.op("dve", lambda e: e.tensor_tensor(g_, sp_, sc6[:, 12:18], ALU.mult), reads=[s_, sc6], writes=[s_])
                k.op("pool", lambda e: e.tensor_tensor(g_g[:].rearrange("p (h d) -> p h d", h=6), g_g[:].rearrange("p (h d) -> p h d", h=6),
                                                       ogain[:].unsqueeze(1).to_broadcast([128, 6, 128]), ALU.mult),
                     reads=[g_g, ogain], writes=[g_g])
                pgc = self.nextbank()
                k.op("pe", lambda e: e.matmul(pgc[:, 0:6], U, g_, start=True, stop=True), reads=[cst, s_], writes=[pgc])
                k.op("pe", lambda e: e.matmul(pgc[:, 8:14], ones, g_, start=True, stop=True), reads=[cst, s_], writes=[pgc])
                k.op("dve", lambda e: e.tensor_copy(gcl, pgc[:, 0:16]), reads=[pgc], writes=[s_])
                k.op("act", lambda e: e.activation(egc, s_[:, 32:38], AF.Exp), reads=[s_], writes=[s_])
                k.op("dve", lambda e: e.tensor_single_scalar(negc, egc, -1.0, ALU.mult), reads=[s_], writes=[s_])
                k.op("dve", lambda e: e.tensor_tensor(dd, s_[:, 40:46], s_[:, 32:38], ALU.subtract), reads=[s_], writes=[s_])
                k.op("act", lambda e: e.activation(etl, dd, AF.Exp), reads=[s_], writes=[s_])
                k.op("act", lambda e: e.activation(egl, s_[:, 40:46], AF.Exp), reads=[s_], writes=[s_])
                yield
                for fc in range(18):
                    o_ = cv[:, fc, :]
                    k.op("dve", lambda e: e.tensor_scalar(o_, pc[:, fc, 0:128], convw[:, fc, 0:1], None, ALU.mult),
                         reads=[pc, convw], writes=[cv])
                    for j in range(1, 4):
                        k.op("dve", lambda e: e.scalar_tensor_tensor(o_, pc[:, fc, j:j + 128], convw[:, fc, j:j + 1], o_, ALU.mult, ALU.add),
                             reads=[pc, convw, cv], writes=[cv])
                k.op("pool", lambda e: e.tensor_copy(pc[:, :, 0:3], pc[:, :, 128:131]), reads=[pc], writes=[pc])
                k.op("act", lambda e: e.activation(cv[:], cv[:], AF.Silu), reads=[cv], writes=[cv])
                qkv = cv
                k.op("act", lambda e: e.activation(sq[:], qkv[:, 0:12, :], AF.Square), reads=[qkv], writes=[sq])
                for g3 in range(3):
                    pb = self.nextbank()
                    k.op("pe", lambda e: e.matmul(pb[:], self.onesb, sq[:, g3 * 4:(g3 + 1) * 4, :], start=True, stop=True),
                         reads=[cstb, sq], writes=[pb])
                    k.op("act", lambda e: e.activation(rs[:, g3 * 4:(g3 + 1) * 4, :], pb[:].rearrange("p (c t) -> p c t", c=4), AF.Ln,
                                                       bias=self.epsb[:, 0:1]), reads=[pb, self.epsbuf], writes=[rs])
                    k.op("act", lambda e: e.activation(rs[:, g3 * 4:(g3 + 1) * 4, :], rs[:, g3 * 4:(g3 + 1) * 4, :], AF.Exp, scale=-0.5),
                         reads=[rs], writes=[rs])
                yield
                qk = qkT[b]
                k.op("dve", lambda e: e.scalar_tensor_tensor(qk[:, :, 1, :], qkv[:, 0:6, :], QS, rs[:, 0:6, :], ALU.mult, ALU.mult),
                     reads=[qkv, rs], writes=[qk])
                k.op("dve", lambda e: e.tensor_tensor(kn32[:], qkv[:, 6:12, :], rs[:, 6:12, :], ALU.mult), reads=[qkv, rs], writes=[kn32])
                k.op("pool", lambda e: e.tensor_copy(qk[:, :, 0, :], kn32[:]), reads=[kn32], writes=[qk])
                yield
                for grp in range(3):
                    pb = self.nextbank()
                    for j in range(4):
                        idx = grp * 4 + j
                        src = qkv[:, 12 + idx, :] if idx < 6 else kn32[:, idx - 6, :]
                        srcb = qkv if idx < 6 else kn32
                        k.op("pe", lambda e: e.transpose(pb[:, j * 128:(j + 1) * 128], src, ident), reads=[srcb, cst], writes=[pb], inc=(j == 3))
                    for j in range(4):
                        idx = grp * 4 + j
                        if idx < 6:
                            k.op("act", lambda e: e.copy(vtok[b][:, idx, :], pb[:, j * 128:(j + 1) * 128]), reads=[pb], writes=[vtok[b]])
                        else:
                            hh = idx - 6
                            k.op("dve", lambda e: e.tensor_scalar(kt[b][:, hh, :], pb[:, j * 128:(j + 1) * 128], etl[:, hh:hh + 1], None, ALU.mult),
                                 reads=[pb, s_], writes=[kt[b]])
                for hp in range(3):
                    pb = self.nextbank()
                    for j in range(2):
                        hh = hp * 2 + j
                        sg = SLg[hh % 2]
                        k.op("dve", lambda e: e.tensor_scalar(sg[:], SL, g_[:, hh:hh + 1], None, ALU.mult), reads=[cst, s_], writes=[sg])
                        k.op("pe", lambda e: e.matmul(pb[:, j * 128:(j + 1) * 128], sg[:], U, start=True, stop=False),
                             reads=[sg, cst], writes=[pb], inc=False)
                        k.op("pe", lambda e: e.matmul(pb[:, j * 128:(j + 1) * 128], ident, NEGs, start=False, stop=True),
                             reads=[cst], writes=[pb])
                    k.op("act", lambda e: e.activation(dm[:, hp * 2:hp * 2 + 2, :], pb[:, 0:256].rearrange("p (c t) -> p c t", c=2), AF.Exp),
                         reads=[pb], writes=[dm])
                k.op("pool", lambda e: e.tensor_tensor(dmi[:], dm[:], ident.unsqueeze(1).to_broadcast([128, 6, 128]), ALU.add),
                     reads=[dm, cst], writes=[dmi])
                for hh in range(6):
                    pb = self.nextbank()
                    W0 = Wq[hh][0]
                    qf = Q0f[hh % 2]
                    k.op("pe", lambda e: e.matmul(pb[:, 0:256], qk[:, hh, 0, :], qk[:, hh, :, :].rearrange("p a t -> p (a t)"),
                                                  start=True, stop=True), reads=[qk], writes=[pb])
                    k.op("dve", lambda e: e.scalar_tensor_tensor(qf[:], pb[:, 0:128], beta[:, hh:hh + 1], dm[:, hh, :], ALU.mult, ALU.mult),
                         reads=[pb, s_, dm], writes=[qf])
                    k.op("dve", lambda e: e.tensor_tensor(attnT[b][:, hh, :], pb[:, 128:256], dmi[:, hh, :], ALU.mult),
                         reads=[pb, dmi], writes=[attnT[b]])
                    k.op("pool", lambda e: e.tensor_copy(W0[:, 0, :], qf[:]), reads=[qf], writes=[W0])
                    k.op("pool", lambda e: e.tensor_tensor(Wq[hh][1][:, 1, :], ident, qf[:], ALU.subtract), reads=[cst, qf], writes=[Wq[hh][1]])
                    pb2 = self.nextbank()
                    k.op("pe", lambda e: e.transpose(pb2[:, 0:128], qf[:], ident), reads=[qf, cst], writes=[pb2])
                    k.op("act", lambda e: e.copy(W0[:, 2, :], pb2[:, 0:128]), reads=[pb2], writes=[W0])
                for lvl in range(7):
                    if lvl in (0, 2, 4, 6):
                        yield
                    for hh in range(6):
                        Wc = Wq[hh][lvl % 2]
                        Wn = Wq[hh][(lvl + 1) % 2]
                        pb = self.nextbank()
                        Qk, Xk, Pk = Wc[:, 0, :], Wc[:, 1, :], Wc[:, 2, :]
                        mm = lambda out, l_, r_, st_, sp_2, inc_: k.op(
                            "pe", lambda e: e.matmul(out, l_, r_, start=st_, stop=sp_2), reads=[Wc, cstb], writes=[pb], inc=inc_)
                        if lvl == 0:
                            mm(pb[:, 0:128], Pk, Qk, True, True, False)
                            mm(pb[:, 256:384], Qk, Pk, True, True, True)
                            k.op("act", lambda e: e.copy(Wn[:, 0, :], pb[:, 0:128]), reads=[pb], writes=[Wn])
                            k.op("act", lambda e: e.copy(Wn[:, 2, :], pb[:, 256:384]), reads=[pb], writes=[Wn])
                        elif lvl < 6:
                            mm(pb[:, 0:128], Pk, Qk, True, True, False)
                            mm(pb[:, 128:256], Pk, Xk, True, False, False)
                            mm(pb[:, 128:256], identb, Xk, False, True, False)
                            mm(pb[:, 256:384], Qk, Pk, True, True, True)
                            if hh % 2 == 0:
                                k.op("act", lambda e: e.copy(Wn[:], pb[:, 0:384].rearrange("p (c t) -> p c t", c=3)), reads=[pb], writes=[Wn])
                            else:
                                k.op("dve", lambda e: e.tensor_copy(Wn[:], pb[:, 0:384].rearrange("p (c t) -> p c t", c=3)), reads=[pb], writes=[Wn])
                        else:
                            mm(pb[:, 128:256], Pk, Xk, True, False, False)
                            mm(pb[:, 128:256], identb, Xk, False, True, True)
                            k.op("act", lambda e: e.copy(TT[b][:, hh, :], pb[:, 128:256]), reads=[pb], writes=[TT[b]])

            def back(t):
                b = t % 2
                h = ht[b]
                s_ = sc[b]
                C = lambda a_, n=6: s_[:, a_:a_ + n]
                beta, egc, negc, egl = C(0), C(48), C(54), C(66)
                qk, g_g = qkT[b], gg[b]
                for hh in range(6):
                    r2 = hh % 2
                    pb = self.nextbank()
                    k.op("pe", lambda e: e.matmul(pb[:, 0:128], qk[:, hh, 0, :], Sb[hh][:], start=True, stop=True), reads=[qk, Sb[hh]], writes=[pb], inc=False)
                    k.op("pe", lambda e: e.matmul(pb[:, 128:256], qk[:, hh, 1, :], Sb[hh][:], start=True, stop=True), reads=[qk, Sb[hh]], writes=[pb])
                    R_ = Rb[r2]
                    k.op("dve", lambda e: e.scalar_tensor_tensor(R_[:], pb[:, 0:128], negc[:, hh:hh + 1], vtok[b][:, hh, :], ALU.mult, ALU.add),
                         reads=[pb, s_, vtok[b]], writes=[R_])
                    k.op("act", lambda e: e.mul(o1[r2][:], pb[:, 128:256], egc[:, hh:hh + 1]), reads=[pb, s_], writes=[o1[r2]])
                    pb2 = self.nextbank()
                    k.op("pe", lambda e: e.matmul(pb2[:, 0:128], TT[b][:, hh, :], R_[:], start=True, stop=True), reads=[TT[b], R_], writes=[pb2])
                    vn = vnew[r2]
                    k.op("dve", lambda e: e.tensor_scalar(vn[:], pb2[:, 0:128], beta[:, hh:hh + 1], None, ALU.mult), reads=[pb2, s_], writes=[vn])
                    pb3 = self.nextbank()
                    k.op("pe", lambda e: e.matmul(pb3[:, 0:128], attnT[b][:, hh, :], vn[:], start=True, stop=True), reads=[attnT[b], vn], writes=[pb3], inc=False)
                    k.op("pe", lambda e: e.matmul(pb3[:, 128:256], kt[b][:, hh, :], vn[:], start=True, stop=True), reads=[kt[b], vn], writes=[pb3])
                    o_ = ob[r2]
                    k.op("dve", lambda e: e.tensor_tensor(o_[:], o1[r2][:], pb3[:, 0:128], ALU.add), reads=[o1[r2], pb3], writes=[o_])
                    k.op("dve", lambda e: e.scalar_tensor_tensor(Sf[hh][:], Sf[hh][:], egl[:, hh:hh + 1], pb3[:, 128:256], ALU.mult, ALU.add),
                         reads=[Sf[hh], s_, pb3], writes=[Sf[hh]])
                    k.op("pool", lambda e: e.tensor_copy(Sb[hh][:], Sf[hh][:]), reads=[Sf[hh]], writes=[Sb[hh]])
                    os_ = ost[r2]
                    k.op("act", lambda e: e.activation(ojunk[:], o_[:], AF.Square, accum_out=os_[:, 0:1]), reads=[o_], writes=[ojunk, os_])
                    k.op("act", lambda e: e.activation(os_[:, 1:2], os_[:, 0:1], AF.Sqrt, bias=self.epsb[:, 0:1], scale=1.0 / 128),
                         reads=[os_, self.epsbuf], writes=[os_])
                    k.op("dve", lambda e: e.reciprocal(os_[:, 2:3], os_[:, 1:2]), reads=[os_], writes=[os_])
                    k.op("dve", lambda e: e.scalar_tensor_tensor(mix[:, hh * 128:(hh + 1) * 128], o_[:], os_[:, 2:3], g_g[:, hh * 128:(hh + 1) * 128],
                                                                 ALU.mult, ALU.mult), reads=[o_, os_, g_g], writes=[mix])
                    yield
                self.mem_attend(MW, mqT[b], 0, memkT, memv, mix)
                self.out_proj(mix, mixT, w_out, h, hn, hout[0][t * 128:(t + 1) * 128, :], hout[1][t])

            def run2(g1, g2):
                gens = [g for g in (g1, g2) if g is not None]
                while gens:
                    for g_ in list(gens):
                        try:
                            next(g_)
                        except StopIteration:
                            gens.remove(g_)

            run2(front(0), None)
            for t in range(NTA):
                run2(front(t + 1) if t + 1 < NTA else None, back(t))
            k.barrier()

    def mixer_b_phase(self, hin, hout):
        nc, k, P = self.nc, self.k, self.P
        NGB = int(os.environ.get("MK_NGB", "8"))
        NHB = int(os.environ.get("MK_NHB", "12"))
        NDUM = int(os.environ.get("MK_NDUM", "0"))
        with ExitStack() as st:
            sb = lambda n, s, d: self.sbuf(st, n, s, d)
            memkT, memv = self.mem_kv(st, 1)
            win_d = self.dscratch("b_w_in_bf", [D, D], BF16)
            wout_d = self.dscratch("b_w_out_bf", [D, D], BF16)
            wdb = [Buf(None, "win_d"), Buf(None, "wout_d")]
            k.dma(win_d, P["b_w_in"][0], writes=[wdb[0]], q="pool")
            k.dma(wout_d, P["b_w_out"][0], writes=[wdb[1]], q="pool")
            KT = sb("KT", [128, 6, S], BF16)
            Vt = sb("Vt", [128, NT, 768], BF16)
            ht = [sb("htB%d" % i, [128, D], F32) for i in range(2)]
            xn = sb("xnB", [128, D], F32)
            stt = [sb("sttB%d" % i, [128, 4], F32) for i in range(2)]
            with ExitStack() as st1:
                sb1 = lambda n, s, d: self.sbuf(st1, n, s, d)
                kvg = sb1("kvg", [128, D], F32)
                k.dma(kvg[:], P["kv_norm"].partition_broadcast(128), writes=[kvg])
                w_kv = sb1("w_kv", [128, 8, 1536], BF16)
                for c in range(8):
                    k.dma(w_kv[:, c, :], P["w_kv"][c * 128:(c + 1) * 128, :], writes=[w_kv], q="pool")
                xT1 = [sb1("xT1_%d" % i, [128, 8, 128], BF16) for i in range(2)]
                for t in range(NT):
                    b = t % 2
                    h = ht[b]
                    k.dma(h[:], hin[0][t * 128:(t + 1) * 128, :], reads=[hin[1][t]], writes=[h])
                    self.rmsnorm(h, kvg, xn, xn, stt[b])
                    xT = xT1[b]
                    self.transpose8(xn, [(xT, lambda half: xT[:, half * 4:(half + 1) * 4, :])], self.nextbank(), self.nextbank())
                    for g4 in range(2):
                        nf = 4 if g4 == 0 else 2
                        pb = self.nextbank()
                        for j in range(nf):
                            fc = g4 * 4 + j
                            for dc in range(8):
                                k.op("pe", lambda e: e.matmul(pb[:, j * 128:(j + 1) * 128], w_kv[:, dc, fc * 128:(fc + 1) * 128], xT[:, dc, :],
                                                              start=(dc == 0), stop=(dc == 7)),
                                     reads=[w_kv, xT], writes=[pb], inc=(dc == 7 and j == nf - 1))
                        dstv = KT[:, g4 * 4:g4 * 4 + nf, t * 128:(t + 1) * 128]
                        srcv = pb[:, 0:nf * 128].rearrange("p (c t) -> p c t", c=nf)
                        k.op("act", lambda e: e.copy(dstv, srcv), reads=[pb], writes=[KT])
                    for half, (c0, c1) in enumerate(((0, 512), (512, 768))):
                        pb = self.nextbank()
                        for dc in range(8):
                            k.op("pe", lambda e: e.matmul(pb[:, 0:c1 - c0], xT[:, dc, :], w_kv[:, dc, 768 + c0:768 + c1],
                                                          start=(dc == 0), stop=(dc == 7)),
                                 reads=[w_kv, xT], writes=[pb], inc=(dc == 7))
                        k.op("dve", lambda e: e.tensor_copy(Vt[:, t, c0:c1], pb[:, 0:c1 - c0]), reads=[pb], writes=[Vt])
                k.barrier()
            bg = sb("bgain", [128, D], F32)
            k.dma(bg[:], P["b_norm"][0].partition_broadcast(128), writes=[bg])
            wB = sb("wB", [128, 8, D], BF16)
            xTg = sb("xTg", [128, 8, 512], BF16)
            qT = sb("qT", [128, 6, 512], BF16)
            mqT = sb("mqTB", [128, 2, 512], BF16)
            mixTg = sb("mixTg", [128, 8, 512], BF16)
            Eb = [sb("Eb%d" % i, [128, 512], F32) for i in range(3)]
            spb = [sb("spb%d" % i, [128, 512], BF16) for i in range(3)]
            eab = [sb("eab%d" % i, [128, 512], F32) for i in range(2)]
            ab = [sb("ab%d" % i, [128, 512], BF16) for i in range(2)]
            MW = self.mem_work(st)
            mmB = sb("mmB", [128, 256], F32)
            hn = sb("hnB", [128, D], F32)
            NGEb = self.cstb[:, 5, :]
            NLTb = self.cstb[:, 6, :]
            strictTb = self.cstb[:, 3, :]
            cstb = self.cstb
            PZ = [self.ps[0], self.ps[1]]
            PC = [self.ps[2], self.ps[3]]
            PO = [self.ps[4], self.ps[5]]
            rot = [0]

            def nb():
                rot[0] = (rot[0] + 1) % 8
                return self.ps[rot[0]]
            self.nextbank = nb
            for g in range(NGB):
                k.dma(wB[:], win_d.rearrange("(c p) n -> p c n", p=128), reads=[wdb[0]], writes=[wB])
                for tt in range(4):
                    t = g * 4 + tt
                    b = t % 2
                    h = ht[b]
                    k.dma(h[:], hin[0][t * 128:(t + 1) * 128, :], reads=[hin[1][t]], writes=[h])
                    self.rmsnorm(h, bg, xn, xn, stt[b])
                    self.transpose8(xn, [(xTg, lambda half: xTg[:, half * 4:(half + 1) * 4, tt * 128:(tt + 1) * 128])], nb(), nb())
                for fc in range(8):
                    pb = nb()
                    for dc in range(8):
                        k.op("pe", lambda e: e.matmul(pb[:], wB[:, dc, fc * 128:(fc + 1) * 128], xTg[:, dc, :], start=(dc == 0), stop=(dc == 7)),
                             reads=[wB, xTg], writes=[pb], inc=(dc == 7))
                    if fc < 6:
                        k.op("act", lambda e: e.mul(qT[:, fc, :], pb[:], 0.125), reads=[pb], writes=[qT])
                    else:
                        k.op("dve", lambda e: e.tensor_copy(mqT[:, fc - 6, :], pb[:]), reads=[pb], writes=[mqT])
                items = [(2 * p + s, kb) for p in range(NHB // 2) for kb in range(4 * g + 3, -1, -1) for s in range(2)]

                def geom(i):
                    hh, kb = items[i]
                    r = max(kb - 4 * g, 0)
                    return hh, kb, hh // 2, hh % 2, r * 128, kb >= 4 * g

                def s1_pe(i):
                    hh, kb, fc, s, c0, diag = geom(i)
                    ps_ = slice(s * 64, (s + 1) * 64)
                    cs = slice(c0, 512)
                    pz = PZ[i % 2]
                    k.op("pe", lambda e: e.matmul(pz[:, cs], KT[ps_, fc, kb * 128:(kb + 1) * 128], qT[ps_, fc, cs], start=True, stop=True),
                         reads=[KT, qT], writes=[pz])

                def s1_act(i):
                    hh, kb, fc, s, c0, diag = geom(i)
                    cs = slice(c0, 512)
                    pz, E, sp = PZ[i % 2], Eb[i % 3], spb[i % 3]
                    k.op("act", lambda e: e.activation(E[:, cs], pz[:, cs], AF.Exp), reads=[pz], writes=[E])
                    k.op("act", lambda e: e.activation(sp[:, cs], E[:, cs], AF.Ln, bias=1.0), reads=[E], writes=[sp])
                    if diag:
                        k.op("dve", lambda e: e.tensor_tensor(sp[:, c0:c0 + 128], sp[:, c0:c0 + 128], strictTb, ALU.mult),
                             reads=[sp, cstb], writes=[sp])

                def s2_peA(i):
                    hh, kb, fc, s, c0, diag = geom(i)
                    cs = slice(c0, 512)
                    C, sp = PC[s], spb[i % 3]
                    if kb == 4 * g + 3:
                        k.op("dve", lambda e: e.memset(C[:], 0.0), writes=[C])
                    k.op("pe", lambda e: e.matmul(C[:, cs], NGEb, sp[:, cs], start=False, stop=False, skip_group_check=True),
                         reads=[cstb, sp], writes=[C])

                def s2_act(i):
                    hh, kb, fc, s, c0, diag = geom(i)
                    cs = slice(c0, 512)
                    C, ea_ = PC[s], eab[i % 2]
                    k.op("act", lambda e: e.activation(ea_[:, cs], C[:, cs], AF.Exp), reads=[C], writes=[ea_])

                def s2_peB(i):
                    hh, kb, fc, s, c0, diag = geom(i)
                    cs = slice(c0, 512)
                    C, sp = PC[s], spb[i % 3]
                    if kb > 0:
                        k.op("pe", lambda e: e.matmul(C[:, cs], NLTb, sp[:, cs], start=False, stop=False, skip_group_check=True),
                             reads=[cstb, sp], writes=[C])

                def s3_pool(i):
                    hh, kb, fc, s, c0, diag = geom(i)
                    cs = slice(c0, 512)
                    E, ea_, a_ = Eb[i % 3], eab[i % 2], ab[i % 2]
                    k.op("pool", lambda e: e.tensor_tensor(a_[:, cs], E[:, cs], ea_[:, cs], ALU.mult), reads=[E, ea_], writes=[a_])
                    if diag:
                        k.op("dve", lambda e: e.tensor_tensor(a_[:, c0:c0 + 128], a_[:, c0:c0 + 128], strictTb, ALU.mult),
                             reads=[a_, cstb], writes=[a_])

                def s3_pe(i):
                    hh, kb, fc, s, c0, diag = geom(i)
                    cs = slice(c0, 512)
                    a_ = ab[i % 2]
                    po = PO[fc % 2]
                    if kb == 4 * g + 3 and s == 0:
                        k.op("dve", lambda e: e.memset(po[:], 0.0), writes=[po])
                    vblk = Vt[:, kb, hh * 64:(hh + 1) * 64]
                    if s == 0:
                        k.op("pe", lambda e: e.matmul(po[0:64, cs], vblk, a_[:, cs], start=False, stop=False, skip_group_check=True),
                             reads=[Vt, a_], writes=[po])
                    else:
                        k.op("pe", lambda e: e.matmul(po[64:128, cs], vblk, a_[:, cs], start=False, stop=False, skip_group_check=True,
                                                      tile_position=(0, 64)), reads=[Vt, a_], writes=[po])
                    if kb == 0 and s == 1:
                        k.op("act", lambda e: e.copy(mixTg[:, fc, :], po[:]), reads=[po], writes=[mixTg])

                n_it = len(items)
                for step in range(-2, n_it):
                    i1, i2_, i3 = step + 2, step + 1, step
                    if 0 <= i3:
                        s3_pool(i3)
                    if i1 < n_it:
                        s1_pe(i1)
                    if 0 <= i2_ < n_it:
                        s2_peA(i2_)
                    if i1 < n_it:
                        s1_act(i1)
                    if 0 <= i2_ < n_it:
                        s2_act(i2_)
                    if 0 <= i3:
                        s2_peB(i3)
                        s3_pe(i3)
                    for _d in range(NDUM):
                        k.op("pe", lambda e: e.matmul(self.ps[6][:], NGEb, spb[0][:], start=True, stop=True), reads=[], writes=[], inc=False)
                k.dma(wB[:], wout_d.rearrange("(c p) n -> p c n", p=128), reads=[wdb[1]], writes=[wB])
                for tt in range(4):
                    t = g * 4 + tt
                    b = t % 2
                    h = ht[b]
                    k.dma(h[:], hin[0][t * 128:(t + 1) * 128, :], reads=[hin[1][t]], writes=[h])
                    self.mem_attend(MW, mqT, tt * 128, memkT, memv, mmB, col0=0)
                    pb = nb()
                    for j in range(2):
                        k.op("pe", lambda e: e.transpose(pb[:, j * 128:(j + 1) * 128], mmB[:, j * 128:(j + 1) * 128], self.ident),
                             reads=[mmB, self.cst], writes=[pb], inc=(j == 1))
                    k.op("act", lambda e: e.copy(mixTg[:, 6:8, tt * 128:(tt + 1) * 128], pb[:, 0:256].rearrange("p (c t) -> p c t", c=2)),
                         reads=[pb], writes=[mixTg])
                    for half in range(2):
                        pb = nb()
                        for fc in range(8):
                            k.op("pe", lambda e: e.matmul(pb[:], mixTg[:, fc, tt * 128:(tt + 1) * 128], wB[:, fc, half * 512:(half + 1) * 512],
                                                          start=(fc == 0), stop=(fc == 7)),
                                 reads=[mixTg, wB], writes=[pb], inc=(fc == 7))
                        k.op("dve", lambda e: e.tensor_tensor(hn[:, half * 512:(half + 1) * 512], h[:, half * 512:(half + 1) * 512], pb[:], ALU.add),
                             reads=[h, pb], writes=[hn])
                    k.dma(hout[0][t * 128:(t + 1) * 128, :], hn[:], reads=[hn], writes=[hout[1][t]])
            k.barrier()
            del self.nextbank

    def build(self, phases):
        nc, k = self.nc, self.k
        P = self.P = {}
        shapes = dict(
            x=[S, D], mem=[256, D], a_norm=[1, D], a_w_in=[1, D, 3340], a_conv=[1, 4, 2304],
            a_log=[1, 6], a_dt_bias=[1, 6], a_out_gain=[1, 128], a_w_out=[1, D, D],
            kv_norm=[D], w_kv=[D, 1536], b_norm=[1, D], b_w_in=[1, D, D], b_w_out=[1, D, D],
            mem_norm=[2, D], w_mem_kv=[2, D, 512], ffn_norm=[2, D], w_group=[2, D, 4], b_group=[2, 4],
            w_router=[2, D, 16], b_router=[2, 16], w1=[2, 16, D, 256], w3=[2, 16, D, 256],
            w2=[2, 16, 256, D], final_norm=[D], wgr=[2, 128, 8, 20], rbias=[2, 20], convw=[128, 18, 4])
        for n, s in shapes.items():
            P[n] = self.din(n, s)
        out = nc.dram_tensor("out", [S, D], F32, kind="ExternalOutput").ap()
        mkbufs = lambda nm: [Buf(None, "%s%d" % (nm, i)) for i in range(NT)]
        hx = (P["x"], mkbufs("x"))
        hA = (self.dscratch("hA", [S, D]), mkbufs("hA"))
        hB = (self.dscratch("hB", [S, D]), mkbufs("hB"))
        ho = (out, mkbufs("out"))
        with ExitStack() as gst:
            self.load_consts(gst)
            self.epsbuf = self.sbuf(gst, "epsb", [128, 1], F32)
            self.epsb = self.epsbuf
            k.op("dve", lambda e: e.memset(self.epsbuf[:], EPS), writes=[self.epsbuf])
            cur = hx
            seq = {"A": hA, "M0": hB, "B": hA, "M1": ho}
            for ph in phases:
                dst = seq[ph] if ph != phases[-1] else ho
                if ph == "M0":
                    self.moe_phase(0, cur, dst, final=False)
                elif ph == "M1":
                    self.moe_phase(1, cur, dst, final=True)
                elif ph == "A":
                    self.mixer_a_phase(cur, dst)
                elif ph == "B":
                    self.mixer_b_phase(cur, dst)
                cur = dst
            for b in ho[1]:
                if b.lw is not None:
                    k._wait("sp", b.lw)
        return nc


def make_consts():
    c = np.zeros((128, 8, 128), np.float32)
    i = np.arange(128)
    c[:, 0, :] = np.eye(128)
    c[:, 1, :] = (i[:, None] <= i[None, :])
    c[:, 2, :] = (i[:, None] > i[None, :])
    c[:, 3, :] = (i[:, None] < i[None, :])
    c[:, 4, :] = 1.0
    c[:, 5, :] = -(i[:, None] >= i[None, :]).astype(np.float32)
    c[:, 6, :] = -(i[:, None] < i[None, :]).astype(np.float32)
    c[:, 7, :] = -30000.0 * (i[:, None] >= i[None, :])
    return c


_CACHE = {}


def run(inputs, phases=("A", "M0", "B", "M1"), ncores=NCORES, trace=False):
    key = tuple(phases)
    if key not in _CACHE:
        mk = MK(phases)
        _CACHE[key] = mk.build(list(phases))
    nc = _CACHE[key]
    consts = make_consts()
    inputs = dict(inputs)
    wg = np.concatenate([np.asarray(inputs["w_group"]), np.asarray(inputs["w_router"])], axis=2)
    inputs["wgr"] = np.ascontiguousarray(wg.reshape(2, 8, 128, 20).transpose(0, 2, 1, 3))
    inputs["rbias"] = np.concatenate([np.asarray(inputs["b_group"]), np.asarray(inputs["b_router"])], axis=1)
    cw = np.asarray(inputs["a_conv"])[0]
    inputs["convw"] = np.ascontiguousarray(cw.reshape(4, 18, 128).transpose(2, 1, 0))
    in_maps = []
    for c in range(ncores):
        m = {"consts": consts}
        for n, v in inputs.items():
            v = np.asarray(v)
            if n in ("x", "mem"):
                m[n] = np.ascontiguousarray(v[c])
            else:
                m[n] = np.ascontiguousarray(v, dtype=np.float32)
        in_maps.append(m)
    res = run_bass_kernel_spmd(nc, in_maps, core_ids=list(range(ncores)), trace=trace)
    outs = np.stack([r["out"] for r in res.results], axis=0)
    return outs, res


def kernel(**inputs):
    outs, _ = run(inputs)
    return outs.astype(np.float32)
```

```python
from contextlib import ExitStack
import os
import numpy as np
import concourse.bass as bass
import concourse.mybir as mybir
from concourse.bass_utils import run_bass_kernel_spmd

F32 = mybir.dt.float32
BF16 = mybir.dt.bfloat16
AF = mybir.ActivationFunctionType
ALU = mybir.AluOpType
AX = mybir.AxisListType

S = 4096
D = 1024
NT = S // 128
EPS = 1e-6
NCORES = 8


class Buf:
    __slots__ = ("ap", "name", "_lw", "_rd", "_excl")
    lw = property(lambda self: self._lw, lambda self, v: setattr(self, "_lw", v))
    rd = property(lambda self: self._rd, lambda self, v: setattr(self, "_rd", v))
    excl = property(lambda self: self._excl, lambda self, v: setattr(self, "_excl", v))

    def __init__(self, ap, name="", excl=False):
        self.ap = ap
        self.name = name
        self.excl = excl
        self.lw = None
        self.rd = []

    def __getitem__(self, idx):
        return self.ap[idx]


class View(Buf):
    __slots__ = ("parent",)

    def __init__(self, parent, ap):
        self.parent = parent
        self.ap = ap
        self.name = parent.name

    lw = property(lambda self: self.parent.lw, lambda self, v: setattr(self.parent, "lw", v))
    rd = property(lambda self: self.parent.rd, lambda self, v: setattr(self.parent, "rd", v))
    excl = property(lambda self: self.parent.excl, lambda self, v: None)


class K:
    NDMA = 48

    def __init__(self, nc):
        self.nc = nc
        self.eng = {"pe": nc.tensor, "act": nc.scalar, "dve": nc.vector,
                    "pool": nc.gpsimd, "sp": nc.sync}
        self.sem = {e: nc.alloc_semaphore("s_" + e) for e in ("pe", "act", "dve", "pool")}
        self.cnt = {e: 0 for e in self.sem}
        self.waited = {}
        self.dsem = [nc.alloc_semaphore("d%d" % i) for i in range(self.NDMA)]
        self.dcnt = [0] * self.NDMA
        self.dnext = 0
        self.nins = 0

    def _semh(self, key):
        return self.sem[key] if isinstance(key, str) else self.dsem[key]

    def _wait(self, e, dep):
        key, val = dep
        if key == e and e == "pe":
            return
        w = self.waited.get((e, key), 0)
        if w >= val:
            return
        self.eng[e].wait_ge(self._semh(key), val)
        self.nins += 1
        self.waited[(e, key)] = val

    def _deps(self, e, reads, writes):
        best = {}
        for r in reads:
            if r.lw is not None:
                if best.get(r.lw[0], 0) < r.lw[1]:
                    best[r.lw[0]] = r.lw[1]
            if r.excl:
                for key, val in r.rd:
                    if key != e and best.get(key, 0) < val:
                        best[key] = val
        for w in writes:
            if w.lw is not None:
                if best.get(w.lw[0], 0) < w.lw[1]:
                    best[w.lw[0]] = w.lw[1]
            for key, val in w.rd:
                if best.get(key, 0) < val:
                    best[key] = val
        for key, val in best.items():
            self._wait(e, (key, val))

    def _mark(self, tag, reads, writes):
        for r in reads:
            r.rd.append(tag)
            if len(r.rd) > 64:
                best = {}
                for key, val in r.rd:
                    if best.get(key, 0) < val:
                        best[key] = val
                r.rd = list(best.items())
        for w in writes:
            w.lw = tag
            w.rd = []

    def op(self, e, fn, reads=(), writes=(), inc=True):
        self._deps(e, reads, writes)
        ins = fn(self.eng[e])
        self.nins += 1
        if inc:
            ins.then_inc(self.sem[e], 1)
            self.cnt[e] += 1
            tag = (e, self.cnt[e])
        else:
            tag = (e, self.cnt[e] + 1)
        self._mark(tag, reads, writes)
        return ins

    def dma(self, out, in_, reads=(), writes=(), q="sp", **kw):
        slot = self.dnext
        self.dnext = (self.dnext + 1) % self.NDMA
        if self.dcnt[slot] > 0:
            self._wait(q, (slot, 16 * self.dcnt[slot]))
        self._deps(q, reads, writes)
        ins = self.eng[q].dma_start(out=out, in_=in_, **kw)
        self.nins += 1
        ins.then_inc(self.dsem[slot], 16)
        self.dcnt[slot] += 1
        tag = (slot, 16 * self.dcnt[slot])
        self._mark(tag, reads, writes)
        return tag

    def barrier(self):
        for e in ("pe", "act", "dve", "pool", "sp"):
            for e2 in ("pe", "act", "dve", "pool"):
                if e2 != e and self.cnt[e2] > 0:
                    self._wait(e, (e2, self.cnt[e2]))
            for slot in range(self.NDMA):
                if self.dcnt[slot] > 0:
                    self._wait(e, (slot, 16 * self.dcnt[slot]))


class MK:
    def __init__(self, phases, h0_from_input=True):
        self.nc = nc = bass.Bass("TRN2", target_bir_lowering=False)
        self.k = K(nc)
        self.uid = 0
        self.ins = {}
        self.ps = [Buf(nc.alloc_psum_tensor("psb%d" % i, [128, 512], F32).ap(), "ps%d" % i, excl=True)
                   for i in range(8)]

    def din(self, name, shape):
        ap = self.nc.dram_tensor(name, list(shape), F32, kind="ExternalInput").ap()
        self.ins[name] = ap
        return ap

    def dscratch(self, name, shape, dt=F32):
        return self.nc.dram_tensor(name, list(shape), dt, kind="Internal").ap()

    def sbuf(self, st, name, shape, dt):
        self.uid += 1
        h = st.enter_context(self.nc.sbuf_tensor("%s_%d" % (name, self.uid), list(shape), dt))
        return Buf(h.ap(), name)

    def load_consts(self, st):
        k = self.k
        c = self.din("consts", [128, 8, 128])
        self.cst = self.sbuf(st, "cst", [128, 8, 128], F32)
        k.dma(self.cst[:], c, writes=[self.cst])
        self.ident = self.cst[:, 0, :]
        self.U = self.cst[:, 1, :]
        self.SL = self.cst[:, 2, :]
        self.strictT = self.cst[:, 3, :]
        self.ones = self.cst[:, 4, :]
        self.cstb = self.sbuf(st, "cstb", [128, 8, 128], BF16)
        k.dma(self.cstb[:], c, writes=[self.cstb], q="pool")
        self.identb = self.cstb[:, 0, :]
        self.onesb = self.cstb[:, 4, :]

    def rmsnorm(self, h, gainb, xn, junk, st2):
        k = self.k
        k.op("act", lambda e: e.activation(junk[:], h[:], AF.Square, accum_out=st2[:, 0:1]),
             reads=[h], writes=[junk, st2])
        k.op("act", lambda e: e.activation(st2[:, 1:2], st2[:, 0:1], AF.Sqrt, bias=self.epsb[:, 0:1], scale=1.0 / D),
             reads=[st2, self.epsbuf], writes=[st2])
        k.op("dve", lambda e: e.reciprocal(st2[:, 2:3], st2[:, 1:2]), reads=[st2], writes=[st2])
        k.op("dve", lambda e: e.scalar_tensor_tensor(xn[:], h[:], st2[:, 2:3], gainb[:], ALU.mult, ALU.mult),
             reads=[h, st2, gainb], writes=[xn])

    def transpose8(self, src, dsts, psa, psb, evac=("act", "dve"), second="pool"):
        k = self.k
        for half, ps in enumerate((psa, psb)):
            for j in range(4):
                c = half * 4 + j
                k.op("pe", lambda e: e.transpose(ps[:, j * 128:(j + 1) * 128], src[:, c * 128:(c + 1) * 128], self.ident),
                     reads=[src, self.cst], writes=[ps], inc=(j == 3))
            dbuf, fn = dsts[0]
            eng = evac[half % len(evac)]
            pv = ps[:].rearrange("p (c t) -> p c t", c=4)
            if eng == "act":
                k.op("act", lambda e: e.copy(fn(half), pv), reads=[ps], writes=[dbuf])
            else:
                k.op(eng, lambda e: e.tensor_copy(fn(half), pv), reads=[ps], writes=[dbuf])
            for dbuf2, fn2 in dsts[1:]:
                k.op(second, lambda e: e.tensor_copy(fn2(half), fn(half)), reads=[dbuf], writes=[dbuf2])

    def moe_phase(self, l, hin, hout, final=False, out_ap=None):
        nc, k = self.nc, self.k
        G = 1024
        NTG = G // 128
        NG = S // G
        NTB = G // 512
        NEX = 16
        P = self.P
        with ExitStack() as st:
            sb = lambda n, s, d: self.sbuf(st, n, s, d)
            gain = sb("gain", [128, D], F32)
            k.dma(gain[:], P["ffn_norm"][l].partition_broadcast(128), writes=[gain])
            if final:
                fgain = sb("fgain", [128, D], F32)
                k.dma(fgain[:], P["final_norm"].partition_broadcast(128), writes=[fgain])
            wgr = sb("wgr", [128, 8, 20], F32)
            k.dma(wgr[:], P["wgr"][l], writes=[wgr])
            rb = sb("rbias", [128, 20], F32)
            k.dma(rb[:], P["rbias"][l].partition_broadcast(128), writes=[rb])
            xnT = [sb("xnT%d" % i, [128, 8, G], BF16) for i in range(2)]
            yacc = [[sb("yacc%d_%d" % (j, i), [128, D], F32) for i in range(NTG)] for j in range(2)]
            comb = [[sb("comb%d_%d" % (j, i), [128, 16], F32) for i in range(NTG)] for j in range(2)]
            w1b = [sb("w1b%d" % i, [128, 8, 256], BF16) for i in range(2)]
            w3b = [sb("w3b%d" % i, [128, 8, 256], BF16) for i in range(2)]
            w2b = [sb("w2b%d" % i, [128, 2, D], BF16) for i in range(2)]
            ht = [sb("ht%d" % i, [128, D], F32) for i in range(2)]
            htc = [sb("htc%d" % i, [128, D], F32) for i in range(2)]
            xn = [sb("xn%d" % i, [128, D], F32) for i in range(2)]
            xnT32 = [sb("xnT32_%d" % i, [128, 8, 128], F32) for i in range(2)]
            stt = [sb("stt%d" % i, [128, 4], F32) for i in range(2)]
            sttc = [sb("sttc%d" % i, [128, 4], F32) for i in range(2)]
            rt = [sb("rt%d" % i, [128, 96], F32) for i in range(2)]
            hid = [sb("hid%d" % i, [128, 2, 512], BF16) for i in range(2)]
            sil = [sb("sil%d" % i, [128, 512], F32) for i in range(2)]
            ps = self.ps
            w1d, w3d, w2d = P["w1"], P["w3"], P["w2"]

            def load_w(e, slot):
                k.dma(w1b[slot][:], w1d[l, e].rearrange("(c p) f -> p c f", p=128), writes=[w1b[slot]], q="pool")
                k.dma(w3b[slot][:], w3d[l, e].rearrange("(c p) f -> p c f", p=128), writes=[w3b[slot]], q="pool")
                k.dma(w2b[slot][:], w2d[l, e].rearrange("(c p) n -> p c n", p=128), writes=[w2b[slot]], q="pool")

            def stage_a(g):
                par = g % 2
                xT = xnT[par]
                for ti in range(NTG):
                    t = g * NTG + ti
                    b = ti % 2
                    h = ht[b]
                    k.dma(h[:], hin[0][t * 128:(t + 1) * 128, :], reads=[hin[1][t]], writes=[h])
                    self.rmsnorm(h, gain, xn[b], xn[b], stt[b])
                    yield
                    x32 = xnT32[b]
                    self.transpose8(
                        xn[b],
                        [(x32, lambda half: x32[:, half * 4:(half + 1) * 4, :]),
                         (xT, lambda half: xT[:, half * 4:(half + 1) * 4, ti * 128:(ti + 1) * 128])],
                        ps[6], ps[7])
                    yield
                    pr = ps[6 + (ti % 2)]
                    for dc in range(8):
                        k.op("pe", lambda e: e.matmul(pr[:, 0:20], x32[:, dc, :], wgr[:, dc, :], start=(dc == 0), stop=(dc == 7)),
                             reads=[x32, wgr], writes=[pr], inc=(dc == 7))
                    yield
                    r = rt[b]
                    R = lambda a, n: r[:, a:a + n]
                    lg, gmax, ngmax, oh, ge, gsum, pg = R(0, 20), R(20, 1), R(21, 1), R(22, 4), R(26, 4), R(30, 1), R(31, 1)
                    tmp, elsel, m1, nm1, ee, mask1, ee2 = R(32, 16), R(48, 4), R(52, 1), R(53, 1), R(54, 4), R(58, 4), R(62, 4)
                    v2, mask2, den, rden, wl, scl = R(66, 1), R(67, 4), R(71, 1), R(72, 1), R(73, 4), R(77, 1)
                    dv = lambda fn, rd=(), wr=(): k.op("dve", fn, reads=[r] + list(rd), writes=[r] + list(wr))
                    dv(lambda e: e.tensor_tensor(lg, pr[:, 0:20], rb[:], ALU.add), rd=[pr, rb])
                    dv(lambda e: e.tensor_reduce(gmax, lg[:, 0:4], AX.X, ALU.max))
                    dv(lambda e: e.tensor_single_scalar(ngmax, gmax, -1.0, ALU.mult))
                    dv(lambda e: e.tensor_scalar(oh, lg[:, 0:4], gmax, None, ALU.is_equal))
                    yield
                    k.op("act", lambda e: e.activation(ge, lg[:, 0:4], AF.Exp, bias=ngmax, accum_out=gsum), reads=[r], writes=[r])
                    dv(lambda e: e.reciprocal(pg, gsum))
                    dv(lambda e: e.tensor_tensor(tmp.rearrange("p (g j) -> p g j", g=4),
                                                 lg[:, 4:20].rearrange("p (g j) -> p g j", g=4),
                                                 oh.unsqueeze(2).to_broadcast([128, 4, 4]), ALU.mult))
                    dv(lambda e: e.tensor_reduce(elsel, tmp.rearrange("p (g j) -> p j g", g=4), AX.X, ALU.add))
                    yield
                    dv(lambda e: e.tensor_reduce(m1, elsel, AX.X, ALU.max))
                    dv(lambda e: e.tensor_single_scalar(nm1, m1, -1.0, ALU.mult))
                    k.op("act", lambda e: e.activation(ee, elsel, AF.Exp, bias=nm1), reads=[r], writes=[r])
                    dv(lambda e: e.tensor_scalar(mask1, elsel, m1, None, ALU.is_equal))
                    yield
                    dv(lambda e: e.scalar_tensor_tensor(ee2, mask1, -2.0, ee, ALU.mult, ALU.add))
                    dv(lambda e: e.tensor_reduce(v2, ee2, AX.X, ALU.max))
                    dv(lambda e: e.tensor_scalar(mask2, ee2, v2, None, ALU.is_equal))
                    dv(lambda e: e.tensor_single_scalar(den, v2, 1.0, ALU.add))
                    yield
                    dv(lambda e: e.reciprocal(rden, den))
                    dv(lambda e: e.scalar_tensor_tensor(wl, mask2, v2, mask1, ALU.mult, ALU.add))
                    dv(lambda e: e.tensor_tensor(scl, pg, rden, ALU.mult))
                    dv(lambda e: e.tensor_scalar(wl, wl, scl, None, ALU.mult))
                    cb = comb[par][ti]
                    dv(lambda e: e.tensor_tensor(cb[:].rearrange("p (g j) -> p g j", g=4),
                                                 oh.unsqueeze(2).to_broadcast([128, 4, 4]),
                                                 wl.unsqueeze(1).to_broadcast([128, 4, 4]), ALU.mult), wr=[cb])
                    yield

            def stage_b(g):
                par = g % 2
                xT = xnT[par]
                work = [(ex, tb) for ex in range(NEX) for tb in range(NTB)]

                def up(idx):
                    ex, tb = work[idx]
                    slot = ex % 2
                    w1, w3 = w1b[slot], w3b[slot]
                    hd = hid[idx % 2]
                    for fc in range(2):
                        p1 = ps[fc]
                        p3 = ps[2 + fc]
                        for wsrc, pdst in ((w1, p1), (w3, p3)):
                            for dc in range(8):
                                k.op("pe", lambda e: e.matmul(pdst[:], wsrc[:, dc, fc * 128:(fc + 1) * 128], xT[:, dc, tb * 512:(tb + 1) * 512],
                                                              start=(dc == 0), stop=(dc == 7)),
                                     reads=[wsrc, xT], writes=[pdst], inc=(dc == 7))
                                if dc % 4 == 3:
                                    yield
                        sl = sil[fc]
                        k.op("act", lambda e: e.activation(sl[:], p1[:], AF.Silu), reads=[p1], writes=[sl])
                        k.op("dve", lambda e: e.tensor_tensor(hd[:, fc, :], sl[:], p3[:], ALU.mult), reads=[sl, p3], writes=[hd])

                def down(idx):
                    ex, tb = work[idx]
                    slot = ex % 2
                    w2 = w2b[slot]
                    hd = hid[idx % 2]
                    for tt in range(4):
                        ti = tb * 4 + tt
                        for half in range(2):
                            py = ps[4 + half]
                            for fc in range(2):
                                k.op("pe", lambda e: e.matmul(py[:], hd[:, fc, tt * 128:(tt + 1) * 128], w2[:, fc, half * 512:(half + 1) * 512],
                                                              start=(fc == 0), stop=(fc == 1)),
                                     reads=[hd, w2], writes=[py], inc=(fc == 1))
                            ya = yacc[par][ti]
                            cs = comb[par][ti][:, ex:ex + 1]
                            if ex == 0:
                                k.op("dve", lambda e: e.tensor_scalar(ya[:, half * 512:(half + 1) * 512], py[:], cs, None, ALU.mult),
                                     reads=[py, comb[par][ti]], writes=[ya])
                            else:
                                k.op("dve", lambda e: e.scalar_tensor_tensor(ya[:, half * 512:(half + 1) * 512], py[:], cs,
                                                                             ya[:, half * 512:(half + 1) * 512], ALU.mult, ALU.add),
                                     reads=[py, comb[par][ti], ya], writes=[ya])
                            yield
                    if tb == NTB - 1:
                        if ex + 2 < NEX:
                            load_w(ex + 2, slot)
                        elif g + 1 < NG:
                            load_w(ex + 2 - NEX, slot)

                def rr(*gens):
                    gens = [g_ for g_ in gens if g_ is not None]
                    while gens:
                        for g_ in list(gens):
                            try:
                                next(g_)
                                yield
                            except StopIteration:
                                gens.remove(g_)

                yield from up(0)
                for idx in range(len(work)):
                    nu = up(idx + 1) if idx + 1 < len(work) else None
                    if nu is not None:
                        next(nu)
                        yield
                    yield from rr(nu, down(idx))

            def stage_c(g):
                par = g % 2
                for ti in range(NTG):
                    t = g * NTG + ti
                    b = ti % 2
                    h = htc[b]
                    ya = yacc[par][ti]
                    k.dma(h[:], hin[0][t * 128:(t + 1) * 128, :], reads=[hin[1][t]], writes=[h])
                    k.op("pool", lambda e: e.tensor_tensor(ya[:], h[:], ya[:], ALU.add), reads=[h, ya], writes=[ya])
                    yield
                    if final:
                        self.rmsnorm(ya, fgain, h, h, sttc[b])
                        k.dma(hout[0][t * 128:(t + 1) * 128, :], h[:], reads=[h], writes=[hout[1][t]])
                    else:
                        k.dma(hout[0][t * 128:(t + 1) * 128, :], ya[:], reads=[ya], writes=[hout[1][t]])
                    yield

            def run_group(bg, ag, cg):
                others = [[g_, per] for g_, per in ((ag, 4), (cg, 24)) if g_ is not None]
                step = 0
                b_alive = bg is not None
                while b_alive or others:
                    if b_alive:
                        try:
                            next(bg)
                        except StopIteration:
                            b_alive = False
                    for item in list(others):
                        if (not b_alive) or step % item[1] == 0:
                            try:
                                next(item[0])
                            except StopIteration:
                                others.remove(item)
                    step += 1

            load_w(0, 0)
            load_w(1, 1)
            run_group(None, stage_a(0), None)
            for g in range(NG):
                run_group(stage_b(g), stage_a(g + 1) if g + 1 < NG else None, stage_c(g - 1) if g >= 1 else None)
            run_group(None, None, stage_c(NG - 1))
            k.barrier()

    def nextbank(self):
        self.pbi = (getattr(self, "pbi", -1) + 1) % 8
        return self.ps[self.pbi]

    def mem_kv(self, st, l):
        k, P = self.k, self.P
        sb = lambda n, s, d: self.sbuf(st, n, s, d)
        memkT = sb("memkT", [128, 2, 256], BF16)
        memv = sb("memv", [128, 2, 256], BF16)
        with ExitStack() as st2:
            sb2 = lambda n, s, d: self.sbuf(st2, n, s, d)
            g = sb2("mg", [128, D], F32)
            k.dma(g[:], P["mem_norm"][l].partition_broadcast(128), writes=[g])
            w = sb2("wmkv", [128, 8, 512], BF16)
            k.dma(w[:], P["w_mem_kv"][l].rearrange("(c p) n -> p c n", p=128), writes=[w], q="pool")
            mT = sb2("memnT", [128, 8, 256], BF16)
            junk = sb2("mjunk", [128, D], F32)
            for mt in range(2):
                h = sb2("mh%d" % mt, [128, D], F32)
                xn = sb2("mxn%d" % mt, [128, D], F32)
                stt = sb2("mst%d" % mt, [128, 4], F32)
                k.dma(h[:], P["mem"][mt * 128:(mt + 1) * 128, :], writes=[h])
                self.rmsnorm(h, g, xn, junk, stt)
                self.transpose8(xn, [(mT, lambda half: mT[:, half * 4:(half + 1) * 4, mt * 128:(mt + 1) * 128])],
                                self.nextbank(), self.nextbank())
            for j in range(2):
                pb = self.nextbank()
                for dc in range(8):
                    k.op("pe", lambda e: e.matmul(pb[:, 0:256], w[:, dc, j * 128:(j + 1) * 128], mT[:, dc, :],
                                                  start=(dc == 0), stop=(dc == 7)),
                         reads=[w, mT], writes=[pb], inc=(dc == 7))
                k.op("act", lambda e: e.copy(memkT[:, j, :], pb[:, 0:256]), reads=[pb], writes=[memkT])
            for mt in range(2):
                pb = self.nextbank()
                for dc in range(8):
                    k.op("pe", lambda e: e.matmul(pb[:, 0:256], mT[:, dc, mt * 128:(mt + 1) * 128], w[:, dc, 256:512],
                                                  start=(dc == 0), stop=(dc == 7)),
                         reads=[w, mT], writes=[pb], inc=(dc == 7))
                k.op("dve", lambda e: e.tensor_copy(memv[:, mt, :], pb[:, 0:256]), reads=[pb], writes=[memv])
            k.barrier()
        return memkT, memv

    def mem_attend(self, W, mqT, qoff, memkT, memv, mix, col0=768):
        k = self.k
        pe_ = W["pexp"]; ms = W["mstat"]; pT = W["pT"]
        banks = [self.nextbank(), self.nextbank()]
        for hh in range(4):
            pair, s = hh // 2, hh % 2
            pb = banks[s]
            k.op("pe", lambda e: e.matmul(pb[:, pair * 256:(pair + 1) * 256], mqT[s * 64:(s + 1) * 64, pair, qoff:qoff + 128],
                                          memkT[s * 64:(s + 1) * 64, pair, :], start=True, stop=True),
                 reads=[mqT, memkT], writes=[pb])
        for s in range(2):
            pb = banks[s]
            k.op("dve", lambda e: e.tensor_reduce(ms[:, s:s + 3:2], pb[:].rearrange("p (h m) -> p h m", h=2), AX.X, ALU.max),
                 reads=[pb], writes=[ms])
        k.op("dve", lambda e: e.tensor_single_scalar(ms[:, 4:8], ms[:, 0:4], -0.125, ALU.mult), reads=[ms], writes=[ms])
        for hh in range(4):
            pair, s = hh // 2, hh % 2
            pb = banks[s]
            k.op("act", lambda e: e.activation(pe_[:, hh, :], pb[:, pair * 256:(pair + 1) * 256], AF.Exp, bias=ms[:, 4 + hh:5 + hh],
                                               scale=0.125, accum_out=ms[:, 8 + hh:9 + hh]),
                 reads=[pb, ms], writes=[pe_, ms])
        k.op("dve", lambda e: e.reciprocal(ms[:, 12:16], ms[:, 8:12]), reads=[ms], writes=[ms])
        for half in range(2):
            pb = self.nextbank()
            for j in range(4):
                idx = half * 4 + j
                hh, mc = idx // 2, idx % 2
                k.op("pe", lambda e: e.transpose(pb[:, j * 128:(j + 1) * 128], pe_[:, hh, mc * 128:(mc + 1) * 128], self.ident),
                     reads=[pe_, self.cst], writes=[pb], inc=(j == 3))
            if half == 0:
                k.op("act", lambda e: e.copy(pT[:, 0:4, :], pb[:].rearrange("p (c t) -> p c t", c=4)), reads=[pb], writes=[pT])
            else:
                k.op("dve", lambda e: e.tensor_copy(pT[:, 4:8, :], pb[:].rearrange("p (c t) -> p c t", c=4)), reads=[pb], writes=[pT])
        pb = self.nextbank()
        for hh in range(4):
            for mc in range(2):
                k.op("pe", lambda e: e.matmul(pb[:, hh * 64:(hh + 1) * 64], pT[:, hh * 2 + mc, :], memv[:, mc, hh * 64:(hh + 1) * 64],
                                              start=(mc == 0), stop=(mc == 1)),
                     reads=[pT, memv], writes=[pb], inc=(mc == 1))
        k.op("dve", lambda e: e.tensor_tensor(mix[:, col0:col0 + 256].rearrange("p (h d) -> p h d", h=4),
                                              pb[:, 0:256].rearrange("p (h d) -> p h d", h=4),
                                              ms[:, 12:16].unsqueeze(2).to_broadcast([128, 4, 64]), ALU.mult),
             reads=[pb, ms], writes=[mix])

    def mem_work(self, st):
        sb = lambda n, s, d: self.sbuf(st, n, s, d)
        return {"pexp": sb("pexp", [128, 4, 256], F32), "mstat": sb("mstat", [128, 16], F32),
                "pT": sb("pT", [128, 8, 128], BF16)}

    def out_proj(self, mix, mixT, w_out, h, hn, dst_ap, dst_buf):
        k = self.k
        self.transpose8(mix, [(mixT, lambda half: mixT[:, half * 4:(half + 1) * 4, :])], self.nextbank(), self.nextbank())
        for half in range(2):
            pb = self.nextbank()
            for fc in range(8):
                k.op("pe", lambda e: e.matmul(pb[:], mixT[:, fc, :], w_out[:, fc, half * 512:(half + 1) * 512],
                                              start=(fc == 0), stop=(fc == 7)),
                     reads=[mixT, w_out], writes=[pb], inc=(fc == 7))
            k.op("dve", lambda e: e.tensor_tensor(hn[:, half * 512:(half + 1) * 512], h[:, half * 512:(half + 1) * 512], pb[:], ALU.add),
                 reads=[h, pb], writes=[hn])
        k.dma(dst_ap, hn[:], reads=[hn], writes=[dst_buf])

    def mixer_a_phase(self, hin, hout):
        nc, k, P = self.nc, self.k, self.P
        NTA = int(os.environ.get("MK_NTA", str(NT)))
        with ExitStack() as st:
            sb = lambda n, s, d: self.sbuf(st, n, s, d)
            memkT, memv = self.mem_kv(st, 0)
            gain = sb("gainA", [128, D], F32)
            k.dma(gain[:], P["a_norm"][0].partition_broadcast(128), writes=[gain])
            w_in = sb("w_inA", [128, 8, 3340], BF16)
            for c in range(8):
                k.dma(w_in[:, c, :], P["a_w_in"][0, c * 128:(c + 1) * 128, :], writes=[w_in], q="pool")
            w_out = sb("w_outA", [128, 8, D], BF16)
            k.dma(w_out[:], P["a_w_out"][0].rearrange("(c p) n -> p c n", p=128), writes=[w_out], q="pool")
            convw = sb("convw", [128, 18, 4], F32)
            k.dma(convw[:], P["convw"], writes=[convw])
            sc6 = sb("sc6", [128, 32], F32)
            k.dma(sc6[:, 0:6], P["a_log"][0].partition_broadcast(128), writes=[sc6])
            k.dma(sc6[:, 6:12], P["a_dt_bias"][0].partition_broadcast(128), writes=[sc6])
            k.op("act", lambda e: e.activation(sc6[:, 12:18], sc6[:, 0:6], AF.Exp), reads=[sc6], writes=[sc6])
            k.op("dve", lambda e: e.tensor_single_scalar(sc6[:, 12:18], sc6[:, 12:18], -1.0, ALU.mult), reads=[sc6], writes=[sc6])
            ogain = sb("ogain", [128, 128], F32)
            k.dma(ogain[:], P["a_out_gain"][0].partition_broadcast(128), writes=[ogain])
            MW = self.mem_work(st)
            pc = sb("pc", [128, 18, 131], F32)
            k.op("dve", lambda e: e.memset(pc[:], 0.0), writes=[pc])
            Sf = [sb("Sf%d" % h, [128, 128], F32) for h in range(6)]
            Sb = [sb("Sb%d" % h, [128, 128], BF16) for h in range(6)]
            for h in range(6):
                k.op("dve", lambda e: e.memset(Sf[h][:], 0.0), writes=[Sf[h]])
                k.op("pool", lambda e: e.memset(Sb[h][:], 0.0), writes=[Sb[h]])
            xn = sb("xnA", [128, D], F32)
            xT = sb("xnTA", [128, 8, 128], BF16)
            cv = sb("cv", [128, 18, 128], F32)
            sq = sb("sq", [128, 12, 128], BF16)
            rs = sb("rs", [128, 12, 128], F32)
            kn32 = sb("kn32", [128, 6, 128], F32)
            SLg = [sb("SLg%d" % i, [128, 128], F32) for i in range(2)]
            dm = sb("dm", [128, 6, 128], F32)
            dmi = sb("dmi", [128, 6, 128], F32)
            Wq = [[sb("W%d_%d" % (h, i), [128, 3, 128], BF16) for i in range(2)] for h in range(6)]
            Q0f = [sb("Q0f%d" % i, [128, 128], F32) for i in range(2)]
            stt = [sb("sttA%d" % i, [128, 4], F32) for i in range(2)]
            ht = [sb("htA%d" % i, [128, D], F32) for i in range(2)]
            qkT = [sb("qkT%d" % i, [128, 6, 2, 128], BF16) for i in range(2)]
            kt = [sb("kt%d" % i, [128, 6, 128], BF16) for i in range(2)]
            vtok = [sb("vtok%d" % i, [128, 6, 128], F32) for i in range(2)]
            attnT = [sb("attnT%d" % i, [128, 6, 128], BF16) for i in range(2)]
            TT = [sb("TT%d" % i, [128, 6, 128], BF16) for i in range(2)]
            sc = [sb("scA%d" % i, [128, 96], F32) for i in range(2)]
            gg = [sb("gg%d" % i, [128, 768], F32) for i in range(2)]
            mqT = [sb("mqTA%d" % i, [128, 2, 128], BF16) for i in range(2)]
            Rb = [sb("R%d" % i, [128, 128], BF16) for i in range(2)]
            vnew = [sb("vnew%d" % i, [128, 128], BF16) for i in range(2)]
            o1 = [sb("o1_%d" % i, [128, 128], F32) for i in range(2)]
            ob = [sb("ob%d" % i, [128, 128], F32) for i in range(2)]
            ost = [sb("ost%d" % i, [128, 4], F32) for i in range(2)]
            ojunk = sb("ojunk", [128, 128], F32)
            mix = sb("mixA", [128, D], F32)
            mixT = sb("mixTA", [128, 8, 128], BF16)
            hn = sb("hnA", [128, D], F32)
            ident, U, SL, ones = self.ident, self.U, self.SL, self.ones
            NEGs = self.cst[:, 7, :]
            identb = self.identb
            cst, cstb = self.cst, self.cstb
            QS = float(128 ** -0.5)

            def front(t):
                b = t % 2
                h = ht[b]
                k.dma(h[:], hin[0][t * 128:(t + 1) * 128, :], reads=[hin[1][t]], writes=[h])
                self.rmsnorm(h, gain, xn, xn, stt[b])
                self.transpose8(xn, [(xT, lambda half: xT[:, half * 4:(half + 1) * 4, :])], self.nextbank(), self.nextbank())
                for g4 in range(5):
                    nf = 4 if g4 < 4 else 2
                    pb = self.nextbank()
                    for j in range(nf):
                        fc = g4 * 4 + j
                        for dc in range(8):
                            k.op("pe", lambda e: e.matmul(pb[:, j * 128:(j + 1) * 128], w_in[:, dc, fc * 128:(fc + 1) * 128], xT[:, dc, :],
                                                          start=(dc == 0), stop=(dc == 7)),
                                 reads=[w_in, xT], writes=[pb], inc=(dc == 7 and j == nf - 1))
                    dstv = pc[:, g4 * 4:g4 * 4 + nf, 3:131]
                    srcv = pb[:, 0:nf * 128].rearrange("p (c t) -> p c t", c=nf)
                    k.op("act", lambda e: e.copy(dstv, srcv), reads=[pb], writes=[pc])
                pb = self.nextbank()
                for j in range(2):
                    for dc in range(8):
                        k.op("pe", lambda e: e.matmul(pb[:, j * 128:(j + 1) * 128], w_in[:, dc, 3084 + j * 128:3084 + (j + 1) * 128], xT[:, dc, :],
                                                      start=(dc == 0), stop=(dc == 7)),
                             reads=[w_in, xT], writes=[pb], inc=(dc == 7 and j == 1))
                k.op("act", lambda e: e.copy(mqT[b][:], pb[:, 0:256].rearrange("p (c t) -> p c t", c=2)), reads=[pb], writes=[mqT[b]])
                pg1 = self.nextbank()
                for dc in range(8):
                    k.op("pe", lambda e: e.matmul(pg1[:], xT[:, dc, :], w_in[:, dc, 2304:2816], start=(dc == 0), stop=(dc == 7)),
                         reads=[w_in, xT], writes=[pg1], inc=(dc == 7))
                pg2 = self.nextbank()
                for dc in range(8):
                    k.op("pe", lambda e: e.matmul(pg2[:, 0:268], xT[:, dc, :], w_in[:, dc, 2816:3084], start=(dc == 0), stop=(dc == 7)),
                         reads=[w_in, xT], writes=[pg2], inc=(dc == 7))
                g_g = gg[b]
                k.op("act", lambda e: e.activation(g_g[:, 0:512], pg1[:], AF.Silu), reads=[pg1], writes=[g_g])
                k.op("act", lambda e: e.activation(g_g[:, 512:768], pg2[:, 0:256], AF.Silu), reads=[pg2], writes=[g_g])
                s_ = sc[b]
                C = lambda a_, n=6: s_[:, a_:a_ + n]
                beta, tt_, ex_, sp_, g_, gcl, egc, negc, etl, egl, dd = (C(0), C(6), C(12), C(18), C(24), C(32, 16), C(48), C(54), C(60), C(66), C(72))
                k.op("act", lambda e: e.activation(beta, pg2[:, 256:262], AF.Sigmoid), reads=[pg2], writes=[s_])
                k.op("dve", lambda e: e.tensor_tensor(tt_, pg2[:, 262:268], sc6[:, 6:12], ALU.add), reads=[pg2, sc6], writes=[s_])
                k.op("act", lambda e: e.activation(ex_, tt_, AF.Exp), reads=[s_], writes=[s_])
                k.op("act", lambda e: e.activation(sp_, ex_, AF.Ln, bias=1.0), reads=[s_], writes=[s_])
                k.op("dve", lambda e: e.tensor_tensor(g_, sp_, sc6[:, 12:18], ALU.mult), reads=[s_, sc6], writes=[s_])
                k.op("pool", lambda e: e.tensor_tensor(g_g[:].rearrange("p (h d) -> p h d", h=6), g_g[:].rearrange("p (h d) -> p h d", h=6),
                                                       ogain[:].unsqueeze(1).to_broadcast([128, 6, 128]), ALU.mult),
                     reads=[g_g, ogain], writes=[g_g])
                pgc = self.nextbank()
                k.op("pe", lambda e: e.matmul(pgc[:, 0:6], U, g_, start=True, stop=True), reads=[cst, s_], writes=[pgc])
                k.op("pe", lambda e: e.matmul(pgc[:, 8:14], ones, g_, start=True, stop=True), reads=[cst, s_], writes=[pgc])
                k.op("dve", lambda e: e.tensor_copy(gcl, pgc[:, 0:16]), reads=[pgc], writes=[s_])
                k.op("act", lambda e: e.activation(egc, s_[:, 32:38], AF.Exp), reads=[s_], writes=[s_])
                k.op("dve", lambda e: e.tensor_single_scalar(negc, egc, -1.0, ALU.mult), reads=[s_], writes=[s_])
                k.op("dve", lambda e: e.tensor_tensor(dd, s_[:, 40:46], s_[:, 32:38], ALU.subtract), reads=[s_], writes=[s_])
                k.op("act", lambda e: e.activation(etl, dd, AF.Exp), reads=[s_], writes=[s_])
                k.op("act", lambda e: e.activation(egl, s_[:, 40:46], AF.Exp), reads=[s_], writes=[s_])
                yield
                for fc in range(18):
                    o_ = cv[:, fc, :]
                    k.op("dve", lambda e: e.tensor_scalar(o_, pc[:, fc, 0:128], convw[:, fc, 0:1], None, ALU.mult),
                         reads=[pc, convw], writes=[cv])
                    for j in range(1, 4):
                        k.op("dve", lambda e: e.scalar_tensor_tensor(o_, pc[:, fc, j:j + 128], convw[:, fc, j:j + 1], o_, ALU.mult, ALU.add),
                             reads=[pc, convw, cv], writes=[cv])
                k.op("pool", lambda e: e.tensor_copy(pc[:, :, 0:3], pc[:, :, 128:131]), reads=[pc], writes=[pc])
                k.op("act", lambda e: e.activation(cv[:], cv[:], AF.Silu), reads=[cv], writes=[cv])
                qkv = cv
                k.op("act", lambda e: e.activation(sq[:], qkv[:, 0:12, :], AF.Square), reads=[qkv], writes=[sq])
                for g3 in range(3):
                    pb = self.nextbank()
                    k.op("pe", lambda e: e.matmul(pb[:], self.onesb, sq[:, g3 * 4:(g3 + 1) * 4, :], start=True, stop=True),
                         reads=[cstb, sq], writes=[pb])
                    k.op("act", lambda e: e.activation(rs[:, g3 * 4:(g3 + 1) * 4, :], pb[:].rearrange("p (c t) -> p c t", c=4), AF.Ln,
                                                       bias=self.epsb[:, 0:1]), reads=[pb, self.epsbuf], writes=[rs])
                    k.op("act", lambda e: e.activation(rs[:, g3 * 4:(g3 + 1) * 4, :], rs[:, g3 * 4:(g3 + 1) * 4, :], AF.Exp, scale=-0.5),
                         reads=[rs], writes=[rs])
                yield
                qk = qkT[b]
                k.op("dve", lambda e: e.scalar_tensor_tensor(qk[:, :, 1, :], qkv[:, 0:6, :], QS, rs[:, 0:6, :], ALU.mult, ALU.mult),
                     reads=[qkv, rs], writes=[qk])
                k.op("dve", lambda e: e.tensor_tensor(kn32[:], qkv[:, 6:12, :], rs[:, 6:12, :], ALU.mult), reads=[qkv, rs], writes=[kn32])
                k.op("pool", lambda e: e.tensor_copy(qk[:, :, 0, :], kn32[:]), reads=[kn32], writes=[qk])
                yield
                for grp in range(3):
                    pb = self.nextbank()
                    for j in range(4):
                        idx = grp * 4 + j
                        src = qkv[:, 12 + idx, :] if idx < 6 else kn32[:, idx - 6, :]
                        srcb = qkv if idx < 6 else kn32
                        k.op("pe", lambda e: e.transpose(pb[:, j * 128:(j + 1) * 128], src, ident), reads=[srcb, cst], writes=[pb], inc=(j == 3))
                    for j in range(4):
                        idx = grp * 4 + j
                        if idx < 6:
                            k.op("act", lambda e: e.copy(vtok[b][:, idx, :], pb[:, j * 128:(j + 1) * 128]), reads=[pb], writes=[vtok[b]])
                        else:
                            hh = idx - 6
                            k.op("dve", lambda e: e.tensor_scalar(kt[b][:, hh, :], pb[:, j * 128:(j + 1) * 128], etl[:, hh:hh + 1], None, ALU.mult),
                                 reads=[pb, s_], writes=[kt[b]])
                for hp in range(3):
                    pb = self.nextbank()
                    for j in range(2):
                        hh = hp * 2 + j
                        sg = SLg[hh % 2]
                        k.op("dve", lambda e: e.tensor_scalar(sg[:], SL, g_[:, hh:hh + 1], None, ALU.mult), reads=[cst, s_], writes=[sg])
                        k.op("pe", lambda e: e.matmul(pb[:, j * 128:(j + 1) * 128], sg[:], U, start=True, stop=False),
                             reads=[sg, cst], writes=[pb], inc=False)
                        k.op("pe", lambda e: e.matmul(pb[:, j * 128:(j + 1) * 128], ident, NEGs, start=False, stop=True),
                             reads=[cst], writes=[pb])
                    k.op("act", lambda e: e.activation(dm[:, hp * 2:hp * 2 + 2, :], pb[:, 0:256].rearrange("p (c t) -> p c t", c=2), AF.Exp),
                         reads=[pb], writes=[dm])
                k.op("pool", lambda e: e.tensor_tensor(dmi[:], dm[:], ident.unsqueeze(1).to_broadcast([128, 6, 128]), ALU.add),
                     reads=[dm, cst], writes=[dmi])
                for hh in range(6):
                    pb = self.nextbank()
                    W0 = Wq[hh][0]
                    qf = Q0f[hh % 2]
                    k.op("pe", lambda e: e.matmul(pb[:, 0:256], qk[:, hh, 0, :], qk[:, hh, :, :].rearrange("p a t -> p (a t)"),
                                                  start=True, stop=True), reads=[qk], writes=[pb])
                    k.op("dve", lambda e: e.scalar_tensor_tensor(qf[:], pb[:, 0:128], beta[:, hh:hh + 1], dm[:, hh, :], ALU.mult, ALU.mult),
                         reads=[pb, s_, dm], writes=[qf])
                    k.op("dve", lambda e: e.tensor_tensor(attnT[b][:, hh, :], pb[:, 128:256], dmi[:, hh, :], ALU.mult),
                         reads=[pb, dmi], writes=[attnT[b]])
                    k.op("pool", lambda e: e.tensor_copy(W0[:, 0, :], qf[:]), reads=[qf], writes=[W0])
                    k.op("pool", lambda e: e.tensor_tensor(Wq[hh][1][:, 1, :], ident, qf[:], ALU.subtract), reads=[cst, qf], writes=[Wq[hh][1]])
                    pb2 = self.nextbank()
                    k.op("pe", lambda e: e.transpose(pb2[:, 0:128], qf[:], ident), reads=[qf, cst], writes=[pb2])
                    k.op("act", lambda e: e.copy(W0[:, 2, :], pb2[:, 0:128]), reads=[pb2], writes=[W0])
                for lvl in range(7):
                    if lvl in (0, 2, 4, 6):
                        yield
                    for hh in range(6):
                        Wc = Wq[hh][lvl % 2]
                        Wn = Wq[hh][(lvl + 1) % 2]
                        pb = self.nextbank()
                        Qk, Xk, Pk = Wc[:, 0, :], Wc[:, 1, :], Wc[:, 2, :]
                        mm = lambda out, l_, r_, st_, sp_2, inc_: k.op(
                            "pe", lambda e: e.matmul(out, l_, r_, start=st_, stop=sp_2), reads=[Wc, cstb], writes=[pb], inc=inc_)
                        if lvl == 0:
                            mm(pb[:, 0:128], Pk, Qk, True, True, False)
                            mm(pb[:, 256:384], Qk, Pk, True, True, True)
                            k.op("act", lambda e: e.copy(Wn[:, 0, :], pb[:, 0:128]), reads=[pb], writes=[Wn])
                            k.op("act", lambda e: e.copy(Wn[:, 2, :], pb[:, 256:384]), reads=[pb], writes=[Wn])
                        elif lvl < 6:
                            mm(pb[:, 0:128], Pk, Qk, True, True, False)
                            mm(pb[:, 128:256], Pk, Xk, True, False, False)
                            mm(pb[:, 128:256], identb, Xk, False, True, False)
                            mm(pb[:, 256:384], Qk, Pk, True, True, True)
                            if hh % 2 == 0:
                                k.op("act", lambda e: e.copy(Wn[:], pb[:, 0:384].rearrange("p (c t) -> p c t", c=3)), reads=[pb], writes=[Wn])
                            else:
                                k.op("dve", lambda e: e.tensor_copy(Wn[:], pb[:, 0:384].rearrange("p (c t) -> p c t", c=3)), reads=[pb], writes=[Wn])
                        else:
                            mm(pb[:, 128:256], Pk, Xk, True, False, False)
                            mm(pb[:, 128:256], identb, Xk, False, True, True)
                            k.op("act", lambda e: e.copy(TT[b][:, hh, :], pb[:, 128:256]), reads=[pb], writes=[TT[b]])

            def back(t):
                b = t % 2
                h = ht[b]
                s_ = sc[b]
                C = lambda a_, n=6: s_[:, a_:a_ + n]
                beta, egc, negc, egl = C(0), C(48), C(54), C(66)
                qk, g_g = qkT[b], gg[b]
                for hh in range(6):
                    r2 = hh % 2
                    pb = self.nextbank()
                    k.op("pe", lambda e: e.matmul(pb[:, 0:128], qk[:, hh, 0, :], Sb[hh][:], start=True, stop=True), reads=[qk, Sb[hh]], writes=[pb], inc=False)
                    k.op("pe", lambda e: e.matmul(pb[:, 128:256], qk[:, hh, 1, :], Sb[hh][:], start=True, stop=True), reads=[qk, Sb[hh]], writes=[pb])
                    R_ = Rb[r2]
                    k.op("dve", lambda e: e.scalar_tensor_tensor(R_[:], pb[:, 0:128], negc[:, hh:hh + 1], vtok[b][:, hh, :], ALU.mult, ALU.add),
                         reads=[pb, s_, vtok[b]], writes=[R_])
                    k.op("act", lambda e: e.mul(o1[r2][:], pb[:, 128:256], egc[:, hh:hh + 1]), reads=[pb, s_], writes=[o1[r2]])
                    pb2 = self.nextbank()
                    k.op("pe", lambda e: e.matmul(pb2[:, 0:128], TT[b][:, hh, :], R_[:], start=True, stop=True), reads=[TT[b], R_], writes=[pb2])
                    vn = vnew[r2]
                    k.op("dve", lambda e: e.tensor_scalar(vn[:], pb2[:, 0:128], beta[:, hh:hh + 1], None, ALU.mult), reads=[pb2, s_], writes=[vn])
                    pb3 = self.nextbank()
                    k.op("pe", lambda e: e.matmul(pb3[:, 0:128], attnT[b][:, hh, :], vn[:], start=True, stop=True), reads=[attnT[b], vn], writes=[pb3], inc=False)
                    k.op("pe", lambda e: e.matmul(pb3[:, 128:256], kt[b][:, hh, :], vn[:], start=True, stop=True), reads=[kt[b], vn], writes=[pb3])
                    o_ = ob[r2]
                    k.op("dve", lambda e: e.tensor_tensor(o_[:], o1[r2][:], pb3[:, 0:128], ALU.add), reads=[o1[r2], pb3], writes=[o_])
                    k.op("dve", lambda e: e.scalar_tensor_tensor(Sf[hh][:], Sf[hh][:], egl[:, hh:hh + 1], pb3[:, 128:256], ALU.mult, ALU.add),
                         reads=[Sf[hh], s_, pb3], writes=[Sf[hh]])
                    k.op("pool", lambda e: e.tensor_copy(Sb[hh][:], Sf[hh][:]), reads=[Sf[hh]], writes=[Sb[hh]])
                    os_ = ost[r2]
                    k.op("act", lambda e: e.activation(ojunk[:], o_[:], AF.Square, accum_out=os_[:, 0:1]), reads=[o_], writes=[ojunk, os_])
                    k.op("act", lambda e: e.activation(os_[:, 1:2], os_[:, 0:1], AF.Sqrt, bias=self.epsb[:, 0:1], scale=1.0 / 128),
                         reads=[os_, self.epsbuf], writes=[os_])
                    k.op("dve", lambda e: e.reciprocal(os_[:, 2:3], os_[:, 1:2]), reads=[os_], writes=[os_])
                    k.op("dve", lambda e: e.scalar_tensor_tensor(mix[:, hh * 128:(hh + 1) * 128], o_[:], os_[:, 2:3], g_g[:, hh * 128:(hh + 1) * 128],
                                                                 ALU.mult, ALU.mult), reads=[o_, os_, g_g], writes=[mix])
                    yield
                self.mem_attend(MW, mqT[b], 0, memkT, memv, mix)
                self.out_proj(mix, mixT, w_out, h, hn, hout[0][t * 128:(t + 1) * 128, :], hout[1][t])

            def run2(g1, g2):
                gens = [g for g in (g1, g2) if g is not None]
                while gens:
                    for g_ in list(gens):
                        try:
                            next(g_)
                        except StopIteration:
                            gens.remove(g_)

            run2(front(0), None)
            for t in range(NTA):
                run2(front(t + 1) if t + 1 < NTA else None, back(t))
            k.barrier()

    def mixer_b_phase(self, hin, hout):
        nc, k, P = self.nc, self.k, self.P
        NGB = int(os.environ.get("MK_NGB", "8"))
        NHB = int(os.environ.get("MK_NHB", "12"))
        NDUM = int(os.environ.get("MK_NDUM", "0"))
        with ExitStack() as st:
            sb = lambda n, s, d: self.sbuf(st, n, s, d)
            memkT, memv = self.mem_kv(st, 1)
            win_d = self.dscratch("b_w_in_bf", [D, D], BF16)
            wout_d = self.dscratch("b_w_out_bf", [D, D], BF16)
            wdb = [Buf(None, "win_d"), Buf(None, "wout_d")]
            k.dma(win_d, P["b_w_in"][0], writes=[wdb[0]], q="pool")
            k.dma(wout_d, P["b_w_out"][0], writes=[wdb[1]], q="pool")
            KT = sb("KT", [128, 6, S], BF16)
            Vt = sb("Vt", [128, NT, 768], BF16)
            ht = [sb("htB%d" % i, [128, D], F32) for i in range(2)]
            xn = sb("xnB", [128, D], F32)
            stt = [sb("sttB%d" % i, [128, 4], F32) for i in range(2)]
            with ExitStack() as st1:
                sb1 = lambda n, s, d: self.sbuf(st1, n, s, d)
                kvg = sb1("kvg", [128, D], F32)
                k.dma(kvg[:], P["kv_norm"].partition_broadcast(128), writes=[kvg])
                w_kv = sb1("w_kv", [128, 8, 1536], BF16)
                for c in range(8):
                    k.dma(w_kv[:, c, :], P["w_kv"][c * 128:(c + 1) * 128, :], writes=[w_kv], q="pool")
                xT1 = [sb1("xT1_%d" % i, [128, 8, 128], BF16) for i in range(2)]
                for t in range(NT):
                    b = t % 2
                    h = ht[b]
                    k.dma(h[:], hin[0][t * 128:(t + 1) * 128, :], reads=[hin[1][t]], writes=[h])
                    self.rmsnorm(h, kvg, xn, xn, stt[b])
                    xT = xT1[b]
                    self.transpose8(xn, [(xT, lambda half: xT[:, half * 4:(half + 1) * 4, :])], self.nextbank(), self.nextbank())
                    for g4 in range(2):
                        nf = 4 if g4 == 0 else 2
                        pb = self.nextbank()
                        for j in range(nf):
                            fc = g4 * 4 + j
                            for dc in range(8):
                                k.op("pe", lambda e: e.matmul(pb[:, j * 128:(j + 1) * 128], w_kv[:, dc, fc * 128:(fc + 1) * 128], xT[:, dc, :],
                                                              start=(dc == 0), stop=(dc == 7)),
                                     reads=[w_kv, xT], writes=[pb], inc=(dc == 7 and j == nf - 1))
                        dstv = KT[:, g4 * 4:g4 * 4 + nf, t * 128:(t + 1) * 128]
                        srcv = pb[:, 0:nf * 128].rearrange("p (c t) -> p c t", c=nf)
                        k.op("act", lambda e: e.copy(dstv, srcv), reads=[pb], writes=[KT])
                    for half, (c0, c1) in enumerate(((0, 512), (512, 768))):
                        pb = self.nextbank()
                        for dc in range(8):
                            k.op("pe", lambda e: e.matmul(pb[:, 0:c1 - c0], xT[:, dc, :], w_kv[:, dc, 768 + c0:768 + c1],
                                                          start=(dc == 0), stop=(dc == 7)),
                                 reads=[w_kv, xT], writes=[pb], inc=(dc == 7))
                        k.op("dve", lambda e: e.tensor_copy(Vt[:, t, c0:c1], pb[:, 0:c1 - c0]), reads=[pb], writes=[Vt])
                k.barrier()
            bg = sb("bgain", [128, D], F32)
            k.dma(bg[:], P["b_norm"][0].partition_broadcast(128), writes=[bg])
            wB = sb("wB", [128, 8, D], BF16)
            xTg = sb("xTg", [128, 8, 512], BF16)
            qT = sb("qT", [128, 6, 512], BF16)
            mqT = sb("mqTB", [128, 2, 512], BF16)
            mixTg = sb("mixTg", [128, 8, 512], BF16)
            Eb = [sb("Eb%d" % i, [128, 512], F32) for i in range(3)]
            spb = [sb("spb%d" % i, [128, 512], BF16) for i in range(3)]
            eab = [sb("eab%d" % i, [128, 512], F32) for i in range(2)]
            ab = [sb("ab%d" % i, [128, 512], BF16) for i in range(2)]
            MW = self.mem_work(st)
            mmB = sb("mmB", [128, 256], F32)
            hn = sb("hnB", [128, D], F32)
            NGEb = self.cstb[:, 5, :]
            NLTb = self.cstb[:, 6, :]
            strictTb = self.cstb[:, 3, :]
            cstb = self.cstb
            PZ = [self.ps[0], self.ps[1]]
            PC = [self.ps[2], self.ps[3]]
            PO = [self.ps[4], self.ps[5]]
            rot = [0]

            def nb():
                rot[0] = (rot[0] + 1) % 8
                return self.ps[rot[0]]
            self.nextbank = nb
            for g in range(NGB):
                k.dma(wB[:], win_d.rearrange("(c p) n -> p c n", p=128), reads=[wdb[0]], writes=[wB])
                for tt in range(4):
                    t = g * 4 + tt
                    b = t % 2
                    h = ht[b]
                    k.dma(h[:], hin[0][t * 128:(t + 1) * 128, :], reads=[hin[1][t]], writes=[h])
                    self.rmsnorm(h, bg, xn, xn, stt[b])
                    self.transpose8(xn, [(xTg, lambda half: xTg[:, half * 4:(half + 1) * 4, tt * 128:(tt + 1) * 128])], nb(), nb())
                for fc in range(8):
                    pb = nb()
                    for dc in range(8):
                        k.op("pe", lambda e: e.matmul(pb[:], wB[:, dc, fc * 128:(fc + 1) * 128], xTg[:, dc, :], start=(dc == 0), stop=(dc == 7)),
                             reads=[wB, xTg], writes=[pb], inc=(dc == 7))
                    if fc < 6:
                        k.op("act", lambda e: e.mul(qT[:, fc, :], pb[:], 0.125), reads=[pb], writes=[qT])
                    else:
                        k.op("dve", lambda e: e.tensor_copy(mqT[:, fc - 6, :], pb[:]), reads=[pb], writes=[mqT])
                items = [(2 * p + s, kb) for p in range(NHB // 2) for kb in range(4 * g + 3, -1, -1) for s in range(2)]

                def geom(i):
                    hh, kb = items[i]
                    r = max(kb - 4 * g, 0)
                    return hh, kb, hh // 2, hh % 2, r * 128, kb >= 4 * g

                def s1_pe(i):
                    hh, kb, fc, s, c0, diag = geom(i)
                    ps_ = slice(s * 64, (s + 1) * 64)
                    cs = slice(c0, 512)
                    pz = PZ[i % 2]
                    k.op("pe", lambda e: e.matmul(pz[:, cs], KT[ps_, fc, kb * 128:(kb + 1) * 128], qT[ps_, fc, cs], start=True, stop=True),
                         reads=[KT, qT], writes=[pz])

                def s1_act(i):
                    hh, kb, fc, s, c0, diag = geom(i)
                    cs = slice(c0, 512)
                    pz, E, sp = PZ[i % 2], Eb[i % 3], spb[i % 3]
                    k.op("act", lambda e: e.activation(E[:, cs], pz[:, cs], AF.Exp), reads=[pz], writes=[E])
                    k.op("act", lambda e: e.activation(sp[:, cs], E[:, cs], AF.Ln, bias=1.0), reads=[E], writes=[sp])
                    if diag:
                        k.op("dve", lambda e: e.tensor_tensor(sp[:, c0:c0 + 128], sp[:, c0:c0 + 128], strictTb, ALU.mult),
                             reads=[sp, cstb], writes=[sp])

                def s2_peA(i):
                    hh, kb, fc, s, c0, diag = geom(i)
                    cs = slice(c0, 512)
                    C, sp = PC[s], spb[i % 3]
                    if kb == 4 * g + 3:
                        k.op("dve", lambda e: e.memset(C[:], 0.0), writes=[C])
                    k.op("pe", lambda e: e.matmul(C[:, cs], NGEb, sp[:, cs], start=False, stop=False, skip_group_check=True),
                         reads=[cstb, sp], writes=[C])

                def s2_act(i):
                    hh, kb, fc, s, c0, diag = geom(i)
                    cs = slice(c0, 512)
                    C, ea_ = PC[s], eab[i % 2]
                    k.op("act", lambda e: e.activation(ea_[:, cs], C[:, cs], AF.Exp), reads=[C], writes=[ea_])

                def s2_peB(i):
                    hh, kb, fc, s, c0, diag = geom(i)
                    cs = slice(c0, 512)
                    C, sp = PC[s], spb[i % 3]
                    if kb > 0:
                        k.op("pe", lambda e: e.matmul(C[:, cs], NLTb, sp[:, cs], start=False, stop=False, skip_group_check=True),
                             reads=[cstb, sp], writes=[C])

                def s3_pool(i):
                    hh, kb, fc, s, c0, diag = geom(i)
                    cs = slice(c0, 512)
                    E, ea_, a_ = Eb[i % 3], eab[i % 2], ab[i % 2]
                    k.op("pool", lambda e: e.tensor_tensor(a_[:, cs], E[:, cs], ea_[:, cs], ALU.mult), reads=[E, ea_], writes=[a_])
                    if diag:
                        k.op("dve", lambda e: e.tensor_tensor(a_[:, c0:c0 + 128], a_[:, c0:c0 + 128], strictTb, ALU.mult),
                             reads=[a_, cstb], writes=[a_])

                def s3_pe(i):
                    hh, kb, fc, s, c0, diag = geom(i)
                    cs = slice(c0, 512)
                    a_ = ab[i % 2]
                    po = PO[fc % 2]
                    if kb == 4 * g + 3 and s == 0:
                        k.op("dve", lambda e: e.memset(po[:], 0.0), writes=[po])
                    vblk = Vt[:, kb, hh * 64:(hh + 1) * 64]
                    if s == 0:
                        k.op("pe", lambda e: e.matmul(po[0:64, cs], vblk, a_[:, cs], start=False, stop=False, skip_group_check=True),
                             reads=[Vt, a_], writes=[po])
                    else:
                        k.op("pe", lambda e: e.matmul(po[64:128, cs], vblk, a_[:, cs], start=False, stop=False, skip_group_check=True,
                                                      tile_position=(0, 64)), reads=[Vt, a_], writes=[po])
                    if kb == 0 and s == 1:
                        k.op("act", lambda e: e.copy(mixTg[:, fc, :], po[:]), reads=[po], writes=[mixTg])

                n_it = len(items)
                for step in range(-2, n_it):
                    i1, i2_, i3 = step + 2, step + 1, step
                    if 0 <= i3:
                        s3_pool(i3)
                    if i1 < n_it:
                        s1_pe(i1)
                    if 0 <= i2_ < n_it:
                        s2_peA(i2_)
                    if i1 < n_it:
                        s1_act(i1)
                    if 0 <= i2_ < n_it:
                        s2_act(i2_)
                    if 0 <= i3:
                        s2_peB(i3)
                        s3_pe(i3)
                    for _d in range(NDUM):
                        k.op("pe", lambda e: e.matmul(self.ps[6][:], NGEb, spb[0][:], start=True, stop=True), reads=[], writes=[], inc=False)
                k.dma(wB[:], wout_d.rearrange("(c p) n -> p c n", p=128), reads=[wdb[1]], writes=[wB])
                for tt in range(4):
                    t = g * 4 + tt
                    b = t % 2
                    h = ht[b]
                    k.dma(h[:], hin[0][t * 128:(t + 1) * 128, :], reads=[hin[1][t]], writes=[h])
                    self.mem_attend(MW, mqT, tt * 128, memkT, memv, mmB, col0=0)
                    pb = nb()
                    for j in range(2):
                        k.op("pe", lambda e: e.transpose(pb[:, j * 128:(j + 1) * 128], mmB[:, j * 128:(j + 1) * 128], self.ident),
                             reads=[mmB, self.cst], writes=[pb], inc=(j == 1))
                    k.op("act", lambda e: e.copy(mixTg[:, 6:8, tt * 128:(tt + 1) * 128], pb[:, 0:256].rearrange("p (c t) -> p c t", c=2)),
                         reads=[pb], writes=[mixTg])
                    for half in range(2):
                        pb = nb()
                        for fc in range(8):
                            k.op("pe", lambda e: e.matmul(pb[:], mixTg[:, fc, tt * 128:(tt + 1) * 128], wB[:, fc, half * 512:(half + 1) * 512],
                                                          start=(fc == 0), stop=(fc == 7)),
                                 reads=[mixTg, wB], writes=[pb], inc=(fc == 7))
                        k.op("dve", lambda e: e.tensor_tensor(hn[:, half * 512:(half + 1) * 512], h[:, half * 512:(half + 1) * 512], pb[:], ALU.add),
                             reads=[h, pb], writes=[hn])
                    k.dma(hout[0][t * 128:(t + 1) * 128, :], hn[:], reads=[hn], writes=[hout[1][t]])
            k.barrier()
            del self.nextbank

    def build(self, phases):
        nc, k = self.nc, self.k
        P = self.P = {}
        shapes = dict(
            x=[S, D], mem=[256, D], a_norm=[1, D], a_w_in=[1, D, 3340], a_conv=[1, 4, 2304],
            a_log=[1, 6], a_dt_bias=[1, 6], a_out_gain=[1, 128], a_w_out=[1, D, D],
            kv_norm=[D], w_kv=[D, 1536], b_norm=[1, D], b_w_in=[1, D, D], b_w_out=[1, D, D],
            mem_norm=[2, D], w_mem_kv=[2, D, 512], ffn_norm=[2, D], w_group=[2, D, 4], b_group=[2, 4],
            w_router=[2, D, 16], b_router=[2, 16], w1=[2, 16, D, 256], w3=[2, 16, D, 256],
            w2=[2, 16, 256, D], final_norm=[D], wgr=[2, 128, 8, 20], rbias=[2, 20], convw=[128, 18, 4])
        for n, s in shapes.items():
            P[n] = self.din(n, s)
        out = nc.dram_tensor("out", [S, D], F32, kind="ExternalOutput").ap()
        mkbufs = lambda nm: [Buf(None, "%s%d" % (nm, i)) for i in range(NT)]
        hx = (P["x"], mkbufs("x"))
        hA = (self.dscratch("hA", [S, D]), mkbufs("hA"))
        hB = (self.dscratch("hB", [S, D]), mkbufs("hB"))
        ho = (out, mkbufs("out"))
        with ExitStack() as gst:
            self.load_consts(gst)
            self.epsbuf = self.sbuf(gst, "epsb", [128, 1], F32)
            self.epsb = self.epsbuf
            k.op("dve", lambda e: e.memset(self.epsbuf[:], EPS), writes=[self.epsbuf])
            cur = hx
            seq = {"A": hA, "M0": hB, "B": hA, "M1": ho}
            for ph in phases:
                dst = seq[ph] if ph != phases[-1] else ho
                if ph == "M0":
                    self.moe_phase(0, cur, dst, final=False)
                elif ph == "M1":
                    self.moe_phase(1, cur, dst, final=True)
                elif ph == "A":
                    self.mixer_a_phase(cur, dst)
                elif ph == "B":
                    self.mixer_b_phase(cur, dst)
                cur = dst
            for b in ho[1]:
                if b.lw is not None:
                    k._wait("sp", b.lw)
        return nc


def make_consts():
    c = np.zeros((128, 8, 128), np.float32)
    i = np.arange(128)
    c[:, 0, :] = np.eye(128)
    c[:, 1, :] = (i[:, None] <= i[None, :])
    c[:, 2, :] = (i[:, None] > i[None, :])
    c[:, 3, :] = (i[:, None] < i[None, :])
    c[:, 4, :] = 1.0
    c[:, 5, :] = -(i[:, None] >= i[None, :]).astype(np.float32)
    c[:, 6, :] = -(i[:, None] < i[None, :]).astype(np.float32)
    c[:, 7, :] = -30000.0 * (i[:, None] >= i[None, :])
    return c


_CACHE = {}


def run(inputs, phases=("A", "M0", "B", "M1"), ncores=NCORES, trace=False):
    key = tuple(phases)
    if key not in _CACHE:
        mk = MK(phases)
        _CACHE[key] = mk.build(list(phases))
    nc = _CACHE[key]
    consts = make_consts()
    inputs = dict(inputs)
    wg = np.concatenate([np.asarray(inputs["w_group"]), np.asarray(inputs["w_router"])], axis=2)
    inputs["wgr"] = np.ascontiguousarray(wg.reshape(2, 8, 128, 20).transpose(0, 2, 1, 3))
    inputs["rbias"] = np.concatenate([np.asarray(inputs["b_group"]), np.asarray(inputs["b_router"])], axis=1)
    cw = np.asarray(inputs["a_conv"])[0]
    inputs["convw"] = np.ascontiguousarray(cw.reshape(4, 18, 128).transpose(2, 1, 0))
    in_maps = []
    for c in range(ncores):
        m = {"consts": consts}
        for n, v in inputs.items():
            v = np.asarray(v)
            if n in ("x", "mem"):
                m[n] = np.ascontiguousarray(v[c])
            else:
                m[n] = np.ascontiguousarray(v, dtype=np.float32)
        in_maps.append(m)
    res = run_bass_kernel_spmd(nc, in_maps, core_ids=list(range(ncores)), trace=trace)
    outs = np.stack([r["out"] for r in res.results], axis=0)
    return outs, res


def kernel(**inputs):
    outs, _ = run(inputs)
    return outs.astype(np.float32)
```

```python
from contextlib import ExitStack
import os
import numpy as np
import concourse.bass as bass
import concourse.mybir as mybir
from concourse.bass_utils import run_bass_kernel_spmd

F32 = mybir.dt.float32
BF16 = mybir.dt.bfloat16
AF = mybir.ActivationFunctionType
ALU = mybir.AluOpType
AX = mybir.AxisListType

S = 4096
D = 1024
NT = S // 128
EPS = 1e-6
NCORES = 8


class Buf:
    __slots__ = ("ap", "name", "_lw", "_rd", "_excl")
    lw = property(lambda self: self._lw, lambda self, v: setattr(self, "_lw", v))
    rd = property(lambda self: self._rd, lambda self, v: setattr(self, "_rd", v))
    excl = property(lambda self: self._excl, lambda self, v: setattr(self, "_excl", v))

    def __init__(self, ap, name="", excl=False):
        self.ap = ap
        self.name = name
        self.excl = excl
        self.lw = None
        self.rd = []

    def __getitem__(self, idx):
        return self.ap[idx]


class View(Buf):
    __slots__ = ("parent",)

    def __init__(self, parent, ap):
        self.parent = parent
        self.ap = ap
        self.name = parent.name

    lw = property(lambda self: self.parent.lw, lambda self, v: setattr(self.parent, "lw", v))
    rd = property(lambda self: self.parent.rd, lambda self, v: setattr(self.parent, "rd", v))
    excl = property(lambda self: self.parent.excl, lambda self, v: None)


class K:
    NDMA = 48

    def __init__(self, nc):
        self.nc = nc
        self.eng = {"pe": nc.tensor, "act": nc.scalar, "dve": nc.vector,
                    "pool": nc.gpsimd, "sp": nc.sync}
        self.sem = {e: nc.alloc_semaphore("s_" + e) for e in ("pe", "act", "dve", "pool")}
        self.cnt = {e: 0 for e in self.sem}
        self.waited = {}
        self.dsem = [nc.alloc_semaphore("d%d" % i) for i in range(self.NDMA)]
        self.dcnt = [0] * self.NDMA
        self.dnext = 0
        self.nins = 0

    def _semh(self, key):
        return self.sem[key] if isinstance(key, str) else self.dsem[key]

    def _wait(self, e, dep):
        key, val = dep
        if key == e and e == "pe":
            return
        w = self.waited.get((e, key), 0)
        if w >= val:
            return
        self.eng[e].wait_ge(self._semh(key), val)
        self.nins += 1
        self.waited[(e, key)] = val

    def _deps(self, e, reads, writes):
        best = {}
        for r in reads:
            if r.lw is not None:
                if best.get(r.lw[0], 0) < r.lw[1]:
                    best[r.lw[0]] = r.lw[1]
            if r.excl:
                for key, val in r.rd:
                    if key != e and best.get(key, 0) < val:
                        best[key] = val
        for w in writes:
            if w.lw is not None:
                if best.get(w.lw[0], 0) < w.lw[1]:
                    best[w.lw[0]] = w.lw[1]
            for key, val in w.rd:
                if best.get(key, 0) < val:
                    best[key] = val
        for key, val in best.items():
            self._wait(e, (key, val))

    def _mark(self, tag, reads, writes):
        for r in reads:
            r.rd.append(tag)
            if len(r.rd) > 64:
                best = {}
                for key, val in r.rd:
                    if best.get(key, 0) < val:
                        best[key] = val
                r.rd = list(best.items())
        for w in writes:
            w.lw = tag
            w.rd = []

    def op(self, e, fn, reads=(), writes=(), inc=True):
        self._deps(e, reads, writes)
        ins = fn(self.eng[e])
        self.nins += 1
        if inc:
            ins.then_inc(self.sem[e], 1)
            self.cnt[e] += 1
            tag = (e, self.cnt[e])
        else:
            tag = (e, self.cnt[e] + 1)
        self._mark(tag, reads, writes)
        return ins

    def dma(self, out, in_, reads=(), writes=(), q="sp", **kw):
        slot = self.dnext
        self.dnext = (self.dnext + 1) % self.NDMA
        if self.dcnt[slot] > 0:
            self._wait(q, (slot, 16 * self.dcnt[slot]))
        self._deps(q, reads, writes)
        ins = self.eng[q].dma_start(out=out, in_=in_, **kw)
        self.nins += 1
        ins.then_inc(self.dsem[slot], 16)
        self.dcnt[slot] += 1
        tag = (slot, 16 * self.dcnt[slot])
        self._mark(tag, reads, writes)
        return tag

    def barrier(self):
        for e in ("pe", "act", "dve", "pool", "sp"):
            for e2 in ("pe", "act", "dve", "pool"):
                if e2 != e and self.cnt[e2] > 0:
                    self._wait(e, (e2, self.cnt[e2]))
            for slot in range(self.NDMA):
                if self.dcnt[slot] > 0:
                    self._wait(e, (slot, 16 * self.dcnt[slot]))


class MK:
    def __init__(self, phases, h0_from_input=True):
        self.nc = nc = bass.Bass("TRN2", target_bir_lowering=False)
        self.k = K(nc)
        self.uid = 0
        self.ins = {}
        self.ps = [Buf(nc.alloc_psum_tensor("psb%d" % i, [128, 512], F32).ap(), "ps%d" % i, excl=True)
                   for i in range(8)]

    def din(self, name, shape):
        ap = self.nc.dram_tensor(name, list(shape), F32, kind="ExternalInput").ap()
        self.ins[name] = ap
        return ap

    def dscratch(self, name, shape, dt=F32):
        return self.nc.dram_tensor(name, list(shape), dt, kind="Internal").ap()

    def sbuf(self, st, name, shape, dt):
        self.uid += 1
        h = st.enter_context(self.nc.sbuf_tensor("%s_%d" % (name, self.uid), list(shape), dt))
        return Buf(h.ap(), name)

    def load_consts(self, st):
        k = self.k
        c = self.din("consts", [128, 8, 128])
        self.cst = self.sbuf(st, "cst", [128, 8, 128], F32)
        k.dma(self.cst[:], c, writes=[self.cst])
        self.ident = self.cst[:, 0, :]
        self.U = self.cst[:, 1, :]
        self.SL = self.cst[:, 2, :]
        self.strictT = self.cst[:, 3, :]
        self.ones = self.cst[:, 4, :]
        self.cstb = self.sbuf(st, "cstb", [128, 8, 128], BF16)
        k.dma(self.cstb[:], c, writes=[self.cstb], q="pool")
        self.identb = self.cstb[:, 0, :]
        self.onesb = self.cstb[:, 4, :]

    def rmsnorm(self, h, gainb, xn, junk, st2):
        k = self.k
        k.op("act", lambda e: e.activation(junk[:], h[:], AF.Square, accum_out=st2[:, 0:1]),
             reads=[h], writes=[junk, st2])
        k.op("act", lambda e: e.activation(st2[:, 1:2], st2[:, 0:1], AF.Sqrt, bias=self.epsb[:, 0:1], scale=1.0 / D),
             reads=[st2, self.epsbuf], writes=[st2])
        k.op("dve", lambda e: e.reciprocal(st2[:, 2:3], st2[:, 1:2]), reads=[st2], writes=[st2])
        k.op("dve", lambda e: e.scalar_tensor_tensor(xn[:], h[:], st2[:, 2:3], gainb[:], ALU.mult, ALU.mult),
             reads=[h, st2, gainb], writes=[xn])

    def transpose8(self, src, dsts, psa, psb, evac=("act", "dve"), second="pool"):
        k = self.k
        for half, ps in enumerate((psa, psb)):
            for j in range(4):
                c = half * 4 + j
                k.op("pe", lambda e: e.transpose(ps[:, j * 128:(j + 1) * 128], src[:, c * 128:(c + 1) * 128], self.ident),
                     reads=[src, self.cst], writes=[ps], inc=(j == 3))
            dbuf, fn = dsts[0]
            eng = evac[half % len(evac)]
            pv = ps[:].rearrange("p (c t) -> p c t", c=4)
            if eng == "act":
                k.op("act", lambda e: e.copy(fn(half), pv), reads=[ps], writes=[dbuf])
            else:
                k.op(eng, lambda e: e.tensor_copy(fn(half), pv), reads=[ps], writes=[dbuf])
            for dbuf2, fn2 in dsts[1:]:
                k.op(second, lambda e: e.tensor_copy(fn2(half), fn(half)), reads=[dbuf], writes=[dbuf2])

    def moe_phase(self, l, hin, hout, final=False, out_ap=None):
        nc, k = self.nc, self.k
        G = 1024
        NTG = G // 128
        NG = S // G
        NTB = G // 512
        NEX = 16
        P = self.P
        with ExitStack() as st:
            sb = lambda n, s, d: self.sbuf(st, n, s, d)
            gain = sb("gain", [128, D], F32)
            k.dma(gain[:], P["ffn_norm"][l].partition_broadcast(128), writes=[gain])
            if final:
                fgain = sb("fgain", [128, D], F32)
                k.dma(fgain[:], P["final_norm"].partition_broadcast(128), writes=[fgain])
            wgr = sb("wgr", [128, 8, 20], F32)
            k.dma(wgr[:], P["wgr"][l], writes=[wgr])
            rb = sb("rbias", [128, 20], F32)
            k.dma(rb[:], P["rbias"][l].partition_broadcast(128), writes=[rb])
            xnT = [sb("xnT%d" % i, [128, 8, G], BF16) for i in range(2)]
            yacc = [[sb("yacc%d_%d" % (j, i), [128, D], F32) for i in range(NTG)] for j in range(2)]
            comb = [[sb("comb%d_%d" % (j, i), [128, 16], F32) for i in range(NTG)] for j in range(2)]
            w1b = [sb("w1b%d" % i, [128, 8, 256], BF16) for i in range(2)]
            w3b = [sb("w3b%d" % i, [128, 8, 256], BF16) for i in range(2)]
            w2b = [sb("w2b%d" % i, [128, 2, D], BF16) for i in range(2)]
            ht = [sb("ht%d" % i, [128, D], F32) for i in range(2)]
            htc = [sb("htc%d" % i, [128, D], F32) for i in range(2)]
            xn = [sb("xn%d" % i, [128, D], F32) for i in range(2)]
            xnT32 = [sb("xnT32_%d" % i, [128, 8, 128], F32) for i in range(2)]
            stt = [sb("stt%d" % i, [128, 4], F32) for i in range(2)]
            sttc = [sb("sttc%d" % i, [128, 4], F32) for i in range(2)]
            rt = [sb("rt%d" % i, [128, 96], F32) for i in range(2)]
            hid = [sb("hid%d" % i, [128, 2, 512], BF16) for i in range(2)]
            sil = [sb("sil%d" % i, [128, 512], F32) for i in range(2)]
            ps = self.ps
            w1d, w3d, w2d = P["w1"], P["w3"], P["w2"]

            def load_w(e, slot):
                k.dma(w1b[slot][:], w1d[l, e].rearrange("(c p) f -> p c f", p=128), writes=[w1b[slot]], q="pool")
                k.dma(w3b[slot][:], w3d[l, e].rearrange("(c p) f -> p c f", p=128), writes=[w3b[slot]], q="pool")
                k.dma(w2b[slot][:], w2d[l, e].rearrange("(c p) n -> p c n", p=128), writes=[w2b[slot]], q="pool")

            def stage_a(g):
                par = g % 2
                xT = xnT[par]
                for ti in range(NTG):
                    t = g * NTG + ti
                    b = ti % 2
                    h = ht[b]
                    k.dma(h[:], hin[0][t * 128:(t + 1) * 128, :], reads=[hin[1][t]], writes=[h])
                    self.rmsnorm(h, gain, xn[b], xn[b], stt[b])
                    yield
                    x32 = xnT32[b]
                    self.transpose8(
                        xn[b],
                        [(x32, lambda half: x32[:, half * 4:(half + 1) * 4, :]),
                         (xT, lambda half: xT[:, half * 4:(half + 1) * 4, ti * 128:(ti + 1) * 128])],
                        ps[6], ps[7])
                    yield
                    pr = ps[6 + (ti % 2)]
                    for dc in range(8):
                        k.op("pe", lambda e: e.matmul(pr[:, 0:20], x32[:, dc, :], wgr[:, dc, :], start=(dc == 0), stop=(dc == 7)),
                             reads=[x32, wgr], writes=[pr], inc=(dc == 7))
                    yield
                    r = rt[b]
                    R = lambda a, n: r[:, a:a + n]
                    lg, gmax, ngmax, oh, ge, gsum, pg = R(0, 20), R(20, 1), R(21, 1), R(22, 4), R(26, 4), R(30, 1), R(31, 1)
                    tmp, elsel, m1, nm1, ee, mask1, ee2 = R(32, 16), R(48, 4), R(52, 1), R(53, 1), R(54, 4), R(58, 4), R(62, 4)
                    v2, mask2, den, rden, wl, scl = R(66, 1), R(67, 4), R(71, 1), R(72, 1), R(73, 4), R(77, 1)
                    dv = lambda fn, rd=(), wr=(): k.op("dve", fn, reads=[r] + list(rd), writes=[r] + list(wr))
                    dv(lambda e: e.tensor_tensor(lg, pr[:, 0:20], rb[:], ALU.add), rd=[pr, rb])
                    dv(lambda e: e.tensor_reduce(gmax, lg[:, 0:4], AX.X, ALU.max))
                    dv(lambda e: e.tensor_single_scalar(ngmax, gmax, -1.0, ALU.mult))
                    dv(lambda e: e.tensor_scalar(oh, lg[:, 0:4], gmax, None, ALU.is_equal))
                    yield
                    k.op("act", lambda e: e.activation(ge, lg[:, 0:4], AF.Exp, bias=ngmax, accum_out=gsum), reads=[r], writes=[r])
                    dv(lambda e: e.reciprocal(pg, gsum))
                    dv(lambda e: e.tensor_tensor(tmp.rearrange("p (g j) -> p g j", g=4),
                                                 lg[:, 4:20].rearrange("p (g j) -> p g j", g=4),
                                                 oh.unsqueeze(2).to_broadcast([128, 4, 4]), ALU.mult))
                    dv(lambda e: e.tensor_reduce(elsel, tmp.rearrange("p (g j) -> p j g", g=4), AX.X, ALU.add))
                    yield
                    dv(lambda e: e.tensor_reduce(m1, elsel, AX.X, ALU.max))
                    dv(lambda e: e.tensor_single_scalar(nm1, m1, -1.0, ALU.mult))
                    k.op("act", lambda e: e.activation(ee, elsel, AF.Exp, bias=nm1), reads=[r], writes=[r])
                    dv(lambda e: e.tensor_scalar(mask1, elsel, m1, None, ALU.is_equal))
                    yield
                    dv(lambda e: e.scalar_tensor_tensor(ee2, mask1, -2.0, ee, ALU.mult, ALU.add))
                    dv(lambda e: e.tensor_reduce(v2, ee2, AX.X, ALU.max))
                    dv(lambda e: e.tensor_scalar(mask2, ee2, v2, None, ALU.is_equal))
                    dv(lambda e: e.tensor_single_scalar(den, v2, 1.0, ALU.add))
                    yield
                    dv(lambda e: e.reciprocal(rden, den))
                    dv(lambda e: e.scalar_tensor_tensor(wl, mask2, v2, mask1, ALU.mult, ALU.add))
                    dv(lambda e: e.tensor_tensor(scl, pg, rden, ALU.mult))
                    dv(lambda e: e.tensor_scalar(wl, wl, scl, None, ALU.mult))
                    cb = comb[par][ti]
                    dv(lambda e: e.tensor_tensor(cb[:].rearrange("p (g j) -> p g j", g=4),
                                                 oh.unsqueeze(2).to_broadcast([128, 4, 4]),
                                                 wl.unsqueeze(1).to_broadcast([128, 4, 4]), ALU.mult), wr=[cb])
                    yield

            def stage_b(g):
                par = g % 2
                xT = xnT[par]
                work = [(ex, tb) for ex in range(NEX) for tb in range(NTB)]

                def up(idx):
                    ex, tb = work[idx]
                    slot = ex % 2
                    w1, w3 = w1b[slot], w3b[slot]
                    hd = hid[idx % 2]
                    for fc in range(2):
                        p1 = ps[fc]
                        p3 = ps[2 + fc]
                        for wsrc, pdst in ((w1, p1), (w3, p3)):
                            for dc in range(8):
                                k.op("pe", lambda e: e.matmul(pdst[:], wsrc[:, dc, fc * 128:(fc + 1) * 128], xT[:, dc, tb * 512:(tb + 1) * 512],
                                                              start=(dc == 0), stop=(dc == 7)),
                                     reads=[wsrc, xT], writes=[pdst], inc=(dc == 7))
                                if dc % 4 == 3:
                                    yield
                        sl = sil[fc]
                        k.op("act", lambda e: e.activation(sl[:], p1[:], AF.Silu), reads=[p1], writes=[sl])
                        k.op("dve", lambda e: e.tensor_tensor(hd[:, fc, :], sl[:], p3[:], ALU.mult), reads=[sl, p3], writes=[hd])

                def down(idx):
                    ex, tb = work[idx]
                    slot = ex % 2
                    w2 = w2b[slot]
                    hd = hid[idx % 2]
                    for tt in range(4):
                        ti = tb * 4 + tt
                        for half in range(2):
                            py = ps[4 + half]
                            for fc in range(2):
                                k.op("pe", lambda e: e.matmul(py[:], hd[:, fc, tt * 128:(tt + 1) * 128], w2[:, fc, half * 512:(half + 1) * 512],
                                                              start=(fc == 0), stop=(fc == 1)),
                                     reads=[hd, w2], writes=[py], inc=(fc == 1))
                            ya = yacc[par][ti]
                            cs = comb[par][ti][:, ex:ex + 1]
                            if ex == 0:
                                k.op("dve", lambda e: e.tensor_scalar(ya[:, half * 512:(half + 1) * 512], py[:], cs, None, ALU.mult),
                                     reads=[py, comb[par][ti]], writes=[ya])
                            else:
                                k.op("dve", lambda e: e.scalar_tensor_tensor(ya[:, half * 512:(half + 1) * 512], py[:], cs,
                                                                             ya[:, half * 512:(half + 1) * 512], ALU.mult, ALU.add),
                                     reads=[py, comb[par][ti], ya], writes=[ya])
                            yield
                    if tb == NTB - 1:
                        if ex + 2 < NEX:
                            load_w(ex + 2, slot)
                        elif g + 1 < NG:
                            load_w(ex + 2 - NEX, slot)

                def rr(*gens):
                    gens = [g_ for g_ in gens if g_ is not None]
                    while gens:
                        for g_ in list(gens):
                            try:
                                next(g_)
                                yield
                            except StopIteration:
                                gens.remove(g_)

                yield from up(0)
                for idx in range(len(work)):
                    nu = up(idx + 1) if idx + 1 < len(work) else None
                    if nu is not None:
                        next(nu)
                        yield
                    yield from rr(nu, down(idx))

            def stage_c(g):
                par = g % 2
                for ti in range(NTG):
                    t = g * NTG + ti
                    b = ti % 2
                    h = htc[b]
                    ya = yacc[par][ti]
                    k.dma(h[:], hin[0][t * 128:(t + 1) * 128, :], reads=[hin[1][t]], writes=[h])
                    k.op("pool", lambda e: e.tensor_tensor(ya[:], h[:], ya[:], ALU.add), reads=[h, ya], writes=[ya])
                    yield
                    if final:
                        self.rmsnorm(ya, fgain, h, h, sttc[b])
                        k.dma(hout[0][t * 128:(t + 1) * 128, :], h[:], reads=[h], writes=[hout[1][t]])
                    else:
                        k.dma(hout[0][t * 128:(t + 1) * 128, :], ya[:], reads=[ya], writes=[hout[1][t]])
                    yield

            def run_group(bg, ag, cg):
                others = [[g_, per] for g_, per in ((ag, 4), (cg, 24)) if g_ is not None]
                step = 0
                b_alive = bg is not None
                while b_alive or others:
                    if b_alive:
                        try:
                            next(bg)
                        except StopIteration:
                            b_alive = False
                    for item in list(others):
                        if (not b_alive) or step % item[1] == 0:
                            try:
                                next(item[0])
                            except StopIteration:
                                others.remove(item)
                    step += 1

            load_w(0, 0)
            load_w(1, 1)
            run_group(None, stage_a(0), None)
            for g in range(NG):
                run_group(stage_b(g), stage_a(g + 1) if g + 1 < NG else None, stage_c(g - 1) if g >= 1 else None)
            run_group(None, None, stage_c(NG - 1))
            k.barrier()

    def nextbank(self):
        self.pbi = (getattr(self, "pbi", -1) + 1) % 8
        return self.ps[self.pbi]

    def mem_kv(self, st, l):
        k, P = self.k, self.P
        sb = lambda n, s, d: self.sbuf(st, n, s, d)
        memkT = sb("memkT", [128, 2, 256], BF16)
        memv = sb("memv", [128, 2, 256], BF16)
        with ExitStack() as st2:
            sb2 = lambda n, s, d: self.sbuf(st2, n, s, d)
            g = sb2("mg", [128, D], F32)
            k.dma(g[:], P["mem_norm"][l].partition_broadcast(128), writes=[g])
            w = sb2("wmkv", [128, 8, 512], BF16)
            k.dma(w[:], P["w_mem_kv"][l].rearrange("(c p) n -> p c n", p=128), writes=[w], q="pool")
            mT = sb2("memnT", [128, 8, 256], BF16)
            junk = sb2("mjunk", [128, D], F32)
            for mt in range(2):
                h = sb2("mh%d" % mt, [128, D], F32)
                xn = sb2("mxn%d" % mt, [128, D], F32)
                stt = sb2("mst%d" % mt, [128, 4], F32)
                k.dma(h[:], P["mem"][mt * 128:(mt + 1) * 128, :], writes=[h])
                self.rmsnorm(h, g, xn, junk, stt)
                self.transpose8(xn, [(mT, lambda half: mT[:, half * 4:(half + 1) * 4, mt * 128:(mt + 1) * 128])],
                                self.nextbank(), self.nextbank())
            for j in range(2):
                pb = self.nextbank()
                for dc in range(8):
                    k.op("pe", lambda e: e.matmul(pb[:, 0:256], w[:, dc, j * 128:(j + 1) * 128], mT[:, dc, :],
                                                  start=(dc == 0), stop=(dc == 7)),
                         reads=[w, mT], writes=[pb], inc=(dc == 7))
                k.op("act", lambda e: e.copy(memkT[:, j, :], pb[:, 0:256]), reads=[pb], writes=[memkT])
            for mt in range(2):
                pb = self.nextbank()
                for dc in range(8):
                    k.op("pe", lambda e: e.matmul(pb[:, 0:256], mT[:, dc, mt * 128:(mt + 1) * 128], w[:, dc, 256:512],
                                                  start=(dc == 0), stop=(dc == 7)),
                         reads=[w, mT], writes=[pb], inc=(dc == 7))
                k.op("dve", lambda e: e.tensor_copy(memv[:, mt, :], pb[:, 0:256]), reads=[pb], writes=[memv])
            k.barrier()
        return memkT, memv

    def mem_attend(self, W, mqT, qoff, memkT, memv, mix, col0=768):
        k = self.k
        pe_ = W["pexp"]; ms = W["mstat"]; pT = W["pT"]
        banks = [self.nextbank(), self.nextbank()]
        for hh in range(4):
            pair, s = hh // 2, hh % 2
            pb = banks[s]
            k.op("pe", lambda e: e.matmul(pb[:, pair * 256:(pair + 1) * 256], mqT[s * 64:(s + 1) * 64, pair, qoff:qoff + 128],
                                          memkT[s * 64:(s + 1) * 64, pair, :], start=True, stop=True),
                 reads=[mqT, memkT], writes=[pb])
        for s in range(2):
            pb = banks[s]
            k.op("dve", lambda e: e.tensor_reduce(ms[:, s:s + 3:2], pb[:].rearrange("p (h m) -> p h m", h=2), AX.X, ALU.max),
                 reads=[pb], writes=[ms])
        k.op("dve", lambda e: e.tensor_single_scalar(ms[:, 4:8], ms[:, 0:4], -0.125, ALU.mult), reads=[ms], writes=[ms])
        for hh in range(4):
            pair, s = hh // 2, hh % 2
            pb = banks[s]
            k.op("act", lambda e: e.activation(pe_[:, hh, :], pb[:, pair * 256:(pair + 1) * 256], AF.Exp, bias=ms[:, 4 + hh:5 + hh],
                                               scale=0.125, accum_out=ms[:, 8 + hh:9 + hh]),
                 reads=[pb, ms], writes=[pe_, ms])
        k.op("dve", lambda e: e.reciprocal(ms[:, 12:16], ms[:, 8:12]), reads=[ms], writes=[ms])
        for half in range(2):
            pb = self.nextbank()
            for j in range(4):
                idx = half * 4 + j
                hh, mc = idx // 2, idx % 2
                k.op("pe", lambda e: e.transpose(pb[:, j * 128:(j + 1) * 128], pe_[:, hh, mc * 128:(mc + 1) * 128], self.ident),
                     reads=[pe_, self.cst], writes=[pb], inc=(j == 3))
            if half == 0:
                k.op("act", lambda e: e.copy(pT[:, 0:4, :], pb[:].rearrange("p (c t) -> p c t", c=4)), reads=[pb], writes=[pT])
            else:
                k.op("dve", lambda e: e.tensor_copy(pT[:, 4:8, :], pb[:].rearrange("p (c t) -> p c t", c=4)), reads=[pb], writes=[pT])
        pb = self.nextbank()
        for hh in range(4):
            for mc in range(2):
                k.op("pe", lambda e: e.matmul(pb[:, hh * 64:(hh + 1) * 64], pT[:, hh * 2 + mc, :], memv[:, mc, hh * 64:(hh + 1) * 64],
                                              start=(mc == 0), stop=(mc == 1)),
                     reads=[pT, memv], writes=[pb], inc=(mc == 1))
        k.op("dve", lambda e: e.tensor_tensor(mix[:, col0:col0 + 256].rearrange("p (h d) -> p h d", h=4),
                                              pb[:, 0:256].rearrange("p (h d) -> p h d", h=4),
                                              ms[:, 12:16].unsqueeze(2).to_broadcast([128, 4, 64]), ALU.mult),
             reads=[pb, ms], writes=[mix])

    def mem_work(self, st):
        sb = lambda n, s, d: self.sbuf(st, n, s, d)
        return {"pexp": sb("pexp", [128, 4, 256], F32), "mstat": sb("mstat", [128, 16], F32),
                "pT": sb("pT", [128, 8, 128], BF16)}

    def out_proj(self, mix, mixT, w_out, h, hn, dst_ap, dst_buf):
        k = self.k
        self.transpose8(mix, [(mixT, lambda half: mixT[:, half * 4:(half + 1) * 4, :])], self.nextbank(), self.nextbank())
        for half in range(2):
            pb = self.nextbank()
            for fc in range(8):
                k.op("pe", lambda e: e.matmul(pb[:], mixT[:, fc, :], w_out[:, fc, half * 512:(half + 1) * 512],
                                              start=(fc == 0), stop=(fc == 7)),
                     reads=[mixT, w_out], writes=[pb], inc=(fc == 7))
            k.op("dve", lambda e: e.tensor_tensor(hn[:, half * 512:(half + 1) * 512], h[:, half * 512:(half + 1) * 512], pb[:], ALU.add),
                 reads=[h, pb], writes=[hn])
        k.dma(dst_ap, hn[:], reads=[hn], writes=[dst_buf])

    def mixer_a_phase(self, hin, hout):
        nc, k, P = self.nc, self.k, self.P
        NTA = int(os.environ.get("MK_NTA", str(NT)))
        with ExitStack() as st:
            sb = lambda n, s, d: self.sbuf(st, n, s, d)
            memkT, memv = self.mem_kv(st, 0)
            gain = sb("gainA", [128, D], F32)
            k.dma(gain[:], P["a_norm"][0].partition_broadcast(128), writes=[gain])
            w_in = sb("w_inA", [128, 8, 3340], BF16)
            for c in range(8):
                k.dma(w_in[:, c, :], P["a_w_in"][0, c * 128:(c + 1) * 128, :], writes=[w_in], q="pool")
            w_out = sb("w_outA", [128, 8, D], BF16)
            k.dma(w_out[:], P["a_w_out"][0].rearrange("(c p) n -> p c n", p=128), writes=[w_out], q="pool")
            convw = sb("convw", [128, 18, 4], F32)
            k.dma(convw[:], P["convw"], writes=[convw])
            sc6 = sb("sc6", [128, 32], F32)
            k.dma(sc6[:, 0:6], P["a_log"][0].partition_broadcast(128), writes=[sc6])
            k.dma(sc6[:, 6:12], P["a_dt_bias"][0].partition_broadcast(128), writes=[sc6])
            k.op("act", lambda e: e.activation(sc6[:, 12:18], sc6[:, 0:6], AF.Exp), reads=[sc6], writes=[sc6])
            k.op("dve", lambda e: e.tensor_single_scalar(sc6[:, 12:18], sc6[:, 12:18], -1.0, ALU.mult), reads=[sc6], writes=[sc6])
            ogain = sb("ogain", [128, 128], F32)
            k.dma(ogain[:], P["a_out_gain"][0].partition_broadcast(128), writes=[ogain])
            MW = self.mem_work(st)
            pc = sb("pc", [128, 18, 131], F32)
            k.op("dve", lambda e: e.memset(pc[:], 0.0), writes=[pc])
            Sf = [sb("Sf%d" % h, [128, 128], F32) for h in range(6)]
            Sb = [sb("Sb%d" % h, [128, 128], BF16) for h in range(6)]
            for h in range(6):
                k.op("dve", lambda e: e.memset(Sf[h][:], 0.0), writes=[Sf[h]])
                k.op("pool", lambda e: e.memset(Sb[h][:], 0.0), writes=[Sb[h]])
            xn = sb("xnA", [128, D], F32)
            xT = sb("xnTA", [128, 8, 128], BF16)
            cv = sb("cv", [128, 18, 128], F32)
            sq = sb("sq", [128, 12, 128], BF16)
            rs = sb("rs", [128, 12, 128], F32)
            kn32 = sb("kn32", [128, 6, 128], F32)
            SLg = [sb("SLg%d" % i, [128, 128], F32) for i in range(2)]
            dm = sb("dm", [128, 6, 128], F32)
            dmi = sb("dmi", [128, 6, 128], F32)
            Wq = [[sb("W%d_%d" % (h, i), [128, 3, 128], BF16) for i in range(2)] for h in range(6)]
            Q0f = [sb("Q0f%d" % i, [128, 128], F32) for i in range(2)]
            stt = [sb("sttA%d" % i, [128, 4], F32) for i in range(2)]
            ht = [sb("htA%d" % i, [128, D], F32) for i in range(2)]
            qkT = [sb("qkT%d" % i, [128, 6, 2, 128], BF16) for i in range(2)]
            kt = [sb("kt%d" % i, [128, 6, 128], BF16) for i in range(2)]
            vtok = [sb("vtok%d" % i, [128, 6, 128], F32) for i in range(2)]
            attnT = [sb("attnT%d" % i, [128, 6, 128], BF16) for i in range(2)]
            TT = [sb("TT%d" % i, [128, 6, 128], BF16) for i in range(2)]
            sc = [sb("scA%d" % i, [128, 96], F32) for i in range(2)]
            gg = [sb("gg%d" % i, [128, 768], F32) for i in range(2)]
            mqT = [sb("mqTA%d" % i, [128, 2, 128], BF16) for i in range(2)]
            Rb = [sb("R%d" % i, [128, 128], BF16) for i in range(2)]
            vnew = [sb("vnew%d" % i, [128, 128], BF16) for i in range(2)]
            o1 = [sb("o1_%d" % i, [128, 128], F32) for i in range(2)]
            ob = [sb("ob%d" % i, [128, 128], F32) for i in range(2)]
            ost = [sb("ost%d" % i, [128, 4], F32) for i in range(2)]
            ojunk = sb("ojunk", [128, 128], F32)
            mix = sb("mixA", [128, D], F32)
            mixT = sb("mixTA", [128, 8, 128], BF16)
            hn = sb("hnA", [128, D], F32)
            ident, U, SL, ones = self.ident, self.U, self.SL, self.ones
            NEGs = self.cst[:, 7, :]
            identb = self.identb
            cst, cstb = self.cst, self.cstb
            QS = float(128 ** -0.5)

            def front(t):
                b = t % 2
                h = ht[b]
                k.dma(h[:], hin[0][t * 128:(t + 1) * 128, :], reads=[hin[1][t]], writes=[h])
                self.rmsnorm(h, gain, xn, xn, stt[b])
                self.transpose8(xn, [(xT, lambda half: xT[:, half * 4:(half + 1) * 4, :])], self.nextbank(), self.nextbank())
                for g4 in range(5):
                    nf = 4 if g4 < 4 else 2
                    pb = self.nextbank()
                    for j in range(nf):
                        fc = g4 * 4 + j
                        for dc in range(8):
                            k.op("pe", lambda e: e.matmul(pb[:, j * 128:(j + 1) * 128], w_in[:, dc, fc * 128:(fc + 1) * 128], xT[:, dc, :],
                                                          start=(dc == 0), stop=(dc == 7)),
                                 reads=[w_in, xT], writes=[pb], inc=(dc == 7 and j == nf - 1))
                    dstv = pc[:, g4 * 4:g4 * 4 + nf, 3:131]
                    srcv = pb[:, 0:nf * 128].rearrange("p (c t) -> p c t", c=nf)
                    k.op("act", lambda e: e.copy(dstv, srcv), reads=[pb], writes=[pc])
                pb = self.nextbank()
                for j in range(2):
                    for dc in range(8):
                        k.op("pe", lambda e: e.matmul(pb[:, j * 128:(j + 1) * 128], w_in[:, dc, 3084 + j * 128:3084 + (j + 1) * 128], xT[:, dc, :],
                                                      start=(dc == 0), stop=(dc == 7)),
                             reads=[w_in, xT], writes=[pb], inc=(dc == 7 and j == 1))
                k.op("act", lambda e: e.copy(mqT[b][:], pb[:, 0:256].rearrange("p (c t) -> p c t", c=2)), reads=[pb], writes=[mqT[b]])
                pg1 = self.nextbank()
                for dc in range(8):
                    k.op("pe", lambda e: e.matmul(pg1[:], xT[:, dc, :], w_in[:, dc, 2304:2816], start=(dc == 0), stop=(dc == 7)),
                         reads=[w_in, xT], writes=[pg1], inc=(dc == 7))
                pg2 = self.nextbank()
                for dc in range(8):
                    k.op("pe", lambda e: e.matmul(pg2[:, 0:268], xT[:, dc, :], w_in[:, dc, 2816:3084], start=(dc == 0), stop=(dc == 7)),
                         reads=[w_in, xT], writes=[pg2], inc=(dc == 7))
                g_g = gg[b]
                k.op("act", lambda e: e.activation(g_g[:, 0:512], pg1[:], AF.Silu), reads=[pg1], writes=[g_g])
                k.op("act", lambda e: e.activation(g_g[:, 512:768], pg2[:, 0:256], AF.Silu), reads=[pg2], writes=[g_g])
                s_ = sc[b]
                C = lambda a_, n=6: s_[:, a_:a_ + n]
                beta, tt_, ex_, sp_, g_, gcl, egc, negc, etl, egl, dd = (C(0), C(6), C(12), C(18), C(24), C(32, 16), C(48), C(54), C(60), C(66), C(72))
                k.op("act", lambda e: e.activation(beta, pg2[:, 256:262], AF.Sigmoid), reads=[pg2], writes=[s_])
                k.op("dve", lambda e: e.tensor_tensor(tt_, pg2[:, 262:268], sc6[:, 6:12], ALU.add), reads=[pg2, sc6], writes=[s_])
                k.op("act", lambda e: e.activation(ex_, tt_, AF.Exp), reads=[s_], writes=[s_])
                k.op("act", lambda e: e.activation(sp_, ex_, AF.Ln, bias=1.0), reads=[s_], writes=[s_])
                k.op("dve", lambda e: e.tensor_tensor(g_, sp_, sc6[:, 12:18], ALU.mult), reads=[s_, sc6], writes=[s_])
                k.op("pool", lambda e: e.tensor_tensor(g_g[:].rearrange("p (h d) -> p h d", h=6), g_g[:].rearrange("p (h d) -> p h d", h=6),
                                                       ogain[:].unsqueeze(1).to_broadcast([128, 6, 128]), ALU.mult),
                     reads=[g_g, ogain], writes=[g_g])
                pgc = self.nextbank()
                k.op("pe", lambda e: e.matmul(pgc[:, 0:6], U, g_, start=True, stop=True), reads=[cst, s_], writes=[pgc])
                k.op("pe", lambda e: e.matmul(pgc[:, 8:14], ones, g_, start=True, stop=True), reads=[cst, s_], writes=[pgc])
                k.op("dve", lambda e: e.tensor_copy(gcl, pgc[:, 0:16]), reads=[pgc], writes=[s_])
                k.op("act", lambda e: e.activation(egc, s_[:, 32:38], AF.Exp), reads=[s_], writes=[s_])
                k.op("dve", lambda e: e.tensor_single_scalar(negc, egc, -1.0, ALU.mult), reads=[s_], writes=[s_])
                k.op("dve", lambda e: e.tensor_tensor(dd, s_[:, 40:46], s_[:, 32:38], ALU.subtract), reads=[s_], writes=[s_])
                k.op("act", lambda e: e.activation(etl, dd, AF.Exp), reads=[s_], writes=[s_])
                k.op("act", lambda e: e.activation(egl, s_[:, 40:46], AF.Exp), reads=[s_], writes=[s_])
                yield
                for fc in range(18):
                    o_ = cv[:, fc, :]
                    k.op("dve", lambda e: e.tensor_scalar(o_, pc[:, fc, 0:128], convw[:, fc, 0:1], None, ALU.mult),
                         reads=[pc, convw], writes=[cv])
                    for j in range(1, 4):
                        k.op("dve", lambda e: e.scalar_tensor_tensor(o_, pc[:, fc, j:j + 128], convw[:, fc, j:j + 1], o_, ALU.mult, ALU.add),
                             reads=[pc, convw, cv], writes=[cv])
                k.op("pool", lambda e: e.tensor_copy(pc[:, :, 0:3], pc[:, :, 128:131]), reads=[pc], writes=[pc])
                k.op("act", lambda e: e.activation(cv[:], cv[:], AF.Silu), reads=[cv], writes=[cv])
                qkv = cv
                k.op("act", lambda e: e.activation(sq[:], qkv[:, 0:12, :], AF.Square), reads=[qkv], writes=[sq])
                for g3 in range(3):
                    pb = self.nextbank()
                    k.op("pe", lambda e: e.matmul(pb[:], self.onesb, sq[:, g3 * 4:(g3 + 1) * 4, :], start=True, stop=True),
                         reads=[cstb, sq], writes=[pb])
                    k.op("act", lambda e: e.activation(rs[:, g3 * 4:(g3 + 1) * 4, :], pb[:].rearrange("p (c t) -> p c t", c=4), AF.Ln,
                                                       bias=self.epsb[:, 0:1]), reads=[pb, self.epsbuf], writes=[rs])
                    k.op("act", lambda e: e.activation(rs[:, g3 * 4:(g3 + 1) * 4, :], rs[:, g3 * 4:(g3 + 1) * 4, :], AF.Exp, scale=-0.5),
                         reads=[rs], writes=[rs])
                yield
                qk = qkT[b]
                k.op("dve", lambda e: e.scalar_tensor_tensor(qk[:, :, 1, :], qkv[:, 0:6, :], QS, rs[:, 0:6, :], ALU.mult, ALU.mult),
                     reads=[qkv, rs], writes=[qk])
                k.op("dve", lambda e: e.tensor_tensor(kn32[:], qkv[:, 6:12, :], rs[:, 6:12, :], ALU.mult), reads=[qkv, rs], writes=[kn32])
                k.op("pool", lambda e: e.tensor_copy(qk[:, :, 0, :], kn32[:]), reads=[kn32], writes=[qk])
                yield
                for grp in range(3):
                    pb = self.nextbank()
                    for j in range(4):
                        idx = grp * 4 + j
                        src = qkv[:, 12 + idx, :] if idx < 6 else kn32[:, idx - 6, :]
                        srcb = qkv if idx < 6 else kn32
                        k.op("pe", lambda e: e.transpose(pb[:, j * 128:(j + 1) * 128], src, ident), reads=[srcb, cst], writes=[pb], inc=(j == 3))
                    for j in range(4):
                        idx = grp * 4 + j
                        if idx < 6:
                            k.op("act", lambda e: e.copy(vtok[b][:, idx, :], pb[:, j * 128:(j + 1) * 128]), reads=[pb], writes=[vtok[b]])
                        else:
                            hh = idx - 6
                            k.op("dve", lambda e: e.tensor_scalar(kt[b][:, hh, :], pb[:, j * 128:(j + 1) * 128], etl[:, hh:hh + 1], None, ALU.mult),
                                 reads=[pb, s_], writes=[kt[b]])
                for hp in range(3):
                    pb = self.nextbank()
                    for j in range(2):
                        hh = hp * 2 + j
                        sg = SLg[hh % 2]
                        k.op("dve", lambda e: e.tensor_scalar(sg[:], SL, g_[:, hh:hh + 1], None, ALU.mult), reads=[cst, s_], writes=[sg])
                        k.op("pe", lambda e: e.matmul(pb[:, j * 128:(j + 1) * 128], sg[:], U, start=True, stop=False),
                             reads=[sg, cst], writes=[pb], inc=False)
                        k.op("pe", lambda e: e.matmul(pb[:, j * 128:(j + 1) * 128], ident, NEGs, start=False, stop=True),
                             reads=[cst], writes=[pb])
                    k.op("act", lambda e: e.activation(dm[:, hp * 2:hp * 2 + 2, :], pb[:, 0:256].rearrange("p (c t) -> p c t", c=2), AF.Exp),
                         reads=[pb], writes=[dm])
                k.op("pool", lambda e: e.tensor_tensor(dmi[:], dm[:], ident.unsqueeze(1).to_broadcast([128, 6, 128]), ALU.add),
                     reads=[dm, cst], writes=[dmi])
                for hh in range(6):
                    pb = self.nextbank()
                    W0 = Wq[hh][0]
                    qf = Q0f[hh % 2]
                    k.op("pe", lambda e: e.matmul(pb[:, 0:256], qk[:, hh, 0, :], qk[:, hh, :, :].rearrange("p a t -> p (a t)"),
                                                  start=True, stop=True), reads=[qk], writes=[pb])
                    k.op("dve", lambda e: e.scalar_tensor_tensor(qf[:], pb[:, 0:128], beta[:, hh:hh + 1], dm[:, hh, :], ALU.mult, ALU.mult),
                         reads=[pb, s_, dm], writes=[qf])
                    k.op("dve", lambda e: e.tensor_tensor(attnT[b][:, hh, :], pb[:, 128:256], dmi[:, hh, :], ALU.mult),
                         reads=[pb, dmi], writes=[attnT[b]])
                    k.op("pool", lambda e: e.tensor_copy(W0[:, 0, :], qf[:]), reads=[qf], writes=[W0])
                    k.op("pool", lambda e: e.tensor_tensor(Wq[hh][1][:, 1, :], ident, qf[:], ALU.subtract), reads=[cst, qf], writes=[Wq[hh][1]])
                    pb2 = self.nextbank()
                    k.op("pe", lambda e: e.transpose(pb2[:, 0:128], qf[:], ident), reads=[qf, cst], writes=[pb2])
                    k.op("act", lambda e: e.copy(W0[:, 2, :], pb2[:, 0:128]), reads=[pb2], writes=[W0])
                for lvl in range(7):
                    if lvl in (0, 2, 4, 6):
                        yield
                    for hh in range(6):
                        Wc = Wq[hh][lvl % 2]
                        Wn = Wq[hh][(lvl + 1) % 2]
                        pb = self.nextbank()
                        Qk, Xk, Pk = Wc[:, 0, :], Wc[:, 1, :], Wc[:, 2, :]
                        mm = lambda out, l_, r_, st_, sp_2, inc_: k.op(
                            "pe", lambda e: e.matmul(out, l_, r_, start=st_, stop=sp_2), reads=[Wc, cstb], writes=[pb], inc=inc_)
                        if lvl == 0:
                            mm(pb[:, 0:128], Pk, Qk, True, True, False)
                            mm(pb[:, 256:384], Qk, Pk, True, True, True)
                            k.op("act", lambda e: e.copy(Wn[:, 0, :], pb[:, 0:128]), reads=[pb], writes=[Wn])
                            k.op("act", lambda e: e.copy(Wn[:, 2, :], pb[:, 256:384]), reads=[pb], writes=[Wn])
                        elif lvl < 6:
                            mm(pb[:, 0:128], Pk, Qk, True, True, False)
                            mm(pb[:, 128:256], Pk, Xk, True, False, False)
                            mm(pb[:, 128:256], identb, Xk, False, True, False)
                            mm(pb[:, 256:384], Qk, Pk, True, True, True)
                            if hh % 2 == 0:
                                k.op("act", lambda e: e.copy(Wn[:], pb[:, 0:384].rearrange("p (c t) -> p c t", c=3)), reads=[pb], writes=[Wn])
                            else:
                                k.op("dve", lambda e: e.tensor_copy(Wn[:], pb[:, 0:384].rearrange("p (c t) -> p c t", c=3)), reads=[pb], writes=[Wn])
                        else:
                            mm(pb[:, 128:256], Pk, Xk, True, False, False)
                            mm(pb[:, 128:256], identb, Xk, False, True, True)
                            k.op("act", lambda e: e.copy(TT[b][:, hh, :], pb[:, 128:256]), reads=[pb], writes=[TT[b]])

            def back(t):
                b = t % 2
                h = ht[b]
                s_ = sc[b]
                C = lambda a_, n=6: s_[:, a_:a_ + n]
                beta, egc, negc, egl = C(0), C(48), C(54), C(66)
                qk, g_g = qkT[b], gg[b]
                for hh in range(6):
                    r2 = hh % 2
                    pb = self.nextbank()
                    k.op("pe", lambda e: e.matmul(pb[:, 0:128], qk[:, hh, 0, :], Sb[hh][:], start=True, stop=True), reads=[qk, Sb[hh]], writes=[pb], inc=False)
                    k.op("pe", lambda e: e.matmul(pb[:, 128:256], qk[:, hh, 1, :], Sb[hh][:], start=True, stop=True), reads=[qk, Sb[hh]], writes=[pb])
                    R_ = Rb[r2]
                    k.op("dve", lambda e: e.scalar_tensor_tensor(R_[:], pb[:, 0:128], negc[:, hh:hh + 1], vtok[b][:, hh, :], ALU.mult, ALU.add),
                         reads=[pb, s_, vtok[b]], writes=[R_])
                    k.op("act", lambda e: e.mul(o1[r2][:], pb[:, 128:256], egc[:, hh:hh + 1]), reads=[pb, s_], writes=[o1[r2]])
                    pb2 = self.nextbank()
                    k.op("pe", lambda e: e.matmul(pb2[:, 0:128], TT[b][:, hh, :], R_[:], start=True, stop=True), reads=[TT[b], R_], writes=[pb2])
                    vn = vnew[r2]
                    k.op("dve", lambda e: e.tensor_scalar(vn[:], pb2[:, 0:128], beta[:, hh:hh + 1], None, ALU.mult), reads=[pb2, s_], writes=[vn])
                    pb3 = self.nextbank()
                    k.op("pe", lambda e: e.matmul(pb3[:, 0:128], attnT[b][:, hh, :], vn[:], start=True, stop=True), reads=[attnT[b], vn], writes=[pb3], inc=False)
                    k.op("pe", lambda e: e.matmul(pb3[:, 128:256], kt[b][:, hh, :], vn[:], start=True, stop=True), reads=[kt[b], vn], writes=[pb3])
                    o_ = ob[r2]
                    k.op("dve", lambda e: e.tensor_tensor(o_[:], o1[r2][:], pb3[:, 0:128], ALU.add), reads=[o1[r2], pb3], writes=[o_])
                    k.op("dve", lambda e: e.scalar_tensor_tensor(Sf[hh][:], Sf[hh][:], egl[:, hh:hh + 1], pb3[:, 128:256], ALU.mult, ALU.add),
                         reads=[Sf[hh], s_, pb3], writes=[Sf[hh]])
                    k.op("pool", lambda e: e.tensor_copy(Sb[hh][:], Sf[hh][:]), reads=[Sf[hh]], writes=[Sb[hh]])
                    os_ = ost[r2]
                    k.op("act", lambda e: e.activation(ojunk[:], o_[:], AF.Square, accum_out=os_[:, 0:1]), reads=[o_], writes=[ojunk, os_])
                    k.op("act", lambda e: e.activation(os_[:, 1:2], os_[:, 0:1], AF.Sqrt, bias=self.epsb[:, 0:1], scale=1.0 / 128),
                         reads=[os_, self.epsbuf], writes=[os_])
                    k.op("dve", lambda e: e.reciprocal(os_[:, 2:3], os_[:, 1:2]), reads=[os_], writes=[os_])
                    k.op("dve", lambda e: e.scalar_tensor_tensor(mix[:, hh * 128:(hh + 1) * 128], o_[:], os_[:, 2:3], g_g[:, hh * 128:(hh + 1) * 128],
                                                                 ALU.mult, ALU.mult), reads=[o_, os_, g_g], writes=[mix])
                    yield
                self.mem_attend(MW, mqT[b], 0, memkT, memv, mix)
                self.out_proj(mix, mixT, w_out, h, hn, hout[0][t * 128:(t + 1) * 128, :], hout[1][t])

            def run2(g1, g2):
                gens = [g for g in (g1, g2) if g is not None]
                while gens:
                    for g_ in list(gens):
                        try:
                            next(g_)
                        except StopIteration:
                            gens.remove(g_)

            run2(front(0), None)
            for t in range(NTA):
                run2(front(t + 1) if t + 1 < NTA else None, back(t))
            k.barrier()

    def mixer_b_phase(self, hin, hout):
        nc, k, P = self.nc, self.k, self.P
        NGB = int(os.environ.get("MK_NGB", "8"))
        NHB = int(os.environ.get("MK_NHB", "12"))
        NDUM = int(os.environ.get("MK_NDUM", "1"))
        with ExitStack() as st:
            sb = lambda n, s, d: self.sbuf(st, n, s, d)
            memkT, memv = self.mem_kv(st, 1)
            win_d = self.dscratch("b_w_in_bf", [D, D], BF16)
            wout_d = self.dscratch("b_w_out_bf", [D, D], BF16)
            wdb = [Buf(None, "win_d"), Buf(None, "wout_d")]
            k.dma(win_d, P["b_w_in"][0], writes=[wdb[0]], q="pool")
            k.dma(wout_d, P["b_w_out"][0], writes=[wdb[1]], q="pool")
            KT = sb("KT", [128, 6, S], BF16)
            Vt = sb("Vt", [128, NT, 768], BF16)
            ht = [sb("htB%d" % i, [128, D], F32) for i in range(2)]
            xn = sb("xnB", [128, D], F32)
            stt = [sb("sttB%d" % i, [128, 4], F32) for i in range(2)]
            with ExitStack() as st1:
                sb1 = lambda n, s, d: self.sbuf(st1, n, s, d)
                kvg = sb1("kvg", [128, D], F32)
                k.dma(kvg[:], P["kv_norm"].partition_broadcast(128), writes=[kvg])
                w_kv = sb1("w_kv", [128, 8, 1536], BF16)
                for c in range(8):
                    k.dma(w_kv[:, c, :], P["w_kv"][c * 128:(c + 1) * 128, :], writes=[w_kv], q="pool")
                xT1 = [sb1("xT1_%d" % i, [128, 8, 128], BF16) for i in range(2)]
                for t in range(NT):
                    b = t % 2
                    h = ht[b]
                    k.dma(h[:], hin[0][t * 128:(t + 1) * 128, :], reads=[hin[1][t]], writes=[h])
                    self.rmsnorm(h, kvg, xn, xn, stt[b])
                    xT = xT1[b]
                    self.transpose8(xn, [(xT, lambda half: xT[:, half * 4:(half + 1) * 4, :])], self.nextbank(), self.nextbank())
                    for g4 in range(2):
                        nf = 4 if g4 == 0 else 2
                        pb = self.nextbank()
                        for j in range(nf):
                            fc = g4 * 4 + j
                            for dc in range(8):
                                k.op("pe", lambda e: e.matmul(pb[:, j * 128:(j + 1) * 128], w_kv[:, dc, fc * 128:(fc + 1) * 128], xT[:, dc, :],
                                                              start=(dc == 0), stop=(dc == 7)),
                                     reads=[w_kv, xT], writes=[pb], inc=(dc == 7 and j == nf - 1))
                        dstv = KT[:, g4 * 4:g4 * 4 + nf, t * 128:(t + 1) * 128]
                        srcv = pb[:, 0:nf * 128].rearrange("p (c t) -> p c t", c=nf)
                        k.op("act", lambda e: e.copy(dstv, srcv), reads=[pb], writes=[KT])
                    for half, (c0, c1) in enumerate(((0, 512), (512, 768))):
                        pb = self.nextbank()
                        for dc in range(8):
                            k.op("pe", lambda e: e.matmul(pb[:, 0:c1 - c0], xT[:, dc, :], w_kv[:, dc, 768 + c0:768 + c1],
                                                          start=(dc == 0), stop=(dc == 7)),
                                 reads=[w_kv, xT], writes=[pb], inc=(dc == 7))
                        k.op("dve", lambda e: e.tensor_copy(Vt[:, t, c0:c1], pb[:, 0:c1 - c0]), reads=[pb], writes=[Vt])
                k.barrier()
            bg = sb("bgain", [128, D], F32)
            k.dma(bg[:], P["b_norm"][0].partition_broadcast(128), writes=[bg])
            wB = sb("wB", [128, 8, D], BF16)
            xTg = sb("xTg", [128, 8, 512], BF16)
            qT = sb("qT", [128, 6, 512], BF16)
            mqT = sb("mqTB", [128, 2, 512], BF16)
            mixTg = sb("mixTg", [128, 8, 512], BF16)
            Eb = [sb("Eb%d" % i, [128, 512], F32) for i in range(3)]
            spb = [sb("spb%d" % i, [128, 512], BF16) for i in range(3)]
            eab = [sb("eab%d" % i, [128, 512], F32) for i in range(2)]
            ab = [sb("ab%d" % i, [128, 512], BF16) for i in range(2)]
            MW = self.mem_work(st)
            mmB = sb("mmB", [128, 256], F32)
            hn = sb("hnB", [128, D], F32)
            NGEb = self.cstb[:, 5, :]
            NLTb = self.cstb[:, 6, :]
            strictTb = self.cstb[:, 3, :]
            cstb = self.cstb
            PZ = [self.ps[0], self.ps[1]]
            PC = [self.ps[2], self.ps[3]]
            PO = [self.ps[4], self.ps[5]]
            rot = [0]

            def nb():
                rot[0] = (rot[0] + 1) % 8
                return self.ps[rot[0]]
            self.nextbank = nb
            for g in range(NGB):
                k.dma(wB[:], win_d.rearrange("(c p) n -> p c n", p=128), reads=[wdb[0]], writes=[wB])
                for tt in range(4):
                    t = g * 4 + tt
                    b = t % 2
                    h = ht[b]
                    k.dma(h[:], hin[0][t * 128:(t + 1) * 128, :], reads=[hin[1][t]], writes=[h])
                    self.rmsnorm(h, bg, xn, xn, stt[b])
                    self.transpose8(xn, [(xTg, lambda half: xTg[:, half * 4:(half + 1) * 4, tt * 128:(tt + 1) * 128])], nb(), nb())
                for fc in range(8):
                    pb = nb()
                    for dc in range(8):
                        k.op("pe", lambda e: e.matmul(pb[:], wB[:, dc, fc * 128:(fc + 1) * 128], xTg[:, dc, :], start=(dc == 0), stop=(dc == 7)),
                             reads=[wB, xTg], writes=[pb], inc=(dc == 7))
                    if fc < 6:
                        k.op("act", lambda e: e.mul(qT[:, fc, :], pb[:], 0.125), reads=[pb], writes=[qT])
                    else:
                        k.op("dve", lambda e: e.tensor_copy(mqT[:, fc - 6, :], pb[:]), reads=[pb], writes=[mqT])
                items = [(2 * p + s, kb) for p in range(NHB // 2) for kb in range(4 * g + 3, -1, -1) for s in range(2)]

                def geom(i):
                    hh, kb = items[i]
                    r = max(kb - 4 * g, 0)
                    return hh, kb, hh // 2, hh % 2, r * 128, kb >= 4 * g

                def s1_pe(i):
                    hh, kb, fc, s, c0, diag = geom(i)
                    ps_ = slice(s * 64, (s + 1) * 64)
                    cs = slice(c0, 512)
                    pz = PZ[i % 2]
                    k.op("pe", lambda e: e.matmul(pz[:, cs], KT[ps_, fc, kb * 128:(kb + 1) * 128], qT[ps_, fc, cs], start=True, stop=True),
                         reads=[KT, qT], writes=[pz])

                def s1_act(i):
                    hh, kb, fc, s, c0, diag = geom(i)
                    cs = slice(c0, 512)
                    pz, E, sp = PZ[i % 2], Eb[i % 3], spb[i % 3]
                    k.op("act", lambda e: e.activation(E[:, cs], pz[:, cs], AF.Exp), reads=[pz], writes=[E])
                    k.op("act", lambda e: e.activation(sp[:, cs], E[:, cs], AF.Ln, bias=1.0), reads=[E], writes=[sp])
                    if diag:
                        k.op("dve", lambda e: e.tensor_tensor(sp[:, c0:c0 + 128], sp[:, c0:c0 + 128], strictTb, ALU.mult),
                             reads=[sp, cstb], writes=[sp])

                def s2_peA(i):
                    hh, kb, fc, s, c0, diag = geom(i)
                    cs = slice(c0, 512)
                    C, sp = PC[s], spb[i % 3]
                    if kb == 4 * g + 3:
                        k.op("dve", lambda e: e.memset(C[:], 0.0), writes=[C])
                    k.op("pe", lambda e: e.matmul(C[:, cs], NGEb, sp[:, cs], start=False, stop=False, skip_group_check=True),
                         reads=[cstb, sp], writes=[C])

                def s2_act(i):
                    hh, kb, fc, s, c0, diag = geom(i)
                    cs = slice(c0, 512)
                    C, ea_ = PC[s], eab[i % 2]
                    k.op("act", lambda e: e.activation(ea_[:, cs], C[:, cs], AF.Exp), reads=[C], writes=[ea_])

                def s2_peB(i):
                    hh, kb, fc, s, c0, diag = geom(i)
                    cs = slice(c0, 512)
                    C, sp = PC[s], spb[i % 3]
                    if kb > 0:
                        k.op("pe", lambda e: e.matmul(C[:, cs], NLTb, sp[:, cs], start=False, stop=False, skip_group_check=True),
                             reads=[cstb, sp], writes=[C])

                def s3_pool(i):
                    hh, kb, fc, s, c0, diag = geom(i)
                    cs = slice(c0, 512)
                    E, ea_, a_ = Eb[i % 3], eab[i % 2], ab[i % 2]
                    k.op("dve", lambda e: e.tensor_tensor(a_[:, cs], E[:, cs], ea_[:, cs], ALU.mult), reads=[E, ea_], writes=[a_])
                    if diag:
                        k.op("dve", lambda e: e.tensor_tensor(a_[:, c0:c0 + 128], a_[:, c0:c0 + 128], strictTb, ALU.mult),
                             reads=[a_, cstb], writes=[a_])

                def s3_pe(i):
                    hh, kb, fc, s, c0, diag = geom(i)
                    cs = slice(c0, 512)
                    a_ = ab[i % 2]
                    po = PO[fc % 2]
                    if kb == 4 * g + 3 and s == 0:
                        k.op("dve", lambda e: e.memset(po[:], 0.0), writes=[po])
                    vblk = Vt[:, kb, hh * 64:(hh + 1) * 64]
                    if s == 0:
                        k.op("pe", lambda e: e.matmul(po[0:64, cs], vblk, a_[:, cs], start=False, stop=False, skip_group_check=True),
                             reads=[Vt, a_], writes=[po])
                    else:
                        k.op("pe", lambda e: e.matmul(po[64:128, cs], vblk, a_[:, cs], start=False, stop=False, skip_group_check=True,
                                                      tile_position=(0, 64)), reads=[Vt, a_], writes=[po])
                    if kb == 0 and s == 1:
                        k.op("act", lambda e: e.copy(mixTg[:, fc, :], po[:]), reads=[po], writes=[mixTg])

                n_it = len(items)
                ok = lambda i: 0 <= i < n_it
                dumrhs = cstb[:, 0:4, :].rearrange("p c t -> p (c t)")
                for step in range(-3, n_it + 1):
                    i0, i1, i2_, i3, i4 = step + 3, step + 2, step + 1, step, step - 1
                    if ok(i3):
                        s3_pool(i3)
                    if ok(i0):
                        s1_pe(i0)
                    if ok(i2_):
                        s2_peA(i2_)
                    if ok(i4):
                        s3_pe(i4)
                    if ok(i1):
                        s1_act(i1)
                    if ok(i2_):
                        s2_act(i2_)
                    if ok(i3):
                        s2_peB(i3)
                    for _d in range(NDUM):
                        k.op("pe", lambda e: e.matmul(self.ps[6][:], NGEb, dumrhs, start=True, stop=True),
                             reads=[cstb], writes=[self.ps[6]], inc=False)
                k.dma(wB[:], wout_d.rearrange("(c p) n -> p c n", p=128), reads=[wdb[1]], writes=[wB])
                for tt in range(4):
                    t = g * 4 + tt
                    b = t % 2
                    h = ht[b]
                    k.dma(h[:], hin[0][t * 128:(t + 1) * 128, :], reads=[hin[1][t]], writes=[h])
                    self.mem_attend(MW, mqT, tt * 128, memkT, memv, mmB, col0=0)
                    pb = nb()
                    for j in range(2):
                        k.op("pe", lambda e: e.transpose(pb[:, j * 128:(j + 1) * 128], mmB[:, j * 128:(j + 1) * 128], self.ident),
                             reads=[mmB, self.cst], writes=[pb], inc=(j == 1))
                    k.op("act", lambda e: e.copy(mixTg[:, 6:8, tt * 128:(tt + 1) * 128], pb[:, 0:256].rearrange("p (c t) -> p c t", c=2)),
                         reads=[pb], writes=[mixTg])
                    for half in range(2):
                        pb = nb()
                        for fc in range(8):
                            k.op("pe", lambda e: e.matmul(pb[:], mixTg[:, fc, tt * 128:(tt + 1) * 128], wB[:, fc, half * 512:(half + 1) * 512],
                                                          start=(fc == 0), stop=(fc == 7)),
                                 reads=[mixTg, wB], writes=[pb], inc=(fc == 7))
                        k.op("dve", lambda e: e.tensor_tensor(hn[:, half * 512:(half + 1) * 512], h[:, half * 512:(half + 1) * 512], pb[:], ALU.add),
                             reads=[h, pb], writes=[hn])
                    k.dma(hout[0][t * 128:(t + 1) * 128, :], hn[:], reads=[hn], writes=[hout[1][t]])
            k.barrier()
            del self.nextbank

    def build(self, phases):
        nc, k = self.nc, self.k
        P = self.P = {}
        shapes = dict(
            x=[S, D], mem=[256, D], a_norm=[1, D], a_w_in=[1, D, 3340], a_conv=[1, 4, 2304],
            a_log=[1, 6], a_dt_bias=[1, 6], a_out_gain=[1, 128], a_w_out=[1, D, D],
            kv_norm=[D], w_kv=[D, 1536], b_norm=[1, D], b_w_in=[1, D, D], b_w_out=[1, D, D],
            mem_norm=[2, D], w_mem_kv=[2, D, 512], ffn_norm=[2, D], w_group=[2, D, 4], b_group=[2, 4],
            w_router=[2, D, 16], b_router=[2, 16], w1=[2, 16, D, 256], w3=[2, 16, D, 256],
            w2=[2, 16, 256, D], final_norm=[D], wgr=[2, 128, 8, 20], rbias=[2, 20], convw=[128, 18, 4])
        for n, s in shapes.items():
            P[n] = self.din(n, s)
        out = nc.dram_tensor("out", [S, D], F32, kind="ExternalOutput").ap()
        mkbufs = lambda nm: [Buf(None, "%s%d" % (nm, i)) for i in range(NT)]
        hx = (P["x"], mkbufs("x"))
        hA = (self.dscratch("hA", [S, D]), mkbufs("hA"))
        hB = (self.dscratch("hB", [S, D]), mkbufs("hB"))
        ho = (out, mkbufs("out"))
        with ExitStack() as gst:
            self.load_consts(gst)
            self.epsbuf = self.sbuf(gst, "epsb", [128, 1], F32)
            self.epsb = self.epsbuf
            k.op("dve", lambda e: e.memset(self.epsbuf[:], EPS), writes=[self.epsbuf])
            cur = hx
            seq = {"A": hA, "M0": hB, "B": hA, "M1": ho}
            for ph in phases:
                dst = seq[ph] if ph != phases[-1] else ho
                if ph == "M0":
                    self.moe_phase(0, cur, dst, final=False)
                elif ph == "M1":
                    self.moe_phase(1, cur, dst, final=True)
                elif ph == "A":
                    self.mixer_a_phase(cur, dst)
                elif ph == "B":
                    self.mixer_b_phase(cur, dst)
                cur = dst
            for b in ho[1]:
                if b.lw is not None:
                    k._wait("sp", b.lw)
        return nc


def make_consts():
    c = np.zeros((128, 8, 128), np.float32)
    i = np.arange(128)
    c[:, 0, :] = np.eye(128)
    c[:, 1, :] = (i[:, None] <= i[None, :])
    c[:, 2, :] = (i[:, None] > i[None, :])
    c[:, 3, :] = (i[:, None] < i[None, :])
    c[:, 4, :] = 1.0
    c[:, 5, :] = -(i[:, None] >= i[None, :]).astype(np.float32)
    c[:, 6, :] = -(i[:, None] < i[None, :]).astype(np.float32)
    c[:, 7, :] = -30000.0 * (i[:, None] >= i[None, :])
    return c


_CACHE = {}


def run(inputs, phases=("A", "M0", "B", "M1"), ncores=NCORES, trace=False):
    key = tuple(phases)
    if key not in _CACHE:
        mk = MK(phases)
        _CACHE[key] = mk.build(list(phases))
    nc = _CACHE[key]
    consts = make_consts()
    inputs = dict(inputs)
    wg = np.concatenate([np.asarray(inputs["w_group"]), np.asarray(inputs["w_router"])], axis=2)
    inputs["wgr"] = np.ascontiguousarray(wg.reshape(2, 8, 128, 20).transpose(0, 2, 1, 3))
    inputs["rbias"] = np.concatenate([np.asarray(inputs["b_group"]), np.asarray(inputs["b_router"])], axis=1)
    cw = np.asarray(inputs["a_conv"])[0]
    inputs["convw"] = np.ascontiguousarray(cw.reshape(4, 18, 128).transpose(2, 1, 0))
    in_maps = []
    for c in range(ncores):
        m = {"consts": consts}
        for n, v in inputs.items():
            v = np.asarray(v)
            if n in ("x", "mem"):
                m[n] = np.ascontiguousarray(v[c])
            else:
                m[n] = np.ascontiguousarray(v, dtype=np.float32)
        in_maps.append(m)
    res = run_bass_kernel_spmd(nc, in_maps, core_ids=list(range(ncores)), trace=trace)
    outs = np.stack([r["out"] for r in res.results], axis=0)
    return outs, res


def kernel(**inputs):
    outs, _ = run(inputs)
    return outs.astype(np.float32)
```

```python
from contextlib import ExitStack
import os
import numpy as np
import concourse.bass as bass
import concourse.mybir as mybir
from concourse.bass_utils import run_bass_kernel_spmd

F32 = mybir.dt.float32
BF16 = mybir.dt.bfloat16
AF = mybir.ActivationFunctionType
ALU = mybir.AluOpType
AX = mybir.AxisListType

S = 4096
D = 1024
NT = S // 128
EPS = 1e-6
NCORES = 8


class Buf:
    __slots__ = ("ap", "name", "_lw", "_rd", "_excl")
    lw = property(lambda self: self._lw, lambda self, v: setattr(self, "_lw", v))
    rd = property(lambda self: self._rd, lambda self, v: setattr(self, "_rd", v))
    excl = property(lambda self: self._excl, lambda self, v: setattr(self, "_excl", v))

    def __init__(self, ap, name="", excl=False):
        self.ap = ap
        self.name = name
        self.excl = excl
        self.lw = None
        self.rd = []

    def __getitem__(self, idx):
        return self.ap[idx]


class View(Buf):
    __slots__ = ("parent",)

    def __init__(self, parent, ap):
        self.parent = parent
        self.ap = ap
        self.name = parent.name

    lw = property(lambda self: self.parent.lw, lambda self, v: setattr(self.parent, "lw", v))
    rd = property(lambda self: self.parent.rd, lambda self, v: setattr(self.parent, "rd", v))
    excl = property(lambda self: self.parent.excl, lambda self, v: None)


class K:
    NDMA = 48

    def __init__(self, nc):
        self.nc = nc
        self.eng = {"pe": nc.tensor, "act": nc.scalar, "dve": nc.vector,
                    "pool": nc.gpsimd, "sp": nc.sync}
        self.sem = {e: nc.alloc_semaphore("s_" + e) for e in ("pe", "act", "dve", "pool")}
        self.cnt = {e: 0 for e in self.sem}
        self.waited = {}
        self.dsem = [nc.alloc_semaphore("d%d" % i) for i in range(self.NDMA)]
        self.dcnt = [0] * self.NDMA
        self.dnext = 0
        self.nins = 0

    def _semh(self, key):
        return self.sem[key] if isinstance(key, str) else self.dsem[key]

    def _wait(self, e, dep):
        key, val = dep
        if key == e and e == "pe":
            return
        w = self.waited.get((e, key), 0)
        if w >= val:
            return
        self.eng[e].wait_ge(self._semh(key), val)
        self.nins += 1
        self.waited[(e, key)] = val

    def _deps(self, e, reads, writes):
        best = {}
        for r in reads:
            if r.lw is not None:
                if best.get(r.lw[0], 0) < r.lw[1]:
                    best[r.lw[0]] = r.lw[1]
            if r.excl:
                for key, val in r.rd:
                    if key != e and best.get(key, 0) < val:
                        best[key] = val
        for w in writes:
            if w.lw is not None:
                if best.get(w.lw[0], 0) < w.lw[1]:
                    best[w.lw[0]] = w.lw[1]
            for key, val in w.rd:
                if best.get(key, 0) < val:
                    best[key] = val
        for key, val in best.items():
            self._wait(e, (key, val))

    def _mark(self, tag, reads, writes):
        for r in reads:
            r.rd.append(tag)
            if len(r.rd) > 64:
                best = {}
                for key, val in r.rd:
                    if best.get(key, 0) < val:
                        best[key] = val
                r.rd = list(best.items())
        for w in writes:
            w.lw = tag
            w.rd = []

    def op(self, e, fn, reads=(), writes=(), inc=True):
        self._deps(e, reads, writes)
        ins = fn(self.eng[e])
        self.nins += 1
        if inc:
            ins.then_inc(self.sem[e], 1)
            self.cnt[e] += 1
            tag = (e, self.cnt[e])
        else:
            tag = (e, self.cnt[e] + 1)
        self._mark(tag, reads, writes)
        return ins

    def dma(self, out, in_, reads=(), writes=(), q="sp", **kw):
        slot = self.dnext
        self.dnext = (self.dnext + 1) % self.NDMA
        if self.dcnt[slot] > 0:
            self._wait(q, (slot, 16 * self.dcnt[slot]))
        self._deps(q, reads, writes)
        ins = self.eng[q].dma_start(out=out, in_=in_, **kw)
        self.nins += 1
        ins.then_inc(self.dsem[slot], 16)
        self.dcnt[slot] += 1
        tag = (slot, 16 * self.dcnt[slot])
        self._mark(tag, reads, writes)
        return tag

    def barrier(self):
        for e in ("pe", "act", "dve", "pool", "sp"):
            for e2 in ("pe", "act", "dve", "pool"):
                if e2 != e and self.cnt[e2] > 0:
                    self._wait(e, (e2, self.cnt[e2]))
            for slot in range(self.NDMA):
                if self.dcnt[slot] > 0:
                    self._wait(e, (slot, 16 * self.dcnt[slot]))


class MK:
    def __init__(self, phases, h0_from_input=True):
        self.nc = nc = bass.Bass("TRN2", target_bir_lowering=False)
        self.k = K(nc)
        self.uid = 0
        self.ins = {}
        self.ps = [Buf(nc.alloc_psum_tensor("psb%d" % i, [128, 512], F32).ap(), "ps%d" % i, excl=True)
                   for i in range(8)]

    def din(self, name, shape):
        ap = self.nc.dram_tensor(name, list(shape), F32, kind="ExternalInput").ap()
        self.ins[name] = ap
        return ap

    def dscratch(self, name, shape, dt=F32):
        return self.nc.dram_tensor(name, list(shape), dt, kind="Internal").ap()

    def sbuf(self, st, name, shape, dt):
        self.uid += 1
        h = st.enter_context(self.nc.sbuf_tensor("%s_%d" % (name, self.uid), list(shape), dt))
        return Buf(h.ap(), name)

    def load_consts(self, st):
        k = self.k
        c = self.din("consts", [128, 8, 128])
        self.cst = self.sbuf(st, "cst", [128, 8, 128], F32)
        k.dma(self.cst[:], c, writes=[self.cst])
        self.ident = self.cst[:, 0, :]
        self.U = self.cst[:, 1, :]
        self.SL = self.cst[:, 2, :]
        self.strictT = self.cst[:, 3, :]
        self.ones = self.cst[:, 4, :]
        self.cstb = self.sbuf(st, "cstb", [128, 8, 128], BF16)
        k.dma(self.cstb[:], c, writes=[self.cstb], q="pool")
        self.identb = self.cstb[:, 0, :]
        self.onesb = self.cstb[:, 4, :]

    def rmsnorm(self, h, gainb, xn, junk, st2):
        k = self.k
        k.op("act", lambda e: e.activation(junk[:], h[:], AF.Square, accum_out=st2[:, 0:1]),
             reads=[h], writes=[junk, st2])
        k.op("act", lambda e: e.activation(st2[:, 1:2], st2[:, 0:1], AF.Ln, bias=self.epsb[:, 0:1], scale=1.0 / D),
             reads=[st2, self.epsbuf], writes=[st2])
        k.op("act", lambda e: e.activation(st2[:, 2:3], st2[:, 1:2], AF.Exp, scale=-0.5), reads=[st2], writes=[st2])
        k.op("dve", lambda e: e.scalar_tensor_tensor(xn[:], h[:], st2[:, 2:3], gainb[:], ALU.mult, ALU.mult),
             reads=[h, st2, gainb], writes=[xn])

    def transpose8(self, src, dsts, psa, psb, evac=("act", "dve"), second="pool"):
        k = self.k
        for half, ps in enumerate((psa, psb)):
            for j in range(4):
                c = half * 4 + j
                k.op("pe", lambda e: e.transpose(ps[:, j * 128:(j + 1) * 128], src[:, c * 128:(c + 1) * 128], self.ident),
                     reads=[src, self.cst], writes=[ps], inc=(j == 3))
            dbuf, fn = dsts[0]
            eng = evac[half % len(evac)]
            pv = ps[:].rearrange("p (c t) -> p c t", c=4)
            if eng == "act":
                k.op("act", lambda e: e.copy(fn(half), pv), reads=[ps], writes=[dbuf])
            else:
                k.op(eng, lambda e: e.tensor_copy(fn(half), pv), reads=[ps], writes=[dbuf])
            for dbuf2, fn2 in dsts[1:]:
                k.op(second, lambda e: e.tensor_copy(fn2(half), fn(half)), reads=[dbuf], writes=[dbuf2])

    def moe_phase(self, l, hin, hout, final=False, out_ap=None):
        nc, k = self.nc, self.k
        G = 1024
        NTG = G // 128
        NG = S // G
        NTB = G // 512
        NEX = 16
        P = self.P
        with ExitStack() as st:
            sb = lambda n, s, d: self.sbuf(st, n, s, d)
            gain = sb("gain", [128, D], F32)
            k.dma(gain[:], P["ffn_norm"][l].partition_broadcast(128), writes=[gain])
            if final:
                fgain = sb("fgain", [128, D], F32)
                k.dma(fgain[:], P["final_norm"].partition_broadcast(128), writes=[fgain])
            wgr = sb("wgr", [128, 8, 20], F32)
            k.dma(wgr[:], P["wgr"][l], writes=[wgr])
            rb = sb("rbias", [128, 20], F32)
            k.dma(rb[:], P["rbias"][l].partition_broadcast(128), writes=[rb])
            xnT = [sb("xnT%d" % i, [128, 8, G], BF16) for i in range(2)]
            yacc = [[sb("yacc%d_%d" % (j, i), [128, D], F32) for i in range(NTG)] for j in range(2)]
            comb = [[sb("comb%d_%d" % (j, i), [128, 16], F32) for i in range(NTG)] for j in range(2)]
            w1b = [sb("w1b%d" % i, [128, 8, 256], BF16) for i in range(2)]
            w3b = [sb("w3b%d" % i, [128, 8, 256], BF16) for i in range(2)]
            w2b = [sb("w2b%d" % i, [128, 2, D], BF16) for i in range(2)]
            ht = [sb("ht%d" % i, [128, D], F32) for i in range(2)]
            htc = [sb("htc%d" % i, [128, D], F32) for i in range(2)]
            xn = [sb("xn%d" % i, [128, D], F32) for i in range(2)]
            xnT32 = [sb("xnT32_%d" % i, [128, 8, 128], F32) for i in range(2)]
            stt = [sb("stt%d" % i, [128, 4], F32) for i in range(2)]
            sttc = [sb("sttc%d" % i, [128, 4], F32) for i in range(2)]
            rt = [sb("rt%d" % i, [128, 96], F32) for i in range(2)]
            hid = [sb("hid%d" % i, [128, 2, 512], BF16) for i in range(2)]
            sil = [sb("sil%d" % i, [128, 512], F32) for i in range(2)]
            ps = self.ps
            w1d, w3d, w2d = P["w1"], P["w3"], P["w2"]

            def load_w(e, slot):
                k.dma(w1b[slot][:], w1d[l, e].rearrange("(c p) f -> p c f", p=128), writes=[w1b[slot]], q="pool")
                k.dma(w3b[slot][:], w3d[l, e].rearrange("(c p) f -> p c f", p=128), writes=[w3b[slot]], q="pool")
                k.dma(w2b[slot][:], w2d[l, e].rearrange("(c p) n -> p c n", p=128), writes=[w2b[slot]], q="pool")

            def stage_a(g):
                par = g % 2
                xT = xnT[par]
                for ti in range(NTG):
                    t = g * NTG + ti
                    b = ti % 2
                    h = ht[b]
                    k.dma(h[:], hin[0][t * 128:(t + 1) * 128, :], reads=[hin[1][t]], writes=[h])
                    self.rmsnorm(h, gain, xn[b], xn[b], stt[b])
                    yield
                    x32 = xnT32[b]
                    self.transpose8(
                        xn[b],
                        [(x32, lambda half: x32[:, half * 4:(half + 1) * 4, :]),
                         (xT, lambda half: xT[:, half * 4:(half + 1) * 4, ti * 128:(ti + 1) * 128])],
                        ps[6], ps[7])
                    yield
                    pr = ps[6 + (ti % 2)]
                    for dc in range(8):
                        k.op("pe", lambda e: e.matmul(pr[:, 0:20], x32[:, dc, :], wgr[:, dc, :], start=(dc == 0), stop=(dc == 7)),
                             reads=[x32, wgr], writes=[pr], inc=(dc == 7))
                    yield
                    r = rt[b]
                    R = lambda a, n: r[:, a:a + n]
                    lg, gmax, ngmax, oh, ge, gsum, pg = R(0, 20), R(20, 1), R(21, 1), R(22, 4), R(26, 4), R(30, 1), R(31, 1)
                    tmp, elsel, m1, nm1, ee, mask1, ee2 = R(32, 16), R(48, 4), R(52, 1), R(53, 1), R(54, 4), R(58, 4), R(62, 4)
                    v2, mask2, den, rden, wl, scl = R(66, 1), R(67, 4), R(71, 1), R(72, 1), R(73, 4), R(77, 1)
                    dv = lambda fn, rd=(), wr=(): k.op("dve", fn, reads=[r] + list(rd), writes=[r] + list(wr))
                    dv(lambda e: e.tensor_tensor(lg, pr[:, 0:20], rb[:], ALU.add), rd=[pr, rb])
                    dv(lambda e: e.tensor_reduce(gmax, lg[:, 0:4], AX.X, ALU.max))
                    dv(lambda e: e.tensor_single_scalar(ngmax, gmax, -1.0, ALU.mult))
                    dv(lambda e: e.tensor_scalar(oh, lg[:, 0:4], gmax, None, ALU.is_equal))
                    yield
                    k.op("act", lambda e: e.activation(ge, lg[:, 0:4], AF.Exp, bias=ngmax, accum_out=gsum), reads=[r], writes=[r])
                    dv(lambda e: e.reciprocal(pg, gsum))
                    dv(lambda e: e.tensor_tensor(tmp.rearrange("p (g j) -> p g j", g=4),
                                                 lg[:, 4:20].rearrange("p (g j) -> p g j", g=4),
                                                 oh.unsqueeze(2).to_broadcast([128, 4, 4]), ALU.mult))
                    dv(lambda e: e.tensor_reduce(elsel, tmp.rearrange("p (g j) -> p j g", g=4), AX.X, ALU.add))
                    yield
                    dv(lambda e: e.tensor_reduce(m1, elsel, AX.X, ALU.max))
                    dv(lambda e: e.tensor_single_scalar(nm1, m1, -1.0, ALU.mult))
                    k.op("act", lambda e: e.activation(ee, elsel, AF.Exp, bias=nm1), reads=[r], writes=[r])
                    dv(lambda e: e.tensor_scalar(mask1, elsel, m1, None, ALU.is_equal))
                    yield
                    dv(lambda e: e.scalar_tensor_tensor(ee2, mask1, -2.0, ee, ALU.mult, ALU.add))
                    dv(lambda e: e.tensor_reduce(v2, ee2, AX.X, ALU.max))
                    dv(lambda e: e.tensor_scalar(mask2, ee2, v2, None, ALU.is_equal))
                    dv(lambda e: e.tensor_single_scalar(den, v2, 1.0, ALU.add))
                    yield
                    dv(lambda e: e.reciprocal(rden, den))
                    dv(lambda e: e.scalar_tensor_tensor(wl, mask2, v2, mask1, ALU.mult, ALU.add))
                    dv(lambda e: e.tensor_tensor(scl, pg, rden, ALU.mult))
                    dv(lambda e: e.tensor_scalar(wl, wl, scl, None, ALU.mult))
                    cb = comb[par][ti]
                    dv(lambda e: e.tensor_tensor(cb[:].rearrange("p (g j) -> p g j", g=4),
                                                 oh.unsqueeze(2).to_broadcast([128, 4, 4]),
                                                 wl.unsqueeze(1).to_broadcast([128, 4, 4]), ALU.mult), wr=[cb])
                    yield

            def stage_b(g):
                par = g % 2
                xT = xnT[par]
                work = [(ex, tb) for ex in range(NEX) for tb in range(NTB)]

                def up(idx):
                    ex, tb = work[idx]
                    slot = ex % 2
                    w1, w3 = w1b[slot], w3b[slot]
                    hd = hid[idx % 2]
                    for fc in range(2):
                        p1 = ps[fc]
                        p3 = ps[2 + fc]
                        for wsrc, pdst in ((w1, p1), (w3, p3)):
                            for dc in range(8):
                                k.op("pe", lambda e: e.matmul(pdst[:], wsrc[:, dc, fc * 128:(fc + 1) * 128], xT[:, dc, tb * 512:(tb + 1) * 512],
                                                              start=(dc == 0), stop=(dc == 7)),
                                     reads=[wsrc, xT], writes=[pdst], inc=(dc == 7))
                                if dc % 4 == 3:
                                    yield
                        sl = sil[fc]
                        k.op("act", lambda e: e.activation(sl[:], p1[:], AF.Silu), reads=[p1], writes=[sl])
                        k.op("dve", lambda e: e.tensor_tensor(hd[:, fc, :], sl[:], p3[:], ALU.mult), reads=[sl, p3], writes=[hd])

                def down(idx):
                    ex, tb = work[idx]
                    slot = ex % 2
                    w2 = w2b[slot]
                    hd = hid[idx % 2]
                    for tt in range(4):
                        ti = tb * 4 + tt
                        for half in range(2):
                            py = ps[4 + half]
                            for fc in range(2):
                                k.op("pe", lambda e: e.matmul(py[:], hd[:, fc, tt * 128:(tt + 1) * 128], w2[:, fc, half * 512:(half + 1) * 512],
                                                              start=(fc == 0), stop=(fc == 1)),
                                     reads=[hd, w2], writes=[py], inc=(fc == 1))
                            ya = yacc[par][ti]
                            cs = comb[par][ti][:, ex:ex + 1]
                            if ex == 0:
                                k.op("dve", lambda e: e.tensor_scalar(ya[:, half * 512:(half + 1) * 512], py[:], cs, None, ALU.mult),
                                     reads=[py, comb[par][ti]], writes=[ya])
                            else:
                                k.op("dve", lambda e: e.scalar_tensor_tensor(ya[:, half * 512:(half + 1) * 512], py[:], cs,
                                                                             ya[:, half * 512:(half + 1) * 512], ALU.mult, ALU.add),
                                     reads=[py, comb[par][ti], ya], writes=[ya])
                            yield
                    if tb == NTB - 1:
                        if ex + 2 < NEX:
                            load_w(ex + 2, slot)
                        elif g + 1 < NG:
                            load_w(ex + 2 - NEX, slot)

                def rr(*gens):
                    gens = [g_ for g_ in gens if g_ is not None]
                    while gens:
                        for g_ in list(gens):
                            try:
                                next(g_)
                                yield
                            except StopIteration:
                                gens.remove(g_)

                yield from up(0)
                for idx in range(len(work)):
                    nu = up(idx + 1) if idx + 1 < len(work) else None
                    if nu is not None:
                        next(nu)
                        yield
                    yield from rr(nu, down(idx))

            def stage_c(g):
                par = g % 2
                for ti in range(NTG):
                    t = g * NTG + ti
                    b = ti % 2
                    h = htc[b]
                    ya = yacc[par][ti]
                    k.dma(h[:], hin[0][t * 128:(t + 1) * 128, :], reads=[hin[1][t]], writes=[h])
                    k.op("pool", lambda e: e.tensor_tensor(ya[:], h[:], ya[:], ALU.add), reads=[h, ya], writes=[ya])
                    yield
                    if final:
                        self.rmsnorm(ya, fgain, h, h, sttc[b])
                        k.dma(hout[0][t * 128:(t + 1) * 128, :], h[:], reads=[h], writes=[hout[1][t]])
                    else:
                        k.dma(hout[0][t * 128:(t + 1) * 128, :], ya[:], reads=[ya], writes=[hout[1][t]])
                    yield

            def run_group(bg, ag, cg):
                others = [[g_, per] for g_, per in ((ag, 4), (cg, 24)) if g_ is not None]
                step = 0
                b_alive = bg is not None
                while b_alive or others:
                    if b_alive:
                        try:
                            next(bg)
                        except StopIteration:
                            b_alive = False
                    for item in list(others):
                        if (not b_alive) or step % item[1] == 0:
                            try:
                                next(item[0])
                            except StopIteration:
                                others.remove(item)
                    step += 1

            load_w(0, 0)
            load_w(1, 1)
            run_group(None, stage_a(0), None)
            for g in range(NG):
                run_group(stage_b(g), stage_a(g + 1) if g + 1 < NG else None, stage_c(g - 1) if g >= 1 else None)
            run_group(None, None, stage_c(NG - 1))
            k.barrier()

    def nextbank(self):
        self.pbi = (getattr(self, "pbi", -1) + 1) % 8
        return self.ps[self.pbi]

    def mem_kv(self, st, l):
        k, P = self.k, self.P
        sb = lambda n, s, d: self.sbuf(st, n, s, d)
        memkT = sb("memkT", [128, 2, 256], BF16)
        memv = sb("memv", [128, 2, 256], BF16)
        with ExitStack() as st2:
            sb2 = lambda n, s, d: self.sbuf(st2, n, s, d)
            g = sb2("mg", [128, D], F32)
            k.dma(g[:], P["mem_norm"][l].partition_broadcast(128), writes=[g])
            w = sb2("wmkv", [128, 8, 512], BF16)
            k.dma(w[:], P["w_mem_kv"][l].rearrange("(c p) n -> p c n", p=128), writes=[w], q="pool")
            mT = sb2("memnT", [128, 8, 256], BF16)
            junk = sb2("mjunk", [128, D], F32)
            for mt in range(2):
                h = sb2("mh%d" % mt, [128, D], F32)
                xn = sb2("mxn%d" % mt, [128, D], F32)
                stt = sb2("mst%d" % mt, [128, 4], F32)
                k.dma(h[:], P["mem"][mt * 128:(mt + 1) * 128, :], writes=[h])
                self.rmsnorm(h, g, xn, junk, stt)
                self.transpose8(xn, [(mT, lambda half: mT[:, half * 4:(half + 1) * 4, mt * 128:(mt + 1) * 128])],
                                self.nextbank(), self.nextbank())
            for j in range(2):
                pb = self.nextbank()
                for dc in range(8):
                    k.op("pe", lambda e: e.matmul(pb[:, 0:256], w[:, dc, j * 128:(j + 1) * 128], mT[:, dc, :],
                                                  start=(dc == 0), stop=(dc == 7)),
                         reads=[w, mT], writes=[pb], inc=(dc == 7))
                k.op("act", lambda e: e.copy(memkT[:, j, :], pb[:, 0:256]), reads=[pb], writes=[memkT])
            for mt in range(2):
                pb = self.nextbank()
                for dc in range(8):
                    k.op("pe", lambda e: e.matmul(pb[:, 0:256], mT[:, dc, mt * 128:(mt + 1) * 128], w[:, dc, 256:512],
                                                  start=(dc == 0), stop=(dc == 7)),
                         reads=[w, mT], writes=[pb], inc=(dc == 7))
                k.op("dve", lambda e: e.tensor_copy(memv[:, mt, :], pb[:, 0:256]), reads=[pb], writes=[memv])
            k.barrier()
        return memkT, memv

    def mem_attend(self, W, mqT, qoff, memkT, memv, mix, col0=768):
        for _ in self.mem_attend_g(W, mqT, qoff, memkT, memv, mix, col0):
            pass

    def mem_attend_g(self, W, mqT, qoff, memkT, memv, mix, col0=768, banks=None):
        k = self.k
        pe_ = W["pexp"]; ms = W["mstat"]; pT = W["pT"]
        if banks is None:
            banks = [self.nextbank(), self.nextbank()]
        for hh in range(4):
            pair, s = hh // 2, hh % 2
            pb = banks[s]
            k.op("pe", lambda e: e.matmul(pb[:, pair * 256:(pair + 1) * 256], mqT[s * 64:(s + 1) * 64, pair, qoff:qoff + 128],
                                          memkT[s * 64:(s + 1) * 64, pair, :], start=True, stop=True),
                 reads=[mqT, memkT], writes=[pb])
        for s in range(2):
            pb = banks[s]
            k.op("dve", lambda e: e.tensor_reduce(ms[:, s:s + 3:2], pb[:].rearrange("p (h m) -> p h m", h=2), AX.X, ALU.max),
                 reads=[pb], writes=[ms])
        k.op("dve", lambda e: e.tensor_single_scalar(ms[:, 4:8], ms[:, 0:4], -0.125, ALU.mult), reads=[ms], writes=[ms])
        yield
        for hh in range(4):
            pair, s = hh // 2, hh % 2
            pb = banks[s]
            k.op("act", lambda e: e.activation(pe_[:, hh, :], pb[:, pair * 256:(pair + 1) * 256], AF.Exp, bias=ms[:, 4 + hh:5 + hh],
                                               scale=0.125, accum_out=ms[:, 8 + hh:9 + hh]),
                 reads=[pb, ms], writes=[pe_, ms])
        k.op("dve", lambda e: e.reciprocal(ms[:, 12:16], ms[:, 8:12]), reads=[ms], writes=[ms])
        yield
        for half in range(2):
            pb = self.nextbank()
            for j in range(4):
                idx = half * 4 + j
                hh, mc = idx // 2, idx % 2
                k.op("pe", lambda e: e.transpose(pb[:, j * 128:(j + 1) * 128], pe_[:, hh, mc * 128:(mc + 1) * 128], self.ident),
                     reads=[pe_, self.cst], writes=[pb], inc=(j == 3))
            if half == 0:
                k.op("act", lambda e: e.copy(pT[:, 0:4, :], pb[:].rearrange("p (c t) -> p c t", c=4)), reads=[pb], writes=[pT])
            else:
                k.op("dve", lambda e: e.tensor_copy(pT[:, 4:8, :], pb[:].rearrange("p (c t) -> p c t", c=4)), reads=[pb], writes=[pT])
        yield
        pb = self.nextbank()
        for hh in range(4):
            for mc in range(2):
                k.op("pe", lambda e: e.matmul(pb[:, hh * 64:(hh + 1) * 64], pT[:, hh * 2 + mc, :], memv[:, mc, hh * 64:(hh + 1) * 64],
                                              start=(mc == 0), stop=(mc == 1)),
                     reads=[pT, memv], writes=[pb], inc=(mc == 1))
        k.op("dve", lambda e: e.tensor_tensor(mix[:, col0:col0 + 256].rearrange("p (h d) -> p h d", h=4),
                                              pb[:, 0:256].rearrange("p (h d) -> p h d", h=4),
                                              ms[:, 12:16].unsqueeze(2).to_broadcast([128, 4, 64]), ALU.mult),
             reads=[pb, ms], writes=[mix])

    def mem_work(self, st):
        sb = lambda n, s, d: self.sbuf(st, n, s, d)
        return {"pexp": sb("pexp", [128, 4, 256], F32), "mstat": sb("mstat", [128, 16], F32),
                "pT": sb("pT", [128, 8, 128], BF16)}

    def out_proj(self, mix, mixT, w_out, h, hn, dst_ap, dst_buf):
        k = self.k
        self.transpose8(mix, [(mixT, lambda half: mixT[:, half * 4:(half + 1) * 4, :])], self.nextbank(), self.nextbank())
        for half in range(2):
            pb = self.nextbank()
            for fc in range(8):
                k.op("pe", lambda e: e.matmul(pb[:], mixT[:, fc, :], w_out[:, fc, half * 512:(half + 1) * 512],
                                              start=(fc == 0), stop=(fc == 7)),
                     reads=[mixT, w_out], writes=[pb], inc=(fc == 7))
            k.op("dve", lambda e: e.tensor_tensor(hn[:, half * 512:(half + 1) * 512], h[:, half * 512:(half + 1) * 512], pb[:], ALU.add),
                 reads=[h, pb], writes=[hn])
        k.dma(dst_ap, hn[:], reads=[hn], writes=[dst_buf])

    def mixer_a_phase(self, hin, hout):
        nc, k, P = self.nc, self.k, self.P
        NTA = int(os.environ.get("MK_NTA", str(NT)))
        with ExitStack() as st:
            sb = lambda n, s, d: self.sbuf(st, n, s, d)
            memkT, memv = self.mem_kv(st, 0)
            gain = sb("gainA", [128, D], F32)
            k.dma(gain[:], P["a_norm"][0].partition_broadcast(128), writes=[gain])
            w_in = sb("w_inA", [128, 8, 3340], BF16)
            for c in range(8):
                k.dma(w_in[:, c, :], P["a_w_in"][0, c * 128:(c + 1) * 128, :], writes=[w_in], q="pool")
            w_out = sb("w_outA", [128, 8, D], BF16)
            k.dma(w_out[:], P["a_w_out"][0].rearrange("(c p) n -> p c n", p=128), writes=[w_out], q="pool")
            convw = sb("convw", [128, 18, 4], F32)
            k.dma(convw[:], P["convw"], writes=[convw])
            sc6 = sb("sc6", [128, 32], F32)
            k.dma(sc6[:, 0:6], P["a_log"][0].partition_broadcast(128), writes=[sc6])
            k.dma(sc6[:, 6:12], P["a_dt_bias"][0].partition_broadcast(128), writes=[sc6])
            k.op("act", lambda e: e.activation(sc6[:, 12:18], sc6[:, 0:6], AF.Exp), reads=[sc6], writes=[sc6])
            k.op("dve", lambda e: e.tensor_single_scalar(sc6[:, 12:18], sc6[:, 12:18], -1.0, ALU.mult), reads=[sc6], writes=[sc6])
            ogain = sb("ogain", [128, 128], F32)
            k.dma(ogain[:], P["a_out_gain"][0].partition_broadcast(128), writes=[ogain])
            MW = self.mem_work(st)
            pc = sb("pc", [128, 18, 131], F32)
            k.op("dve", lambda e: e.memset(pc[:], 0.0), writes=[pc])
            Sf = [sb("Sf%d" % h, [128, 128], F32) for h in range(6)]
            Sb = [sb("Sb%d" % h, [128, 128], BF16) for h in range(6)]
            for h in range(6):
                k.op("dve", lambda e: e.memset(Sf[h][:], 0.0), writes=[Sf[h]])
                k.op("pool", lambda e: e.memset(Sb[h][:], 0.0), writes=[Sb[h]])
            hA = sb("htA", [128, D], F32)
            xT = sb("xnTA", [128, 8, 128], BF16)
            cv = sb("cv", [128, 12, 128], F32)
            cvv = sb("cvv", [128, 6, 128], F32)
            ctmp = sb("ctmp", [128, 128], F32)
            cvjunk = View(cv, cv[:, 0:8, :].rearrange("p c t -> p (c t)"))
            sq = sb("sq", [128, 12, 128], BF16)
            rs = sb("rs", [128, 12, 128], F32)
            kn32 = sb("kn32", [128, 6, 128], F32)
            sttA = sb("sttA", [128, 4], F32)
            qkT = [sb("qkT%d" % i, [128, 6, 2, 128], BF16) for i in range(3)]
            sc = [sb("scA%d" % i, [128, 96], F32) for i in range(3)]
            gg = [sb("gg%d" % i, [128, 768], F32) for i in range(3)]
            mqT = [sb("mqTA%d" % i, [128, 2, 128], BF16) for i in range(3)]
            SLg = [sb("SLg%d" % i, [128, 128], F32) for i in range(6)]
            dm = sb("dm", [128, 6, 128], F32)
            dmi = sb("dmi", [128, 6, 128], F32)
            Wq = [[sb("W%d_%d" % (h, i), [128, 3, 128], BF16) for i in range(2)] for h in range(6)]
            Q0f = [sb("Q0f%d" % i, [128, 128], F32) for i in range(6)]
            kt = [sb("kt%d" % i, [128, 6, 128], BF16) for i in range(2)]
            vtok = [sb("vtok%d" % i, [128, 6, 128], F32) for i in range(2)]
            attnT = [sb("attnT%d" % i, [128, 6, 128], BF16) for i in range(2)]
            TT = [sb("TT%d" % i, [128, 6, 128], BF16) for i in range(2)]
            Rall = sb("Rall", [128, 6, 128], BF16)
            vnew = sb("vnewA", [128, 6, 128], BF16)
            oall = sb("oall", [128, 6, 128], F32)
            ost = sb("ost", [128, 24], F32)
            ojunk = sb("ojunk", [128, 128], F32)
            mix = sb("mixA", [128, D], F32)
            mixT = sb("mixTA", [128, 8, 128], BF16)
            hC = sb("htC", [128, D], F32)
            ident, U, SL, ones = self.ident, self.U, self.SL, self.ones
            NEGs = self.cst[:, 7, :]
            identb = self.identb
            cst, cstb = self.cst, self.cstb
            QS = float(128 ** -0.5)
            ctm = View(rs, rs[:, 0:9, :])
            rotA = [0]

            def nbA():
                rotA[0] = (rotA[0] + 1) % 6
                return self.ps[rotA[0]]
            self.nextbank = nbA
            membanks = [self.ps[6], self.ps[7]]

            def frontA(t):
                i3 = t % 3
                h = hA
                k.dma(h[:], hin[0][t * 128:(t + 1) * 128, :], reads=[hin[1][t]], writes=[h])
                self.rmsnorm(h, gain, h, cvjunk, sttA)
                yield
                self.transpose8(h, [(xT, lambda half: xT[:, half * 4:(half + 1) * 4, :])], self.nextbank(), self.nextbank())
                yield
                for g4 in range(5):
                    nf = 4 if g4 < 4 else 2
                    pb = self.nextbank()
                    for j in range(nf):
                        fc = g4 * 4 + j
                        for dc in range(8):
                            k.op("pe", lambda e: e.matmul(pb[:, j * 128:(j + 1) * 128], w_in[:, dc, fc * 128:(fc + 1) * 128], xT[:, dc, :],
                                                          start=(dc == 0), stop=(dc == 7)),
                                 reads=[w_in, xT], writes=[pb], inc=(dc == 7 and j == nf - 1))
                    dstv = pc[:, g4 * 4:g4 * 4 + nf, 3:131]
                    srcv = pb[:, 0:nf * 128].rearrange("p (c t) -> p c t", c=nf)
                    k.op("act", lambda e: e.copy(dstv, srcv), reads=[pb], writes=[pc])
                    yield
                pb = self.nextbank()
                for j in range(2):
                    for dc in range(8):
                        k.op("pe", lambda e: e.matmul(pb[:, j * 128:(j + 1) * 128], w_in[:, dc, 3084 + j * 128:3084 + (j + 1) * 128], xT[:, dc, :],
                                                      start=(dc == 0), stop=(dc == 7)),
                             reads=[w_in, xT], writes=[pb], inc=(dc == 7 and j == 1))
                k.op("act", lambda e: e.copy(mqT[i3][:], pb[:, 0:256].rearrange("p (c t) -> p c t", c=2)), reads=[pb], writes=[mqT[i3]])
                yield
                pg1 = self.nextbank()
                for dc in range(8):
                    k.op("pe", lambda e: e.matmul(pg1[:], xT[:, dc, :], w_in[:, dc, 2304:2816], start=(dc == 0), stop=(dc == 7)),
                         reads=[w_in, xT], writes=[pg1], inc=(dc == 7))
                pg2 = self.nextbank()
                for dc in range(8):
                    k.op("pe", lambda e: e.matmul(pg2[:, 0:268], xT[:, dc, :], w_in[:, dc, 2816:3084], start=(dc == 0), stop=(dc == 7)),
                         reads=[w_in, xT], writes=[pg2], inc=(dc == 7))
                g_g = gg[i3]
                k.op("act", lambda e: e.activation(g_g[:, 0:512], pg1[:], AF.Silu), reads=[pg1], writes=[g_g])
                k.op("act", lambda e: e.activation(g_g[:, 512:768], pg2[:, 0:256], AF.Silu), reads=[pg2], writes=[g_g])
                s_ = sc[i3]
                C = lambda a_, n=6: s_[:, a_:a_ + n]
                beta, tt_, ex_, sp_, g_, gcl, egc, negc, etl, egl, dd, eb_ = (C(0), C(6), C(12), C(18), C(24), C(32, 16), C(48), C(54), C(60), C(66), C(72), C(78))
                k.op("act", lambda e: e.activation(eb_, pg2[:, 256:262], AF.Exp, scale=-1.0), reads=[pg2], writes=[s_])
                k.op("dve", lambda e: e.tensor_tensor(tt_, pg2[:, 262:268], sc6[:, 6:12], ALU.add), reads=[pg2, sc6], writes=[s_])
                k.op("dve", lambda e: e.tensor_single_scalar(eb_, eb_, 1.0, ALU.add), reads=[s_], writes=[s_])
                k.op("dve", lambda e: e.reciprocal(beta, eb_), reads=[s_], writes=[s_])
                k.op("pool", lambda e: e.tensor_tensor(g_g[:].rearrange("p (h d) -> p h d", h=6), g_g[:].rearrange("p (h d) -> p h d", h=6),
                                                       ogain[:].unsqueeze(1).to_broadcast([128, 6, 128]), ALU.mult),
                     reads=[g_g, ogain], writes=[g_g])
                yield
                k.op("act", lambda e: e.activation(ex_, tt_, AF.Exp), reads=[s_], writes=[s_])
                k.op("act", lambda e: e.activation(sp_, ex_, AF.Ln, bias=1.0), reads=[s_], writes=[s_])
                k.op("dve", lambda e: e.tensor_tensor(g_, sp_, sc6[:, 12:18], ALU.mult), reads=[s_, sc6], writes=[s_])
                yield
                pgc = self.nextbank()
                k.op("pe", lambda e: e.matmul(pgc[:, 0:6], U, g_, start=True, stop=True), reads=[cst, s_], writes=[pgc])
                k.op("pe", lambda e: e.matmul(pgc[:, 8:14], ones, g_, start=True, stop=True), reads=[cst, s_], writes=[pgc])
                k.op("dve", lambda e: e.tensor_copy(gcl, pgc[:, 0:16]), reads=[pgc], writes=[s_])
                k.op("dve", lambda e: e.tensor_tensor(dd, s_[:, 40:46], s_[:, 32:38], ALU.subtract), reads=[s_], writes=[s_])
                yield
                k.op("act", lambda e: e.activation(egc, s_[:, 32:38], AF.Exp), reads=[s_], writes=[s_])
                k.op("act", lambda e: e.activation(etl, dd, AF.Exp), reads=[s_], writes=[s_])
                k.op("act", lambda e: e.activation(egl, s_[:, 40:46], AF.Exp), reads=[s_], writes=[s_])
                k.op("dve", lambda e: e.tensor_single_scalar(negc, egc, -1.0, ALU.mult), reads=[s_], writes=[s_])
                yield
                for (c0, nchk, dstb, dst) in ((0, 9, cv, cv[:, 0:9, :]), (9, 3, cv, cv[:, 9:12, :]), (12, 6, cvv, cvv[:, 0:6, :])):
                    wv = lambda j: convw[:, c0:c0 + nchk, j:j + 1].to_broadcast([128, nchk, 128])
                    k.op("dve", lambda e: e.tensor_tensor(dst, pc[:, c0:c0 + nchk, 0:128], wv(0), ALU.mult), reads=[pc, convw], writes=[dstb])
                    for j in range(1, 4):
                        tv = ctm[:, 0:nchk, :]
                        k.op("dve", lambda e: e.tensor_tensor(tv, pc[:, c0:c0 + nchk, j:j + 128], wv(j), ALU.mult), reads=[pc, convw], writes=[ctm])
                        k.op("dve", lambda e: e.tensor_tensor(dst, dst, tv, ALU.add), reads=[ctm, dstb], writes=[dstb])
                        yield
                k.op("pool", lambda e: e.tensor_copy(pc[:, :, 0:3], pc[:, :, 128:131]), reads=[pc], writes=[pc])
                k.op("act", lambda e: e.activation(cv[:], cv[:], AF.Silu), reads=[cv], writes=[cv])
                k.op("act", lambda e: e.activation(cvv[:], cvv[:], AF.Silu), reads=[cvv], writes=[cvv])
                qkv = cv
                k.op("act", lambda e: e.activation(sq[:], qkv[:, 0:12, :], AF.Square), reads=[qkv], writes=[sq])
                yield
                for g3 in range(3):
                    pb = self.nextbank()
                    k.op("pe", lambda e: e.matmul(pb[:], self.onesb, sq[:, g3 * 4:(g3 + 1) * 4, :], start=True, stop=True),
                         reads=[cstb, sq], writes=[pb])
                    k.op("act", lambda e: e.activation(rs[:, g3 * 4:(g3 + 1) * 4, :], pb[:].rearrange("p (c t) -> p c t", c=4), AF.Ln,
                                                       bias=self.epsb[:, 0:1]), reads=[pb, self.epsbuf], writes=[rs])
                    k.op("act", lambda e: e.activation(rs[:, g3 * 4:(g3 + 1) * 4, :], rs[:, g3 * 4:(g3 + 1) * 4, :], AF.Exp, scale=-0.5),
                         reads=[rs], writes=[rs])
                yield
                qk = qkT[i3]
                k.op("dve", lambda e: e.scalar_tensor_tensor(qk[:, :, 1, :], qkv[:, 0:6, :], QS, rs[:, 0:6, :], ALU.mult, ALU.mult),
                     reads=[qkv, rs], writes=[qk])
                k.op("dve", lambda e: e.tensor_tensor(kn32[:], qkv[:, 6:12, :], rs[:, 6:12, :], ALU.mult), reads=[qkv, rs], writes=[kn32])
                k.op("pool", lambda e: e.tensor_copy(qk[:, :, 0, :], kn32[:]), reads=[kn32], writes=[qk])
                yield
                b = t % 2
                s_etl = etl
                for grp in range(3):
                    pb = self.nextbank()
                    for j in range(4):
                        idx = grp * 4 + j
                        src = cvv[:, idx, :] if idx < 6 else kn32[:, idx - 6, :]
                        srcb = cvv if idx < 6 else kn32
                        k.op("pe", lambda e: e.transpose(pb[:, j * 128:(j + 1) * 128], src, ident), reads=[srcb, cst], writes=[pb], inc=(j == 3))
                    for j in range(4):
                        idx = grp * 4 + j
                        if idx < 6:
                            k.op("act", lambda e: e.copy(vtok[b][:, idx, :], pb[:, j * 128:(j + 1) * 128]), reads=[pb], writes=[vtok[b]])
                        else:
                            hh = idx - 6
                            k.op("dve", lambda e: e.tensor_scalar(kt[b][:, hh, :], pb[:, j * 128:(j + 1) * 128], s_etl[:, hh:hh + 1], None, ALU.mult),
                                 reads=[pb, s_], writes=[kt[b]])
                    yield

            def frontB(t):
                i3 = t % 3
                b = t % 2
                s_ = sc[i3]
                C = lambda a_, n=6: s_[:, a_:a_ + n]
                beta, g_ = C(0), C(24)
                qk = qkT[i3]
                for hh in range(6):
                    sg = SLg[hh]
                    k.op("dve", lambda e: e.tensor_scalar(sg[:], SL, g_[:, hh:hh + 1], None, ALU.mult), reads=[cst, s_], writes=[sg])
                yield
                for hp in range(3):
                    pb = self.nextbank()
                    for j in range(2):
                        hh = hp * 2 + j
                        sg = SLg[hh]
                        k.op("pe", lambda e: e.matmul(pb[:, j * 128:(j + 1) * 128], sg[:], U, start=True, stop=False),
                             reads=[sg, cst], writes=[pb], inc=False)
                        k.op("pe", lambda e: e.matmul(pb[:, j * 128:(j + 1) * 128], ident, NEGs, start=False, stop=True),
                             reads=[cst], writes=[pb])
                    k.op("act", lambda e: e.activation(dm[:, hp * 2:hp * 2 + 2, :], pb[:, 0:256].rearrange("p (c t) -> p c t", c=2), AF.Exp),
                         reads=[pb], writes=[dm])
                    yield
                k.op("pool", lambda e: e.tensor_tensor(dmi[:], dm[:], ident.unsqueeze(1).to_broadcast([128, 6, 128]), ALU.add),
                     reads=[dm, cst], writes=[dmi])
                for hh in range(6):
                    pb = self.nextbank()
                    W0 = Wq[hh][0]
                    qf = Q0f[hh]
                    k.op("pe", lambda e: e.matmul(pb[:, 0:256], qk[:, hh, 0, :], qk[:, hh, :, :].rearrange("p a t -> p (a t)"),
                                                  start=True, stop=True), reads=[qk], writes=[pb])
                    k.op("dve", lambda e: e.scalar_tensor_tensor(qf[:], pb[:, 0:128], beta[:, hh:hh + 1], dm[:, hh, :], ALU.mult, ALU.mult),
                         reads=[pb, s_, dm], writes=[qf])
                    k.op("dve", lambda e: e.tensor_tensor(attnT[b][:, hh, :], pb[:, 128:256], dmi[:, hh, :], ALU.mult),
                         reads=[pb, dmi], writes=[attnT[b]])
                    if hh % 3 == 2:
                        yield
                for hh in range(6):
                    W0 = Wq[hh][0]
                    qf = Q0f[hh]
                    k.op("pool", lambda e: e.tensor_copy(W0[:, 0, :], qf[:]), reads=[qf], writes=[W0])
                    k.op("pool", lambda e: e.tensor_tensor(Wq[hh][1][:, 1, :], ident, qf[:], ALU.subtract), reads=[cst, qf], writes=[Wq[hh][1]])
                    pb2 = self.nextbank()
                    k.op("pe", lambda e: e.transpose(pb2[:, 0:128], qf[:], ident), reads=[qf, cst], writes=[pb2])
                    k.op("act", lambda e: e.copy(W0[:, 2, :], pb2[:, 0:128]), reads=[pb2], writes=[W0])
                    if hh % 3 == 2:
                        yield
                for lvl in range(7):
                    for hh in range(6):
                        Wc = Wq[hh][lvl % 2]
                        Wn = Wq[hh][(lvl + 1) % 2]
                        pb = self.nextbank()
                        Qk, Xk, Pk = Wc[:, 0, :], Wc[:, 1, :], Wc[:, 2, :]
                        mm = lambda out, l_, r_, st_, sp_2, inc_: k.op(
                            "pe", lambda e: e.matmul(out, l_, r_, start=st_, stop=sp_2), reads=[Wc, cstb], writes=[pb], inc=inc_)
                        if lvl == 0:
                            mm(pb[:, 0:128], Pk, Qk, True, True, False)
                            mm(pb[:, 256:384], Qk, Pk, True, True, True)
                            k.op("act", lambda e: e.copy(Wn[:, 0, :], pb[:, 0:128]), reads=[pb], writes=[Wn])
                            k.op("act", lambda e: e.copy(Wn[:, 2, :], pb[:, 256:384]), reads=[pb], writes=[Wn])
                        elif lvl < 6:
                            mm(pb[:, 0:128], Pk, Qk, True, True, False)
                            mm(pb[:, 128:256], Pk, Xk, True, False, False)
                            mm(pb[:, 128:256], identb, Xk, False, True, False)
                            mm(pb[:, 256:384], Qk, Pk, True, True, True)
                            k.op("act", lambda e: e.copy(Wn[:], pb[:, 0:384].rearrange("p (c t) -> p c t", c=3)), reads=[pb], writes=[Wn])
                        else:
                            mm(pb[:, 128:256], Pk, Xk, True, False, False)
                            mm(pb[:, 128:256], identb, Xk, False, True, True)
                            k.op("act", lambda e: e.copy(TT[b][:, hh, :], pb[:, 128:256]), reads=[pb], writes=[TT[b]])
                        if hh % 3 == 2:
                            yield

            def back(t):
                i3 = t % 3
                b = t % 2
                s_ = sc[i3]
                C = lambda a_, n=6: s_[:, a_:a_ + n]
                beta, egc, negc, egl = C(0), C(48), C(54), C(66)
                qk, g_g = qkT[i3], gg[i3]
                k.dma(hC[:], hin[0][t * 128:(t + 1) * 128, :], reads=[hin[1][t]], writes=[hC])
                for hp in range(3):
                    pb = self.nextbank()
                    for j in range(2):
                        hh = hp * 2 + j
                        o = j * 256
                        k.op("pe", lambda e: e.matmul(pb[:, o:o + 128], qk[:, hh, 0, :], Sb[hh][:], start=True, stop=True),
                             reads=[qk, Sb[hh]], writes=[pb], inc=False)
                        k.op("pe", lambda e: e.matmul(pb[:, o + 128:o + 256], qk[:, hh, 1, :], Sb[hh][:], start=True, stop=True),
                             reads=[qk, Sb[hh]], writes=[pb], inc=(j == 1))
                    for j in range(2):
                        hh = hp * 2 + j
                        o = j * 256
                        k.op("dve", lambda e: e.scalar_tensor_tensor(Rall[:, hh, :], pb[:, o:o + 128], negc[:, hh:hh + 1], vtok[b][:, hh, :], ALU.mult, ALU.add),
                             reads=[pb, s_, vtok[b]], writes=[Rall])
                        k.op("dve", lambda e: e.tensor_scalar(oall[:, hh, :], pb[:, o + 128:o + 256], egc[:, hh:hh + 1], None, ALU.mult),
                             reads=[pb, s_], writes=[oall])
                yield
                for (h0, nh) in ((0, 4), (4, 2)):
                    pb = self.nextbank()
                    for j in range(nh):
                        hh = h0 + j
                        k.op("pe", lambda e: e.matmul(pb[:, j * 128:(j + 1) * 128], TT[b][:, hh, :], Rall[:, hh, :], start=True, stop=True),
                             reads=[TT[b], Rall], writes=[pb], inc=(j == nh - 1))
                    k.op("dve", lambda e: e.tensor_tensor(vnew[:, h0:h0 + nh, :], pb[:, 0:nh * 128].rearrange("p (c t) -> p c t", c=nh),
                                                          beta[:, h0:h0 + nh].unsqueeze(2).to_broadcast([128, nh, 128]), ALU.mult),
                         reads=[pb, s_], writes=[vnew])
                yield
                for hp in range(3):
                    pb = self.nextbank()
                    for j in range(2):
                        hh = hp * 2 + j
                        o = j * 256
                        k.op("pe", lambda e: e.matmul(pb[:, o:o + 128], attnT[b][:, hh, :], vnew[:, hh, :], start=True, stop=True),
                             reads=[attnT[b], vnew], writes=[pb], inc=False)
                        k.op("pe", lambda e: e.matmul(pb[:, o + 128:o + 256], kt[b][:, hh, :], vnew[:, hh, :], start=True, stop=True),
                             reads=[kt[b], vnew], writes=[pb], inc=(j == 1))
                    for j in range(2):
                        hh = hp * 2 + j
                        o = j * 256
                        k.op("dve", lambda e: e.scalar_tensor_tensor(Sb[hh][:], Sf[hh][:], egl[:, hh:hh + 1], pb[:, o + 128:o + 256], ALU.mult, ALU.add),
                             reads=[Sf[hh], s_, pb], writes=[Sb[hh]])
                        k.op("dve", lambda e: e.scalar_tensor_tensor(Sf[hh][:], Sf[hh][:], egl[:, hh:hh + 1], pb[:, o + 128:o + 256], ALU.mult, ALU.add),
                             reads=[Sf[hh], s_, pb], writes=[Sf[hh]])
                        k.op("dve", lambda e: e.tensor_tensor(oall[:, hh, :], oall[:, hh, :], pb[:, o:o + 128], ALU.add),
                             reads=[oall, pb], writes=[oall])
                yield
                for hh in range(6):
                    k.op("act", lambda e: e.activation(ojunk[:], oall[:, hh, :], AF.Square, accum_out=ost[:, hh:hh + 1]),
                         reads=[oall], writes=[ojunk, ost])
                k.op("act", lambda e: e.activation(ost[:, 8:14], ost[:, 0:6], AF.Ln, bias=self.epsb[:, 0:1], scale=1.0 / 128),
                     reads=[ost, self.epsbuf], writes=[ost])
                k.op("act", lambda e: e.activation(ost[:, 16:22], ost[:, 8:14], AF.Exp, scale=-0.5), reads=[ost], writes=[ost])
                yield
                for hh in range(6):
                    k.op("dve", lambda e: e.scalar_tensor_tensor(mix[:, hh * 128:(hh + 1) * 128], oall[:, hh, :], ost[:, 16 + hh:17 + hh],
                                                                 g_g[:, hh * 128:(hh + 1) * 128], ALU.mult, ALU.mult),
                         reads=[oall, ost, g_g], writes=[mix])
                yield
                yield from self.mem_attend_g(MW, mqT[i3], 0, memkT, memv, mix, banks=membanks)
                yield
                self.transpose8(mix, [(mixT, lambda half: mixT[:, half * 4:(half + 1) * 4, :])], self.nextbank(), self.nextbank())
                yield
                for half in range(2):
                    pb = self.nextbank()
                    for fc in range(8):
                        k.op("pe", lambda e: e.matmul(pb[:], mixT[:, fc, :], w_out[:, fc, half * 512:(half + 1) * 512],
                                                      start=(fc == 0), stop=(fc == 7)),
                             reads=[mixT, w_out], writes=[pb], inc=(fc == 7))
                    k.op("dve", lambda e: e.tensor_tensor(hC[:, half * 512:(half + 1) * 512], hC[:, half * 512:(half + 1) * 512], pb[:], ALU.add),
                         reads=[hC, pb], writes=[hC])
                k.dma(hout[0][t * 128:(t + 1) * 128, :], hC[:], reads=[hC], writes=[hout[1][t]])

            def run(*gens):
                gens = [g for g in gens if g is not None]
                while gens:
                    for g_ in list(gens):
                        try:
                            next(g_)
                        except StopIteration:
                            gens.remove(g_)

            mk = lambda fn, t: fn(t) if 0 <= t < NTA else None
            for step in range(-2, NTA):
                run(mk(back, step), mk(frontB, step + 1), mk(frontA, step + 2))
            k.barrier()
            del self.nextbank

    def mixer_b_phase(self, hin, hout):
        nc, k, P = self.nc, self.k, self.P
        NGB = int(os.environ.get("MK_NGB", "8"))
        NHB = int(os.environ.get("MK_NHB", "12"))
        NDUM = int(os.environ.get("MK_NDUM", "1"))
        with ExitStack() as st:
            sb = lambda n, s, d: self.sbuf(st, n, s, d)
            memkT, memv = self.mem_kv(st, 1)
            win_d = self.dscratch("b_w_in_bf", [D, D], BF16)
            wout_d = self.dscratch("b_w_out_bf", [D, D], BF16)
            wdb = [Buf(None, "win_d"), Buf(None, "wout_d")]
            k.dma(win_d, P["b_w_in"][0], writes=[wdb[0]], q="pool")
            k.dma(wout_d, P["b_w_out"][0], writes=[wdb[1]], q="pool")
            KT = sb("KT", [128, 6, S], BF16)
            Vt = sb("Vt", [128, NT, 768], BF16)
            ht = [sb("htB%d" % i, [128, D], F32) for i in range(2)]
            xn = sb("xnB", [128, D], F32)
            stt = [sb("sttB%d" % i, [128, 4], F32) for i in range(2)]
            with ExitStack() as st1:
                sb1 = lambda n, s, d: self.sbuf(st1, n, s, d)
                kvg = sb1("kvg", [128, D], F32)
                k.dma(kvg[:], P["kv_norm"].partition_broadcast(128), writes=[kvg])
                w_kv = sb1("w_kv", [128, 8, 1536], BF16)
                for c in range(8):
                    k.dma(w_kv[:, c, :], P["w_kv"][c * 128:(c + 1) * 128, :], writes=[w_kv], q="pool")
                xT1 = [sb1("xT1_%d" % i, [128, 8, 128], BF16) for i in range(2)]
                for t in range(NT):
                    b = t % 2
                    h = ht[b]
                    k.dma(h[:], hin[0][t * 128:(t + 1) * 128, :], reads=[hin[1][t]], writes=[h])
                    self.rmsnorm(h, kvg, xn, xn, stt[b])
                    xT = xT1[b]
                    self.transpose8(xn, [(xT, lambda half: xT[:, half * 4:(half + 1) * 4, :])], self.nextbank(), self.nextbank())
                    for g4 in range(2):
                        nf = 4 if g4 == 0 else 2
                        pb = self.nextbank()
                        for j in range(nf):
                            fc = g4 * 4 + j
                            for dc in range(8):
                                k.op("pe", lambda e: e.matmul(pb[:, j * 128:(j + 1) * 128], w_kv[:, dc, fc * 128:(fc + 1) * 128], xT[:, dc, :],
                                                              start=(dc == 0), stop=(dc == 7)),
                                     reads=[w_kv, xT], writes=[pb], inc=(dc == 7 and j == nf - 1))
                        dstv = KT[:, g4 * 4:g4 * 4 + nf, t * 128:(t + 1) * 128]
                        srcv = pb[:, 0:nf * 128].rearrange("p (c t) -> p c t", c=nf)
                        k.op("act", lambda e: e.copy(dstv, srcv), reads=[pb], writes=[KT])
                    for half, (c0, c1) in enumerate(((0, 512), (512, 768))):
                        pb = self.nextbank()
                        for dc in range(8):
                            k.op("pe", lambda e: e.matmul(pb[:, 0:c1 - c0], xT[:, dc, :], w_kv[:, dc, 768 + c0:768 + c1],
                                                          start=(dc == 0), stop=(dc == 7)),
                                 reads=[w_kv, xT], writes=[pb], inc=(dc == 7))
                        k.op("dve", lambda e: e.tensor_copy(Vt[:, t, c0:c1], pb[:, 0:c1 - c0]), reads=[pb], writes=[Vt])
                k.barrier()
            bg = sb("bgain", [128, D], F32)
            k.dma(bg[:], P["b_norm"][0].partition_broadcast(128), writes=[bg])
            wB = sb("wB", [128, 8, D], BF16)
            xTg = sb("xTg", [128, 8, 512], BF16)
            qT = sb("qT", [128, 6, 512], BF16)
            mqT = sb("mqTB", [128, 2, 512], BF16)
            mixTg = sb("mixTg", [128, 8, 512], BF16)
            Eb = [sb("Eb%d" % i, [128, 512], F32) for i in range(3)]
            spb = [sb("spb%d" % i, [128, 512], BF16) for i in range(3)]
            eab = [sb("eab%d" % i, [128, 512], F32) for i in range(2)]
            ab = [sb("ab%d" % i, [128, 512], BF16) for i in range(2)]
            MW = self.mem_work(st)
            mmB = sb("mmB", [128, 256], F32)
            hn = sb("hnB", [128, D], F32)
            NGEb = self.cstb[:, 5, :]
            NLTb = self.cstb[:, 6, :]
            strictTb = self.cstb[:, 3, :]
            cstb = self.cstb
            PZ = [self.ps[0], self.ps[1]]
            PC = [self.ps[2], self.ps[3]]
            PO = [self.ps[4], self.ps[5]]
            rot = [0]

            def nb():
                rot[0] = (rot[0] + 1) % 8
                return self.ps[rot[0]]
            self.nextbank = nb
            for g in range(NGB):
                k.dma(wB[:], win_d.rearrange("(c p) n -> p c n", p=128), reads=[wdb[0]], writes=[wB])
                for tt in range(4):
                    t = g * 4 + tt
                    b = t % 2
                    h = ht[b]
                    k.dma(h[:], hin[0][t * 128:(t + 1) * 128, :], reads=[hin[1][t]], writes=[h])
                    self.rmsnorm(h, bg, xn, xn, stt[b])
                    self.transpose8(xn, [(xTg, lambda half: xTg[:, half * 4:(half + 1) * 4, tt * 128:(tt + 1) * 128])], nb(), nb())
                for fc in range(8):
                    pb = nb()
                    for dc in range(8):
                        k.op("pe", lambda e: e.matmul(pb[:], wB[:, dc, fc * 128:(fc + 1) * 128], xTg[:, dc, :], start=(dc == 0), stop=(dc == 7)),
                             reads=[wB, xTg], writes=[pb], inc=(dc == 7))
                    if fc < 6:
                        k.op("act", lambda e: e.mul(qT[:, fc, :], pb[:], 0.125), reads=[pb], writes=[qT])
                    else:
                        k.op("dve", lambda e: e.tensor_copy(mqT[:, fc - 6, :], pb[:]), reads=[pb], writes=[mqT])
                items = [(2 * p + s, kb) for p in range(NHB // 2) for kb in range(4 * g + 3, -1, -1) for s in range(2)]

                def geom(i):
                    hh, kb = items[i]
                    r = max(kb - 4 * g, 0)
                    return hh, kb, hh // 2, hh % 2, r * 128, kb >= 4 * g

                def s1_pe(i):
                    hh, kb, fc, s, c0, diag = geom(i)
                    ps_ = slice(s * 64, (s + 1) * 64)
                    cs = slice(c0, 512)
                    pz = PZ[i % 2]
                    k.op("pe", lambda e: e.matmul(pz[:, cs], KT[ps_, fc, kb * 128:(kb + 1) * 128], qT[ps_, fc, cs], start=True, stop=True),
                         reads=[KT, qT], writes=[pz])

                def s1_act(i):
                    hh, kb, fc, s, c0, diag = geom(i)
                    cs = slice(c0, 512)
                    pz, E, sp = PZ[i % 2], Eb[i % 3], spb[i % 3]
                    k.op("act", lambda e: e.activation(E[:, cs], pz[:, cs], AF.Exp), reads=[pz], writes=[E])
                    k.op("act", lambda e: e.activation(sp[:, cs], E[:, cs], AF.Ln, bias=1.0), reads=[E], writes=[sp])
                    if diag:
                        k.op("dve", lambda e: e.tensor_tensor(sp[:, c0:c0 + 128], sp[:, c0:c0 + 128], strictTb, ALU.mult),
                             reads=[sp, cstb], writes=[sp])

                def s2_peA(i):
                    hh, kb, fc, s, c0, diag = geom(i)
                    cs = slice(c0, 512)
                    C, sp = PC[s], spb[i % 3]
                    if kb == 4 * g + 3:
                        k.op("dve", lambda e: e.memset(C[:], 0.0), writes=[C])
                    k.op("pe", lambda e: e.matmul(C[:, cs], NGEb, sp[:, cs], start=False, stop=False, skip_group_check=True),
                         reads=[cstb, sp], writes=[C])

                def s2_act(i):
                    hh, kb, fc, s, c0, diag = geom(i)
                    cs = slice(c0, 512)
                    C, ea_ = PC[s], eab[i % 2]
                    k.op("act", lambda e: e.activation(ea_[:, cs], C[:, cs], AF.Exp), reads=[C], writes=[ea_])

                def s2_peB(i):
                    hh, kb, fc, s, c0, diag = geom(i)
                    cs = slice(c0, 512)
                    C, sp = PC[s], spb[i % 3]
                    if kb > 0:
                        k.op("pe", lambda e: e.matmul(C[:, cs], NLTb, sp[:, cs], start=False, stop=False, skip_group_check=True),
                             reads=[cstb, sp], writes=[C])

                def s3_pool(i):
                    hh, kb, fc, s, c0, diag = geom(i)
                    cs = slice(c0, 512)
                    E, ea_, a_ = Eb[i % 3], eab[i % 2], ab[i % 2]
                    k.op("dve", lambda e: e.tensor_tensor(a_[:, cs], E[:, cs], ea_[:, cs], ALU.mult), reads=[E, ea_], writes=[a_])
                    if diag:
                        k.op("dve", lambda e: e.tensor_tensor(a_[:, c0:c0 + 128], a_[:, c0:c0 + 128], strictTb, ALU.mult),
                             reads=[a_, cstb], writes=[a_])

                def s3_pe(i):
                    hh, kb, fc, s, c0, diag = geom(i)
                    cs = slice(c0, 512)
                    a_ = ab[i % 2]
                    po = PO[fc % 2]
                    if kb == 4 * g + 3 and s == 0:
                        k.op("dve", lambda e: e.memset(po[:], 0.0), writes=[po])
                    vblk = Vt[:, kb, hh * 64:(hh + 1) * 64]
                    if s == 0:
                        k.op("pe", lambda e: e.matmul(po[0:64, cs], vblk, a_[:, cs], start=False, stop=False, skip_group_check=True),
                             reads=[Vt, a_], writes=[po])
                    else:
                        k.op("pe", lambda e: e.matmul(po[64:128, cs], vblk, a_[:, cs], start=False, stop=False, skip_group_check=True,
                                                      tile_position=(0, 64)), reads=[Vt, a_], writes=[po])
                    if kb == 0 and s == 1:
                        k.op("act", lambda e: e.copy(mixTg[:, fc, :], po[:]), reads=[po], writes=[mixTg])

                n_it = len(items)
                ok = lambda i: 0 <= i < n_it
                dumrhs = cstb[:, 0:4, :].rearrange("p c t -> p (c t)")
                for step in range(-3, n_it + 1):
                    i0, i1, i2_, i3, i4 = step + 3, step + 2, step + 1, step, step - 1
                    if ok(i3):
                        s3_pool(i3)
                    if ok(i0):
                        s1_pe(i0)
                    if ok(i2_):
                        s2_peA(i2_)
                    if ok(i4):
                        s3_pe(i4)
                    if ok(i1):
                        s1_act(i1)
                    if ok(i2_):
                        s2_act(i2_)
                    if ok(i3):
                        s2_peB(i3)
                    for _d in range(NDUM):
                        k.op("pe", lambda e: e.matmul(self.ps[6][:], NGEb, dumrhs, start=True, stop=True),
                             reads=[cstb], writes=[self.ps[6]], inc=False)
                k.dma(wB[:], wout_d.rearrange("(c p) n -> p c n", p=128), reads=[wdb[1]], writes=[wB])
                for tt in range(4):
                    t = g * 4 + tt
                    b = t % 2
                    h = ht[b]
                    k.dma(h[:], hin[0][t * 128:(t + 1) * 128, :], reads=[hin[1][t]], writes=[h])
                    self.mem_attend(MW, mqT, tt * 128, memkT, memv, mmB, col0=0)
                    pb = nb()
                    for j in range(2):
                        k.op("pe", lambda e: e.transpose(pb[:, j * 128:(j + 1) * 128], mmB[:, j * 128:(j + 1) * 128], self.ident),
                             reads=[mmB, self.cst], writes=[pb], inc=(j == 1))
                    k.op("act", lambda e: e.copy(mixTg[:, 6:8, tt * 128:(tt + 1) * 128], pb[:, 0:256].rearrange("p (c t) -> p c t", c=2)),
                         reads=[pb], writes=[mixTg])
                    for half in range(2):
                        pb = nb()
                        for fc in range(8):
                            k.op("pe", lambda e: e.matmul(pb[:], mixTg[:, fc, tt * 128:(tt + 1) * 128], wB[:, fc, half * 512:(half + 1) * 512],
                                                          start=(fc == 0), stop=(fc == 7)),
                                 reads=[mixTg, wB], writes=[pb], inc=(fc == 7))
                        k.op("dve", lambda e: e.tensor_tensor(hn[:, half * 512:(half + 1) * 512], h[:, half * 512:(half + 1) * 512], pb[:], ALU.add),
                             reads=[h, pb], writes=[hn])
                    k.dma(hout[0][t * 128:(t + 1) * 128, :], hn[:], reads=[hn], writes=[hout[1][t]])
            k.barrier()
            del self.nextbank

    def build(self, phases):
        nc, k = self.nc, self.k
        P = self.P = {}
        shapes = dict(
            x=[S, D], mem=[256, D], a_norm=[1, D], a_w_in=[1, D, 3340], a_conv=[1, 4, 2304],
            a_log=[1, 6], a_dt_bias=[1, 6], a_out_gain=[1, 128], a_w_out=[1, D, D],
            kv_norm=[D], w_kv=[D, 1536], b_norm=[1, D], b_w_in=[1, D, D], b_w_out=[1, D, D],
            mem_norm=[2, D], w_mem_kv=[2, D, 512], ffn_norm=[2, D], w_group=[2, D, 4], b_group=[2, 4],
            w_router=[2, D, 16], b_router=[2, 16], w1=[2, 16, D, 256], w3=[2, 16, D, 256],
            w2=[2, 16, 256, D], final_norm=[D], wgr=[2, 128, 8, 20], rbias=[2, 20], convw=[128, 18, 4])
        for n, s in shapes.items():
            P[n] = self.din(n, s)
        out = nc.dram_tensor("out", [S, D], F32, kind="ExternalOutput").ap()
        mkbufs = lambda nm: [Buf(None, "%s%d" % (nm, i)) for i in range(NT)]
        hx = (P["x"], mkbufs("x"))
        hA = (self.dscratch("hA", [S, D]), mkbufs("hA"))
        hB = (self.dscratch("hB", [S, D]), mkbufs("hB"))
        ho = (out, mkbufs("out"))
        with ExitStack() as gst:
            self.load_consts(gst)
            self.epsbuf = self.sbuf(gst, "epsb", [128, 1], F32)
            self.epsb = self.epsbuf
            k.op("dve", lambda e: e.memset(self.epsbuf[:], EPS), writes=[self.epsbuf])
            cur = hx
            seq = {"A": hA, "M0": hB, "B": hA, "M1": ho}
            for ph in phases:
                dst = seq[ph] if ph != phases[-1] else ho
                if ph == "M0":
                    self.moe_phase(0, cur, dst, final=False)
                elif ph == "M1":
                    self.moe_phase(1, cur, dst, final=True)
                elif ph == "A":
                    self.mixer_a_phase(cur, dst)
                elif ph == "B":
                    self.mixer_b_phase(cur, dst)
                cur = dst
            for b in ho[1]:
                if b.lw is not None:
                    k._wait("sp", b.lw)
        return nc


def make_consts():
    c = np.zeros((128, 8, 128), np.float32)
    i = np.arange(128)
    c[:, 0, :] = np.eye(128)
    c[:, 1, :] = (i[:, None] <= i[None, :])
    c[:, 2, :] = (i[:, None] > i[None, :])
    c[:, 3, :] = (i[:, None] < i[None, :])
    c[:, 4, :] = 1.0
    c[:, 5, :] = -(i[:, None] >= i[None, :]).astype(np.float32)
    c[:, 6, :] = -(i[:, None] < i[None, :]).astype(np.float32)
    c[:, 7, :] = -30000.0 * (i[:, None] >= i[None, :])
    return c


_CACHE = {}


def run(inputs, phases=("A", "M0", "B", "M1"), ncores=NCORES, trace=False):
    key = tuple(phases)
    if key not in _CACHE:
        mk = MK(phases)
        _CACHE[key] = mk.build(list(phases))
    nc = _CACHE[key]
    consts = make_consts()
    inputs = dict(inputs)
    wg = np.concatenate([np.asarray(inputs["w_group"]), np.asarray(inputs["w_router"])], axis=2)
    inputs["wgr"] = np.ascontiguousarray(wg.reshape(2, 8, 128, 20).transpose(0, 2, 1, 3))
    inputs["rbias"] = np.concatenate([np.asarray(inputs["b_group"]), np.asarray(inputs["b_router"])], axis=1)
    cw = np.asarray(inputs["a_conv"])[0]
    inputs["convw"] = np.ascontiguousarray(cw.reshape(4, 18, 128).transpose(2, 1, 0))
    in_maps = []
    for c in range(ncores):
        m = {"consts": consts}
        for n, v in inputs.items():
            v = np.asarray(v)
            if n in ("x", "mem"):
                m[n] = np.ascontiguousarray(v[c])
            else:
                m[n] = np.ascontiguousarray(v, dtype=np.float32)
        in_maps.append(m)
    res = run_bass_kernel_spmd(nc, in_maps, core_ids=list(range(ncores)), trace=trace)
    outs = np.stack([r["out"] for r in res.results], axis=0)
    return outs, res


def kernel(**inputs):
    outs, _ = run(inputs)
    return outs.astype(np.float32)
```

```python
from contextlib import ExitStack
import os
import numpy as np
import concourse.bass as bass
import concourse.mybir as mybir
from concourse.bass_utils import run_bass_kernel_spmd

F32 = mybir.dt.float32
BF16 = mybir.dt.bfloat16
AF = mybir.ActivationFunctionType
ALU = mybir.AluOpType
AX = mybir.AxisListType

S = 4096
D = 1024
NT = S // 128
EPS = 1e-6
NCORES = 8


class Buf:
    __slots__ = ("ap", "name", "_lw", "_rd", "_excl")
    lw = property(lambda self: self._lw, lambda self, v: setattr(self, "_lw", v))
    rd = property(lambda self: self._rd, lambda self, v: setattr(self, "_rd", v))
    excl = property(lambda self: self._excl, lambda self, v: setattr(self, "_excl", v))

    def __init__(self, ap, name="", excl=False):
        self.ap = ap
        self.name = name
        self.excl = excl
        self.lw = None
        self.rd = []

    def __getitem__(self, idx):
        return self.ap[idx]


class View(Buf):
    __slots__ = ("parent",)

    def __init__(self, parent, ap):
        self.parent = parent
        self.ap = ap
        self.name = parent.name

    lw = property(lambda self: self.parent.lw, lambda self, v: setattr(self.parent, "lw", v))
    rd = property(lambda self: self.parent.rd, lambda self, v: setattr(self.parent, "rd", v))
    excl = property(lambda self: self.parent.excl, lambda self, v: None)


class K:
    NDMA = 48

    def __init__(self, nc):
        self.nc = nc
        self.eng = {"pe": nc.tensor, "act": nc.scalar, "dve": nc.vector,
                    "pool": nc.gpsimd, "sp": nc.sync}
        self.sem = {e: nc.alloc_semaphore("s_" + e) for e in ("pe", "act", "dve", "pool")}
        self.cnt = {e: 0 for e in self.sem}
        self.waited = {}
        self.dsem = [nc.alloc_semaphore("d%d" % i) for i in range(self.NDMA)]
        self.dcnt = [0] * self.NDMA
        self.dnext = 0
        self.nins = 0

    def _semh(self, key):
        return self.sem[key] if isinstance(key, str) else self.dsem[key]

    def _wait(self, e, dep):
        key, val = dep
        if key == e and e == "pe":
            return
        w = self.waited.get((e, key), 0)
        if w >= val:
            return
        self.eng[e].wait_ge(self._semh(key), val)
        self.nins += 1
        self.waited[(e, key)] = val

    def _deps(self, e, reads, writes):
        best = {}
        for r in reads:
            if r.lw is not None:
                if best.get(r.lw[0], 0) < r.lw[1]:
                    best[r.lw[0]] = r.lw[1]
            if r.excl:
                for key, val in r.rd:
                    if key != e and best.get(key, 0) < val:
                        best[key] = val
        for w in writes:
            if w.lw is not None:
                if best.get(w.lw[0], 0) < w.lw[1]:
                    best[w.lw[0]] = w.lw[1]
            for key, val in w.rd:
                if best.get(key, 0) < val:
                    best[key] = val
        for key, val in best.items():
            self._wait(e, (key, val))

    def _mark(self, tag, reads, writes):
        for r in reads:
            r.rd.append(tag)
            if len(r.rd) > 64:
                best = {}
                for key, val in r.rd:
                    if best.get(key, 0) < val:
                        best[key] = val
                r.rd = list(best.items())
        for w in writes:
            w.lw = tag
            w.rd = []

    def op(self, e, fn, reads=(), writes=(), inc=True):
        self._deps(e, reads, writes)
        ins = fn(self.eng[e])
        self.nins += 1
        if inc:
            ins.then_inc(self.sem[e], 1)
            self.cnt[e] += 1
            tag = (e, self.cnt[e])
        else:
            tag = (e, self.cnt[e] + 1)
        self._mark(tag, reads, writes)
        return ins

    def dma(self, out, in_, reads=(), writes=(), q="sp", **kw):
        slot = self.dnext
        self.dnext = (self.dnext + 1) % self.NDMA
        if self.dcnt[slot] > 0:
            self._wait(q, (slot, 16 * self.dcnt[slot]))
        self._deps(q, reads, writes)
        ins = self.eng[q].dma_start(out=out, in_=in_, **kw)
        self.nins += 1
        ins.then_inc(self.dsem[slot], 16)
        self.dcnt[slot] += 1
        tag = (slot, 16 * self.dcnt[slot])
        self._mark(tag, reads, writes)
        return tag

    def barrier(self):
        for e in ("pe", "act", "dve", "pool", "sp"):
            for e2 in ("pe", "act", "dve", "pool"):
                if e2 != e and self.cnt[e2] > 0:
                    self._wait(e, (e2, self.cnt[e2]))
            for slot in range(self.NDMA):
                if self.dcnt[slot] > 0:
                    self._wait(e, (slot, 16 * self.dcnt[slot]))


class MK:
    def __init__(self, phases, h0_from_input=True):
        self.nc = nc = bass.Bass("TRN2", target_bir_lowering=False)
        self.k = K(nc)
        self.uid = 0
        self.ins = {}
        self.ps = [Buf(nc.alloc_psum_tensor("psb%d" % i, [128, 512], F32).ap(), "ps%d" % i, excl=True)
                   for i in range(8)]

    def din(self, name, shape):
        ap = self.nc.dram_tensor(name, list(shape), F32, kind="ExternalInput").ap()
        self.ins[name] = ap
        return ap

    def dscratch(self, name, shape, dt=F32):
        return self.nc.dram_tensor(name, list(shape), dt, kind="Internal").ap()

    def sbuf(self, st, name, shape, dt):
        self.uid += 1
        h = st.enter_context(self.nc.sbuf_tensor("%s_%d" % (name, self.uid), list(shape), dt))
        return Buf(h.ap(), name)

    def load_consts(self, st):
        k = self.k
        c = self.din("consts", [128, 8, 128])
        self.cst = self.sbuf(st, "cst", [128, 8, 128], F32)
        k.dma(self.cst[:], c, writes=[self.cst])
        self.ident = self.cst[:, 0, :]
        self.U = self.cst[:, 1, :]
        self.SL = self.cst[:, 2, :]
        self.strictT = self.cst[:, 3, :]
        self.ones = self.cst[:, 4, :]
        self.cstb = self.sbuf(st, "cstb", [128, 8, 128], BF16)
        k.dma(self.cstb[:], c, writes=[self.cstb], q="pool")
        self.identb = self.cstb[:, 0, :]
        self.onesb = self.cstb[:, 4, :]

    def rmsnorm(self, h, gainb, xn, junk, st2):
        k = self.k
        k.op("act", lambda e: e.activation(junk[:], h[:], AF.Square, accum_out=st2[:, 0:1]),
             reads=[h], writes=[junk, st2])
        k.op("act", lambda e: e.activation(st2[:, 1:2], st2[:, 0:1], AF.Ln, bias=self.epsb[:, 0:1], scale=1.0 / D),
             reads=[st2, self.epsbuf], writes=[st2])
        k.op("act", lambda e: e.activation(st2[:, 2:3], st2[:, 1:2], AF.Exp, scale=-0.5), reads=[st2], writes=[st2])
        k.op("dve", lambda e: e.scalar_tensor_tensor(xn[:], h[:], st2[:, 2:3], gainb[:], ALU.mult, ALU.mult),
             reads=[h, st2, gainb], writes=[xn])

    def transpose8(self, src, dsts, psa, psb, evac=("act", "dve"), second="pool"):
        k = self.k
        for half, ps in enumerate((psa, psb)):
            for j in range(4):
                c = half * 4 + j
                k.op("pe", lambda e: e.transpose(ps[:, j * 128:(j + 1) * 128], src[:, c * 128:(c + 1) * 128], self.ident),
                     reads=[src, self.cst], writes=[ps], inc=(j == 3))
            dbuf, fn = dsts[0]
            eng = evac[half % len(evac)]
            pv = ps[:].rearrange("p (c t) -> p c t", c=4)
            if eng == "act":
                k.op("act", lambda e: e.copy(fn(half), pv), reads=[ps], writes=[dbuf])
            else:
                k.op(eng, lambda e: e.tensor_copy(fn(half), pv), reads=[ps], writes=[dbuf])
            for dbuf2, fn2 in dsts[1:]:
                k.op(second, lambda e: e.tensor_copy(fn2(half), fn(half)), reads=[dbuf], writes=[dbuf2])

    def moe_phase(self, l, hin, hout, final=False, out_ap=None):
        nc, k = self.nc, self.k
        G = 1024
        NTG = G // 128
        NG = S // G
        NTB = G // 512
        NEX = 16
        P = self.P
        with ExitStack() as st:
            sb = lambda n, s, d: self.sbuf(st, n, s, d)
            gain = sb("gain", [128, D], F32)
            k.dma(gain[:], P["ffn_norm"][l].partition_broadcast(128), writes=[gain])
            if final:
                fgain = sb("fgain", [128, D], F32)
                k.dma(fgain[:], P["final_norm"].partition_broadcast(128), writes=[fgain])
            wgr = sb("wgr", [128, 8, 20], F32)
            k.dma(wgr[:], P["wgr"][l], writes=[wgr])
            rb = sb("rbias", [128, 20], F32)
            k.dma(rb[:], P["rbias"][l].partition_broadcast(128), writes=[rb])
            xnT = [sb("xnT%d" % i, [128, 8, G], BF16) for i in range(2)]
            yacc = [[sb("yacc%d_%d" % (j, i), [128, D], F32) for i in range(NTG)] for j in range(2)]
            comb = [[sb("comb%d_%d" % (j, i), [128, 16], F32) for i in range(NTG)] for j in range(2)]
            w1b = [sb("w1b%d" % i, [128, 8, 256], BF16) for i in range(2)]
            w3b = [sb("w3b%d" % i, [128, 8, 256], BF16) for i in range(2)]
            w2b = [sb("w2b%d" % i, [128, 2, D], BF16) for i in range(2)]
            ht = [sb("ht%d" % i, [128, D], F32) for i in range(2)]
            htc = [sb("htc%d" % i, [128, D], F32) for i in range(2)]
            xn = [sb("xn%d" % i, [128, D], F32) for i in range(2)]
            xnT32 = [sb("xnT32_%d" % i, [128, 8, 128], F32) for i in range(2)]
            stt = [sb("stt%d" % i, [128, 4], F32) for i in range(2)]
            sttc = [sb("sttc%d" % i, [128, 4], F32) for i in range(2)]
            rt = [sb("rt%d" % i, [128, 96], F32) for i in range(2)]
            hid = [sb("hid%d" % i, [128, 2, 512], BF16) for i in range(2)]
            sil = [sb("sil%d" % i, [128, 512], F32) for i in range(2)]
            ps = self.ps
            w1d, w3d, w2d = P["w1"], P["w3"], P["w2"]

            def load_w(e, slot):
                k.dma(w1b[slot][:], w1d[l, e].rearrange("(c p) f -> p c f", p=128), writes=[w1b[slot]], q="pool")
                k.dma(w3b[slot][:], w3d[l, e].rearrange("(c p) f -> p c f", p=128), writes=[w3b[slot]], q="pool")
                k.dma(w2b[slot][:], w2d[l, e].rearrange("(c p) n -> p c n", p=128), writes=[w2b[slot]], q="pool")

            def stage_a(g):
                par = g % 2
                xT = xnT[par]
                for ti in range(NTG):
                    t = g * NTG + ti
                    b = ti % 2
                    h = ht[b]
                    k.dma(h[:], hin[0][t * 128:(t + 1) * 128, :], reads=[hin[1][t]], writes=[h])
                    self.rmsnorm(h, gain, xn[b], xn[b], stt[b])
                    yield
                    x32 = xnT32[b]
                    self.transpose8(
                        xn[b],
                        [(x32, lambda half: x32[:, half * 4:(half + 1) * 4, :]),
                         (xT, lambda half: xT[:, half * 4:(half + 1) * 4, ti * 128:(ti + 1) * 128])],
                        ps[6], ps[7])
                    yield
                    pr = ps[6 + (ti % 2)]
                    for dc in range(8):
                        k.op("pe", lambda e: e.matmul(pr[:, 0:20], x32[:, dc, :], wgr[:, dc, :], start=(dc == 0), stop=(dc == 7)),
                             reads=[x32, wgr], writes=[pr], inc=(dc == 7))
                    yield
                    r = rt[b]
                    R = lambda a, n: r[:, a:a + n]
                    lg, gmax, ngmax, oh, ge, gsum, pg = R(0, 20), R(20, 1), R(21, 1), R(22, 4), R(26, 4), R(30, 1), R(31, 1)
                    tmp, elsel, m1, nm1, ee, mask1, ee2 = R(32, 16), R(48, 4), R(52, 1), R(53, 1), R(54, 4), R(58, 4), R(62, 4)
                    v2, mask2, den, rden, wl, scl = R(66, 1), R(67, 4), R(71, 1), R(72, 1), R(73, 4), R(77, 1)
                    dv = lambda fn, rd=(), wr=(): k.op("dve", fn, reads=[r] + list(rd), writes=[r] + list(wr))
                    dv(lambda e: e.tensor_tensor(lg, pr[:, 0:20], rb[:], ALU.add), rd=[pr, rb])
                    dv(lambda e: e.tensor_reduce(gmax, lg[:, 0:4], AX.X, ALU.max))
                    dv(lambda e: e.tensor_single_scalar(ngmax, gmax, -1.0, ALU.mult))
                    dv(lambda e: e.tensor_scalar(oh, lg[:, 0:4], gmax, None, ALU.is_equal))
                    yield
                    k.op("act", lambda e: e.activation(ge, lg[:, 0:4], AF.Exp, bias=ngmax, accum_out=gsum), reads=[r], writes=[r])
                    dv(lambda e: e.reciprocal(pg, gsum))
                    dv(lambda e: e.tensor_tensor(tmp.rearrange("p (g j) -> p g j", g=4),
                                                 lg[:, 4:20].rearrange("p (g j) -> p g j", g=4),
                                                 oh.unsqueeze(2).to_broadcast([128, 4, 4]), ALU.mult))
                    dv(lambda e: e.tensor_reduce(elsel, tmp.rearrange("p (g j) -> p j g", g=4), AX.X, ALU.add))
                    yield
                    dv(lambda e: e.tensor_reduce(m1, elsel, AX.X, ALU.max))
                    dv(lambda e: e.tensor_single_scalar(nm1, m1, -1.0, ALU.mult))
                    k.op("act", lambda e: e.activation(ee, elsel, AF.Exp, bias=nm1), reads=[r], writes=[r])
                    dv(lambda e: e.tensor_scalar(mask1, elsel, m1, None, ALU.is_equal))
                    yield
                    dv(lambda e: e.scalar_tensor_tensor(ee2, mask1, -2.0, ee, ALU.mult, ALU.add))
                    dv(lambda e: e.tensor_reduce(v2, ee2, AX.X, ALU.max))
                    dv(lambda e: e.tensor_scalar(mask2, ee2, v2, None, ALU.is_equal))
                    dv(lambda e: e.tensor_single_scalar(den, v2, 1.0, ALU.add))
                    yield
                    dv(lambda e: e.reciprocal(rden, den))
                    dv(lambda e: e.scalar_tensor_tensor(wl, mask2, v2, mask1, ALU.mult, ALU.add))
                    dv(lambda e: e.tensor_tensor(scl, pg, rden, ALU.mult))
                    dv(lambda e: e.tensor_scalar(wl, wl, scl, None, ALU.mult))
                    cb = comb[par][ti]
                    dv(lambda e: e.tensor_tensor(cb[:].rearrange("p (g j) -> p g j", g=4),
                                                 oh.unsqueeze(2).to_broadcast([128, 4, 4]),
                                                 wl.unsqueeze(1).to_broadcast([128, 4, 4]), ALU.mult), wr=[cb])
                    yield

            def stage_b(g):
                par = g % 2
                xT = xnT[par]
                work = [(ex, tb) for ex in range(NEX) for tb in range(NTB)]

                def up(idx):
                    ex, tb = work[idx]
                    slot = ex % 2
                    w1, w3 = w1b[slot], w3b[slot]
                    hd = hid[idx % 2]
                    for fc in range(2):
                        p1 = ps[fc]
                        p3 = ps[2 + fc]
                        for wsrc, pdst in ((w1, p1), (w3, p3)):
                            for dc in range(8):
                                k.op("pe", lambda e: e.matmul(pdst[:], wsrc[:, dc, fc * 128:(fc + 1) * 128], xT[:, dc, tb * 512:(tb + 1) * 512],
                                                              start=(dc == 0), stop=(dc == 7)),
                                     reads=[wsrc, xT], writes=[pdst], inc=(dc == 7))
                                if dc % 4 == 3:
                                    yield
                        sl = sil[fc]
                        k.op("act", lambda e: e.activation(sl[:], p1[:], AF.Silu), reads=[p1], writes=[sl])
                        k.op("dve", lambda e: e.tensor_tensor(hd[:, fc, :], sl[:], p3[:], ALU.mult), reads=[sl, p3], writes=[hd])

                def down(idx):
                    ex, tb = work[idx]
                    slot = ex % 2
                    w2 = w2b[slot]
                    hd = hid[idx % 2]
                    for tt in range(4):
                        ti = tb * 4 + tt
                        for half in range(2):
                            py = ps[4 + half]
                            for fc in range(2):
                                k.op("pe", lambda e: e.matmul(py[:], hd[:, fc, tt * 128:(tt + 1) * 128], w2[:, fc, half * 512:(half + 1) * 512],
                                                              start=(fc == 0), stop=(fc == 1)),
                                     reads=[hd, w2], writes=[py], inc=(fc == 1))
                            ya = yacc[par][ti]
                            cs = comb[par][ti][:, ex:ex + 1]
                            if ex == 0:
                                k.op("dve", lambda e: e.tensor_scalar(ya[:, half * 512:(half + 1) * 512], py[:], cs, None, ALU.mult),
                                     reads=[py, comb[par][ti]], writes=[ya])
                            else:
                                k.op("dve", lambda e: e.scalar_tensor_tensor(ya[:, half * 512:(half + 1) * 512], py[:], cs,
                                                                             ya[:, half * 512:(half + 1) * 512], ALU.mult, ALU.add),
                                     reads=[py, comb[par][ti], ya], writes=[ya])
                            yield
                    if tb == NTB - 1:
                        if ex + 2 < NEX:
                            load_w(ex + 2, slot)
                        elif g + 1 < NG:
                            load_w(ex + 2 - NEX, slot)

                def rr(*gens):
                    gens = [g_ for g_ in gens if g_ is not None]
                    while gens:
                        for g_ in list(gens):
                            try:
                                next(g_)
                                yield
                            except StopIteration:
                                gens.remove(g_)

                yield from up(0)
                for idx in range(len(work)):
                    nu = up(idx + 1) if idx + 1 < len(work) else None
                    if nu is not None:
                        next(nu)
                        yield
                    yield from rr(nu, down(idx))

            def stage_c(g):
                par = g % 2
                for ti in range(NTG):
                    t = g * NTG + ti
                    b = ti % 2
                    h = htc[b]
                    ya = yacc[par][ti]
                    k.dma(h[:], hin[0][t * 128:(t + 1) * 128, :], reads=[hin[1][t]], writes=[h])
                    k.op("pool", lambda e: e.tensor_tensor(ya[:], h[:], ya[:], ALU.add), reads=[h, ya], writes=[ya])
                    yield
                    if final:
                        self.rmsnorm(ya, fgain, h, h, sttc[b])
                        k.dma(hout[0][t * 128:(t + 1) * 128, :], h[:], reads=[h], writes=[hout[1][t]])
                    else:
                        k.dma(hout[0][t * 128:(t + 1) * 128, :], ya[:], reads=[ya], writes=[hout[1][t]])
                    yield

            def run_group(bg, ag, cg):
                others = [[g_, per] for g_, per in ((ag, 4), (cg, 24)) if g_ is not None]
                step = 0
                b_alive = bg is not None
                while b_alive or others:
                    if b_alive:
                        try:
                            next(bg)
                        except StopIteration:
                            b_alive = False
                    for item in list(others):
                        if (not b_alive) or step % item[1] == 0:
                            try:
                                next(item[0])
                            except StopIteration:
                                others.remove(item)
                    step += 1

            load_w(0, 0)
            load_w(1, 1)
            run_group(None, stage_a(0), None)
            for g in range(NG):
                run_group(stage_b(g), stage_a(g + 1) if g + 1 < NG else None, stage_c(g - 1) if g >= 1 else None)
            run_group(None, None, stage_c(NG - 1))
            k.barrier()

    def nextbank(self):
        self.pbi = (getattr(self, "pbi", -1) + 1) % 8
        return self.ps[self.pbi]

    def mem_kv(self, st, l):
        k, P = self.k, self.P
        sb = lambda n, s, d: self.sbuf(st, n, s, d)
        memkT = sb("memkT", [128, 2, 256], BF16)
        memv = sb("memv", [128, 2, 256], BF16)
        with ExitStack() as st2:
            sb2 = lambda n, s, d: self.sbuf(st2, n, s, d)
            g = sb2("mg", [128, D], F32)
            k.dma(g[:], P["mem_norm"][l].partition_broadcast(128), writes=[g])
            w = sb2("wmkv", [128, 8, 512], BF16)
            k.dma(w[:], P["w_mem_kv"][l].rearrange("(c p) n -> p c n", p=128), writes=[w], q="pool")
            mT = sb2("memnT", [128, 8, 256], BF16)
            junk = sb2("mjunk", [128, D], F32)
            for mt in range(2):
                h = sb2("mh%d" % mt, [128, D], F32)
                xn = sb2("mxn%d" % mt, [128, D], F32)
                stt = sb2("mst%d" % mt, [128, 4], F32)
                k.dma(h[:], P["mem"][mt * 128:(mt + 1) * 128, :], writes=[h])
                self.rmsnorm(h, g, xn, junk, stt)
                self.transpose8(xn, [(mT, lambda half: mT[:, half * 4:(half + 1) * 4, mt * 128:(mt + 1) * 128])],
                                self.nextbank(), self.nextbank())
            for j in range(2):
                pb = self.nextbank()
                for dc in range(8):
                    k.op("pe", lambda e: e.matmul(pb[:, 0:256], w[:, dc, j * 128:(j + 1) * 128], mT[:, dc, :],
                                                  start=(dc == 0), stop=(dc == 7)),
                         reads=[w, mT], writes=[pb], inc=(dc == 7))
                k.op("act", lambda e: e.copy(memkT[:, j, :], pb[:, 0:256]), reads=[pb], writes=[memkT])
            for mt in range(2):
                pb = self.nextbank()
                for dc in range(8):
                    k.op("pe", lambda e: e.matmul(pb[:, 0:256], mT[:, dc, mt * 128:(mt + 1) * 128], w[:, dc, 256:512],
                                                  start=(dc == 0), stop=(dc == 7)),
                         reads=[w, mT], writes=[pb], inc=(dc == 7))
                k.op("dve", lambda e: e.tensor_copy(memv[:, mt, :], pb[:, 0:256]), reads=[pb], writes=[memv])
            k.barrier()
        return memkT, memv

    def mem_attend(self, W, mqT, qoff, memkT, memv, mix, col0=768):
        for _ in self.mem_attend_g(W, mqT, qoff, memkT, memv, mix, col0):
            pass

    def mem_attend_g(self, W, mqT, qoff, memkT, memv, mix, col0=768, banks=None):
        k = self.k
        pe_ = W["pexp"]; ms = W["mstat"]; pT = W["pT"]
        if banks is None:
            banks = [self.nextbank(), self.nextbank()]
        for hh in range(4):
            pair, s = hh // 2, hh % 2
            pb = banks[s]
            k.op("pe", lambda e: e.matmul(pb[:, pair * 256:(pair + 1) * 256], mqT[s * 64:(s + 1) * 64, pair, qoff:qoff + 128],
                                          memkT[s * 64:(s + 1) * 64, pair, :], start=True, stop=True),
                 reads=[mqT, memkT], writes=[pb])
        for s in range(2):
            pb = banks[s]
            k.op("dve", lambda e: e.tensor_reduce(ms[:, s:s + 3:2], pb[:].rearrange("p (h m) -> p h m", h=2), AX.X, ALU.max),
                 reads=[pb], writes=[ms])
        k.op("dve", lambda e: e.tensor_single_scalar(ms[:, 4:8], ms[:, 0:4], -0.125, ALU.mult), reads=[ms], writes=[ms])
        yield
        for hh in range(4):
            pair, s = hh // 2, hh % 2
            pb = banks[s]
            k.op("act", lambda e: e.activation(pe_[:, hh, :], pb[:, pair * 256:(pair + 1) * 256], AF.Exp, bias=ms[:, 4 + hh:5 + hh],
                                               scale=0.125, accum_out=ms[:, 8 + hh:9 + hh]),
                 reads=[pb, ms], writes=[pe_, ms])
        k.op("dve", lambda e: e.reciprocal(ms[:, 12:16], ms[:, 8:12]), reads=[ms], writes=[ms])
        yield
        for half in range(2):
            pb = self.nextbank()
            for j in range(4):
                idx = half * 4 + j
                hh, mc = idx // 2, idx % 2
                k.op("pe", lambda e: e.transpose(pb[:, j * 128:(j + 1) * 128], pe_[:, hh, mc * 128:(mc + 1) * 128], self.ident),
                     reads=[pe_, self.cst], writes=[pb], inc=(j == 3))
            if half == 0:
                k.op("act", lambda e: e.copy(pT[:, 0:4, :], pb[:].rearrange("p (c t) -> p c t", c=4)), reads=[pb], writes=[pT])
            else:
                k.op("dve", lambda e: e.tensor_copy(pT[:, 4:8, :], pb[:].rearrange("p (c t) -> p c t", c=4)), reads=[pb], writes=[pT])
        yield
        pb = self.nextbank()
        for hh in range(4):
            for mc in range(2):
                k.op("pe", lambda e: e.matmul(pb[:, hh * 64:(hh + 1) * 64], pT[:, hh * 2 + mc, :], memv[:, mc, hh * 64:(hh + 1) * 64],
                                              start=(mc == 0), stop=(mc == 1)),
                     reads=[pT, memv], writes=[pb], inc=(mc == 1))
        k.op("dve", lambda e: e.tensor_tensor(mix[:, col0:col0 + 256].rearrange("p (h d) -> p h d", h=4),
                                              pb[:, 0:256].rearrange("p (h d) -> p h d", h=4),
                                              ms[:, 12:16].unsqueeze(2).to_broadcast([128, 4, 64]), ALU.mult),
             reads=[pb, ms], writes=[mix])

    def mem_work(self, st):
        sb = lambda n, s, d: self.sbuf(st, n, s, d)
        return {"pexp": sb("pexp", [128, 4, 256], F32), "mstat": sb("mstat", [128, 16], F32),
                "pT": sb("pT", [128, 8, 128], BF16)}

    def out_proj(self, mix, mixT, w_out, h, hn, dst_ap, dst_buf):
        k = self.k
        self.transpose8(mix, [(mixT, lambda half: mixT[:, half * 4:(half + 1) * 4, :])], self.nextbank(), self.nextbank())
        for half in range(2):
            pb = self.nextbank()
            for fc in range(8):
                k.op("pe", lambda e: e.matmul(pb[:], mixT[:, fc, :], w_out[:, fc, half * 512:(half + 1) * 512],
                                              start=(fc == 0), stop=(fc == 7)),
                     reads=[mixT, w_out], writes=[pb], inc=(fc == 7))
            k.op("dve", lambda e: e.tensor_tensor(hn[:, half * 512:(half + 1) * 512], h[:, half * 512:(half + 1) * 512], pb[:], ALU.add),
                 reads=[h, pb], writes=[hn])
        k.dma(dst_ap, hn[:], reads=[hn], writes=[dst_buf])

    def mixer_a_phase(self, hin, hout):
        nc, k, P = self.nc, self.k, self.P
        NTA = int(os.environ.get("MK_NTA", str(NT)))
        with ExitStack() as st:
            sb = lambda n, s, d: self.sbuf(st, n, s, d)
            memkT, memv = self.mem_kv(st, 0)
            gain = sb("gainA", [128, D], F32)
            k.dma(gain[:], P["a_norm"][0].partition_broadcast(128), writes=[gain])
            w_in = sb("w_inA", [128, 8, 3340], BF16)
            for c in range(8):
                k.dma(w_in[:, c, :], P["a_w_in"][0, c * 128:(c + 1) * 128, :], writes=[w_in], q="pool")
            w_out = sb("w_outA", [128, 8, D], BF16)
            k.dma(w_out[:], P["a_w_out"][0].rearrange("(c p) n -> p c n", p=128), writes=[w_out], q="pool")
            convw = sb("convw", [128, 18, 4], F32)
            k.dma(convw[:], P["convw"], writes=[convw])
            sc6 = sb("sc6", [128, 32], F32)
            k.dma(sc6[:, 0:6], P["a_log"][0].partition_broadcast(128), writes=[sc6])
            k.dma(sc6[:, 6:12], P["a_dt_bias"][0].partition_broadcast(128), writes=[sc6])
            k.op("act", lambda e: e.activation(sc6[:, 12:18], sc6[:, 0:6], AF.Exp), reads=[sc6], writes=[sc6])
            k.op("dve", lambda e: e.tensor_single_scalar(sc6[:, 12:18], sc6[:, 12:18], -1.0, ALU.mult), reads=[sc6], writes=[sc6])
            ogain = sb("ogain", [128, 128], F32)
            k.dma(ogain[:], P["a_out_gain"][0].partition_broadcast(128), writes=[ogain])
            MW = self.mem_work(st)
            pc = sb("pc", [128, 18, 131], F32)
            k.op("dve", lambda e: e.memset(pc[:], 0.0), writes=[pc])
            Sf = [sb("Sf%d" % h, [128, 128], F32) for h in range(6)]
            Sb = [sb("Sb%d" % h, [128, 128], BF16) for h in range(6)]
            for h in range(6):
                k.op("dve", lambda e: e.memset(Sf[h][:], 0.0), writes=[Sf[h]])
                k.op("pool", lambda e: e.memset(Sb[h][:], 0.0), writes=[Sb[h]])
            hA = sb("htA", [128, D], F32)
            xT = sb("xnTA", [128, 8, 128], BF16)
            cv = sb("cv", [128, 12, 128], F32)
            cvv = sb("cvv", [128, 6, 128], F32)
            ctmp = sb("ctmp", [128, 128], F32)
            cvjunk = View(cv, cv[:, 0:8, :].rearrange("p c t -> p (c t)"))
            sq = sb("sq", [128, 12, 128], BF16)
            rs = sb("rs", [128, 12, 128], F32)
            kn32 = sb("kn32", [128, 6, 128], F32)
            sttA = sb("sttA", [128, 4], F32)
            qkT = [sb("qkT%d" % i, [128, 6, 2, 128], BF16) for i in range(3)]
            sc = [sb("scA%d" % i, [128, 96], F32) for i in range(3)]
            gg = [sb("gg%d" % i, [128, 768], F32) for i in range(3)]
            mqT = [sb("mqTA%d" % i, [128, 2, 128], BF16) for i in range(3)]
            SLg = [sb("SLg%d" % i, [128, 128], F32) for i in range(6)]
            dm = sb("dm", [128, 6, 128], F32)
            dmi = sb("dmi", [128, 6, 128], F32)
            Wq = [[sb("W%d_%d" % (h, i), [128, 3, 128], BF16) for i in range(2)] for h in range(6)]
            Q0f = [sb("Q0f%d" % i, [128, 128], F32) for i in range(6)]
            kt = [sb("kt%d" % i, [128, 6, 128], BF16) for i in range(2)]
            vtok = [sb("vtok%d" % i, [128, 6, 128], F32) for i in range(2)]
            attnT = [sb("attnT%d" % i, [128, 6, 128], BF16) for i in range(2)]
            TT = [sb("TT%d" % i, [128, 6, 128], BF16) for i in range(2)]
            Rall = sb("Rall", [128, 6, 128], BF16)
            vnew = sb("vnewA", [128, 6, 128], BF16)
            oall = sb("oall", [128, 6, 128], F32)
            ost = sb("ost", [128, 24], F32)
            ojunk = sb("ojunk", [128, 128], F32)
            mix = sb("mixA", [128, D], F32)
            mixT = sb("mixTA", [128, 8, 128], BF16)
            hC = sb("htC", [128, D], F32)
            ident, U, SL, ones = self.ident, self.U, self.SL, self.ones
            NEGs = self.cst[:, 7, :]
            identb = self.identb
            cst, cstb = self.cst, self.cstb
            QS = float(128 ** -0.5)
            ctm = View(rs, rs[:, 0:9, :])
            rotA = [0]

            def nbA():
                rotA[0] = (rotA[0] + 1) % 6
                return self.ps[rotA[0]]
            self.nextbank = nbA
            membanks = [self.ps[6], self.ps[7]]

            def frontA(t):
                i3 = t % 3
                h = hA
                k.dma(h[:], hin[0][t * 128:(t + 1) * 128, :], reads=[hin[1][t]], writes=[h])
                self.rmsnorm(h, gain, h, cvjunk, sttA)
                yield
                self.transpose8(h, [(xT, lambda half: xT[:, half * 4:(half + 1) * 4, :])], self.nextbank(), self.nextbank())
                yield
                for g4 in range(5):
                    nf = 4 if g4 < 4 else 2
                    pb = self.nextbank()
                    for j in range(nf):
                        fc = g4 * 4 + j
                        for dc in range(8):
                            k.op("pe", lambda e: e.matmul(pb[:, j * 128:(j + 1) * 128], w_in[:, dc, fc * 128:(fc + 1) * 128], xT[:, dc, :],
                                                          start=(dc == 0), stop=(dc == 7)),
                                 reads=[w_in, xT], writes=[pb], inc=(dc == 7 and j == nf - 1))
                    dstv = pc[:, g4 * 4:g4 * 4 + nf, 3:131]
                    srcv = pb[:, 0:nf * 128].rearrange("p (c t) -> p c t", c=nf)
                    k.op("act", lambda e: e.copy(dstv, srcv), reads=[pb], writes=[pc])
                    yield
                pb = self.nextbank()
                for j in range(2):
                    for dc in range(8):
                        k.op("pe", lambda e: e.matmul(pb[:, j * 128:(j + 1) * 128], w_in[:, dc, 3084 + j * 128:3084 + (j + 1) * 128], xT[:, dc, :],
                                                      start=(dc == 0), stop=(dc == 7)),
                             reads=[w_in, xT], writes=[pb], inc=(dc == 7 and j == 1))
                k.op("act", lambda e: e.copy(mqT[i3][:], pb[:, 0:256].rearrange("p (c t) -> p c t", c=2)), reads=[pb], writes=[mqT[i3]])
                yield
                pg1 = self.nextbank()
                for dc in range(8):
                    k.op("pe", lambda e: e.matmul(pg1[:], xT[:, dc, :], w_in[:, dc, 2304:2816], start=(dc == 0), stop=(dc == 7)),
                         reads=[w_in, xT], writes=[pg1], inc=(dc == 7))
                pg2 = self.nextbank()
                for dc in range(8):
                    k.op("pe", lambda e: e.matmul(pg2[:, 0:268], xT[:, dc, :], w_in[:, dc, 2816:3084], start=(dc == 0), stop=(dc == 7)),
                         reads=[w_in, xT], writes=[pg2], inc=(dc == 7))
                g_g = gg[i3]
                k.op("act", lambda e: e.activation(g_g[:, 0:512], pg1[:], AF.Silu), reads=[pg1], writes=[g_g])
                k.op("act", lambda e: e.activation(g_g[:, 512:768], pg2[:, 0:256], AF.Silu), reads=[pg2], writes=[g_g])
                s_ = sc[i3]
                C = lambda a_, n=6: s_[:, a_:a_ + n]
                beta, tt_, ex_, sp_, g_, gcl, egc, negc, etl, egl, dd, eb_ = (C(0), C(6), C(12), C(18), C(24), C(32, 16), C(48), C(54), C(60), C(66), C(72), C(78))
                k.op("act", lambda e: e.activation(eb_, pg2[:, 256:262], AF.Exp, scale=-1.0), reads=[pg2], writes=[s_])
                k.op("dve", lambda e: e.tensor_tensor(tt_, pg2[:, 262:268], sc6[:, 6:12], ALU.add), reads=[pg2, sc6], writes=[s_])
                k.op("dve", lambda e: e.tensor_single_scalar(eb_, eb_, 1.0, ALU.add), reads=[s_], writes=[s_])
                k.op("dve", lambda e: e.reciprocal(beta, eb_), reads=[s_], writes=[s_])
                k.op("pool", lambda e: e.tensor_tensor(g_g[:].rearrange("p (h d) -> p h d", h=6), g_g[:].rearrange("p (h d) -> p h d", h=6),
                                                       ogain[:].unsqueeze(1).to_broadcast([128, 6, 128]), ALU.mult),
                     reads=[g_g, ogain], writes=[g_g])
                yield
                k.op("act", lambda e: e.activation(ex_, tt_, AF.Exp), reads=[s_], writes=[s_])
                k.op("act", lambda e: e.activation(sp_, ex_, AF.Ln, bias=1.0), reads=[s_], writes=[s_])
                k.op("dve", lambda e: e.tensor_tensor(g_, sp_, sc6[:, 12:18], ALU.mult), reads=[s_, sc6], writes=[s_])
                yield
                pgc = self.nextbank()
                k.op("pe", lambda e: e.matmul(pgc[:, 0:6], U, g_, start=True, stop=True), reads=[cst, s_], writes=[pgc])
                k.op("pe", lambda e: e.matmul(pgc[:, 8:14], ones, g_, start=True, stop=True), reads=[cst, s_], writes=[pgc])
                k.op("dve", lambda e: e.tensor_copy(gcl, pgc[:, 0:16]), reads=[pgc], writes=[s_])
                k.op("dve", lambda e: e.tensor_tensor(dd, s_[:, 40:46], s_[:, 32:38], ALU.subtract), reads=[s_], writes=[s_])
                yield
                k.op("act", lambda e: e.activation(egc, s_[:, 32:38], AF.Exp), reads=[s_], writes=[s_])
                k.op("act", lambda e: e.activation(etl, dd, AF.Exp), reads=[s_], writes=[s_])
                k.op("act", lambda e: e.activation(egl, s_[:, 40:46], AF.Exp), reads=[s_], writes=[s_])
                k.op("dve", lambda e: e.tensor_single_scalar(negc, egc, -1.0, ALU.mult), reads=[s_], writes=[s_])
                yield
                for (c0, nchk, dstb, dst) in ((0, 9, cv, cv[:, 0:9, :]), (9, 3, cv, cv[:, 9:12, :]), (12, 6, cvv, cvv[:, 0:6, :])):
                    wv = lambda j: convw[:, c0:c0 + nchk, j:j + 1].to_broadcast([128, nchk, 128])
                    k.op("dve", lambda e: e.tensor_tensor(dst, pc[:, c0:c0 + nchk, 0:128], wv(0), ALU.mult), reads=[pc, convw], writes=[dstb])
                    for j in range(1, 4):
                        tv = ctm[:, 0:nchk, :]
                        k.op("dve", lambda e: e.tensor_tensor(tv, pc[:, c0:c0 + nchk, j:j + 128], wv(j), ALU.mult), reads=[pc, convw], writes=[ctm])
                        k.op("dve", lambda e: e.tensor_tensor(dst, dst, tv, ALU.add), reads=[ctm, dstb], writes=[dstb])
                        yield
                k.op("pool", lambda e: e.tensor_copy(pc[:, :, 0:3], pc[:, :, 128:131]), reads=[pc], writes=[pc])
                k.op("act", lambda e: e.activation(cv[:], cv[:], AF.Silu), reads=[cv], writes=[cv])
                k.op("act", lambda e: e.activation(cvv[:], cvv[:], AF.Silu), reads=[cvv], writes=[cvv])
                qkv = cv
                k.op("act", lambda e: e.activation(sq[:], qkv[:, 0:12, :], AF.Square), reads=[qkv], writes=[sq])
                yield
                for g3 in range(3):
                    pb = self.nextbank()
                    k.op("pe", lambda e: e.matmul(pb[:], self.onesb, sq[:, g3 * 4:(g3 + 1) * 4, :], start=True, stop=True),
                         reads=[cstb, sq], writes=[pb])
                    k.op("act", lambda e: e.activation(rs[:, g3 * 4:(g3 + 1) * 4, :], pb[:].rearrange("p (c t) -> p c t", c=4), AF.Ln,
                                                       bias=self.epsb[:, 0:1]), reads=[pb, self.epsbuf], writes=[rs])
                    k.op("act", lambda e: e.activation(rs[:, g3 * 4:(g3 + 1) * 4, :], rs[:, g3 * 4:(g3 + 1) * 4, :], AF.Exp, scale=-0.5),
                         reads=[rs], writes=[rs])
                yield
                qk = qkT[i3]
                k.op("dve", lambda e: e.scalar_tensor_tensor(qk[:, :, 1, :], qkv[:, 0:6, :], QS, rs[:, 0:6, :], ALU.mult, ALU.mult),
                     reads=[qkv, rs], writes=[qk])
                k.op("dve", lambda e: e.tensor_tensor(kn32[:], qkv[:, 6:12, :], rs[:, 6:12, :], ALU.mult), reads=[qkv, rs], writes=[kn32])
                k.op("pool", lambda e: e.tensor_copy(qk[:, :, 0, :], kn32[:]), reads=[kn32], writes=[qk])
                yield
                b = t % 2
                s_etl = etl
                for grp in range(3):
                    pb = self.nextbank()
                    for j in range(4):
                        idx = grp * 4 + j
                        src = cvv[:, idx, :] if idx < 6 else kn32[:, idx - 6, :]
                        srcb = cvv if idx < 6 else kn32
                        k.op("pe", lambda e: e.transpose(pb[:, j * 128:(j + 1) * 128], src, ident), reads=[srcb, cst], writes=[pb], inc=(j == 3))
                    for j in range(4):
                        idx = grp * 4 + j
                        if idx < 6:
                            k.op("act", lambda e: e.copy(vtok[b][:, idx, :], pb[:, j * 128:(j + 1) * 128]), reads=[pb], writes=[vtok[b]])
                        else:
                            hh = idx - 6
                            k.op("dve", lambda e: e.tensor_scalar(kt[b][:, hh, :], pb[:, j * 128:(j + 1) * 128], s_etl[:, hh:hh + 1], None, ALU.mult),
                                 reads=[pb, s_], writes=[kt[b]])
                    yield

            def frontB(t):
                i3 = t % 3
                b = t % 2
                s_ = sc[i3]
                C = lambda a_, n=6: s_[:, a_:a_ + n]
                beta, g_ = C(0), C(24)
                qk = qkT[i3]
                for hh in range(6):
                    sg = SLg[hh]
                    k.op("dve", lambda e: e.tensor_scalar(sg[:], SL, g_[:, hh:hh + 1], None, ALU.mult), reads=[cst, s_], writes=[sg])
                yield
                for hp in range(3):
                    pb = self.nextbank()
                    for j in range(2):
                        hh = hp * 2 + j
                        sg = SLg[hh]
                        k.op("pe", lambda e: e.matmul(pb[:, j * 128:(j + 1) * 128], sg[:], U, start=True, stop=False),
                             reads=[sg, cst], writes=[pb], inc=False)
                        k.op("pe", lambda e: e.matmul(pb[:, j * 128:(j + 1) * 128], ident, NEGs, start=False, stop=True),
                             reads=[cst], writes=[pb])
                    k.op("act", lambda e: e.activation(dm[:, hp * 2:hp * 2 + 2, :], pb[:, 0:256].rearrange("p (c t) -> p c t", c=2), AF.Exp),
                         reads=[pb], writes=[dm])
                    yield
                k.op("pool", lambda e: e.tensor_tensor(dmi[:], dm[:], ident.unsqueeze(1).to_broadcast([128, 6, 128]), ALU.add),
                     reads=[dm, cst], writes=[dmi])
                for hh in range(6):
                    pb = self.nextbank()
                    W0 = Wq[hh][0]
                    qf = Q0f[hh]
                    k.op("pe", lambda e: e.matmul(pb[:, 0:256], qk[:, hh, 0, :], qk[:, hh, :, :].rearrange("p a t -> p (a t)"),
                                                  start=True, stop=True), reads=[qk], writes=[pb])
                    k.op("dve", lambda e: e.scalar_tensor_tensor(qf[:], pb[:, 0:128], beta[:, hh:hh + 1], dm[:, hh, :], ALU.mult, ALU.mult),
                         reads=[pb, s_, dm], writes=[qf])
                    k.op("dve", lambda e: e.tensor_tensor(attnT[b][:, hh, :], pb[:, 128:256], dmi[:, hh, :], ALU.mult),
                         reads=[pb, dmi], writes=[attnT[b]])
                    if hh % 3 == 2:
                        yield
                for hh in range(6):
                    W0 = Wq[hh][0]
                    qf = Q0f[hh]
                    k.op("pool", lambda e: e.tensor_copy(W0[:, 0, :], qf[:]), reads=[qf], writes=[W0])
                    k.op("pool", lambda e: e.tensor_tensor(Wq[hh][1][:, 1, :], ident, qf[:], ALU.subtract), reads=[cst, qf], writes=[Wq[hh][1]])
                    pb2 = self.nextbank()
                    k.op("pe", lambda e: e.transpose(pb2[:, 0:128], qf[:], ident), reads=[qf, cst], writes=[pb2])
                    k.op("act", lambda e: e.copy(W0[:, 2, :], pb2[:, 0:128]), reads=[pb2], writes=[W0])
                    if hh % 3 == 2:
                        yield
                for lvl in range(7):
                    for hh in range(6):
                        Wc = Wq[hh][lvl % 2]
                        Wn = Wq[hh][(lvl + 1) % 2]
                        pb = self.nextbank()
                        Qk, Xk, Pk = Wc[:, 0, :], Wc[:, 1, :], Wc[:, 2, :]
                        mm = lambda out, l_, r_, st_, sp_2, inc_: k.op(
                            "pe", lambda e: e.matmul(out, l_, r_, start=st_, stop=sp_2), reads=[Wc, cstb], writes=[pb], inc=inc_)
                        if lvl == 0:
                            mm(pb[:, 0:128], Pk, Qk, True, True, False)
                            mm(pb[:, 256:384], Qk, Pk, True, True, True)
                            k.op("act", lambda e: e.copy(Wn[:, 0, :], pb[:, 0:128]), reads=[pb], writes=[Wn])
                            k.op("act", lambda e: e.copy(Wn[:, 2, :], pb[:, 256:384]), reads=[pb], writes=[Wn])
                        elif lvl < 6:
                            mm(pb[:, 0:128], Pk, Qk, True, True, False)
                            mm(pb[:, 128:256], Pk, Xk, True, False, False)
                            mm(pb[:, 128:256], identb, Xk, False, True, False)
                            mm(pb[:, 256:384], Qk, Pk, True, True, True)
                            k.op("act", lambda e: e.copy(Wn[:], pb[:, 0:384].rearrange("p (c t) -> p c t", c=3)), reads=[pb], writes=[Wn])
                        else:
                            mm(pb[:, 128:256], Pk, Xk, True, False, False)
                            mm(pb[:, 128:256], identb, Xk, False, True, True)
                            k.op("act", lambda e: e.copy(TT[b][:, hh, :], pb[:, 128:256]), reads=[pb], writes=[TT[b]])
                        if hh % 3 == 2:
                            yield

            def back(t):
                i3 = t % 3
                b = t % 2
                s_ = sc[i3]
                C = lambda a_, n=6: s_[:, a_:a_ + n]
                beta, egc, negc, egl = C(0), C(48), C(54), C(66)
                qk, g_g = qkT[i3], gg[i3]
                k.dma(hC[:], hin[0][t * 128:(t + 1) * 128, :], reads=[hin[1][t]], writes=[hC])
                for hp in range(3):
                    pb = self.nextbank()
                    for j in range(2):
                        hh = hp * 2 + j
                        o = j * 256
                        k.op("pe", lambda e: e.matmul(pb[:, o:o + 128], qk[:, hh, 0, :], Sb[hh][:], start=True, stop=True),
                             reads=[qk, Sb[hh]], writes=[pb], inc=False)
                        k.op("pe", lambda e: e.matmul(pb[:, o + 128:o + 256], qk[:, hh, 1, :], Sb[hh][:], start=True, stop=True),
                             reads=[qk, Sb[hh]], writes=[pb], inc=(j == 1))
                    for j in range(2):
                        hh = hp * 2 + j
                        o = j * 256
                        k.op("dve", lambda e: e.scalar_tensor_tensor(Rall[:, hh, :], pb[:, o:o + 128], negc[:, hh:hh + 1], vtok[b][:, hh, :], ALU.mult, ALU.add),
                             reads=[pb, s_, vtok[b]], writes=[Rall])
                        k.op("dve", lambda e: e.tensor_scalar(oall[:, hh, :], pb[:, o + 128:o + 256], egc[:, hh:hh + 1], None, ALU.mult),
                             reads=[pb, s_], writes=[oall])
                yield
                for (h0, nh) in ((0, 4), (4, 2)):
                    pb = self.nextbank()
                    for j in range(nh):
                        hh = h0 + j
                        k.op("pe", lambda e: e.matmul(pb[:, j * 128:(j + 1) * 128], TT[b][:, hh, :], Rall[:, hh, :], start=True, stop=True),
                             reads=[TT[b], Rall], writes=[pb], inc=(j == nh - 1))
                    k.op("dve", lambda e: e.tensor_tensor(vnew[:, h0:h0 + nh, :], pb[:, 0:nh * 128].rearrange("p (c t) -> p c t", c=nh),
                                                          beta[:, h0:h0 + nh].unsqueeze(2).to_broadcast([128, nh, 128]), ALU.mult),
                         reads=[pb, s_], writes=[vnew])
                yield
                for hp in range(3):
                    pb = self.nextbank()
                    for j in range(2):
                        hh = hp * 2 + j
                        o = j * 256
                        k.op("pe", lambda e: e.matmul(pb[:, o:o + 128], attnT[b][:, hh, :], vnew[:, hh, :], start=True, stop=True),
                             reads=[attnT[b], vnew], writes=[pb], inc=False)
                        k.op("pe", lambda e: e.matmul(pb[:, o + 128:o + 256], kt[b][:, hh, :], vnew[:, hh, :], start=True, stop=True),
                             reads=[kt[b], vnew], writes=[pb], inc=(j == 1))
                    for j in range(2):
                        hh = hp * 2 + j
                        o = j * 256
                        k.op("dve", lambda e: e.scalar_tensor_tensor(Sb[hh][:], Sf[hh][:], egl[:, hh:hh + 1], pb[:, o + 128:o + 256], ALU.mult, ALU.add),
                             reads=[Sf[hh], s_, pb], writes=[Sb[hh]])
                        k.op("dve", lambda e: e.scalar_tensor_tensor(Sf[hh][:], Sf[hh][:], egl[:, hh:hh + 1], pb[:, o + 128:o + 256], ALU.mult, ALU.add),
                             reads=[Sf[hh], s_, pb], writes=[Sf[hh]])
                        k.op("dve", lambda e: e.tensor_tensor(oall[:, hh, :], oall[:, hh, :], pb[:, o:o + 128], ALU.add),
                             reads=[oall, pb], writes=[oall])
                yield
                for hh in range(6):
                    k.op("act", lambda e: e.activation(ojunk[:], oall[:, hh, :], AF.Square, accum_out=ost[:, hh:hh + 1]),
                         reads=[oall], writes=[ojunk, ost])
                k.op("act", lambda e: e.activation(ost[:, 8:14], ost[:, 0:6], AF.Ln, bias=self.epsb[:, 0:1], scale=1.0 / 128),
                     reads=[ost, self.epsbuf], writes=[ost])
                k.op("act", lambda e: e.activation(ost[:, 16:22], ost[:, 8:14], AF.Exp, scale=-0.5), reads=[ost], writes=[ost])
                yield
                for hh in range(6):
                    k.op("dve", lambda e: e.scalar_tensor_tensor(mix[:, hh * 128:(hh + 1) * 128], oall[:, hh, :], ost[:, 16 + hh:17 + hh],
                                                                 g_g[:, hh * 128:(hh + 1) * 128], ALU.mult, ALU.mult),
                         reads=[oall, ost, g_g], writes=[mix])
                yield
                yield from self.mem_attend_g(MW, mqT[i3], 0, memkT, memv, mix, banks=membanks)
                yield
                self.transpose8(mix, [(mixT, lambda half: mixT[:, half * 4:(half + 1) * 4, :])], self.nextbank(), self.nextbank())
                yield
                for half in range(2):
                    pb = self.nextbank()
                    for fc in range(8):
                        k.op("pe", lambda e: e.matmul(pb[:], mixT[:, fc, :], w_out[:, fc, half * 512:(half + 1) * 512],
                                                      start=(fc == 0), stop=(fc == 7)),
                             reads=[mixT, w_out], writes=[pb], inc=(fc == 7))
                    k.op("dve", lambda e: e.tensor_tensor(hC[:, half * 512:(half + 1) * 512], hC[:, half * 512:(half + 1) * 512], pb[:], ALU.add),
                         reads=[hC, pb], writes=[hC])
                k.dma(hout[0][t * 128:(t + 1) * 128, :], hC[:], reads=[hC], writes=[hout[1][t]])

            def run(*gens):
                gens = [g for g in gens if g is not None]
                while gens:
                    for g_ in list(gens):
                        try:
                            next(g_)
                        except StopIteration:
                            gens.remove(g_)

            mk = lambda fn, t: fn(t) if 0 <= t < NTA else None
            for step in range(-2, NTA):
                run(mk(back, step), mk(frontB, step + 1), mk(frontA, step + 2))
            k.barrier()
            del self.nextbank

    def mixer_b_phase(self, hin, hout):
        nc, k, P = self.nc, self.k, self.P
        NGB = int(os.environ.get("MK_NGB", "8"))
        NHB = int(os.environ.get("MK_NHB", "12"))
        NDUM = int(os.environ.get("MK_NDUM", "1"))
        with ExitStack() as st:
            sb = lambda n, s, d: self.sbuf(st, n, s, d)
            memkT, memv = self.mem_kv(st, 1)
            win_d = self.dscratch("b_w_in_bf", [D, D], BF16)
            wout_d = self.dscratch("b_w_out_bf", [D, D], BF16)
            wdb = [Buf(None, "win_d"), Buf(None, "wout_d")]
            k.dma(win_d, P["b_w_in"][0], writes=[wdb[0]], q="pool")
            k.dma(wout_d, P["b_w_out"][0], writes=[wdb[1]], q="pool")
            KT = sb("KT", [128, 6, S], BF16)
            Vt = sb("Vt", [128, NT, 768], BF16)
            ht = [sb("htB%d" % i, [128, D], F32) for i in range(2)]
            xn = sb("xnB", [128, D], F32)
            stt = [sb("sttB%d" % i, [128, 4], F32) for i in range(2)]
            with ExitStack() as st1:
                sb1 = lambda n, s, d: self.sbuf(st1, n, s, d)
                kvg = sb1("kvg", [128, D], F32)
                k.dma(kvg[:], P["kv_norm"].partition_broadcast(128), writes=[kvg])
                w_kv = sb1("w_kv", [128, 8, 1536], BF16)
                for c in range(8):
                    k.dma(w_kv[:, c, :], P["w_kv"][c * 128:(c + 1) * 128, :], writes=[w_kv], q="pool")
                xT1 = [sb1("xT1_%d" % i, [128, 8, 128], BF16) for i in range(2)]
                for t in range(NT):
                    b = t % 2
                    h = ht[b]
                    k.dma(h[:], hin[0][t * 128:(t + 1) * 128, :], reads=[hin[1][t]], writes=[h])
                    self.rmsnorm(h, kvg, xn, xn, stt[b])
                    xT = xT1[b]
                    self.transpose8(xn, [(xT, lambda half: xT[:, half * 4:(half + 1) * 4, :])], self.nextbank(), self.nextbank())
                    for g4 in range(2):
                        nf = 4 if g4 == 0 else 2
                        pb = self.nextbank()
                        for j in range(nf):
                            fc = g4 * 4 + j
                            for dc in range(8):
                                k.op("pe", lambda e: e.matmul(pb[:, j * 128:(j + 1) * 128], w_kv[:, dc, fc * 128:(fc + 1) * 128], xT[:, dc, :],
                                                              start=(dc == 0), stop=(dc == 7)),
                                     reads=[w_kv, xT], writes=[pb], inc=(dc == 7 and j == nf - 1))
                        dstv = KT[:, g4 * 4:g4 * 4 + nf, t * 128:(t + 1) * 128]
                        srcv = pb[:, 0:nf * 128].rearrange("p (c t) -> p c t", c=nf)
                        k.op("act", lambda e: e.copy(dstv, srcv), reads=[pb], writes=[KT])
                    for half, (c0, c1) in enumerate(((0, 512), (512, 768))):
                        pb = self.nextbank()
                        for dc in range(8):
                            k.op("pe", lambda e: e.matmul(pb[:, 0:c1 - c0], xT[:, dc, :], w_kv[:, dc, 768 + c0:768 + c1],
                                                          start=(dc == 0), stop=(dc == 7)),
                                 reads=[w_kv, xT], writes=[pb], inc=(dc == 7))
                        k.op("dve", lambda e: e.tensor_copy(Vt[:, t, c0:c1], pb[:, 0:c1 - c0]), reads=[pb], writes=[Vt])
                k.barrier()
            bg = sb("bgain", [128, D], F32)
            k.dma(bg[:], P["b_norm"][0].partition_broadcast(128), writes=[bg])
            wB = sb("wB", [128, 8, D], BF16)
            xTg = sb("xTg", [128, 8, 512], BF16)
            qT = [sb("qT%d" % i, [128, 6, 512], BF16) for i in range(2)]
            mqT = [sb("mqTB%d" % i, [128, 2, 512], BF16) for i in range(3)]
            mixTg = [sb("mixTg%d" % i, [128, 8, 512], BF16) for i in range(2)]
            Eb = [sb("Eb%d" % i, [128, 512], F32) for i in range(3)]
            spb = [sb("spb%d" % i, [128, 512], BF16) for i in range(3)]
            eab = [sb("eab%d" % i, [128, 512], F32) for i in range(2)]
            ab = [sb("ab%d" % i, [128, 512], BF16) for i in range(2)]
            MW = self.mem_work(st)
            mmB = sb("mmB", [128, 256], F32)
            NGEb = self.cstb[:, 5, :]
            NLTb = self.cstb[:, 6, :]
            strictTb = self.cstb[:, 3, :]
            cstb = self.cstb
            PZ = [self.ps[0], self.ps[1]]
            PC = [self.ps[2], self.ps[3]]
            PO = [self.ps[4], self.ps[5]]
            rot = [0]

            def nb():
                rot[0] = (rot[0] + 1) % 2
                return self.ps[6 + rot[0]]
            self.nextbank = nb
            wcur = [None]

            def load_wB(which):
                if wcur[0] != which:
                    srcd = win_d if which == "in" else wout_d
                    k.dma(wB[:], srcd.rearrange("(c p) n -> p c n", p=128), reads=[wdb[0 if which == "in" else 1]], writes=[wB])
                    wcur[0] = which

            def prologue(g):
                load_wB("in")
                qTg, mqTg = qT[g % 2], mqT[g % 3]
                h = ht[0]
                for tt in range(4):
                    t = g * 4 + tt
                    k.dma(h[:], hin[0][t * 128:(t + 1) * 128, :], reads=[hin[1][t]], writes=[h])
                    self.rmsnorm(h, bg, xn, xn, stt[0])
                    yield
                    self.transpose8(xn, [(xTg, lambda half: xTg[:, half * 4:(half + 1) * 4, tt * 128:(tt + 1) * 128])], nb(), nb())
                    yield
                for fc in range(8):
                    pb = nb()
                    for dc in range(8):
                        k.op("pe", lambda e: e.matmul(pb[:], wB[:, dc, fc * 128:(fc + 1) * 128], xTg[:, dc, :], start=(dc == 0), stop=(dc == 7)),
                             reads=[wB, xTg], writes=[pb], inc=(dc == 7))
                    if fc < 6:
                        k.op("act", lambda e: e.mul(qTg[:, fc, :], pb[:], 0.125), reads=[pb], writes=[qTg])
                    else:
                        k.op("dve", lambda e: e.tensor_copy(mqTg[:, fc - 6, :], pb[:]), reads=[pb], writes=[mqTg])
                    yield

            def epilogue(g):
                load_wB("out")
                mixg, mqTg = mixTg[g % 2], mqT[g % 3]
                h = ht[1]
                for tt in range(4):
                    t = g * 4 + tt
                    k.dma(h[:], hin[0][t * 128:(t + 1) * 128, :], reads=[hin[1][t]], writes=[h])
                    yield from self.mem_attend_g(MW, mqTg, tt * 128, memkT, memv, mmB, col0=0)
                    yield
                    pb = nb()
                    for j in range(2):
                        k.op("pe", lambda e: e.transpose(pb[:, j * 128:(j + 1) * 128], mmB[:, j * 128:(j + 1) * 128], self.ident),
                             reads=[mmB, self.cst], writes=[pb], inc=(j == 1))
                    k.op("act", lambda e: e.copy(mixg[:, 6:8, tt * 128:(tt + 1) * 128], pb[:, 0:256].rearrange("p (c t) -> p c t", c=2)),
                         reads=[pb], writes=[mixg])
                    yield
                    for half in range(2):
                        pb = nb()
                        for fc in range(8):
                            k.op("pe", lambda e: e.matmul(pb[:], mixg[:, fc, tt * 128:(tt + 1) * 128], wB[:, fc, half * 512:(half + 1) * 512],
                                                          start=(fc == 0), stop=(fc == 7)),
                                 reads=[mixg, wB], writes=[pb], inc=(fc == 7))
                        k.op("dve", lambda e: e.tensor_tensor(h[:, half * 512:(half + 1) * 512], h[:, half * 512:(half + 1) * 512], pb[:], ALU.add),
                             reads=[h, pb], writes=[h])
                        yield
                    k.dma(hout[0][t * 128:(t + 1) * 128, :], h[:], reads=[h], writes=[hout[1][t]])

            def side_thread(g):
                if g >= 1:
                    yield from epilogue(g - 1)
                if g + 1 < NGB:
                    yield from prologue(g + 1)

            for _ in prologue(0):
                pass
            for g in range(NGB):
                qTg, mixg = qT[g % 2], mixTg[g % 2]
                side = side_thread(g)
                items = [(2 * p + s, kb) for p in range(NHB // 2) for kb in range(4 * g + 3, -1, -1) for s in range(2)]

                def geom(i):
                    hh, kb = items[i]
                    r = max(kb - 4 * g, 0)
                    return hh, kb, hh // 2, hh % 2, r * 128, kb >= 4 * g

                def s1_pe(i):
                    hh, kb, fc, s, c0, diag = geom(i)
                    ps_ = slice(s * 64, (s + 1) * 64)
                    cs = slice(c0, 512)
                    pz = PZ[i % 2]
                    k.op("pe", lambda e: e.matmul(pz[:, cs], KT[ps_, fc, kb * 128:(kb + 1) * 128], qTg[ps_, fc, cs], start=True, stop=True),
                         reads=[KT, qTg], writes=[pz])

                def s1_act(i):
                    hh, kb, fc, s, c0, diag = geom(i)
                    cs = slice(c0, 512)
                    pz, E, sp = PZ[i % 2], Eb[i % 3], spb[i % 3]
                    k.op("act", lambda e: e.activation(E[:, cs], pz[:, cs], AF.Exp), reads=[pz], writes=[E])
                    k.op("act", lambda e: e.activation(sp[:, cs], E[:, cs], AF.Ln, bias=1.0), reads=[E], writes=[sp])
                    if diag:
                        k.op("dve", lambda e: e.tensor_tensor(sp[:, c0:c0 + 128], sp[:, c0:c0 + 128], strictTb, ALU.mult),
                             reads=[sp, cstb], writes=[sp])

                def s2_peA(i):
                    hh, kb, fc, s, c0, diag = geom(i)
                    cs = slice(c0, 512)
                    C, sp = PC[s], spb[i % 3]
                    if kb == 4 * g + 3:
                        k.op("dve", lambda e: e.memset(C[:], 0.0), writes=[C])
                    k.op("pe", lambda e: e.matmul(C[:, cs], NGEb, sp[:, cs], start=False, stop=False, skip_group_check=True),
                         reads=[cstb, sp], writes=[C])

                def s2_act(i):
                    hh, kb, fc, s, c0, diag = geom(i)
                    cs = slice(c0, 512)
                    C, ea_ = PC[s], eab[i % 2]
                    k.op("act", lambda e: e.activation(ea_[:, cs], C[:, cs], AF.Exp), reads=[C], writes=[ea_])

                def s2_peB(i):
                    hh, kb, fc, s, c0, diag = geom(i)
                    cs = slice(c0, 512)
                    C, sp = PC[s], spb[i % 3]
                    if kb > 0:
                        k.op("pe", lambda e: e.matmul(C[:, cs], NLTb, sp[:, cs], start=False, stop=False, skip_group_check=True),
                             reads=[cstb, sp], writes=[C])

                def s3_pool(i):
                    hh, kb, fc, s, c0, diag = geom(i)
                    cs = slice(c0, 512)
                    E, ea_, a_ = Eb[i % 3], eab[i % 2], ab[i % 2]
                    k.op("dve", lambda e: e.tensor_tensor(a_[:, cs], E[:, cs], ea_[:, cs], ALU.mult), reads=[E, ea_], writes=[a_])
                    if diag:
                        k.op("dve", lambda e: e.tensor_tensor(a_[:, c0:c0 + 128], a_[:, c0:c0 + 128], strictTb, ALU.mult),
                             reads=[a_, cstb], writes=[a_])

                def s3_pe(i):
                    hh, kb, fc, s, c0, diag = geom(i)
                    cs = slice(c0, 512)
                    a_ = ab[i % 2]
                    po = PO[fc % 2]
                    if kb == 4 * g + 3 and s == 0:
                        k.op("dve", lambda e: e.memset(po[:], 0.0), writes=[po])
                    vblk = Vt[:, kb, hh * 64:(hh + 1) * 64]
                    if s == 0:
                        k.op("pe", lambda e: e.matmul(po[0:64, cs], vblk, a_[:, cs], start=False, stop=False, skip_group_check=True),
                             reads=[Vt, a_], writes=[po])
                    else:
                        k.op("pe", lambda e: e.matmul(po[64:128, cs], vblk, a_[:, cs], start=False, stop=False, skip_group_check=True,
                                                      tile_position=(0, 64)), reads=[Vt, a_], writes=[po])
                    if kb == 0 and s == 1:
                        k.op("act", lambda e: e.copy(mixg[:, fc, :], po[:]), reads=[po], writes=[mixg])

                n_it = len(items)
                ok = lambda i: 0 <= i < n_it
                dumrhs = cstb[:, 0:4, :].rearrange("p c t -> p (c t)")
                stride = max(1, n_it // 72)
                for step in range(-3, n_it + 1):
                    i0, i1, i2_, i3, i4 = step + 3, step + 2, step + 1, step, step - 1
                    if ok(i3):
                        s3_pool(i3)
                    if ok(i0):
                        for _d in range(NDUM):
                            k.op("pe", lambda e: e.matmul(PZ[i0 % 2][:], NGEb, dumrhs, start=True, stop=True),
                                 reads=[cstb], writes=[PZ[i0 % 2]], inc=False)
                        s1_pe(i0)
                    if ok(i2_):
                        s2_peA(i2_)
                    if ok(i4):
                        s3_pe(i4)
                    if ok(i1):
                        s1_act(i1)
                    if ok(i2_):
                        s2_act(i2_)
                    if ok(i3):
                        s2_peB(i3)
                    if side is not None and step >= 0 and step % stride == 0:
                        try:
                            next(side)
                        except StopIteration:
                            side = None
                if side is not None:
                    for _ in side:
                        pass
            for _ in epilogue(NGB - 1):
                pass
            k.barrier()
            del self.nextbank

    def build(self, phases):
        nc, k = self.nc, self.k
        P = self.P = {}
        shapes = dict(
            x=[S, D], mem=[256, D], a_norm=[1, D], a_w_in=[1, D, 3340], a_conv=[1, 4, 2304],
            a_log=[1, 6], a_dt_bias=[1, 6], a_out_gain=[1, 128], a_w_out=[1, D, D],
            kv_norm=[D], w_kv=[D, 1536], b_norm=[1, D], b_w_in=[1, D, D], b_w_out=[1, D, D],
            mem_norm=[2, D], w_mem_kv=[2, D, 512], ffn_norm=[2, D], w_group=[2, D, 4], b_group=[2, 4],
            w_router=[2, D, 16], b_router=[2, 16], w1=[2, 16, D, 256], w3=[2, 16, D, 256],
            w2=[2, 16, 256, D], final_norm=[D], wgr=[2, 128, 8, 20], rbias=[2, 20], convw=[128, 18, 4])
        for n, s in shapes.items():
            P[n] = self.din(n, s)
        out = nc.dram_tensor("out", [S, D], F32, kind="ExternalOutput").ap()
        mkbufs = lambda nm: [Buf(None, "%s%d" % (nm, i)) for i in range(NT)]
        hx = (P["x"], mkbufs("x"))
        hA = (self.dscratch("hA", [S, D]), mkbufs("hA"))
        hB = (self.dscratch("hB", [S, D]), mkbufs("hB"))
        ho = (out, mkbufs("out"))
        with ExitStack() as gst:
            self.load_consts(gst)
            self.epsbuf = self.sbuf(gst, "epsb", [128, 1], F32)
            self.epsb = self.epsbuf
            k.op("dve", lambda e: e.memset(self.epsbuf[:], EPS), writes=[self.epsbuf])
            cur = hx
            seq = {"A": hA, "M0": hB, "B": hA, "M1": ho}
            for ph in phases:
                dst = seq[ph] if ph != phases[-1] else ho
                if ph == "M0":
                    self.moe_phase(0, cur, dst, final=False)
                elif ph == "M1":
                    self.moe_phase(1, cur, dst, final=True)
                elif ph == "A":
                    self.mixer_a_phase(cur, dst)
                elif ph == "B":
                    self.mixer_b_phase(cur, dst)
                cur = dst
            for b in ho[1]:
                if b.lw is not None:
                    k._wait("sp", b.lw)
        return nc


def make_consts():
    c = np.zeros((128, 8, 128), np.float32)
    i = np.arange(128)
    c[:, 0, :] = np.eye(128)
    c[:, 1, :] = (i[:, None] <= i[None, :])
    c[:, 2, :] = (i[:, None] > i[None, :])
    c[:, 3, :] = (i[:, None] < i[None, :])
    c[:, 4, :] = 1.0
    c[:, 5, :] = -(i[:, None] >= i[None, :]).astype(np.float32)
    c[:, 6, :] = -(i[:, None] < i[None, :]).astype(np.float32)
    c[:, 7, :] = -30000.0 * (i[:, None] >= i[None, :])
    return c


_CACHE = {}


def run(inputs, phases=("A", "M0", "B", "M1"), ncores=NCORES, trace=False):
    key = tuple(phases)
    if key not in _CACHE:
        mk = MK(phases)
        _CACHE[key] = mk.build(list(phases))
    nc = _CACHE[key]
    consts = make_consts()
    inputs = dict(inputs)
    wg = np.concatenate([np.asarray(inputs["w_group"]), np.asarray(inputs["w_router"])], axis=2)
    inputs["wgr"] = np.ascontiguousarray(wg.reshape(2, 8, 128, 20).transpose(0, 2, 1, 3))
    inputs["rbias"] = np.concatenate([np.asarray(inputs["b_group"]), np.asarray(inputs["b_router"])], axis=1)
    cw = np.asarray(inputs["a_conv"])[0]
    inputs["convw"] = np.ascontiguousarray(cw.reshape(4, 18, 128).transpose(2, 1, 0))
    in_maps = []
    for c in range(ncores):
        m = {"consts": consts}
        for n, v in inputs.items():
            v = np.asarray(v)
            if n in ("x", "mem"):
                m[n] = np.ascontiguousarray(v[c])
            else:
                m[n] = np.ascontiguousarray(v, dtype=np.float32)
        in_maps.append(m)
    res = run_bass_kernel_spmd(nc, in_maps, core_ids=list(range(ncores)), trace=trace)
    outs = np.stack([r["out"] for r in res.results], axis=0)
    return outs, res


def kernel(**inputs):
    outs, _ = run(inputs)
    return outs.astype(np.float32)
```

```python
from contextlib import ExitStack
import os
import numpy as np
import concourse.bass as bass
import concourse.mybir as mybir
from concourse.bass_utils import run_bass_kernel_spmd

F32 = mybir.dt.float32
BF16 = mybir.dt.bfloat16
AF = mybir.ActivationFunctionType
ALU = mybir.AluOpType
AX = mybir.AxisListType

S = 4096
D = 1024
NT = S // 128
EPS = 1e-6
NCORES = 8


class Buf:
    __slots__ = ("ap", "name", "_lw", "_rd", "_excl")
    lw = property(lambda self: self._lw, lambda self, v: setattr(self, "_lw", v))
    rd = property(lambda self: self._rd, lambda self, v: setattr(self, "_rd", v))
    excl = property(lambda self: self._excl, lambda self, v: setattr(self, "_excl", v))

    def __init__(self, ap, name="", excl=False):
        self.ap = ap
        self.name = name
        self.excl = excl
        self.lw = None
        self.rd = []

    def __getitem__(self, idx):
        return self.ap[idx]


class View(Buf):
    __slots__ = ("parent",)

    def __init__(self, parent, ap):
        self.parent = parent
        self.ap = ap
        self.name = parent.name

    lw = property(lambda self: self.parent.lw, lambda self, v: setattr(self.parent, "lw", v))
    rd = property(lambda self: self.parent.rd, lambda self, v: setattr(self.parent, "rd", v))
    excl = property(lambda self: self.parent.excl, lambda self, v: None)


class K:
    NDMA = 48

    def __init__(self, nc):
        self.nc = nc
        self.eng = {"pe": nc.tensor, "act": nc.scalar, "dve": nc.vector,
                    "pool": nc.gpsimd, "sp": nc.sync}
        self.sem = {e: nc.alloc_semaphore("s_" + e) for e in ("pe", "act", "dve", "pool")}
        self.cnt = {e: 0 for e in self.sem}
        self.waited = {}
        self.dsem = [nc.alloc_semaphore("d%d" % i) for i in range(self.NDMA)]
        self.dcnt = [0] * self.NDMA
        self.dnext = 0
        self.nins = 0

    def _semh(self, key):
        return self.sem[key] if isinstance(key, str) else self.dsem[key]

    def _wait(self, e, dep):
        key, val = dep
        if key == e and e == "pe":
            return
        w = self.waited.get((e, key), 0)
        if w >= val:
            return
        self.eng[e].wait_ge(self._semh(key), val)
        self.nins += 1
        self.waited[(e, key)] = val

    def _deps(self, e, reads, writes):
        best = {}
        for r in reads:
            if r.lw is not None:
                if best.get(r.lw[0], 0) < r.lw[1]:
                    best[r.lw[0]] = r.lw[1]
            if r.excl:
                for key, val in r.rd:
                    if key != e and best.get(key, 0) < val:
                        best[key] = val
        for w in writes:
            if w.lw is not None:
                if best.get(w.lw[0], 0) < w.lw[1]:
                    best[w.lw[0]] = w.lw[1]
            for key, val in w.rd:
                if best.get(key, 0) < val:
                    best[key] = val
        for key, val in best.items():
            self._wait(e, (key, val))

    def _mark(self, tag, reads, writes):
        for r in reads:
            r.rd.append(tag)
            if len(r.rd) > 64:
                best = {}
                for key, val in r.rd:
                    if best.get(key, 0) < val:
                        best[key] = val
                r.rd = list(best.items())
        for w in writes:
            w.lw = tag
            w.rd = []

    def op(self, e, fn, reads=(), writes=(), inc=True):
        self._deps(e, reads, writes)
        ins = fn(self.eng[e])
        self.nins += 1
        if inc:
            ins.then_inc(self.sem[e], 1)
            self.cnt[e] += 1
            tag = (e, self.cnt[e])
        else:
            tag = (e, self.cnt[e] + 1)
        self._mark(tag, reads, writes)
        return ins

    def dma(self, out, in_, reads=(), writes=(), q="sp", **kw):
        slot = self.dnext
        self.dnext = (self.dnext + 1) % self.NDMA
        if self.dcnt[slot] > 0:
            self._wait(q, (slot, 16 * self.dcnt[slot]))
        self._deps(q, reads, writes)
        ins = self.eng[q].dma_start(out=out, in_=in_, **kw)
        self.nins += 1
        ins.then_inc(self.dsem[slot], 16)
        self.dcnt[slot] += 1
        tag = (slot, 16 * self.dcnt[slot])
        self._mark(tag, reads, writes)
        return tag

    def barrier(self):
        for e in ("pe", "act", "dve", "pool", "sp"):
            for e2 in ("pe", "act", "dve", "pool"):
                if e2 != e and self.cnt[e2] > 0:
                    self._wait(e, (e2, self.cnt[e2]))
            for slot in range(self.NDMA):
                if self.dcnt[slot] > 0:
                    self._wait(e, (slot, 16 * self.dcnt[slot]))


class MK:
    def __init__(self, phases, h0_from_input=True):
        self.nc = nc = bass.Bass("TRN2", target_bir_lowering=False)
        self.k = K(nc)
        self.uid = 0
        self.ins = {}
        self.ps = [Buf(nc.alloc_psum_tensor("psb%d" % i, [128, 512], F32).ap(), "ps%d" % i, excl=True)
                   for i in range(8)]

    def din(self, name, shape):
        ap = self.nc.dram_tensor(name, list(shape), F32, kind="ExternalInput").ap()
        self.ins[name] = ap
        return ap

    def dscratch(self, name, shape, dt=F32):
        return self.nc.dram_tensor(name, list(shape), dt, kind="Internal").ap()

    def sbuf(self, st, name, shape, dt):
        self.uid += 1
        h = st.enter_context(self.nc.sbuf_tensor("%s_%d" % (name, self.uid), list(shape), dt))
        return Buf(h.ap(), name)

    def load_consts(self, st):
        k = self.k
        c = self.din("consts", [128, 8, 128])
        self.cst = self.sbuf(st, "cst", [128, 8, 128], F32)
        k.dma(self.cst[:], c, writes=[self.cst])
        self.ident = self.cst[:, 0, :]
        self.U = self.cst[:, 1, :]
        self.SL = self.cst[:, 2, :]
        self.strictT = self.cst[:, 3, :]
        self.ones = self.cst[:, 4, :]
        self.cstb = self.sbuf(st, "cstb", [128, 8, 128], BF16)
        k.dma(self.cstb[:], c, writes=[self.cstb], q="pool")
        self.identb = self.cstb[:, 0, :]
        self.onesb = self.cstb[:, 4, :]

    def rmsnorm(self, h, gainb, xn, junk, st2):
        k = self.k
        k.op("act", lambda e: e.activation(junk[:], h[:], AF.Square, accum_out=st2[:, 0:1]),
             reads=[h], writes=[junk, st2])
        k.op("act", lambda e: e.activation(st2[:, 1:2], st2[:, 0:1], AF.Ln, bias=self.epsb[:, 0:1], scale=1.0 / D),
             reads=[st2, self.epsbuf], writes=[st2])
        k.op("act", lambda e: e.activation(st2[:, 2:3], st2[:, 1:2], AF.Exp, scale=-0.5), reads=[st2], writes=[st2])
        k.op("dve", lambda e: e.scalar_tensor_tensor(xn[:], h[:], st2[:, 2:3], gainb[:], ALU.mult, ALU.mult),
             reads=[h, st2, gainb], writes=[xn])

    def transpose8(self, src, dsts, psa, psb, evac=("act", "dve"), second="pool"):
        k = self.k
        for half, ps in enumerate((psa, psb)):
            for j in range(4):
                c = half * 4 + j
                k.op("pe", lambda e: e.transpose(ps[:, j * 128:(j + 1) * 128], src[:, c * 128:(c + 1) * 128], self.ident),
                     reads=[src, self.cst], writes=[ps], inc=(j == 3))
            dbuf, fn = dsts[0]
            eng = evac[half % len(evac)]
            pv = ps[:].rearrange("p (c t) -> p c t", c=4)
            if eng == "act":
                k.op("act", lambda e: e.copy(fn(half), pv), reads=[ps], writes=[dbuf])
            else:
                k.op(eng, lambda e: e.tensor_copy(fn(half), pv), reads=[ps], writes=[dbuf])
            for dbuf2, fn2 in dsts[1:]:
                k.op(second, lambda e: e.tensor_copy(fn2(half), fn(half)), reads=[dbuf], writes=[dbuf2])

    def moe_phase(self, l, hin, hout, final=False, out_ap=None):
        nc, k = self.nc, self.k
        G = 1024
        NTG = G // 128
        NG = S // G
        NTB = G // 512
        NEX = 16
        P = self.P
        with ExitStack() as st:
            sb = lambda n, s, d: self.sbuf(st, n, s, d)
            gain = sb("gain", [128, D], F32)
            k.dma(gain[:], P["ffn_norm"][l].partition_broadcast(128), writes=[gain])
            if final:
                fgain = sb("fgain", [128, D], F32)
                k.dma(fgain[:], P["final_norm"].partition_broadcast(128), writes=[fgain])
            wgr = sb("wgr", [128, 8, 20], F32)
            k.dma(wgr[:], P["wgr"][l], writes=[wgr])
            rb = sb("rbias", [128, 20], F32)
            k.dma(rb[:], P["rbias"][l].partition_broadcast(128), writes=[rb])
            xnT = [sb("xnT%d" % i, [128, 8, G], BF16) for i in range(2)]
            yacc = [[sb("yacc%d_%d" % (j, i), [128, D], F32) for i in range(NTG)] for j in range(2)]
            comb = [[sb("comb%d_%d" % (j, i), [128, 16], F32) for i in range(NTG)] for j in range(2)]
            w1b = [sb("w1b%d" % i, [128, 8, 256], BF16) for i in range(2)]
            w3b = [sb("w3b%d" % i, [128, 8, 256], BF16) for i in range(2)]
            w2b = [sb("w2b%d" % i, [128, 2, D], BF16) for i in range(2)]
            ht = [sb("ht%d" % i, [128, D], F32) for i in range(2)]
            htc = [sb("htc%d" % i, [128, D], F32) for i in range(2)]
            xn = [sb("xn%d" % i, [128, D], F32) for i in range(2)]
            xnT32 = [sb("xnT32_%d" % i, [128, 8, 128], F32) for i in range(2)]
            stt = [sb("stt%d" % i, [128, 4], F32) for i in range(2)]
            sttc = [sb("sttc%d" % i, [128, 4], F32) for i in range(2)]
            rt = [sb("rt%d" % i, [128, 96], F32) for i in range(2)]
            hid = [sb("hid%d" % i, [128, 2, 512], BF16) for i in range(2)]
            sil = [sb("sil%d" % i, [128, 512], F32) for i in range(2)]
            ps = self.ps
            w1d, w3d, w2d = P["w1"], P["w3"], P["w2"]

            def load_w(e, slot):
                k.dma(w1b[slot][:], w1d[l, e].rearrange("(c p) f -> p c f", p=128), writes=[w1b[slot]], q="pool")
                k.dma(w3b[slot][:], w3d[l, e].rearrange("(c p) f -> p c f", p=128), writes=[w3b[slot]], q="pool")
                k.dma(w2b[slot][:], w2d[l, e].rearrange("(c p) n -> p c n", p=128), writes=[w2b[slot]], q="pool")

            def stage_a(g, tis=None, banks=None):
                par = g % 2
                xT = xnT[par]
                for ti in (range(NTG) if tis is None else tis):
                    t = g * NTG + ti
                    b = ti % 2
                    h = ht[b]
                    k.dma(h[:], hin[0][t * 128:(t + 1) * 128, :], reads=[hin[1][t]], writes=[h])
                    self.rmsnorm(h, gain, xn[b], xn[b], stt[b])
                    yield
                    x32 = xnT32[b]
                    self.transpose8(
                        xn[b],
                        [(x32, lambda half: x32[:, half * 4:(half + 1) * 4, :]),
                         (xT, lambda half: xT[:, half * 4:(half + 1) * 4, ti * 128:(ti + 1) * 128])],
                        ps[6] if banks is None else banks[0], ps[7] if banks is None else banks[1])
                    yield
                    pr = ps[6 + (ti % 2)] if banks is None else banks[2]
                    for dc in range(8):
                        k.op("pe", lambda e: e.matmul(pr[:, 0:20], x32[:, dc, :], wgr[:, dc, :], start=(dc == 0), stop=(dc == 7)),
                             reads=[x32, wgr], writes=[pr], inc=(dc == 7))
                    yield
                    r = rt[b]
                    R = lambda a, n: r[:, a:a + n]
                    lg, gmax, ngmax, oh, ge, gsum, pg = R(0, 20), R(20, 1), R(21, 1), R(22, 4), R(26, 4), R(30, 1), R(31, 1)
                    tmp, elsel, m1, nm1, ee, mask1, ee2 = R(32, 16), R(48, 4), R(52, 1), R(53, 1), R(54, 4), R(58, 4), R(62, 4)
                    v2, mask2, den, rden, wl, scl = R(66, 1), R(67, 4), R(71, 1), R(72, 1), R(73, 4), R(77, 1)
                    dv = lambda fn, rd=(), wr=(): k.op("dve", fn, reads=[r] + list(rd), writes=[r] + list(wr))
                    dv(lambda e: e.tensor_tensor(lg, pr[:, 0:20], rb[:], ALU.add), rd=[pr, rb])
                    dv(lambda e: e.tensor_reduce(gmax, lg[:, 0:4], AX.X, ALU.max))
                    dv(lambda e: e.tensor_single_scalar(ngmax, gmax, -1.0, ALU.mult))
                    dv(lambda e: e.tensor_scalar(oh, lg[:, 0:4], gmax, None, ALU.is_equal))
                    yield
                    k.op("act", lambda e: e.activation(ge, lg[:, 0:4], AF.Exp, bias=ngmax, accum_out=gsum), reads=[r], writes=[r])
                    dv(lambda e: e.reciprocal(pg, gsum))
                    dv(lambda e: e.tensor_tensor(tmp.rearrange("p (g j) -> p g j", g=4),
                                                 lg[:, 4:20].rearrange("p (g j) -> p g j", g=4),
                                                 oh.unsqueeze(2).to_broadcast([128, 4, 4]), ALU.mult))
                    dv(lambda e: e.tensor_reduce(elsel, tmp.rearrange("p (g j) -> p j g", g=4), AX.X, ALU.add))
                    yield
                    dv(lambda e: e.tensor_reduce(m1, elsel, AX.X, ALU.max))
                    dv(lambda e: e.tensor_single_scalar(nm1, m1, -1.0, ALU.mult))
                    k.op("act", lambda e: e.activation(ee, elsel, AF.Exp, bias=nm1), reads=[r], writes=[r])
                    dv(lambda e: e.tensor_scalar(mask1, elsel, m1, None, ALU.is_equal))
                    yield
                    dv(lambda e: e.scalar_tensor_tensor(ee2, mask1, -2.0, ee, ALU.mult, ALU.add))
                    dv(lambda e: e.tensor_reduce(v2, ee2, AX.X, ALU.max))
                    dv(lambda e: e.tensor_scalar(mask2, ee2, v2, None, ALU.is_equal))
                    dv(lambda e: e.tensor_single_scalar(den, v2, 1.0, ALU.add))
                    yield
                    dv(lambda e: e.reciprocal(rden, den))
                    dv(lambda e: e.scalar_tensor_tensor(wl, mask2, v2, mask1, ALU.mult, ALU.add))
                    dv(lambda e: e.tensor_tensor(scl, pg, rden, ALU.mult))
                    dv(lambda e: e.tensor_scalar(wl, wl, scl, None, ALU.mult))
                    cb = comb[par][ti]
                    dv(lambda e: e.tensor_tensor(cb[:].rearrange("p (g j) -> p g j", g=4),
                                                 oh.unsqueeze(2).to_broadcast([128, 4, 4]),
                                                 wl.unsqueeze(1).to_broadcast([128, 4, 4]), ALU.mult), wr=[cb])
                    yield

            def stage_b(g):
                par = g % 2
                xT = xnT[par]
                work = [(ex, tb) for ex in range(NEX) for tb in range(NTB)]

                def up(idx):
                    ex, tb = work[idx]
                    slot = ex % 2
                    w1, w3 = w1b[slot], w3b[slot]
                    hd = hid[idx % 2]
                    for fc in range(2):
                        p1 = ps[fc]
                        p3 = ps[2 + fc]
                        for wsrc, pdst in ((w1, p1), (w3, p3)):
                            for dc in range(8):
                                k.op("pe", lambda e: e.matmul(pdst[:], wsrc[:, dc, fc * 128:(fc + 1) * 128], xT[:, dc, tb * 512:(tb + 1) * 512],
                                                              start=(dc == 0), stop=(dc == 7)),
                                     reads=[wsrc, xT], writes=[pdst], inc=(dc == 7))
                                if dc % 4 == 3:
                                    yield
                        sl = sil[fc]
                        k.op("act", lambda e: e.activation(sl[:], p1[:], AF.Silu), reads=[p1], writes=[sl])
                        k.op("dve", lambda e: e.tensor_tensor(hd[:, fc, :], sl[:], p3[:], ALU.mult), reads=[sl, p3], writes=[hd])

                def down(idx):
                    ex, tb = work[idx]
                    slot = ex % 2
                    w2 = w2b[slot]
                    hd = hid[idx % 2]
                    for tt in range(4):
                        ti = tb * 4 + tt
                        for half in range(2):
                            py = ps[4 + half]
                            for fc in range(2):
                                k.op("pe", lambda e: e.matmul(py[:], hd[:, fc, tt * 128:(tt + 1) * 128], w2[:, fc, half * 512:(half + 1) * 512],
                                                              start=(fc == 0), stop=(fc == 1)),
                                     reads=[hd, w2], writes=[py], inc=(fc == 1))
                            ya = yacc[par][ti]
                            cs = comb[par][ti][:, ex:ex + 1]
                            if ex == 0:
                                k.op("dve", lambda e: e.tensor_scalar(ya[:, half * 512:(half + 1) * 512], py[:], cs, None, ALU.mult),
                                     reads=[py, comb[par][ti]], writes=[ya])
                            else:
                                k.op("dve", lambda e: e.scalar_tensor_tensor(ya[:, half * 512:(half + 1) * 512], py[:], cs,
                                                                             ya[:, half * 512:(half + 1) * 512], ALU.mult, ALU.add),
                                     reads=[py, comb[par][ti], ya], writes=[ya])
                            yield
                    if tb == NTB - 1:
                        if ex + 2 < NEX:
                            load_w(ex + 2, slot)
                        elif g + 1 < NG:
                            load_w(ex + 2 - NEX, slot)

                def rr(*gens):
                    gens = [g_ for g_ in gens if g_ is not None]
                    while gens:
                        for g_ in list(gens):
                            try:
                                next(g_)
                                yield
                            except StopIteration:
                                gens.remove(g_)

                yield from up(0)
                for idx in range(len(work)):
                    nu = up(idx + 1) if idx + 1 < len(work) else None
                    if nu is not None:
                        next(nu)
                        yield
                    yield from rr(nu, down(idx))

            def stage_c(g, tis=None):
                par = g % 2
                for ti in (range(NTG) if tis is None else tis):
                    t = g * NTG + ti
                    b = ti % 2
                    h = htc[b]
                    ya = yacc[par][ti]
                    k.dma(h[:], hin[0][t * 128:(t + 1) * 128, :], reads=[hin[1][t]], writes=[h])
                    k.op("pool", lambda e: e.tensor_tensor(ya[:], h[:], ya[:], ALU.add), reads=[h, ya], writes=[ya])
                    yield
                    if final:
                        self.rmsnorm(ya, fgain, h, h, sttc[b])
                        k.dma(hout[0][t * 128:(t + 1) * 128, :], h[:], reads=[h], writes=[hout[1][t]])
                    else:
                        k.dma(hout[0][t * 128:(t + 1) * 128, :], ya[:], reads=[ya], writes=[hout[1][t]])
                    yield

            def run_group(bg, ag, cg):
                others = [[g_, per] for g_, per in ((ag, 4), (cg, 24)) if g_ is not None]
                step = 0
                b_alive = bg is not None
                while b_alive or others:
                    if b_alive:
                        try:
                            next(bg)
                        except StopIteration:
                            b_alive = False
                    for item in list(others):
                        if (not b_alive) or step % item[1] == 0:
                            try:
                                next(item[0])
                            except StopIteration:
                                others.remove(item)
                    step += 1

            load_w(0, 0)
            load_w(1, 1)
            ga = [stage_a(0, range(0, NTG, 2), (ps[0], ps[1], ps[2])), stage_a(0, range(1, NTG, 2), (ps[3], ps[4], ps[5]))]
            while ga:
                for g_ in list(ga):
                    try:
                        next(g_)
                    except StopIteration:
                        ga.remove(g_)
            for g in range(NG):
                run_group(stage_b(g), stage_a(g + 1) if g + 1 < NG else None, stage_c(g - 1) if g >= 1 else None)
            gc = [stage_c(NG - 1, range(0, NTG, 2)), stage_c(NG - 1, range(1, NTG, 2))]
            while gc:
                for g_ in list(gc):
                    try:
                        next(g_)
                    except StopIteration:
                        gc.remove(g_)
            k.barrier()

    def nextbank(self):
        self.pbi = (getattr(self, "pbi", -1) + 1) % 8
        return self.ps[self.pbi]

    def mem_kv(self, st, l):
        k, P = self.k, self.P
        sb = lambda n, s, d: self.sbuf(st, n, s, d)
        memkT = sb("memkT", [128, 2, 256], BF16)
        memv = sb("memv", [128, 2, 256], BF16)
        with ExitStack() as st2:
            sb2 = lambda n, s, d: self.sbuf(st2, n, s, d)
            g = sb2("mg", [128, D], F32)
            k.dma(g[:], P["mem_norm"][l].partition_broadcast(128), writes=[g])
            w = sb2("wmkv", [128, 8, 512], BF16)
            k.dma(w[:], P["w_mem_kv"][l].rearrange("(c p) n -> p c n", p=128), writes=[w], q="pool")
            mT = sb2("memnT", [128, 8, 256], BF16)
            junk = sb2("mjunk", [128, D], F32)
            for mt in range(2):
                h = sb2("mh%d" % mt, [128, D], F32)
                xn = sb2("mxn%d" % mt, [128, D], F32)
                stt = sb2("mst%d" % mt, [128, 4], F32)
                k.dma(h[:], P["mem"][mt * 128:(mt + 1) * 128, :], writes=[h])
                self.rmsnorm(h, g, xn, junk, stt)
                self.transpose8(xn, [(mT, lambda half: mT[:, half * 4:(half + 1) * 4, mt * 128:(mt + 1) * 128])],
                                self.nextbank(), self.nextbank())
            for j in range(2):
                pb = self.nextbank()
                for dc in range(8):
                    k.op("pe", lambda e: e.matmul(pb[:, 0:256], w[:, dc, j * 128:(j + 1) * 128], mT[:, dc, :],
                                                  start=(dc == 0), stop=(dc == 7)),
                         reads=[w, mT], writes=[pb], inc=(dc == 7))
                k.op("act", lambda e: e.copy(memkT[:, j, :], pb[:, 0:256]), reads=[pb], writes=[memkT])
            for mt in range(2):
                pb = self.nextbank()
                for dc in range(8):
                    k.op("pe", lambda e: e.matmul(pb[:, 0:256], mT[:, dc, mt * 128:(mt + 1) * 128], w[:, dc, 256:512],
                                                  start=(dc == 0), stop=(dc == 7)),
                         reads=[w, mT], writes=[pb], inc=(dc == 7))
                k.op("dve", lambda e: e.tensor_copy(memv[:, mt, :], pb[:, 0:256]), reads=[pb], writes=[memv])
            k.barrier()
        return memkT, memv

    def mem_attend(self, W, mqT, qoff, memkT, memv, mix, col0=768):
        for _ in self.mem_attend_g(W, mqT, qoff, memkT, memv, mix, col0):
            pass

    def mem_attend_g(self, W, mqT, qoff, memkT, memv, mix, col0=768, banks=None):
        k = self.k
        pe_ = W["pexp"]; ms = W["mstat"]; pT = W["pT"]
        if banks is None:
            banks = [self.nextbank(), self.nextbank()]
        for hh in range(4):
            pair, s = hh // 2, hh % 2
            pb = banks[s]
            k.op("pe", lambda e: e.matmul(pb[:, pair * 256:(pair + 1) * 256], mqT[s * 64:(s + 1) * 64, pair, qoff:qoff + 128],
                                          memkT[s * 64:(s + 1) * 64, pair, :], start=True, stop=True),
                 reads=[mqT, memkT], writes=[pb])
        for s in range(2):
            pb = banks[s]
            k.op("dve", lambda e: e.tensor_reduce(ms[:, s:s + 3:2], pb[:].rearrange("p (h m) -> p h m", h=2), AX.X, ALU.max),
                 reads=[pb], writes=[ms])
        k.op("dve", lambda e: e.tensor_single_scalar(ms[:, 4:8], ms[:, 0:4], -0.125, ALU.mult), reads=[ms], writes=[ms])
        yield
        for hh in range(4):
            pair, s = hh // 2, hh % 2
            pb = banks[s]
            k.op("act", lambda e: e.activation(pe_[:, hh, :], pb[:, pair * 256:(pair + 1) * 256], AF.Exp, bias=ms[:, 4 + hh:5 + hh],
                                               scale=0.125, accum_out=ms[:, 8 + hh:9 + hh]),
                 reads=[pb, ms], writes=[pe_, ms])
        k.op("dve", lambda e: e.reciprocal(ms[:, 12:16], ms[:, 8:12]), reads=[ms], writes=[ms])
        yield
        for half in range(2):
            pb = self.nextbank()
            for j in range(4):
                idx = half * 4 + j
                hh, mc = idx // 2, idx % 2
                k.op("pe", lambda e: e.transpose(pb[:, j * 128:(j + 1) * 128], pe_[:, hh, mc * 128:(mc + 1) * 128], self.ident),
                     reads=[pe_, self.cst], writes=[pb], inc=(j == 3))
            if half == 0:
                k.op("act", lambda e: e.copy(pT[:, 0:4, :], pb[:].rearrange("p (c t) -> p c t", c=4)), reads=[pb], writes=[pT])
            else:
                k.op("dve", lambda e: e.tensor_copy(pT[:, 4:8, :], pb[:].rearrange("p (c t) -> p c t", c=4)), reads=[pb], writes=[pT])
        yield
        pb = self.nextbank()
        for hh in range(4):
            for mc in range(2):
                k.op("pe", lambda e: e.matmul(pb[:, hh * 64:(hh + 1) * 64], pT[:, hh * 2 + mc, :], memv[:, mc, hh * 64:(hh + 1) * 64],
                                              start=(mc == 0), stop=(mc == 1)),
                     reads=[pT, memv], writes=[pb], inc=(mc == 1))
        k.op("dve", lambda e: e.tensor_tensor(mix[:, col0:col0 + 256].rearrange("p (h d) -> p h d", h=4),
                                              pb[:, 0:256].rearrange("p (h d) -> p h d", h=4),
                                              ms[:, 12:16].unsqueeze(2).to_broadcast([128, 4, 64]), ALU.mult),
             reads=[pb, ms], writes=[mix])

    def mem_work(self, st):
        sb = lambda n, s, d: self.sbuf(st, n, s, d)
        return {"pexp": sb("pexp", [128, 4, 256], F32), "mstat": sb("mstat", [128, 16], F32),
                "pT": sb("pT", [128, 8, 128], BF16)}

    def out_proj(self, mix, mixT, w_out, h, hn, dst_ap, dst_buf):
        k = self.k
        self.transpose8(mix, [(mixT, lambda half: mixT[:, half * 4:(half + 1) * 4, :])], self.nextbank(), self.nextbank())
        for half in range(2):
            pb = self.nextbank()
            for fc in range(8):
                k.op("pe", lambda e: e.matmul(pb[:], mixT[:, fc, :], w_out[:, fc, half * 512:(half + 1) * 512],
                                              start=(fc == 0), stop=(fc == 7)),
                     reads=[mixT, w_out], writes=[pb], inc=(fc == 7))
            k.op("dve", lambda e: e.tensor_tensor(hn[:, half * 512:(half + 1) * 512], h[:, half * 512:(half + 1) * 512], pb[:], ALU.add),
                 reads=[h, pb], writes=[hn])
        k.dma(dst_ap, hn[:], reads=[hn], writes=[dst_buf])

    def mixer_a_phase(self, hin, hout):
        nc, k, P = self.nc, self.k, self.P
        NTA = int(os.environ.get("MK_NTA", str(NT)))
        with ExitStack() as st:
            sb = lambda n, s, d: self.sbuf(st, n, s, d)
            memkT, memv = self.mem_kv(st, 0)
            gain = sb("gainA", [128, D], F32)
            k.dma(gain[:], P["a_norm"][0].partition_broadcast(128), writes=[gain])
            w_in = sb("w_inA", [128, 8, 3340], BF16)
            for c in range(8):
                k.dma(w_in[:, c, :], P["a_w_in"][0, c * 128:(c + 1) * 128, :], writes=[w_in], q="pool")
            w_out = sb("w_outA", [128, 8, D], BF16)
            k.dma(w_out[:], P["a_w_out"][0].rearrange("(c p) n -> p c n", p=128), writes=[w_out], q="pool")
            convw = sb("convw", [128, 18, 4], F32)
            k.dma(convw[:], P["convw"], writes=[convw])
            sc6 = sb("sc6", [128, 32], F32)
            k.dma(sc6[:, 0:6], P["a_log"][0].partition_broadcast(128), writes=[sc6])
            k.dma(sc6[:, 6:12], P["a_dt_bias"][0].partition_broadcast(128), writes=[sc6])
            k.op("act", lambda e: e.activation(sc6[:, 12:18], sc6[:, 0:6], AF.Exp), reads=[sc6], writes=[sc6])
            k.op("dve", lambda e: e.tensor_single_scalar(sc6[:, 12:18], sc6[:, 12:18], -1.0, ALU.mult), reads=[sc6], writes=[sc6])
            ogain = sb("ogain", [128, 128], F32)
            k.dma(ogain[:], P["a_out_gain"][0].partition_broadcast(128), writes=[ogain])
            MW = self.mem_work(st)
            pc = sb("pc", [128, 18, 131], F32)
            k.op("dve", lambda e: e.memset(pc[:], 0.0), writes=[pc])
            Sf = [sb("Sf%d" % h, [128, 128], F32) for h in range(6)]
            Sb = [sb("Sb%d" % h, [128, 128], BF16) for h in range(6)]
            for h in range(6):
                k.op("dve", lambda e: e.memset(Sf[h][:], 0.0), writes=[Sf[h]])
                k.op("pool", lambda e: e.memset(Sb[h][:], 0.0), writes=[Sb[h]])
            hA = sb("htA", [128, D], F32)
            xT = sb("xnTA", [128, 8, 128], BF16)
            cv = sb("cv", [128, 12, 128], F32)
            cvv = sb("cvv", [128, 6, 128], F32)
            ctmp = sb("ctmp", [128, 128], F32)
            cvjunk = View(cv, cv[:, 0:8, :].rearrange("p c t -> p (c t)"))
            sq = sb("sq", [128, 12, 128], BF16)
            rs = sb("rs", [128, 12, 128], F32)
            kn32 = sb("kn32", [128, 6, 128], F32)
            sttA = sb("sttA", [128, 4], F32)
            qkT = [sb("qkT%d" % i, [128, 6, 2, 128], BF16) for i in range(3)]
            sc = [sb("scA%d" % i, [128, 96], F32) for i in range(3)]
            gg = [sb("gg%d" % i, [128, 768], F32) for i in range(3)]
            mqT = [sb("mqTA%d" % i, [128, 2, 128], BF16) for i in range(3)]
            SLg = [sb("SLg%d" % i, [128, 128], F32) for i in range(6)]
            dm = sb("dm", [128, 6, 128], F32)
            dmi = sb("dmi", [128, 6, 128], F32)
            Wq = [[sb("W%d_%d" % (h, i), [128, 3, 128], BF16) for i in range(2)] for h in range(6)]
            Q0f = [sb("Q0f%d" % i, [128, 128], F32) for i in range(6)]
            kt = [sb("kt%d" % i, [128, 6, 128], BF16) for i in range(2)]
            vtok = [sb("vtok%d" % i, [128, 6, 128], F32) for i in range(2)]
            attnT = [sb("attnT%d" % i, [128, 6, 128], BF16) for i in range(2)]
            TT = [sb("TT%d" % i, [128, 6, 128], BF16) for i in range(2)]
            Rall = sb("Rall", [128, 6, 128], BF16)
            vnew = sb("vnewA", [128, 6, 128], BF16)
            oall = sb("oall", [128, 6, 128], F32)
            ost = sb("ost", [128, 24], F32)
            ojunk = sb("ojunk", [128, 128], F32)
            mix = sb("mixA", [128, D], F32)
            mixT = sb("mixTA", [128, 8, 128], BF16)
            hC = sb("htC", [128, D], F32)
            ident, U, SL, ones = self.ident, self.U, self.SL, self.ones
            NEGs = self.cst[:, 7, :]
            identb = self.identb
            cst, cstb = self.cst, self.cstb
            QS = float(128 ** -0.5)
            ctm = View(rs, rs[:, 0:9, :])
            rotA = [0]

            def nbA():
                rotA[0] = (rotA[0] + 1) % 6
                return self.ps[rotA[0]]
            self.nextbank = nbA
            membanks = [self.ps[6], self.ps[7]]

            def frontA(t):
                i3 = t % 3
                h = hA
                k.dma(h[:], hin[0][t * 128:(t + 1) * 128, :], reads=[hin[1][t]], writes=[h])
                self.rmsnorm(h, gain, h, cvjunk, sttA)
                yield
                self.transpose8(h, [(xT, lambda half: xT[:, half * 4:(half + 1) * 4, :])], self.nextbank(), self.nextbank())
                yield
                for g4 in range(5):
                    nf = 4 if g4 < 4 else 2
                    pb = self.nextbank()
                    for j in range(nf):
                        fc = g4 * 4 + j
                        for dc in range(8):
                            k.op("pe", lambda e: e.matmul(pb[:, j * 128:(j + 1) * 128], w_in[:, dc, fc * 128:(fc + 1) * 128], xT[:, dc, :],
                                                          start=(dc == 0), stop=(dc == 7)),
                                 reads=[w_in, xT], writes=[pb], inc=(dc == 7 and j == nf - 1))
                    dstv = pc[:, g4 * 4:g4 * 4 + nf, 3:131]
                    srcv = pb[:, 0:nf * 128].rearrange("p (c t) -> p c t", c=nf)
                    k.op("act", lambda e: e.copy(dstv, srcv), reads=[pb], writes=[pc])
                    yield
                pb = self.nextbank()
                for j in range(2):
                    for dc in range(8):
                        k.op("pe", lambda e: e.matmul(pb[:, j * 128:(j + 1) * 128], w_in[:, dc, 3084 + j * 128:3084 + (j + 1) * 128], xT[:, dc, :],
                                                      start=(dc == 0), stop=(dc == 7)),
                             reads=[w_in, xT], writes=[pb], inc=(dc == 7 and j == 1))
                k.op("act", lambda e: e.copy(mqT[i3][:], pb[:, 0:256].rearrange("p (c t) -> p c t", c=2)), reads=[pb], writes=[mqT[i3]])
                yield
                pg1 = self.nextbank()
                for dc in range(8):
                    k.op("pe", lambda e: e.matmul(pg1[:], xT[:, dc, :], w_in[:, dc, 2304:2816], start=(dc == 0), stop=(dc == 7)),
                         reads=[w_in, xT], writes=[pg1], inc=(dc == 7))
                pg2 = self.nextbank()
                for dc in range(8):
                    k.op("pe", lambda e: e.matmul(pg2[:, 0:268], xT[:, dc, :], w_in[:, dc, 2816:3084], start=(dc == 0), stop=(dc == 7)),
                         reads=[w_in, xT], writes=[pg2], inc=(dc == 7))
                g_g = gg[i3]
                k.op("act", lambda e: e.activation(g_g[:, 0:512], pg1[:], AF.Silu), reads=[pg1], writes=[g_g])
                k.op("act", lambda e: e.activation(g_g[:, 512:768], pg2[:, 0:256], AF.Silu), reads=[pg2], writes=[g_g])
                s_ = sc[i3]
                C = lambda a_, n=6: s_[:, a_:a_ + n]
                beta, tt_, ex_, sp_, g_, gcl, egc, negc, etl, egl, dd, eb_ = (C(0), C(6), C(12), C(18), C(24), C(32, 16), C(48), C(54), C(60), C(66), C(72), C(78))
                k.op("act", lambda e: e.activation(eb_, pg2[:, 256:262], AF.Exp, scale=-1.0), reads=[pg2], writes=[s_])
                k.op("dve", lambda e: e.tensor_tensor(tt_, pg2[:, 262:268], sc6[:, 6:12], ALU.add), reads=[pg2, sc6], writes=[s_])
                k.op("dve", lambda e: e.tensor_single_scalar(eb_, eb_, 1.0, ALU.add), reads=[s_], writes=[s_])
                k.op("dve", lambda e: e.reciprocal(beta, eb_), reads=[s_], writes=[s_])
                k.op("pool", lambda e: e.tensor_tensor(g_g[:].rearrange("p (h d) -> p h d", h=6), g_g[:].rearrange("p (h d) -> p h d", h=6),
                                                       ogain[:].unsqueeze(1).to_broadcast([128, 6, 128]), ALU.mult),
                     reads=[g_g, ogain], writes=[g_g])
                yield
                k.op("act", lambda e: e.activation(ex_, tt_, AF.Exp), reads=[s_], writes=[s_])
                k.op("act", lambda e: e.activation(sp_, ex_, AF.Ln, bias=1.0), reads=[s_], writes=[s_])
                k.op("dve", lambda e: e.tensor_tensor(g_, sp_, sc6[:, 12:18], ALU.mult), reads=[s_, sc6], writes=[s_])
                yield
                pgc = self.nextbank()
                k.op("pe", lambda e: e.matmul(pgc[:, 0:6], U, g_, start=True, stop=True), reads=[cst, s_], writes=[pgc])
                k.op("pe", lambda e: e.matmul(pgc[:, 8:14], ones, g_, start=True, stop=True), reads=[cst, s_], writes=[pgc])
                k.op("dve", lambda e: e.tensor_copy(gcl, pgc[:, 0:16]), reads=[pgc], writes=[s_])
                k.op("dve", lambda e: e.tensor_tensor(dd, s_[:, 40:46], s_[:, 32:38], ALU.subtract), reads=[s_], writes=[s_])
                yield
                k.op("act", lambda e: e.activation(egc, s_[:, 32:38], AF.Exp), reads=[s_], writes=[s_])
                k.op("act", lambda e: e.activation(etl, dd, AF.Exp), reads=[s_], writes=[s_])
                k.op("act", lambda e: e.activation(egl, s_[:, 40:46], AF.Exp), reads=[s_], writes=[s_])
                k.op("dve", lambda e: e.tensor_single_scalar(negc, egc, -1.0, ALU.mult), reads=[s_], writes=[s_])
                yield
                for (c0, nchk, dstb, dst) in ((0, 9, cv, cv[:, 0:9, :]), (9, 3, cv, cv[:, 9:12, :]), (12, 6, cvv, cvv[:, 0:6, :])):
                    wv = lambda j: convw[:, c0:c0 + nchk, j:j + 1].to_broadcast([128, nchk, 128])
                    k.op("dve", lambda e: e.tensor_tensor(dst, pc[:, c0:c0 + nchk, 0:128], wv(0), ALU.mult), reads=[pc, convw], writes=[dstb])
                    for j in range(1, 4):
                        tv = ctm[:, 0:nchk, :]
                        k.op("dve", lambda e: e.tensor_tensor(tv, pc[:, c0:c0 + nchk, j:j + 128], wv(j), ALU.mult), reads=[pc, convw], writes=[ctm])
                        k.op("dve", lambda e: e.tensor_tensor(dst, dst, tv, ALU.add), reads=[ctm, dstb], writes=[dstb])
                        yield
                k.op("pool", lambda e: e.tensor_copy(pc[:, :, 0:3], pc[:, :, 128:131]), reads=[pc], writes=[pc])
                k.op("act", lambda e: e.activation(cv[:], cv[:], AF.Silu), reads=[cv], writes=[cv])
                k.op("act", lambda e: e.activation(cvv[:], cvv[:], AF.Silu), reads=[cvv], writes=[cvv])
                qkv = cv
                k.op("act", lambda e: e.activation(sq[:], qkv[:, 0:12, :], AF.Square), reads=[qkv], writes=[sq])
                yield
                for g3 in range(3):
                    pb = self.nextbank()
                    k.op("pe", lambda e: e.matmul(pb[:], self.onesb, sq[:, g3 * 4:(g3 + 1) * 4, :], start=True, stop=True),
                         reads=[cstb, sq], writes=[pb])
                    k.op("act", lambda e: e.activation(rs[:, g3 * 4:(g3 + 1) * 4, :], pb[:].rearrange("p (c t) -> p c t", c=4), AF.Ln,
                                                       bias=self.epsb[:, 0:1]), reads=[pb, self.epsbuf], writes=[rs])
                    k.op("act", lambda e: e.activation(rs[:, g3 * 4:(g3 + 1) * 4, :], rs[:, g3 * 4:(g3 + 1) * 4, :], AF.Exp, scale=-0.5),
                         reads=[rs], writes=[rs])
                yield
                qk = qkT[i3]
                k.op("dve", lambda e: e.scalar_tensor_tensor(qk[:, :, 1, :], qkv[:, 0:6, :], QS, rs[:, 0:6, :], ALU.mult, ALU.mult),
                     reads=[qkv, rs], writes=[qk])
                k.op("dve", lambda e: e.tensor_tensor(kn32[:], qkv[:, 6:12, :], rs[:, 6:12, :], ALU.mult), reads=[qkv, rs], writes=[kn32])
                k.op("pool", lambda e: e.tensor_copy(qk[:, :, 0, :], kn32[:]), reads=[kn32], writes=[qk])
                yield
                b = t % 2
                s_etl = etl
                for grp in range(3):
                    pb = self.nextbank()
                    for j in range(4):
                        idx = grp * 4 + j
                        src = cvv[:, idx, :] if idx < 6 else kn32[:, idx - 6, :]
                        srcb = cvv if idx < 6 else kn32
                        k.op("pe", lambda e: e.transpose(pb[:, j * 128:(j + 1) * 128], src, ident), reads=[srcb, cst], writes=[pb], inc=(j == 3))
                    for j in range(4):
                        idx = grp * 4 + j
                        if idx < 6:
                            k.op("act", lambda e: e.copy(vtok[b][:, idx, :], pb[:, j * 128:(j + 1) * 128]), reads=[pb], writes=[vtok[b]])
                        else:
                            hh = idx - 6
                            k.op("dve", lambda e: e.tensor_scalar(kt[b][:, hh, :], pb[:, j * 128:(j + 1) * 128], s_etl[:, hh:hh + 1], None, ALU.mult),
                                 reads=[pb, s_], writes=[kt[b]])
                    yield

            def frontB(t):
                i3 = t % 3
                b = t % 2
                s_ = sc[i3]
                C = lambda a_, n=6: s_[:, a_:a_ + n]
                beta, g_ = C(0), C(24)
                qk = qkT[i3]
                for hh in range(6):
                    sg = SLg[hh]
                    k.op("dve", lambda e: e.tensor_scalar(sg[:], SL, g_[:, hh:hh + 1], None, ALU.mult), reads=[cst, s_], writes=[sg])
                yield
                for hp in range(3):
                    pb = self.nextbank()
                    for j in range(2):
                        hh = hp * 2 + j
                        sg = SLg[hh]
                        k.op("pe", lambda e: e.matmul(pb[:, j * 128:(j + 1) * 128], sg[:], U, start=True, stop=False),
                             reads=[sg, cst], writes=[pb], inc=False)
                        k.op("pe", lambda e: e.matmul(pb[:, j * 128:(j + 1) * 128], ident, NEGs, start=False, stop=True),
                             reads=[cst], writes=[pb])
                    k.op("act", lambda e: e.activation(dm[:, hp * 2:hp * 2 + 2, :], pb[:, 0:256].rearrange("p (c t) -> p c t", c=2), AF.Exp),
                         reads=[pb], writes=[dm])
                    yield
                k.op("pool", lambda e: e.tensor_tensor(dmi[:], dm[:], ident.unsqueeze(1).to_broadcast([128, 6, 128]), ALU.add),
                     reads=[dm, cst], writes=[dmi])
                for hh in range(6):
                    pb = self.nextbank()
                    W0 = Wq[hh][0]
                    qf = Q0f[hh]
                    k.op("pe", lambda e: e.matmul(pb[:, 0:256], qk[:, hh, 0, :], qk[:, hh, :, :].rearrange("p a t -> p (a t)"),
                                                  start=True, stop=True), reads=[qk], writes=[pb])
                    k.op("dve", lambda e: e.scalar_tensor_tensor(qf[:], pb[:, 0:128], beta[:, hh:hh + 1], dm[:, hh, :], ALU.mult, ALU.mult),
                         reads=[pb, s_, dm], writes=[qf])
                    k.op("dve", lambda e: e.tensor_tensor(attnT[b][:, hh, :], pb[:, 128:256], dmi[:, hh, :], ALU.mult),
                         reads=[pb, dmi], writes=[attnT[b]])
                    if hh % 3 == 2:
                        yield
                for hh in range(6):
                    W0 = Wq[hh][0]
                    qf = Q0f[hh]
                    k.op("pool", lambda e: e.tensor_copy(W0[:, 0, :], qf[:]), reads=[qf], writes=[W0])
                    k.op("pool", lambda e: e.tensor_tensor(Wq[hh][1][:, 1, :], ident, qf[:], ALU.subtract), reads=[cst, qf], writes=[Wq[hh][1]])
                    pb2 = self.nextbank()
                    k.op("pe", lambda e: e.transpose(pb2[:, 0:128], qf[:], ident), reads=[qf, cst], writes=[pb2])
                    k.op("act", lambda e: e.copy(W0[:, 2, :], pb2[:, 0:128]), reads=[pb2], writes=[W0])
                    if hh % 3 == 2:
                        yield
                for lvl in range(7):
                    for hh in range(6):
                        Wc = Wq[hh][lvl % 2]
                        Wn = Wq[hh][(lvl + 1) % 2]
                        pb = self.nextbank()
                        Qk, Xk, Pk = Wc[:, 0, :], Wc[:, 1, :], Wc[:, 2, :]
                        mm = lambda out, l_, r_, st_, sp_2, inc_: k.op(
                            "pe", lambda e: e.matmul(out, l_, r_, start=st_, stop=sp_2), reads=[Wc, cstb], writes=[pb], inc=inc_)
                        if lvl == 0:
                            mm(pb[:, 0:128], Pk, Qk, True, True, False)
                            mm(pb[:, 256:384], Qk, Pk, True, True, True)
                            k.op("act", lambda e: e.copy(Wn[:, 0, :], pb[:, 0:128]), reads=[pb], writes=[Wn])
                            k.op("act", lambda e: e.copy(Wn[:, 2, :], pb[:, 256:384]), reads=[pb], writes=[Wn])
                        elif lvl < 6:
                            mm(pb[:, 0:128], Pk, Qk, True, True, False)
                            mm(pb[:, 128:256], Pk, Xk, True, False, False)
                            mm(pb[:, 128:256], identb, Xk, False, True, False)
                            mm(pb[:, 256:384], Qk, Pk, True, True, True)
                            k.op("act", lambda e: e.copy(Wn[:], pb[:, 0:384].rearrange("p (c t) -> p c t", c=3)), reads=[pb], writes=[Wn])
                        else:
                            mm(pb[:, 128:256], Pk, Xk, True, False, False)
                            mm(pb[:, 128:256], identb, Xk, False, True, True)
                            k.op("act", lambda e: e.copy(TT[b][:, hh, :], pb[:, 128:256]), reads=[pb], writes=[TT[b]])
                        if hh % 3 == 2:
                            yield

            def back(t):
                i3 = t % 3
                b = t % 2
                s_ = sc[i3]
                C = lambda a_, n=6: s_[:, a_:a_ + n]
                beta, egc, negc, egl = C(0), C(48), C(54), C(66)
                qk, g_g = qkT[i3], gg[i3]
                k.dma(hC[:], hin[0][t * 128:(t + 1) * 128, :], reads=[hin[1][t]], writes=[hC])
                for hp in range(3):
                    pb = self.nextbank()
                    for j in range(2):
                        hh = hp * 2 + j
                        o = j * 256
                        k.op("pe", lambda e: e.matmul(pb[:, o:o + 128], qk[:, hh, 0, :], Sb[hh][:], start=True, stop=True),
                             reads=[qk, Sb[hh]], writes=[pb], inc=False)
                        k.op("pe", lambda e: e.matmul(pb[:, o + 128:o + 256], qk[:, hh, 1, :], Sb[hh][:], start=True, stop=True),
                             reads=[qk, Sb[hh]], writes=[pb], inc=(j == 1))
                    for j in range(2):
                        hh = hp * 2 + j
                        o = j * 256
                        k.op("dve", lambda e: e.scalar_tensor_tensor(Rall[:, hh, :], pb[:, o:o + 128], negc[:, hh:hh + 1], vtok[b][:, hh, :], ALU.mult, ALU.add),
                             reads=[pb, s_, vtok[b]], writes=[Rall])
                        k.op("dve", lambda e: e.tensor_scalar(oall[:, hh, :], pb[:, o + 128:o + 256], egc[:, hh:hh + 1], None, ALU.mult),
                             reads=[pb, s_], writes=[oall])
                yield
                for (h0, nh) in ((0, 4), (4, 2)):
                    pb = self.nextbank()
                    for j in range(nh):
                        hh = h0 + j
                        k.op("pe", lambda e: e.matmul(pb[:, j * 128:(j + 1) * 128], TT[b][:, hh, :], Rall[:, hh, :], start=True, stop=True),
                             reads=[TT[b], Rall], writes=[pb], inc=(j == nh - 1))
                    k.op("dve", lambda e: e.tensor_tensor(vnew[:, h0:h0 + nh, :], pb[:, 0:nh * 128].rearrange("p (c t) -> p c t", c=nh),
                                                          beta[:, h0:h0 + nh].unsqueeze(2).to_broadcast([128, nh, 128]), ALU.mult),
                         reads=[pb, s_], writes=[vnew])
                yield
                for hp in range(3):
                    pb = self.nextbank()
                    for j in range(2):
                        hh = hp * 2 + j
                        o = j * 256
                        k.op("pe", lambda e: e.matmul(pb[:, o:o + 128], attnT[b][:, hh, :], vnew[:, hh, :], start=True, stop=True),
                             reads=[attnT[b], vnew], writes=[pb], inc=False)
                        k.op("pe", lambda e: e.matmul(pb[:, o + 128:o + 256], kt[b][:, hh, :], vnew[:, hh, :], start=True, stop=True),
                             reads=[kt[b], vnew], writes=[pb], inc=(j == 1))
                    for j in range(2):
                        hh = hp * 2 + j
                        o = j * 256
                        k.op("dve", lambda e: e.scalar_tensor_tensor(Sb[hh][:], Sf[hh][:], egl[:, hh:hh + 1], pb[:, o + 128:o + 256], ALU.mult, ALU.add),
                             reads=[Sf[hh], s_, pb], writes=[Sb[hh]])
                        k.op("dve", lambda e: e.scalar_tensor_tensor(Sf[hh][:], Sf[hh][:], egl[:, hh:hh + 1], pb[:, o + 128:o + 256], ALU.mult, ALU.add),
                             reads=[Sf[hh], s_, pb], writes=[Sf[hh]])
                        k.op("dve", lambda e: e.tensor_tensor(oall[:, hh, :], oall[:, hh, :], pb[:, o:o + 128], ALU.add),
                             reads=[oall, pb], writes=[oall])
                yield
                for hh in range(6):
                    k.op("act", lambda e: e.activation(ojunk[:], oall[:, hh, :], AF.Square, accum_out=ost[:, hh:hh + 1]),
                         reads=[oall], writes=[ojunk, ost])
                k.op("act", lambda e: e.activation(ost[:, 8:14], ost[:, 0:6], AF.Ln, bias=self.epsb[:, 0:1], scale=1.0 / 128),
                     reads=[ost, self.epsbuf], writes=[ost])
                k.op("act", lambda e: e.activation(ost[:, 16:22], ost[:, 8:14], AF.Exp, scale=-0.5), reads=[ost], writes=[ost])
                yield
                for hh in range(6):
                    k.op("dve", lambda e: e.scalar_tensor_tensor(mix[:, hh * 128:(hh + 1) * 128], oall[:, hh, :], ost[:, 16 + hh:17 + hh],
                                                                 g_g[:, hh * 128:(hh + 1) * 128], ALU.mult, ALU.mult),
                         reads=[oall, ost, g_g], writes=[mix])
                yield
                yield from self.mem_attend_g(MW, mqT[i3], 0, memkT, memv, mix, banks=membanks)
                yield
                self.transpose8(mix, [(mixT, lambda half: mixT[:, half * 4:(half + 1) * 4, :])], self.nextbank(), self.nextbank())
                yield
                for half in range(2):
                    pb = self.nextbank()
                    for fc in range(8):
                        k.op("pe", lambda e: e.matmul(pb[:], mixT[:, fc, :], w_out[:, fc, half * 512:(half + 1) * 512],
                                                      start=(fc == 0), stop=(fc == 7)),
                             reads=[mixT, w_out], writes=[pb], inc=(fc == 7))
                    k.op("dve", lambda e: e.tensor_tensor(hC[:, half * 512:(half + 1) * 512], hC[:, half * 512:(half + 1) * 512], pb[:], ALU.add),
                         reads=[hC, pb], writes=[hC])
                k.dma(hout[0][t * 128:(t + 1) * 128, :], hC[:], reads=[hC], writes=[hout[1][t]])

            def run(*gens):
                gens = [g for g in gens if g is not None]
                while gens:
                    for g_ in list(gens):
                        try:
                            next(g_)
                        except StopIteration:
                            gens.remove(g_)

            mk = lambda fn, t: fn(t) if 0 <= t < NTA else None
            for step in range(-2, NTA):
                run(mk(back, step), mk(frontB, step + 1), mk(frontA, step + 2))
            k.barrier()
            del self.nextbank

    def mixer_b_phase(self, hin, hout):
        nc, k, P = self.nc, self.k, self.P
        NGB = int(os.environ.get("MK_NGB", "8"))
        NHB = int(os.environ.get("MK_NHB", "12"))
        NDUM = int(os.environ.get("MK_NDUM", "1"))
        with ExitStack() as st:
            sb = lambda n, s, d: self.sbuf(st, n, s, d)
            memkT, memv = self.mem_kv(st, 1)
            win_d = self.dscratch("b_w_in_bf", [D, D], BF16)
            wout_d = self.dscratch("b_w_out_bf", [D, D], BF16)
            wdb = [Buf(None, "win_d"), Buf(None, "wout_d")]
            k.dma(win_d, P["b_w_in"][0], writes=[wdb[0]], q="pool")
            k.dma(wout_d, P["b_w_out"][0], writes=[wdb[1]], q="pool")
            KT = sb("KT", [128, 6, S], BF16)
            Vt = sb("Vt", [128, NT, 768], BF16)
            ht = [sb("htB%d" % i, [128, D], F32) for i in range(2)]
            xn = sb("xnB", [128, D], F32)
            stt = [sb("sttB%d" % i, [128, 4], F32) for i in range(2)]
            with ExitStack() as st1:
                sb1 = lambda n, s, d: self.sbuf(st1, n, s, d)
                kvg = sb1("kvg", [128, D], F32)
                k.dma(kvg[:], P["kv_norm"].partition_broadcast(128), writes=[kvg])
                w_kv = sb1("w_kv", [128, 8, 1536], BF16)
                for c in range(8):
                    k.dma(w_kv[:, c, :], P["w_kv"][c * 128:(c + 1) * 128, :], writes=[w_kv], q="pool")
                xTg1 = [sb1("xTg1_%d" % i, [128, 8, 512], BF16) for i in range(2)]

                def p1_x(g):
                    xTg_ = xTg1[g % 2]
                    for tt in range(4):
                        t = g * 4 + tt
                        b = tt % 2
                        h = ht[b]
                        k.dma(h[:], hin[0][t * 128:(t + 1) * 128, :], reads=[hin[1][t]], writes=[h])
                        self.rmsnorm(h, kvg, xn, xn, stt[b])
                        yield
                        self.transpose8(xn, [(xTg_, lambda half: xTg_[:, half * 4:(half + 1) * 4, tt * 128:(tt + 1) * 128])],
                                        self.nextbank(), self.nextbank())
                        yield

                def p1_y(g):
                    xTg_ = xTg1[g % 2]
                    for fc in range(6):
                        pb = self.nextbank()
                        for dc in range(8):
                            k.op("pe", lambda e: e.matmul(pb[:], w_kv[:, dc, fc * 128:(fc + 1) * 128], xTg_[:, dc, :],
                                                          start=(dc == 0), stop=(dc == 7)),
                                 reads=[w_kv, xTg_], writes=[pb], inc=(dc == 7))
                        if fc % 2 == 0:
                            k.op("act", lambda e: e.copy(KT[:, fc, g * 512:(g + 1) * 512], pb[:]), reads=[pb], writes=[KT])
                        else:
                            k.op("dve", lambda e: e.tensor_copy(KT[:, fc, g * 512:(g + 1) * 512], pb[:]), reads=[pb], writes=[KT])
                        yield
                    for tt in range(4):
                        t = g * 4 + tt
                        for half, (c0, c1) in enumerate(((0, 512), (512, 768))):
                            pb = self.nextbank()
                            for dc in range(8):
                                k.op("pe", lambda e: e.matmul(pb[:, 0:c1 - c0], xTg_[:, dc, tt * 128:(tt + 1) * 128], w_kv[:, dc, 768 + c0:768 + c1],
                                                              start=(dc == 0), stop=(dc == 7)),
                                     reads=[w_kv, xTg_], writes=[pb], inc=(dc == 7))
                            if half == 0:
                                k.op("dve", lambda e: e.tensor_copy(Vt[:, t, c0:c1], pb[:, 0:c1 - c0]), reads=[pb], writes=[Vt])
                            else:
                                k.op("act", lambda e: e.copy(Vt[:, t, c0:c1], pb[:, 0:c1 - c0]), reads=[pb], writes=[Vt])
                        yield

                def rr1(*gens):
                    gens = [g_ for g_ in gens if g_ is not None]
                    while gens:
                        for g_ in list(gens):
                            try:
                                next(g_)
                            except StopIteration:
                                gens.remove(g_)

                rr1(p1_x(0))
                for g in range(NT // 4):
                    rr1(p1_y(g), p1_x(g + 1) if g + 1 < NT // 4 else None)
                k.barrier()
            bg = sb("bgain", [128, D], F32)
            k.dma(bg[:], P["b_norm"][0].partition_broadcast(128), writes=[bg])
            wB = sb("wB", [128, 8, D], BF16)
            xTg = sb("xTg", [128, 8, 512], BF16)
            qT = [sb("qT%d" % i, [128, 6, 512], BF16) for i in range(2)]
            mqT = [sb("mqTB%d" % i, [128, 2, 512], BF16) for i in range(3)]
            mixTg = [sb("mixTg%d" % i, [128, 8, 512], BF16) for i in range(2)]
            Eb = [sb("Eb%d" % i, [128, 512], F32) for i in range(3)]
            spb = [sb("spb%d" % i, [128, 512], BF16) for i in range(3)]
            eab = [sb("eab%d" % i, [128, 512], F32) for i in range(2)]
            ab = [sb("ab%d" % i, [128, 512], BF16) for i in range(2)]
            MW = self.mem_work(st)
            mmB = sb("mmB", [128, 256], F32)
            NGEb = self.cstb[:, 5, :]
            NLTb = self.cstb[:, 6, :]
            strictTb = self.cstb[:, 3, :]
            cstb = self.cstb
            PZ = [self.ps[0], self.ps[1]]
            PC = [self.ps[2], self.ps[3]]
            PO = [self.ps[4], self.ps[5]]
            rot = [0]

            def nb():
                rot[0] = (rot[0] + 1) % 2
                return self.ps[6 + rot[0]]
            self.nextbank = nb
            wcur = [None]

            def load_wB(which):
                if wcur[0] != which:
                    srcd = win_d if which == "in" else wout_d
                    k.dma(wB[:], srcd.rearrange("(c p) n -> p c n", p=128), reads=[wdb[0 if which == "in" else 1]], writes=[wB])
                    wcur[0] = which

            def prologue(g):
                load_wB("in")
                qTg, mqTg = qT[g % 2], mqT[g % 3]
                h = ht[0]
                for tt in range(4):
                    t = g * 4 + tt
                    k.dma(h[:], hin[0][t * 128:(t + 1) * 128, :], reads=[hin[1][t]], writes=[h])
                    self.rmsnorm(h, bg, xn, xn, stt[0])
                    yield
                    self.transpose8(xn, [(xTg, lambda half: xTg[:, half * 4:(half + 1) * 4, tt * 128:(tt + 1) * 128])], nb(), nb())
                    yield
                for fc in range(8):
                    pb = nb()
                    for dc in range(8):
                        k.op("pe", lambda e: e.matmul(pb[:], wB[:, dc, fc * 128:(fc + 1) * 128], xTg[:, dc, :], start=(dc == 0), stop=(dc == 7)),
                             reads=[wB, xTg], writes=[pb], inc=(dc == 7))
                    if fc < 6:
                        k.op("act", lambda e: e.mul(qTg[:, fc, :], pb[:], 0.125), reads=[pb], writes=[qTg])
                    else:
                        k.op("dve", lambda e: e.tensor_copy(mqTg[:, fc - 6, :], pb[:]), reads=[pb], writes=[mqTg])
                    yield

            def epilogue(g):
                load_wB("out")
                mixg, mqTg = mixTg[g % 2], mqT[g % 3]
                h = ht[1]
                for tt in range(4):
                    t = g * 4 + tt
                    k.dma(h[:], hin[0][t * 128:(t + 1) * 128, :], reads=[hin[1][t]], writes=[h])
                    yield from self.mem_attend_g(MW, mqTg, tt * 128, memkT, memv, mmB, col0=0)
                    yield
                    pb = nb()
                    for j in range(2):
                        k.op("pe", lambda e: e.transpose(pb[:, j * 128:(j + 1) * 128], mmB[:, j * 128:(j + 1) * 128], self.ident),
                             reads=[mmB, self.cst], writes=[pb], inc=(j == 1))
                    k.op("act", lambda e: e.copy(mixg[:, 6:8, tt * 128:(tt + 1) * 128], pb[:, 0:256].rearrange("p (c t) -> p c t", c=2)),
                         reads=[pb], writes=[mixg])
                    yield
                    for half in range(2):
                        pb = nb()
                        for fc in range(8):
                            k.op("pe", lambda e: e.matmul(pb[:], mixg[:, fc, tt * 128:(tt + 1) * 128], wB[:, fc, half * 512:(half + 1) * 512],
                                                          start=(fc == 0), stop=(fc == 7)),
                                 reads=[mixg, wB], writes=[pb], inc=(fc == 7))
                        k.op("dve", lambda e: e.tensor_tensor(h[:, half * 512:(half + 1) * 512], h[:, half * 512:(half + 1) * 512], pb[:], ALU.add),
                             reads=[h, pb], writes=[h])
                        yield
                    k.dma(hout[0][t * 128:(t + 1) * 128, :], h[:], reads=[h], writes=[hout[1][t]])

            def side_thread(g):
                if g >= 1:
                    yield from epilogue(g - 1)
                if g + 1 < NGB:
                    yield from prologue(g + 1)

            for _ in prologue(0):
                pass
            for g in range(NGB):
                qTg, mixg = qT[g % 2], mixTg[g % 2]
                side = side_thread(g)
                items = [(2 * p + s, kb) for p in range(NHB // 2) for kb in range(4 * g + 3, -1, -1) for s in range(2)]

                def geom(i):
                    hh, kb = items[i]
                    r = max(kb - 4 * g, 0)
                    return hh, kb, hh // 2, hh % 2, r * 128, kb >= 4 * g

                def s1_pe(i):
                    hh, kb, fc, s, c0, diag = geom(i)
                    ps_ = slice(s * 64, (s + 1) * 64)
                    cs = slice(c0, 512)
                    pz = PZ[i % 2]
                    k.op("pe", lambda e: e.matmul(pz[:, cs], KT[ps_, fc, kb * 128:(kb + 1) * 128], qTg[ps_, fc, cs], start=True, stop=True),
                         reads=[KT, qTg], writes=[pz])

                def s1_act(i):
                    hh, kb, fc, s, c0, diag = geom(i)
                    cs = slice(c0, 512)
                    pz, E, sp = PZ[i % 2], Eb[i % 3], spb[i % 3]
                    k.op("act", lambda e: e.activation(E[:, cs], pz[:, cs], AF.Exp), reads=[pz], writes=[E])
                    k.op("act", lambda e: e.activation(sp[:, cs], E[:, cs], AF.Ln, bias=1.0), reads=[E], writes=[sp])
                    if diag:
                        k.op("dve", lambda e: e.tensor_tensor(sp[:, c0:c0 + 128], sp[:, c0:c0 + 128], strictTb, ALU.mult),
                             reads=[sp, cstb], writes=[sp])

                def s2_peA(i):
                    hh, kb, fc, s, c0, diag = geom(i)
                    cs = slice(c0, 512)
                    C, sp = PC[s], spb[i % 3]
                    if kb == 4 * g + 3:
                        k.op("dve", lambda e: e.memset(C[:], 0.0), writes=[C])
                    k.op("pe", lambda e: e.matmul(C[:, cs], NGEb, sp[:, cs], start=False, stop=False, skip_group_check=True),
                         reads=[cstb, sp], writes=[C])

                def s2_act(i):
                    hh, kb, fc, s, c0, diag = geom(i)
                    cs = slice(c0, 512)
                    C, ea_ = PC[s], eab[i % 2]
                    k.op("act", lambda e: e.activation(ea_[:, cs], C[:, cs], AF.Exp), reads=[C], writes=[ea_])

                def s2_peB(i):
                    hh, kb, fc, s, c0, diag = geom(i)
                    cs = slice(c0, 512)
                    C, sp = PC[s], spb[i % 3]
                    if kb > 0:
                        k.op("pe", lambda e: e.matmul(C[:, cs], NLTb, sp[:, cs], start=False, stop=False, skip_group_check=True),
                             reads=[cstb, sp], writes=[C])

                def s3_pool(i):
                    hh, kb, fc, s, c0, diag = geom(i)
                    cs = slice(c0, 512)
                    E, ea_, a_ = Eb[i % 3], eab[i % 2], ab[i % 2]
                    k.op("dve", lambda e: e.tensor_tensor(a_[:, cs], E[:, cs], ea_[:, cs], ALU.mult), reads=[E, ea_], writes=[a_])
                    if diag:
                        k.op("dve", lambda e: e.tensor_tensor(a_[:, c0:c0 + 128], a_[:, c0:c0 + 128], strictTb, ALU.mult),
                             reads=[a_, cstb], writes=[a_])

                def s3_pe(i):
                    hh, kb, fc, s, c0, diag = geom(i)
                    cs = slice(c0, 512)
                    a_ = ab[i % 2]
                    po = PO[fc % 2]
                    if kb == 4 * g + 3 and s == 0:
                        k.op("dve", lambda e: e.memset(po[:], 0.0), writes=[po])
                    vblk = Vt[:, kb, hh * 64:(hh + 1) * 64]
                    if s == 0:
                        k.op("pe", lambda e: e.matmul(po[0:64, cs], vblk, a_[:, cs], start=False, stop=False, skip_group_check=True),
                             reads=[Vt, a_], writes=[po])
                    else:
                        k.op("pe", lambda e: e.matmul(po[64:128, cs], vblk, a_[:, cs], start=False, stop=False, skip_group_check=True,
                                                      tile_position=(0, 64)), reads=[Vt, a_], writes=[po])
                    if kb == 0 and s == 1:
                        k.op("act", lambda e: e.copy(mixg[:, fc, :], po[:]), reads=[po], writes=[mixg])

                n_it = len(items)
                ok = lambda i: 0 <= i < n_it
                dumrhs = cstb[:, 0:4, :].rearrange("p c t -> p (c t)")
                stride = max(1, n_it // 72)
                for step in range(-3, n_it + 1):
                    i0, i1, i2_, i3, i4 = step + 3, step + 2, step + 1, step, step - 1
                    if ok(i3):
                        s3_pool(i3)
                    if ok(i0):
                        for _d in range(NDUM):
                            k.op("pe", lambda e: e.matmul(PZ[i0 % 2][:], NGEb, dumrhs, start=True, stop=True),
                                 reads=[cstb], writes=[PZ[i0 % 2]], inc=False)
                        s1_pe(i0)
                    if ok(i2_):
                        s2_peA(i2_)
                    if ok(i4):
                        s3_pe(i4)
                    if ok(i1):
                        s1_act(i1)
                    if ok(i2_):
                        s2_act(i2_)
                    if ok(i3):
                        s2_peB(i3)
                    if side is not None and step >= 0 and step % stride == 0:
                        try:
                            next(side)
                        except StopIteration:
                            side = None
                if side is not None:
                    for _ in side:
                        pass
            for _ in epilogue(NGB - 1):
                pass
            k.barrier()
            del self.nextbank

    def build(self, phases):
        nc, k = self.nc, self.k
        P = self.P = {}
        shapes = dict(
            x=[S, D], mem=[256, D], a_norm=[1, D], a_w_in=[1, D, 3340], a_conv=[1, 4, 2304],
            a_log=[1, 6], a_dt_bias=[1, 6], a_out_gain=[1, 128], a_w_out=[1, D, D],
            kv_norm=[D], w_kv=[D, 1536], b_norm=[1, D], b_w_in=[1, D, D], b_w_out=[1, D, D],
            mem_norm=[2, D], w_mem_kv=[2, D, 512], ffn_norm=[2, D], w_group=[2, D, 4], b_group=[2, 4],
            w_router=[2, D, 16], b_router=[2, 16], w1=[2, 16, D, 256], w3=[2, 16, D, 256],
            w2=[2, 16, 256, D], final_norm=[D], wgr=[2, 128, 8, 20], rbias=[2, 20], convw=[128, 18, 4])
        for n, s in shapes.items():
            P[n] = self.din(n, s)
        out = nc.dram_tensor("out", [S, D], F32, kind="ExternalOutput").ap()
        mkbufs = lambda nm: [Buf(None, "%s%d" % (nm, i)) for i in range(NT)]
        hx = (P["x"], mkbufs("x"))
        hA = (self.dscratch("hA", [S, D]), mkbufs("hA"))
        hB = (self.dscratch("hB", [S, D]), mkbufs("hB"))
        ho = (out, mkbufs("out"))
        with ExitStack() as gst:
            self.load_consts(gst)
            self.epsbuf = self.sbuf(gst, "epsb", [128, 1], F32)
            self.epsb = self.epsbuf
            k.op("dve", lambda e: e.memset(self.epsbuf[:], EPS), writes=[self.epsbuf])
            cur = hx
            seq = {"A": hA, "M0": hB, "B": hA, "M1": ho}
            for ph in phases:
                dst = seq[ph] if ph != phases[-1] else ho
                if ph == "M0":
                    self.moe_phase(0, cur, dst, final=False)
                elif ph == "M1":
                    self.moe_phase(1, cur, dst, final=True)
                elif ph == "A":
                    self.mixer_a_phase(cur, dst)
                elif ph == "B":
                    self.mixer_b_phase(cur, dst)
                cur = dst
            for b in ho[1]:
                if b.lw is not None:
                    k._wait("sp", b.lw)
        return nc


def make_consts():
    c = np.zeros((128, 8, 128), np.float32)
    i = np.arange(128)
    c[:, 0, :] = np.eye(128)
    c[:, 1, :] = (i[:, None] <= i[None, :])
    c[:, 2, :] = (i[:, None] > i[None, :])
    c[:, 3, :] = (i[:, None] < i[None, :])
    c[:, 4, :] = 1.0
    c[:, 5, :] = -(i[:, None] >= i[None, :]).astype(np.float32)
    c[:, 6, :] = -(i[:, None] < i[None, :]).astype(np.float32)
    c[:, 7, :] = -30000.0 * (i[:, None] >= i[None, :])
    return c


_CACHE = {}


def run(inputs, phases=("A", "M0", "B", "M1"), ncores=NCORES, trace=False):
    key = tuple(phases)
    if key not in _CACHE:
        mk = MK(phases)
        _CACHE[key] = mk.build(list(phases))
    nc = _CACHE[key]
    consts = make_consts()
    inputs = dict(inputs)
    wg = np.concatenate([np.asarray(inputs["w_group"]), np.asarray(inputs["w_router"])], axis=2)
    inputs["wgr"] = np.ascontiguousarray(wg.reshape(2, 8, 128, 20).transpose(0, 2, 1, 3))
    inputs["rbias"] = np.concatenate([np.asarray(inputs["b_group"]), np.asarray(inputs["b_router"])], axis=1)
    cw = np.asarray(inputs["a_conv"])[0]
    inputs["convw"] = np.ascontiguousarray(cw.reshape(4, 18, 128).transpose(2, 1, 0))
    in_maps = []
    for c in range(ncores):
        m = {"consts": consts}
        for n, v in inputs.items():
            v = np.asarray(v)
            if n in ("x", "mem"):
                m[n] = np.ascontiguousarray(v[c])
            else:
                m[n] = np.ascontiguousarray(v, dtype=np.float32)
        in_maps.append(m)
    res = run_bass_kernel_spmd(nc, in_maps, core_ids=list(range(ncores)), trace=trace)
    outs = np.stack([r["out"] for r in res.results], axis=0)
    return outs, res


def kernel(**inputs):
    outs, _ = run(inputs)
    return outs.astype(np.float32)
```

```python
from contextlib import ExitStack
import os
import numpy as np
import concourse.bass as bass
import concourse.mybir as mybir
from concourse.bass_utils import run_bass_kernel_spmd

F32 = mybir.dt.float32
BF16 = mybir.dt.bfloat16
AF = mybir.ActivationFunctionType
ALU = mybir.AluOpType
AX = mybir.AxisListType

S = 4096
D = 1024
NT = S // 128
EPS = 1e-6
NCORES = 8


class Buf:
    __slots__ = ("ap", "name", "_lw", "_rd", "_excl")
    lw = property(lambda self: self._lw, lambda self, v: setattr(self, "_lw", v))
    rd = property(lambda self: self._rd, lambda self, v: setattr(self, "_rd", v))
    excl = property(lambda self: self._excl, lambda self, v: setattr(self, "_excl", v))

    def __init__(self, ap, name="", excl=False):
        self.ap = ap
        self.name = name
        self.excl = excl
        self.lw = None
        self.rd = []

    def __getitem__(self, idx):
        return self.ap[idx]


class View(Buf):
    __slots__ = ("parent",)

    def __init__(self, parent, ap):
        self.parent = parent
        self.ap = ap
        self.name = parent.name

    lw = property(lambda self: self.parent.lw, lambda self, v: setattr(self.parent, "lw", v))
    rd = property(lambda self: self.parent.rd, lambda self, v: setattr(self.parent, "rd", v))
    excl = property(lambda self: self.parent.excl, lambda self, v: None)


class K:
    NDMA = 48

    def __init__(self, nc):
        self.nc = nc
        self.eng = {"pe": nc.tensor, "act": nc.scalar, "dve": nc.vector,
                    "pool": nc.gpsimd, "sp": nc.sync}
        self.sem = {e: nc.alloc_semaphore("s_" + e) for e in ("pe", "act", "dve", "pool")}
        self.cnt = {e: 0 for e in self.sem}
        self.waited = {}
        self.dsem = [nc.alloc_semaphore("d%d" % i) for i in range(self.NDMA)]
        self.dcnt = [0] * self.NDMA
        self.dnext = 0
        self.dnext_sw = 0
        self.nins = 0

    def _semh(self, key):
        return self.sem[key] if isinstance(key, str) else self.dsem[key]

    def _wait(self, e, dep):
        key, val = dep
        if key == e and e == "pe":
            return
        w = self.waited.get((e, key), 0)
        if w >= val:
            return
        self.eng[e].wait_ge(self._semh(key), val)
        self.nins += 1
        self.waited[(e, key)] = val

    def _deps(self, e, reads, writes):
        best = {}
        for r in reads:
            if r.lw is not None:
                if best.get(r.lw[0], 0) < r.lw[1]:
                    best[r.lw[0]] = r.lw[1]
            if r.excl:
                for key, val in r.rd:
                    if key != e and best.get(key, 0) < val:
                        best[key] = val
        for w in writes:
            if w.lw is not None:
                if best.get(w.lw[0], 0) < w.lw[1]:
                    best[w.lw[0]] = w.lw[1]
            for key, val in w.rd:
                if best.get(key, 0) < val:
                    best[key] = val
        for key, val in best.items():
            self._wait(e, (key, val))

    def _mark(self, tag, reads, writes):
        for r in reads:
            r.rd.append(tag)
            if len(r.rd) > 64:
                best = {}
                for key, val in r.rd:
                    if best.get(key, 0) < val:
                        best[key] = val
                r.rd = list(best.items())
        for w in writes:
            w.lw = tag
            w.rd = []

    def op(self, e, fn, reads=(), writes=(), inc=True):
        self._deps(e, reads, writes)
        ins = fn(self.eng[e])
        self.nins += 1
        if inc:
            ins.then_inc(self.sem[e], 1)
            self.cnt[e] += 1
            tag = (e, self.cnt[e])
        else:
            tag = (e, self.cnt[e] + 1)
        self._mark(tag, reads, writes)
        return ins

    def dma(self, out, in_, reads=(), writes=(), q="sp", **kw):
        if q == "pool":
            slot = 32 + self.dnext_sw
            self.dnext_sw = (self.dnext_sw + 1) % (self.NDMA - 32)
        else:
            slot = self.dnext
            self.dnext = (self.dnext + 1) % 32
        if self.dcnt[slot] > 0:
            self._wait(q, (slot, 16 * self.dcnt[slot]))
        self._deps(q, reads, writes)
        ins = self.eng[q].dma_start(out=out, in_=in_, **kw)
        self.nins += 1
        ins.then_inc(self.dsem[slot], 16)
        self.dcnt[slot] += 1
        tag = (slot, 16 * self.dcnt[slot])
        self._mark(tag, reads, writes)
        return tag

    def barrier(self):
        for e in ("pe", "act", "dve", "pool", "sp"):
            for e2 in ("pe", "act", "dve", "pool"):
                if e2 != e and self.cnt[e2] > 0:
                    self._wait(e, (e2, self.cnt[e2]))
            for slot in range(self.NDMA):
                if self.dcnt[slot] > 0:
                    self._wait(e, (slot, 16 * self.dcnt[slot]))


class MK:
    def __init__(self, phases, h0_from_input=True):
        self.nc = nc = bass.Bass("TRN2", target_bir_lowering=False)
        self.k = K(nc)
        self.uid = 0
        self.ins = {}
        self.ps = [Buf(nc.alloc_psum_tensor("psb%d" % i, [128, 512], F32).ap(), "ps%d" % i, excl=True)
                   for i in range(8)]

    def din(self, name, shape):
        ap = self.nc.dram_tensor(name, list(shape), F32, kind="ExternalInput").ap()
        self.ins[name] = ap
        return ap

    def dscratch(self, name, shape, dt=F32):
        return self.nc.dram_tensor(name, list(shape), dt, kind="Internal").ap()

    def sbuf(self, st, name, shape, dt):
        self.uid += 1
        h = st.enter_context(self.nc.sbuf_tensor("%s_%d" % (name, self.uid), list(shape), dt))
        return Buf(h.ap(), name)

    def load_consts(self, st):
        k = self.k
        c = self.din("consts", [128, 8, 128])
        self.cst = self.sbuf(st, "cst", [128, 8, 128], F32)
        k.dma(self.cst[:], c, writes=[self.cst])
        self.ident = self.cst[:, 0, :]
        self.U = self.cst[:, 1, :]
        self.SL = self.cst[:, 2, :]
        self.strictT = self.cst[:, 3, :]
        self.ones = self.cst[:, 4, :]
        self.cstb = self.sbuf(st, "cstb", [128, 8, 128], BF16)
        k.dma(self.cstb[:], c, writes=[self.cstb], q="pool")
        self.identb = self.cstb[:, 0, :]
        self.onesb = self.cstb[:, 4, :]

    def rmsnorm(self, h, gainb, xn, junk, st2):
        k = self.k
        k.op("act", lambda e: e.activation(junk[:], h[:], AF.Square, accum_out=st2[:, 0:1]),
             reads=[h], writes=[junk, st2])
        k.op("act", lambda e: e.activation(st2[:, 1:2], st2[:, 0:1], AF.Ln, bias=self.epsb[:, 0:1], scale=1.0 / D),
             reads=[st2, self.epsbuf], writes=[st2])
        k.op("act", lambda e: e.activation(st2[:, 2:3], st2[:, 1:2], AF.Exp, scale=-0.5), reads=[st2], writes=[st2])
        k.op("dve", lambda e: e.scalar_tensor_tensor(xn[:], h[:], st2[:, 2:3], gainb[:], ALU.mult, ALU.mult),
             reads=[h, st2, gainb], writes=[xn])

    def transpose8(self, src, dsts, psa, psb, evac=("act", "dve"), second="pool"):
        k = self.k
        for half, ps in enumerate((psa, psb)):
            for j in range(4):
                c = half * 4 + j
                k.op("pe", lambda e: e.transpose(ps[:, j * 128:(j + 1) * 128], src[:, c * 128:(c + 1) * 128], self.ident),
                     reads=[src, self.cst], writes=[ps], inc=(j == 3))
            dbuf, fn = dsts[0]
            eng = evac[half % len(evac)]
            pv = ps[:].rearrange("p (c t) -> p c t", c=4)
            if eng == "act":
                k.op("act", lambda e: e.copy(fn(half), pv), reads=[ps], writes=[dbuf])
            else:
                k.op(eng, lambda e: e.tensor_copy(fn(half), pv), reads=[ps], writes=[dbuf])
            for dbuf2, fn2 in dsts[1:]:
                k.op(second, lambda e: e.tensor_copy(fn2(half), fn(half)), reads=[dbuf], writes=[dbuf2])

    def moe_phase(self, l, hin, hout, final=False, out_ap=None):
        nc, k = self.nc, self.k
        G = 1024
        NTG = G // 128
        NG = S // G
        NTB = G // 512
        NEX = 16
        P = self.P
        with ExitStack() as st:
            sb = lambda n, s, d: self.sbuf(st, n, s, d)
            gain = sb("gain", [128, D], F32)
            k.dma(gain[:], P["ffn_norm"][l].partition_broadcast(128), writes=[gain])
            if final:
                fgain = sb("fgain", [128, D], F32)
                k.dma(fgain[:], P["final_norm"].partition_broadcast(128), writes=[fgain])
            wgr = sb("wgr", [128, 8, 20], F32)
            k.dma(wgr[:], P["wgr"][l], writes=[wgr])
            rb = sb("rbias", [128, 20], F32)
            k.dma(rb[:], P["rbias"][l].partition_broadcast(128), writes=[rb])
            xnT = [sb("xnT%d" % i, [128, 8, G], BF16) for i in range(2)]
            yacc = [[sb("yacc%d_%d" % (j, i), [128, D], F32) for i in range(NTG)] for j in range(2)]
            comb = [[sb("comb%d_%d" % (j, i), [128, 16], F32) for i in range(NTG)] for j in range(2)]
            w1b = [sb("w1b%d" % i, [128, 8, 256], BF16) for i in range(2)]
            w3b = [sb("w3b%d" % i, [128, 8, 256], BF16) for i in range(2)]
            w2b = [sb("w2b%d" % i, [128, 2, D], BF16) for i in range(2)]
            ht = [sb("ht%d" % i, [128, D], F32) for i in range(2)]
            htc = [sb("htc%d" % i, [128, D], F32) for i in range(2)]
            xn = [sb("xn%d" % i, [128, D], F32) for i in range(2)]
            xnT32 = [sb("xnT32_%d" % i, [128, 8, 128], F32) for i in range(2)]
            stt = [sb("stt%d" % i, [128, 4], F32) for i in range(2)]
            sttc = [sb("sttc%d" % i, [128, 4], F32) for i in range(2)]
            rt = [sb("rt%d" % i, [128, 96], F32) for i in range(2)]
            hid = [sb("hid%d" % i, [128, 2, 512], BF16) for i in range(2)]
            sil = [sb("sil%d" % i, [128, 512], F32) for i in range(2)]
            ps = self.ps
            w1d, w3d, w2d = P["w1"], P["w3"], P["w2"]

            def load_w(e, slot):
                k.dma(w1b[slot][:], w1d[l, e].rearrange("(c p) f -> p c f", p=128), writes=[w1b[slot]], q="pool")
                k.dma(w3b[slot][:], w3d[l, e].rearrange("(c p) f -> p c f", p=128), writes=[w3b[slot]], q="pool")
                k.dma(w2b[slot][:], w2d[l, e].rearrange("(c p) n -> p c n", p=128), writes=[w2b[slot]], q="pool")

            def stage_a(g, tis=None, banks=None):
                par = g % 2
                xT = xnT[par]
                for ti in (range(NTG) if tis is None else tis):
                    t = g * NTG + ti
                    b = ti % 2
                    h = ht[b]
                    k.dma(h[:], hin[0][t * 128:(t + 1) * 128, :], reads=[hin[1][t]], writes=[h])
                    self.rmsnorm(h, gain, xn[b], xn[b], stt[b])
                    yield
                    x32 = xnT32[b]
                    self.transpose8(
                        xn[b],
                        [(x32, lambda half: x32[:, half * 4:(half + 1) * 4, :]),
                         (xT, lambda half: xT[:, half * 4:(half + 1) * 4, ti * 128:(ti + 1) * 128])],
                        ps[6] if banks is None else banks[0], ps[7] if banks is None else banks[1])
                    yield
                    pr = ps[6 + (ti % 2)] if banks is None else banks[2]
                    for dc in range(8):
                        k.op("pe", lambda e: e.matmul(pr[:, 0:20], x32[:, dc, :], wgr[:, dc, :], start=(dc == 0), stop=(dc == 7)),
                             reads=[x32, wgr], writes=[pr], inc=(dc == 7))
                    yield
                    r = rt[b]
                    R = lambda a, n: r[:, a:a + n]
                    lg, gmax, ngmax, oh, ge, gsum, pg = R(0, 20), R(20, 1), R(21, 1), R(22, 4), R(26, 4), R(30, 1), R(31, 1)
                    tmp, elsel, m1, nm1, ee, mask1, ee2 = R(32, 16), R(48, 4), R(52, 1), R(53, 1), R(54, 4), R(58, 4), R(62, 4)
                    v2, mask2, den, rden, wl, scl = R(66, 1), R(67, 4), R(71, 1), R(72, 1), R(73, 4), R(77, 1)
                    dv = lambda fn, rd=(), wr=(): k.op("dve", fn, reads=[r] + list(rd), writes=[r] + list(wr))
                    dv(lambda e: e.tensor_tensor(lg, pr[:, 0:20], rb[:], ALU.add), rd=[pr, rb])
                    dv(lambda e: e.tensor_reduce(gmax, lg[:, 0:4], AX.X, ALU.max))
                    dv(lambda e: e.tensor_single_scalar(ngmax, gmax, -1.0, ALU.mult))
                    dv(lambda e: e.tensor_scalar(oh, lg[:, 0:4], gmax, None, ALU.is_equal))
                    yield
                    k.op("act", lambda e: e.activation(ge, lg[:, 0:4], AF.Exp, bias=ngmax, accum_out=gsum), reads=[r], writes=[r])
                    dv(lambda e: e.reciprocal(pg, gsum))
                    dv(lambda e: e.tensor_tensor(tmp.rearrange("p (g j) -> p g j", g=4),
                                                 lg[:, 4:20].rearrange("p (g j) -> p g j", g=4),
                                                 oh.unsqueeze(2).to_broadcast([128, 4, 4]), ALU.mult))
                    dv(lambda e: e.tensor_reduce(elsel, tmp.rearrange("p (g j) -> p j g", g=4), AX.X, ALU.add))
                    yield
                    dv(lambda e: e.tensor_reduce(m1, elsel, AX.X, ALU.max))
                    dv(lambda e: e.tensor_single_scalar(nm1, m1, -1.0, ALU.mult))
                    k.op("act", lambda e: e.activation(ee, elsel, AF.Exp, bias=nm1), reads=[r], writes=[r])
                    dv(lambda e: e.tensor_scalar(mask1, elsel, m1, None, ALU.is_equal))
                    yield
                    dv(lambda e: e.scalar_tensor_tensor(ee2, mask1, -2.0, ee, ALU.mult, ALU.add))
                    dv(lambda e: e.tensor_reduce(v2, ee2, AX.X, ALU.max))
                    dv(lambda e: e.tensor_scalar(mask2, ee2, v2, None, ALU.is_equal))
                    dv(lambda e: e.tensor_single_scalar(den, v2, 1.0, ALU.add))
                    yield
                    dv(lambda e: e.reciprocal(rden, den))
                    dv(lambda e: e.scalar_tensor_tensor(wl, mask2, v2, mask1, ALU.mult, ALU.add))
                    dv(lambda e: e.tensor_tensor(scl, pg, rden, ALU.mult))
                    dv(lambda e: e.tensor_scalar(wl, wl, scl, None, ALU.mult))
                    cb = comb[par][ti]
                    dv(lambda e: e.tensor_tensor(cb[:].rearrange("p (g j) -> p g j", g=4),
                                                 oh.unsqueeze(2).to_broadcast([128, 4, 4]),
                                                 wl.unsqueeze(1).to_broadcast([128, 4, 4]), ALU.mult), wr=[cb])
                    yield

            def stage_b(g):
                par = g % 2
                xT = xnT[par]
                work = [(ex, tb) for ex in range(NEX) for tb in range(NTB)]

                def up(idx):
                    ex, tb = work[idx]
                    slot = ex % 2
                    w1, w3 = w1b[slot], w3b[slot]
                    hd = hid[idx % 2]
                    for fc in range(2):
                        p1 = ps[fc]
                        p3 = ps[2 + fc]
                        for wsrc, pdst in ((w1, p1), (w3, p3)):
                            for dc in range(8):
                                k.op("pe", lambda e: e.matmul(pdst[:], wsrc[:, dc, fc * 128:(fc + 1) * 128], xT[:, dc, tb * 512:(tb + 1) * 512],
                                                              start=(dc == 0), stop=(dc == 7)),
                                     reads=[wsrc, xT], writes=[pdst], inc=(dc == 7))
                                if dc % 4 == 3:
                                    yield
                        sl = sil[fc]
                        k.op("act", lambda e: e.activation(sl[:], p1[:], AF.Silu), reads=[p1], writes=[sl])
                        k.op("dve", lambda e: e.tensor_tensor(hd[:, fc, :], sl[:], p3[:], ALU.mult), reads=[sl, p3], writes=[hd])

                def down(idx):
                    ex, tb = work[idx]
                    slot = ex % 2
                    w2 = w2b[slot]
                    hd = hid[idx % 2]
                    for tt in range(4):
                        ti = tb * 4 + tt
                        for half in range(2):
                            py = ps[4 + half]
                            for fc in range(2):
                                k.op("pe", lambda e: e.matmul(py[:], hd[:, fc, tt * 128:(tt + 1) * 128], w2[:, fc, half * 512:(half + 1) * 512],
                                                              start=(fc == 0), stop=(fc == 1)),
                                     reads=[hd, w2], writes=[py], inc=(fc == 1))
                            ya = yacc[par][ti]
                            cs = comb[par][ti][:, ex:ex + 1]
                            if ex == 0:
                                k.op("dve", lambda e: e.tensor_scalar(ya[:, half * 512:(half + 1) * 512], py[:], cs, None, ALU.mult),
                                     reads=[py, comb[par][ti]], writes=[ya])
                            else:
                                k.op("dve", lambda e: e.scalar_tensor_tensor(ya[:, half * 512:(half + 1) * 512], py[:], cs,
                                                                             ya[:, half * 512:(half + 1) * 512], ALU.mult, ALU.add),
                                     reads=[py, comb[par][ti], ya], writes=[ya])
                            yield
                    if tb == NTB - 1:
                        if ex + 2 < NEX:
                            load_w(ex + 2, slot)
                        elif g + 1 < NG:
                            load_w(ex + 2 - NEX, slot)

                def rr(*gens):
                    gens = [g_ for g_ in gens if g_ is not None]
                    while gens:
                        for g_ in list(gens):
                            try:
                                next(g_)
                                yield
                            except StopIteration:
                                gens.remove(g_)

                yield from up(0)
                for idx in range(len(work)):
                    nu = up(idx + 1) if idx + 1 < len(work) else None
                    if nu is not None:
                        next(nu)
                        yield
                    yield from rr(nu, down(idx))

            def stage_c(g, tis=None):
                par = g % 2
                for ti in (range(NTG) if tis is None else tis):
                    t = g * NTG + ti
                    b = ti % 2
                    h = htc[b]
                    ya = yacc[par][ti]
                    k.dma(h[:], hin[0][t * 128:(t + 1) * 128, :], reads=[hin[1][t]], writes=[h])
                    k.op("pool", lambda e: e.tensor_tensor(ya[:], h[:], ya[:], ALU.add), reads=[h, ya], writes=[ya])
                    yield
                    if final:
                        self.rmsnorm(ya, fgain, h, h, sttc[b])
                        k.dma(hout[0][t * 128:(t + 1) * 128, :], h[:], reads=[h], writes=[hout[1][t]])
                    else:
                        k.dma(hout[0][t * 128:(t + 1) * 128, :], ya[:], reads=[ya], writes=[hout[1][t]])
                    yield

            def run_group(bg, ag, cg):
                others = [[g_, per] for g_, per in ((ag, 4), (cg, 24)) if g_ is not None]
                step = 0
                b_alive = bg is not None
                while b_alive or others:
                    if b_alive:
                        try:
                            next(bg)
                        except StopIteration:
                            b_alive = False
                    for item in list(others):
                        if (not b_alive) or step % item[1] == 0:
                            try:
                                next(item[0])
                            except StopIteration:
                                others.remove(item)
                    step += 1

            load_w(0, 0)
            load_w(1, 1)
            ga = [stage_a(0, range(0, NTG, 2), (ps[0], ps[1], ps[2])), stage_a(0, range(1, NTG, 2), (ps[3], ps[4], ps[5]))]
            while ga:
                for g_ in list(ga):
                    try:
                        next(g_)
                    except StopIteration:
                        ga.remove(g_)
            for g in range(NG):
                run_group(stage_b(g), stage_a(g + 1) if g + 1 < NG else None, stage_c(g - 1) if g >= 1 else None)
            gc = [stage_c(NG - 1, range(0, NTG, 2)), stage_c(NG - 1, range(1, NTG, 2))]
            while gc:
                for g_ in list(gc):
                    try:
                        next(g_)
                    except StopIteration:
                        gc.remove(g_)
            k.barrier()

    def nextbank(self):
        self.pbi = (getattr(self, "pbi", -1) + 1) % 8
        return self.ps[self.pbi]

    def mem_kv(self, st, l):
        k, P = self.k, self.P
        sb = lambda n, s, d: self.sbuf(st, n, s, d)
        memkT = sb("memkT", [128, 2, 256], BF16)
        memv = sb("memv", [128, 2, 256], BF16)
        with ExitStack() as st2:
            sb2 = lambda n, s, d: self.sbuf(st2, n, s, d)
            g = sb2("mg", [128, D], F32)
            k.dma(g[:], P["mem_norm"][l].partition_broadcast(128), writes=[g])
            w = sb2("wmkv", [128, 8, 512], BF16)
            k.dma(w[:], P["w_mem_kv"][l].rearrange("(c p) n -> p c n", p=128), writes=[w], q="pool")
            mT = sb2("memnT", [128, 8, 256], BF16)
            junk = sb2("mjunk", [128, D], F32)
            for mt in range(2):
                h = sb2("mh%d" % mt, [128, D], F32)
                xn = sb2("mxn%d" % mt, [128, D], F32)
                stt = sb2("mst%d" % mt, [128, 4], F32)
                k.dma(h[:], P["mem"][mt * 128:(mt + 1) * 128, :], writes=[h])
                self.rmsnorm(h, g, xn, junk, stt)
                self.transpose8(xn, [(mT, lambda half: mT[:, half * 4:(half + 1) * 4, mt * 128:(mt + 1) * 128])],
                                self.nextbank(), self.nextbank())
            for j in range(2):
                pb = self.nextbank()
                for dc in range(8):
                    k.op("pe", lambda e: e.matmul(pb[:, 0:256], w[:, dc, j * 128:(j + 1) * 128], mT[:, dc, :],
                                                  start=(dc == 0), stop=(dc == 7)),
                         reads=[w, mT], writes=[pb], inc=(dc == 7))
                k.op("act", lambda e: e.copy(memkT[:, j, :], pb[:, 0:256]), reads=[pb], writes=[memkT])
            for mt in range(2):
                pb = self.nextbank()
                for dc in range(8):
                    k.op("pe", lambda e: e.matmul(pb[:, 0:256], mT[:, dc, mt * 128:(mt + 1) * 128], w[:, dc, 256:512],
                                                  start=(dc == 0), stop=(dc == 7)),
                         reads=[w, mT], writes=[pb], inc=(dc == 7))
                k.op("dve", lambda e: e.tensor_copy(memv[:, mt, :], pb[:, 0:256]), reads=[pb], writes=[memv])
            k.barrier()
        return memkT, memv

    def mem_attend(self, W, mqT, qoff, memkT, memv, mix, col0=768):
        for _ in self.mem_attend_g(W, mqT, qoff, memkT, memv, mix, col0):
            pass

    def mem_attend_g(self, W, mqT, qoff, memkT, memv, mix, col0=768, banks=None):
        k = self.k
        pe_ = W["pexp"]; ms = W["mstat"]; pT = W["pT"]
        if banks is None:
            banks = [self.nextbank(), self.nextbank()]
        for hh in range(4):
            pair, s = hh // 2, hh % 2
            pb = banks[s]
            k.op("pe", lambda e: e.matmul(pb[:, pair * 256:(pair + 1) * 256], mqT[s * 64:(s + 1) * 64, pair, qoff:qoff + 128],
                                          memkT[s * 64:(s + 1) * 64, pair, :], start=True, stop=True),
                 reads=[mqT, memkT], writes=[pb])
        for s in range(2):
            pb = banks[s]
            k.op("dve", lambda e: e.tensor_reduce(ms[:, s:s + 3:2], pb[:].rearrange("p (h m) -> p h m", h=2), AX.X, ALU.max),
                 reads=[pb], writes=[ms])
        k.op("dve", lambda e: e.tensor_single_scalar(ms[:, 4:8], ms[:, 0:4], -0.125, ALU.mult), reads=[ms], writes=[ms])
        yield
        for hh in range(4):
            pair, s = hh // 2, hh % 2
            pb = banks[s]
            k.op("act", lambda e: e.activation(pe_[:, hh, :], pb[:, pair * 256:(pair + 1) * 256], AF.Exp, bias=ms[:, 4 + hh:5 + hh],
                                               scale=0.125, accum_out=ms[:, 8 + hh:9 + hh]),
                 reads=[pb, ms], writes=[pe_, ms])
        k.op("dve", lambda e: e.reciprocal(ms[:, 12:16], ms[:, 8:12]), reads=[ms], writes=[ms])
        yield
        for half in range(2):
            pb = self.nextbank()
            for j in range(4):
                idx = half * 4 + j
                hh, mc = idx // 2, idx % 2
                k.op("pe", lambda e: e.transpose(pb[:, j * 128:(j + 1) * 128], pe_[:, hh, mc * 128:(mc + 1) * 128], self.ident),
                     reads=[pe_, self.cst], writes=[pb], inc=(j == 3))
            if half == 0:
                k.op("act", lambda e: e.copy(pT[:, 0:4, :], pb[:].rearrange("p (c t) -> p c t", c=4)), reads=[pb], writes=[pT])
            else:
                k.op("dve", lambda e: e.tensor_copy(pT[:, 4:8, :], pb[:].rearrange("p (c t) -> p c t", c=4)), reads=[pb], writes=[pT])
        yield
        pb = self.nextbank()
        for hh in range(4):
            for mc in range(2):
                k.op("pe", lambda e: e.matmul(pb[:, hh * 64:(hh + 1) * 64], pT[:, hh * 2 + mc, :], memv[:, mc, hh * 64:(hh + 1) * 64],
                                              start=(mc == 0), stop=(mc == 1)),
                     reads=[pT, memv], writes=[pb], inc=(mc == 1))
        k.op("dve", lambda e: e.tensor_tensor(mix[:, col0:col0 + 256].rearrange("p (h d) -> p h d", h=4),
                                              pb[:, 0:256].rearrange("p (h d) -> p h d", h=4),
                                              ms[:, 12:16].unsqueeze(2).to_broadcast([128, 4, 64]), ALU.mult),
             reads=[pb, ms], writes=[mix])

    def mem_work(self, st):
        sb = lambda n, s, d: self.sbuf(st, n, s, d)
        return {"pexp": sb("pexp", [128, 4, 256], F32), "mstat": sb("mstat", [128, 16], F32),
                "pT": sb("pT", [128, 8, 128], BF16)}

    def out_proj(self, mix, mixT, w_out, h, hn, dst_ap, dst_buf):
        k = self.k
        self.transpose8(mix, [(mixT, lambda half: mixT[:, half * 4:(half + 1) * 4, :])], self.nextbank(), self.nextbank())
        for half in range(2):
            pb = self.nextbank()
            for fc in range(8):
                k.op("pe", lambda e: e.matmul(pb[:], mixT[:, fc, :], w_out[:, fc, half * 512:(half + 1) * 512],
                                              start=(fc == 0), stop=(fc == 7)),
                     reads=[mixT, w_out], writes=[pb], inc=(fc == 7))
            k.op("dve", lambda e: e.tensor_tensor(hn[:, half * 512:(half + 1) * 512], h[:, half * 512:(half + 1) * 512], pb[:], ALU.add),
                 reads=[h, pb], writes=[hn])
        k.dma(dst_ap, hn[:], reads=[hn], writes=[dst_buf])

    def mixer_a_phase(self, hin, hout):
        nc, k, P = self.nc, self.k, self.P
        NTA = int(os.environ.get("MK_NTA", str(NT)))
        with ExitStack() as st:
            sb = lambda n, s, d: self.sbuf(st, n, s, d)
            memkT, memv = self.mem_kv(st, 0)
            gain = sb("gainA", [128, D], F32)
            k.dma(gain[:], P["a_norm"][0].partition_broadcast(128), writes=[gain])
            w_in = sb("w_inA", [128, 8, 3340], BF16)
            for c in range(8):
                k.dma(w_in[:, c, :], P["a_w_in"][0, c * 128:(c + 1) * 128, :], writes=[w_in], q="pool")
            w_out = sb("w_outA", [128, 8, D], BF16)
            k.dma(w_out[:], P["a_w_out"][0].rearrange("(c p) n -> p c n", p=128), writes=[w_out], q="pool")
            convw = sb("convw", [128, 18, 4], F32)
            k.dma(convw[:], P["convw"], writes=[convw])
            sc6 = sb("sc6", [128, 32], F32)
            k.dma(sc6[:, 0:6], P["a_log"][0].partition_broadcast(128), writes=[sc6])
            k.dma(sc6[:, 6:12], P["a_dt_bias"][0].partition_broadcast(128), writes=[sc6])
            k.op("act", lambda e: e.activation(sc6[:, 12:18], sc6[:, 0:6], AF.Exp), reads=[sc6], writes=[sc6])
            k.op("dve", lambda e: e.tensor_single_scalar(sc6[:, 12:18], sc6[:, 12:18], -1.0, ALU.mult), reads=[sc6], writes=[sc6])
            ogain = sb("ogain", [128, 128], F32)
            k.dma(ogain[:], P["a_out_gain"][0].partition_broadcast(128), writes=[ogain])
            MW = self.mem_work(st)
            pc = sb("pc", [128, 18, 131], F32)
            k.op("dve", lambda e: e.memset(pc[:], 0.0), writes=[pc])
            Sf = [sb("Sf%d" % h, [128, 128], F32) for h in range(6)]
            Sb = [sb("Sb%d" % h, [128, 128], BF16) for h in range(6)]
            for h in range(6):
                k.op("dve", lambda e: e.memset(Sf[h][:], 0.0), writes=[Sf[h]])
                k.op("pool", lambda e: e.memset(Sb[h][:], 0.0), writes=[Sb[h]])
            hA = sb("htA", [128, D], F32)
            xT = sb("xnTA", [128, 8, 128], BF16)
            cv = sb("cv", [128, 12, 128], F32)
            cvv = sb("cvv", [128, 6, 128], F32)
            ctmp = sb("ctmp", [128, 128], F32)
            cvjunk = View(cv, cv[:, 0:8, :].rearrange("p c t -> p (c t)"))
            sq = sb("sq", [128, 12, 128], BF16)
            rs = sb("rs", [128, 12, 128], F32)
            kn32 = sb("kn32", [128, 6, 128], F32)
            sttA = sb("sttA", [128, 4], F32)
            qkT = [sb("qkT%d" % i, [128, 6, 2, 128], BF16) for i in range(3)]
            sc = [sb("scA%d" % i, [128, 96], F32) for i in range(3)]
            gg = [sb("gg%d" % i, [128, 768], F32) for i in range(3)]
            mqT = [sb("mqTA%d" % i, [128, 2, 128], BF16) for i in range(3)]
            SLg = [sb("SLg%d" % i, [128, 128], F32) for i in range(6)]
            dm = sb("dm", [128, 6, 128], F32)
            dmi = sb("dmi", [128, 6, 128], F32)
            Wq = [[sb("W%d_%d" % (h, i), [128, 3, 128], BF16) for i in range(2)] for h in range(6)]
            Q0f = [sb("Q0f%d" % i, [128, 128], F32) for i in range(6)]
            kt = [sb("kt%d" % i, [128, 6, 128], BF16) for i in range(2)]
            vtok = [sb("vtok%d" % i, [128, 6, 128], F32) for i in range(2)]
            attnT = [sb("attnT%d" % i, [128, 6, 128], BF16) for i in range(2)]
            TT = [sb("TT%d" % i, [128, 6, 128], BF16) for i in range(2)]
            Rall = sb("Rall", [128, 6, 128], BF16)
            vnew = sb("vnewA", [128, 6, 128], BF16)
            oall = sb("oall", [128, 6, 128], F32)
            ost = sb("ost", [128, 24], F32)
            ojunk = sb("ojunk", [128, 128], F32)
            mix = sb("mixA", [128, D], F32)
            mixT = sb("mixTA", [128, 8, 128], BF16)
            hC = sb("htC", [128, D], F32)
            ident, U, SL, ones = self.ident, self.U, self.SL, self.ones
            NEGs = self.cst[:, 7, :]
            identb = self.identb
            cst, cstb = self.cst, self.cstb
            QS = float(128 ** -0.5)
            ctm = View(rs, rs[:, 0:9, :])
            rotA = [0]

            def nbA():
                rotA[0] = (rotA[0] + 1) % 6
                return self.ps[rotA[0]]
            self.nextbank = nbA
            membanks = [self.ps[6], self.ps[7]]

            def frontA(t):
                i3 = t % 3
                h = hA
                k.dma(h[:], hin[0][t * 128:(t + 1) * 128, :], reads=[hin[1][t]], writes=[h])
                self.rmsnorm(h, gain, h, cvjunk, sttA)
                yield
                self.transpose8(h, [(xT, lambda half: xT[:, half * 4:(half + 1) * 4, :])], self.nextbank(), self.nextbank())
                yield
                for g4 in range(5):
                    nf = 4 if g4 < 4 else 2
                    pb = self.nextbank()
                    for j in range(nf):
                        fc = g4 * 4 + j
                        for dc in range(8):
                            k.op("pe", lambda e: e.matmul(pb[:, j * 128:(j + 1) * 128], w_in[:, dc, fc * 128:(fc + 1) * 128], xT[:, dc, :],
                                                          start=(dc == 0), stop=(dc == 7)),
                                 reads=[w_in, xT], writes=[pb], inc=(dc == 7 and j == nf - 1))
                    dstv = pc[:, g4 * 4:g4 * 4 + nf, 3:131]
                    srcv = pb[:, 0:nf * 128].rearrange("p (c t) -> p c t", c=nf)
                    k.op("act", lambda e: e.copy(dstv, srcv), reads=[pb], writes=[pc])
                    yield
                pb = self.nextbank()
                for j in range(2):
                    for dc in range(8):
                        k.op("pe", lambda e: e.matmul(pb[:, j * 128:(j + 1) * 128], w_in[:, dc, 3084 + j * 128:3084 + (j + 1) * 128], xT[:, dc, :],
                                                      start=(dc == 0), stop=(dc == 7)),
                             reads=[w_in, xT], writes=[pb], inc=(dc == 7 and j == 1))
                k.op("act", lambda e: e.copy(mqT[i3][:], pb[:, 0:256].rearrange("p (c t) -> p c t", c=2)), reads=[pb], writes=[mqT[i3]])
                yield
                pg1 = self.nextbank()
                for dc in range(8):
                    k.op("pe", lambda e: e.matmul(pg1[:], xT[:, dc, :], w_in[:, dc, 2304:2816], start=(dc == 0), stop=(dc == 7)),
                         reads=[w_in, xT], writes=[pg1], inc=(dc == 7))
                pg2 = self.nextbank()
                for dc in range(8):
                    k.op("pe", lambda e: e.matmul(pg2[:, 0:268], xT[:, dc, :], w_in[:, dc, 2816:3084], start=(dc == 0), stop=(dc == 7)),
                         reads=[w_in, xT], writes=[pg2], inc=(dc == 7))
                g_g = gg[i3]
                k.op("act", lambda e: e.activation(g_g[:, 0:512], pg1[:], AF.Silu), reads=[pg1], writes=[g_g])
                k.op("act", lambda e: e.activation(g_g[:, 512:768], pg2[:, 0:256], AF.Silu), reads=[pg2], writes=[g_g])
                s_ = sc[i3]
                C = lambda a_, n=6: s_[:, a_:a_ + n]
                beta, tt_, ex_, sp_, g_, gcl, egc, negc, etl, egl, dd, eb_ = (C(0), C(6), C(12), C(18), C(24), C(32, 16), C(48), C(54), C(60), C(66), C(72), C(78))
                k.op("act", lambda e: e.activation(eb_, pg2[:, 256:262], AF.Exp, scale=-1.0), reads=[pg2], writes=[s_])
                k.op("dve", lambda e: e.tensor_tensor(tt_, pg2[:, 262:268], sc6[:, 6:12], ALU.add), reads=[pg2, sc6], writes=[s_])
                k.op("dve", lambda e: e.tensor_single_scalar(eb_, eb_, 1.0, ALU.add), reads=[s_], writes=[s_])
                k.op("dve", lambda e: e.reciprocal(beta, eb_), reads=[s_], writes=[s_])
                k.op("pool", lambda e: e.tensor_tensor(g_g[:].rearrange("p (h d) -> p h d", h=6), g_g[:].rearrange("p (h d) -> p h d", h=6),
                                                       ogain[:].unsqueeze(1).to_broadcast([128, 6, 128]), ALU.mult),
                     reads=[g_g, ogain], writes=[g_g])
                yield
                k.op("act", lambda e: e.activation(ex_, tt_, AF.Exp), reads=[s_], writes=[s_])
                k.op("act", lambda e: e.activation(sp_, ex_, AF.Ln, bias=1.0), reads=[s_], writes=[s_])
                k.op("dve", lambda e: e.tensor_tensor(g_, sp_, sc6[:, 12:18], ALU.mult), reads=[s_, sc6], writes=[s_])
                yield
                pgc = self.nextbank()
                k.op("pe", lambda e: e.matmul(pgc[:, 0:6], U, g_, start=True, stop=True), reads=[cst, s_], writes=[pgc])
                k.op("pe", lambda e: e.matmul(pgc[:, 8:14], ones, g_, start=True, stop=True), reads=[cst, s_], writes=[pgc])
                k.op("dve", lambda e: e.tensor_copy(gcl, pgc[:, 0:16]), reads=[pgc], writes=[s_])
                k.op("dve", lambda e: e.tensor_tensor(dd, s_[:, 40:46], s_[:, 32:38], ALU.subtract), reads=[s_], writes=[s_])
                yield
                k.op("act", lambda e: e.activation(egc, s_[:, 32:38], AF.Exp), reads=[s_], writes=[s_])
                k.op("act", lambda e: e.activation(etl, dd, AF.Exp), reads=[s_], writes=[s_])
                k.op("act", lambda e: e.activation(egl, s_[:, 40:46], AF.Exp), reads=[s_], writes=[s_])
                k.op("dve", lambda e: e.tensor_single_scalar(negc, egc, -1.0, ALU.mult), reads=[s_], writes=[s_])
                yield
                for (c0, nchk, dstb, dst) in ((0, 9, cv, cv[:, 0:9, :]), (9, 3, cv, cv[:, 9:12, :]), (12, 6, cvv, cvv[:, 0:6, :])):
                    wv = lambda j: convw[:, c0:c0 + nchk, j:j + 1].to_broadcast([128, nchk, 128])
                    k.op("dve", lambda e: e.tensor_tensor(dst, pc[:, c0:c0 + nchk, 0:128], wv(0), ALU.mult), reads=[pc, convw], writes=[dstb])
                    for j in range(1, 4):
                        tv = ctm[:, 0:nchk, :]
                        k.op("dve", lambda e: e.tensor_tensor(tv, pc[:, c0:c0 + nchk, j:j + 128], wv(j), ALU.mult), reads=[pc, convw], writes=[ctm])
                        k.op("dve", lambda e: e.tensor_tensor(dst, dst, tv, ALU.add), reads=[ctm, dstb], writes=[dstb])
                        yield
                k.op("pool", lambda e: e.tensor_copy(pc[:, :, 0:3], pc[:, :, 128:131]), reads=[pc], writes=[pc])
                k.op("act", lambda e: e.activation(cv[:], cv[:], AF.Silu), reads=[cv], writes=[cv])
                k.op("act", lambda e: e.activation(cvv[:], cvv[:], AF.Silu), reads=[cvv], writes=[cvv])
                qkv = cv
                k.op("act", lambda e: e.activation(sq[:], qkv[:, 0:12, :], AF.Square), reads=[qkv], writes=[sq])
                yield
                for g3 in range(3):
                    pb = self.nextbank()
                    k.op("pe", lambda e: e.matmul(pb[:], self.onesb, sq[:, g3 * 4:(g3 + 1) * 4, :], start=True, stop=True),
                         reads=[cstb, sq], writes=[pb])
                    k.op("act", lambda e: e.activation(rs[:, g3 * 4:(g3 + 1) * 4, :], pb[:].rearrange("p (c t) -> p c t", c=4), AF.Ln,
                                                       bias=self.epsb[:, 0:1]), reads=[pb, self.epsbuf], writes=[rs])
                    k.op("act", lambda e: e.activation(rs[:, g3 * 4:(g3 + 1) * 4, :], rs[:, g3 * 4:(g3 + 1) * 4, :], AF.Exp, scale=-0.5),
                         reads=[rs], writes=[rs])
                yield
                qk = qkT[i3]
                k.op("dve", lambda e: e.scalar_tensor_tensor(qk[:, :, 1, :], qkv[:, 0:6, :], QS, rs[:, 0:6, :], ALU.mult, ALU.mult),
                     reads=[qkv, rs], writes=[qk])
                k.op("dve", lambda e: e.tensor_tensor(kn32[:], qkv[:, 6:12, :], rs[:, 6:12, :], ALU.mult), reads=[qkv, rs], writes=[kn32])
                k.op("pool", lambda e: e.tensor_copy(qk[:, :, 0, :], kn32[:]), reads=[kn32], writes=[qk])
                yield
                b = t % 2
                s_etl = etl
                for grp in range(3):
                    pb = self.nextbank()
                    for j in range(4):
                        idx = grp * 4 + j
                        src = cvv[:, idx, :] if idx < 6 else kn32[:, idx - 6, :]
                        srcb = cvv if idx < 6 else kn32
                        k.op("pe", lambda e: e.transpose(pb[:, j * 128:(j + 1) * 128], src, ident), reads=[srcb, cst], writes=[pb], inc=(j == 3))
                    for j in range(4):
                        idx = grp * 4 + j
                        if idx < 6:
                            k.op("act", lambda e: e.copy(vtok[b][:, idx, :], pb[:, j * 128:(j + 1) * 128]), reads=[pb], writes=[vtok[b]])
                        else:
                            hh = idx - 6
                            k.op("dve", lambda e: e.tensor_scalar(kt[b][:, hh, :], pb[:, j * 128:(j + 1) * 128], s_etl[:, hh:hh + 1], None, ALU.mult),
                                 reads=[pb, s_], writes=[kt[b]])
                    yield

            def frontB(t):
                i3 = t % 3
                b = t % 2
                s_ = sc[i3]
                C = lambda a_, n=6: s_[:, a_:a_ + n]
                beta, g_ = C(0), C(24)
                qk = qkT[i3]
                for hh in range(6):
                    sg = SLg[hh]
                    k.op("dve", lambda e: e.tensor_scalar(sg[:], SL, g_[:, hh:hh + 1], None, ALU.mult), reads=[cst, s_], writes=[sg])
                yield
                for hp in range(3):
                    pb = self.nextbank()
                    for j in range(2):
                        hh = hp * 2 + j
                        sg = SLg[hh]
                        k.op("pe", lambda e: e.matmul(pb[:, j * 128:(j + 1) * 128], sg[:], U, start=True, stop=False),
                             reads=[sg, cst], writes=[pb], inc=False)
                        k.op("pe", lambda e: e.matmul(pb[:, j * 128:(j + 1) * 128], ident, NEGs, start=False, stop=True),
                             reads=[cst], writes=[pb])
                    k.op("act", lambda e: e.activation(dm[:, hp * 2:hp * 2 + 2, :], pb[:, 0:256].rearrange("p (c t) -> p c t", c=2), AF.Exp),
                         reads=[pb], writes=[dm])
                    yield
                k.op("pool", lambda e: e.tensor_tensor(dmi[:], dm[:], ident.unsqueeze(1).to_broadcast([128, 6, 128]), ALU.add),
                     reads=[dm, cst], writes=[dmi])
                for hh in range(6):
                    pb = self.nextbank()
                    W0 = Wq[hh][0]
                    qf = Q0f[hh]
                    k.op("pe", lambda e: e.matmul(pb[:, 0:256], qk[:, hh, 0, :], qk[:, hh, :, :].rearrange("p a t -> p (a t)"),
                                                  start=True, stop=True), reads=[qk], writes=[pb])
                    k.op("dve", lambda e: e.scalar_tensor_tensor(qf[:], pb[:, 0:128], beta[:, hh:hh + 1], dm[:, hh, :], ALU.mult, ALU.mult),
                         reads=[pb, s_, dm], writes=[qf])
                    k.op("dve", lambda e: e.tensor_tensor(attnT[b][:, hh, :], pb[:, 128:256], dmi[:, hh, :], ALU.mult),
                         reads=[pb, dmi], writes=[attnT[b]])
                    if hh % 3 == 2:
                        yield
                for hh in range(6):
                    W0 = Wq[hh][0]
                    qf = Q0f[hh]
                    k.op("pool", lambda e: e.tensor_copy(W0[:, 0, :], qf[:]), reads=[qf], writes=[W0])
                    k.op("pool", lambda e: e.tensor_tensor(Wq[hh][1][:, 1, :], ident, qf[:], ALU.subtract), reads=[cst, qf], writes=[Wq[hh][1]])
                    pb2 = self.nextbank()
                    k.op("pe", lambda e: e.transpose(pb2[:, 0:128], qf[:], ident), reads=[qf, cst], writes=[pb2])
                    k.op("act", lambda e: e.copy(W0[:, 2, :], pb2[:, 0:128]), reads=[pb2], writes=[W0])
                    if hh % 3 == 2:
                        yield
                for lvl in range(7):
                    for hh in range(6):
                        Wc = Wq[hh][lvl % 2]
                        Wn = Wq[hh][(lvl + 1) % 2]
                        pb = self.nextbank()
                        Qk, Xk, Pk = Wc[:, 0, :], Wc[:, 1, :], Wc[:, 2, :]
                        mm = lambda out, l_, r_, st_, sp_2, inc_: k.op(
                            "pe", lambda e: e.matmul(out, l_, r_, start=st_, stop=sp_2), reads=[Wc, cstb], writes=[pb], inc=inc_)
                        if lvl == 0:
                            mm(pb[:, 0:128], Pk, Qk, True, True, False)
                            mm(pb[:, 256:384], Qk, Pk, True, True, True)
                            k.op("act", lambda e: e.copy(Wn[:, 0, :], pb[:, 0:128]), reads=[pb], writes=[Wn])
                            k.op("act", lambda e: e.copy(Wn[:, 2, :], pb[:, 256:384]), reads=[pb], writes=[Wn])
                        elif lvl < 6:
                            mm(pb[:, 0:128], Pk, Qk, True, True, False)
                            mm(pb[:, 128:256], Pk, Xk, True, False, False)
                            mm(pb[:, 128:256], identb, Xk, False, True, False)
                            mm(pb[:, 256:384], Qk, Pk, True, True, True)
                            k.op("act", lambda e: e.copy(Wn[:], pb[:, 0:384].rearrange("p (c t) -> p c t", c=3)), reads=[pb], writes=[Wn])
                        else:
                            mm(pb[:, 128:256], Pk, Xk, True, False, False)
                            mm(pb[:, 128:256], identb, Xk, False, True, True)
                            k.op("act", lambda e: e.copy(TT[b][:, hh, :], pb[:, 128:256]), reads=[pb], writes=[TT[b]])
                        if hh % 3 == 2:
                            yield

            def back(t):
                i3 = t % 3
                b = t % 2
                s_ = sc[i3]
                C = lambda a_, n=6: s_[:, a_:a_ + n]
                beta, egc, negc, egl = C(0), C(48), C(54), C(66)
                qk, g_g = qkT[i3], gg[i3]
                k.dma(hC[:], hin[0][t * 128:(t + 1) * 128, :], reads=[hin[1][t]], writes=[hC])
                for hp in range(3):
                    pb = self.nextbank()
                    for j in range(2):
                        hh = hp * 2 + j
                        o = j * 256
                        k.op("pe", lambda e: e.matmul(pb[:, o:o + 128], qk[:, hh, 0, :], Sb[hh][:], start=True, stop=True),
                             reads=[qk, Sb[hh]], writes=[pb], inc=False)
                        k.op("pe", lambda e: e.matmul(pb[:, o + 128:o + 256], qk[:, hh, 1, :], Sb[hh][:], start=True, stop=True),
                             reads=[qk, Sb[hh]], writes=[pb], inc=(j == 1))
                    for j in range(2):
                        hh = hp * 2 + j
                        o = j * 256
                        k.op("dve", lambda e: e.scalar_tensor_tensor(Rall[:, hh, :], pb[:, o:o + 128], negc[:, hh:hh + 1], vtok[b][:, hh, :], ALU.mult, ALU.add),
                             reads=[pb, s_, vtok[b]], writes=[Rall])
                        k.op("dve", lambda e: e.tensor_scalar(oall[:, hh, :], pb[:, o + 128:o + 256], egc[:, hh:hh + 1], None, ALU.mult),
                             reads=[pb, s_], writes=[oall])
                yield
                for (h0, nh) in ((0, 4), (4, 2)):
                    pb = self.nextbank()
                    for j in range(nh):
                        hh = h0 + j
                        k.op("pe", lambda e: e.matmul(pb[:, j * 128:(j + 1) * 128], TT[b][:, hh, :], Rall[:, hh, :], start=True, stop=True),
                             reads=[TT[b], Rall], writes=[pb], inc=(j == nh - 1))
                    k.op("dve", lambda e: e.tensor_tensor(vnew[:, h0:h0 + nh, :], pb[:, 0:nh * 128].rearrange("p (c t) -> p c t", c=nh),
                                                          beta[:, h0:h0 + nh].unsqueeze(2).to_broadcast([128, nh, 128]), ALU.mult),
                         reads=[pb, s_], writes=[vnew])
                yield
                for hp in range(3):
                    pb = self.nextbank()
                    for j in range(2):
                        hh = hp * 2 + j
                        o = j * 256
                        k.op("pe", lambda e: e.matmul(pb[:, o:o + 128], attnT[b][:, hh, :], vnew[:, hh, :], start=True, stop=True),
                             reads=[attnT[b], vnew], writes=[pb], inc=False)
                        k.op("pe", lambda e: e.matmul(pb[:, o + 128:o + 256], kt[b][:, hh, :], vnew[:, hh, :], start=True, stop=True),
                             reads=[kt[b], vnew], writes=[pb], inc=(j == 1))
                    for j in range(2):
                        hh = hp * 2 + j
                        o = j * 256
                        k.op("dve", lambda e: e.scalar_tensor_tensor(Sb[hh][:], Sf[hh][:], egl[:, hh:hh + 1], pb[:, o + 128:o + 256], ALU.mult, ALU.add),
                             reads=[Sf[hh], s_, pb], writes=[Sb[hh]])
                        k.op("dve", lambda e: e.scalar_tensor_tensor(Sf[hh][:], Sf[hh][:], egl[:, hh:hh + 1], pb[:, o + 128:o + 256], ALU.mult, ALU.add),
                             reads=[Sf[hh], s_, pb], writes=[Sf[hh]])
                        k.op("dve", lambda e: e.tensor_tensor(oall[:, hh, :], oall[:, hh, :], pb[:, o:o + 128], ALU.add),
                             reads=[oall, pb], writes=[oall])
                yield
                for hh in range(6):
                    k.op("act", lambda e: e.activation(ojunk[:], oall[:, hh, :], AF.Square, accum_out=ost[:, hh:hh + 1]),
                         reads=[oall], writes=[ojunk, ost])
                k.op("act", lambda e: e.activation(ost[:, 8:14], ost[:, 0:6], AF.Ln, bias=self.epsb[:, 0:1], scale=1.0 / 128),
                     reads=[ost, self.epsbuf], writes=[ost])
                k.op("act", lambda e: e.activation(ost[:, 16:22], ost[:, 8:14], AF.Exp, scale=-0.5), reads=[ost], writes=[ost])
                yield
                for hh in range(6):
                    k.op("dve", lambda e: e.scalar_tensor_tensor(mix[:, hh * 128:(hh + 1) * 128], oall[:, hh, :], ost[:, 16 + hh:17 + hh],
                                                                 g_g[:, hh * 128:(hh + 1) * 128], ALU.mult, ALU.mult),
                         reads=[oall, ost, g_g], writes=[mix])
                yield
                yield from self.mem_attend_g(MW, mqT[i3], 0, memkT, memv, mix, banks=membanks)
                yield
                self.transpose8(mix, [(mixT, lambda half: mixT[:, half * 4:(half + 1) * 4, :])], self.nextbank(), self.nextbank())
                yield
                for half in range(2):
                    pb = self.nextbank()
                    for fc in range(8):
                        k.op("pe", lambda e: e.matmul(pb[:], mixT[:, fc, :], w_out[:, fc, half * 512:(half + 1) * 512],
                                                      start=(fc == 0), stop=(fc == 7)),
                             reads=[mixT, w_out], writes=[pb], inc=(fc == 7))
                    k.op("dve", lambda e: e.tensor_tensor(hC[:, half * 512:(half + 1) * 512], hC[:, half * 512:(half + 1) * 512], pb[:], ALU.add),
                         reads=[hC, pb], writes=[hC])
                k.dma(hout[0][t * 128:(t + 1) * 128, :], hC[:], reads=[hC], writes=[hout[1][t]])

            AMULT = [int(v) for v in os.environ.get("MK_AMULT", "1,1,1").split(",")]

            def run(*gens):
                gens = [[g, m] for g, m in zip(gens, AMULT) if g is not None]
                while gens:
                    for item in list(gens):
                        for _ in range(item[1]):
                            try:
                                next(item[0])
                            except StopIteration:
                                gens.remove(item)
                                break

            mk = lambda fn, t: fn(t) if 0 <= t < NTA else None
            for step in range(-2, NTA):
                run(mk(back, step), mk(frontB, step + 1), mk(frontA, step + 2))
            k.barrier()
            del self.nextbank

    def mixer_b_phase(self, hin, hout):
        nc, k, P = self.nc, self.k, self.P
        NGB = int(os.environ.get("MK_NGB", "8"))
        NHB = int(os.environ.get("MK_NHB", "12"))
        NDUM = int(os.environ.get("MK_NDUM", "1"))
        with ExitStack() as st:
            sb = lambda n, s, d: self.sbuf(st, n, s, d)
            memkT, memv = self.mem_kv(st, 1)
            win_d = self.dscratch("b_w_in_bf", [D, D], BF16)
            wout_d = self.dscratch("b_w_out_bf", [D, D], BF16)
            wdb = [Buf(None, "win_d"), Buf(None, "wout_d")]
            k.dma(win_d, P["b_w_in"][0], writes=[wdb[0]], q="pool")
            k.dma(wout_d, P["b_w_out"][0], writes=[wdb[1]], q="pool")
            KT = sb("KT", [128, 6, S], BF16)
            Vt = sb("Vt", [128, NT, 768], BF16)
            ht = [sb("htB%d" % i, [128, D], F32) for i in range(2)]
            xn = sb("xnB", [128, D], F32)
            stt = [sb("sttB%d" % i, [128, 4], F32) for i in range(2)]
            with ExitStack() as st1:
                sb1 = lambda n, s, d: self.sbuf(st1, n, s, d)
                kvg = sb1("kvg", [128, D], F32)
                k.dma(kvg[:], P["kv_norm"].partition_broadcast(128), writes=[kvg])
                w_kv = sb1("w_kv", [128, 8, 1536], BF16)
                for c in range(8):
                    k.dma(w_kv[:, c, :], P["w_kv"][c * 128:(c + 1) * 128, :], writes=[w_kv], q="pool")
                xTg1 = [sb1("xTg1_%d" % i, [128, 8, 512], BF16) for i in range(2)]

                def p1_x(g):
                    xTg_ = xTg1[g % 2]
                    for tt in range(4):
                        t = g * 4 + tt
                        b = tt % 2
                        h = ht[b]
                        k.dma(h[:], hin[0][t * 128:(t + 1) * 128, :], reads=[hin[1][t]], writes=[h])
                        self.rmsnorm(h, kvg, xn, xn, stt[b])
                        yield
                        self.transpose8(xn, [(xTg_, lambda half: xTg_[:, half * 4:(half + 1) * 4, tt * 128:(tt + 1) * 128])],
                                        self.nextbank(), self.nextbank())
                        yield

                def p1_y(g):
                    xTg_ = xTg1[g % 2]
                    for fc in range(6):
                        pb = self.nextbank()
                        for dc in range(8):
                            k.op("pe", lambda e: e.matmul(pb[:], w_kv[:, dc, fc * 128:(fc + 1) * 128], xTg_[:, dc, :],
                                                          start=(dc == 0), stop=(dc == 7)),
                                 reads=[w_kv, xTg_], writes=[pb], inc=(dc == 7))
                        if fc % 2 == 0:
                            k.op("act", lambda e: e.copy(KT[:, fc, g * 512:(g + 1) * 512], pb[:]), reads=[pb], writes=[KT])
                        else:
                            k.op("dve", lambda e: e.tensor_copy(KT[:, fc, g * 512:(g + 1) * 512], pb[:]), reads=[pb], writes=[KT])
                        yield
                    for tt in range(4):
                        t = g * 4 + tt
                        for half, (c0, c1) in enumerate(((0, 512), (512, 768))):
                            pb = self.nextbank()
                            for dc in range(8):
                                k.op("pe", lambda e: e.matmul(pb[:, 0:c1 - c0], xTg_[:, dc, tt * 128:(tt + 1) * 128], w_kv[:, dc, 768 + c0:768 + c1],
                                                              start=(dc == 0), stop=(dc == 7)),
                                     reads=[w_kv, xTg_], writes=[pb], inc=(dc == 7))
                            if half == 0:
                                k.op("dve", lambda e: e.tensor_copy(Vt[:, t, c0:c1], pb[:, 0:c1 - c0]), reads=[pb], writes=[Vt])
                            else:
                                k.op("act", lambda e: e.copy(Vt[:, t, c0:c1], pb[:, 0:c1 - c0]), reads=[pb], writes=[Vt])
                        yield

                def rr1(*gens):
                    gens = [g_ for g_ in gens if g_ is not None]
                    while gens:
                        for g_ in list(gens):
                            try:
                                next(g_)
                            except StopIteration:
                                gens.remove(g_)

                rr1(p1_x(0))
                for g in range(NT // 4):
                    rr1(p1_y(g), p1_x(g + 1) if g + 1 < NT // 4 else None)
                k.barrier()
            bg = sb("bgain", [128, D], F32)
            k.dma(bg[:], P["b_norm"][0].partition_broadcast(128), writes=[bg])
            wB = sb("wB", [128, 8, D], BF16)
            xTg = sb("xTg", [128, 8, 512], BF16)
            qT = [sb("qT%d" % i, [128, 6, 512], BF16) for i in range(2)]
            mqT = [sb("mqTB%d" % i, [128, 2, 512], BF16) for i in range(3)]
            mixTg = [sb("mixTg%d" % i, [128, 8, 512], BF16) for i in range(2)]
            Eb = [sb("Eb%d" % i, [128, 512], F32) for i in range(3)]
            spb = [sb("spb%d" % i, [128, 512], BF16) for i in range(3)]
            eab = [sb("eab%d" % i, [128, 512], F32) for i in range(2)]
            ab = [sb("ab%d" % i, [128, 512], BF16) for i in range(2)]
            MW = self.mem_work(st)
            mmB = sb("mmB", [128, 256], F32)
            NGEb = self.cstb[:, 5, :]
            NLTb = self.cstb[:, 6, :]
            strictTb = self.cstb[:, 3, :]
            cstb = self.cstb
            PZ = [self.ps[0], self.ps[1]]
            PC = [self.ps[2], self.ps[3]]
            PO = [self.ps[4], self.ps[5]]
            rot = [0]

            def nb():
                rot[0] = (rot[0] + 1) % 2
                return self.ps[6 + rot[0]]
            self.nextbank = nb
            wcur = [None]

            def load_wB(which):
                if wcur[0] != which:
                    srcd = win_d if which == "in" else wout_d
                    k.dma(wB[:], srcd.rearrange("(c p) n -> p c n", p=128), reads=[wdb[0 if which == "in" else 1]], writes=[wB])
                    wcur[0] = which

            def prologue(g):
                load_wB("in")
                qTg, mqTg = qT[g % 2], mqT[g % 3]
                h = ht[0]
                for tt in range(4):
                    t = g * 4 + tt
                    k.dma(h[:], hin[0][t * 128:(t + 1) * 128, :], reads=[hin[1][t]], writes=[h])
                    self.rmsnorm(h, bg, xn, xn, stt[0])
                    yield
                    self.transpose8(xn, [(xTg, lambda half: xTg[:, half * 4:(half + 1) * 4, tt * 128:(tt + 1) * 128])], nb(), nb(),
                                    evac=("dve", "dve"))
                    yield
                for fc in range(8):
                    pb = nb()
                    for dc in range(8):
                        k.op("pe", lambda e: e.matmul(pb[:], wB[:, dc, fc * 128:(fc + 1) * 128], xTg[:, dc, :], start=(dc == 0), stop=(dc == 7)),
                             reads=[wB, xTg], writes=[pb], inc=(dc == 7))
                    if fc < 6:
                        k.op("dve", lambda e: e.tensor_single_scalar(qTg[:, fc, :], pb[:], 0.125, ALU.mult), reads=[pb], writes=[qTg])
                    else:
                        k.op("dve", lambda e: e.tensor_copy(mqTg[:, fc - 6, :], pb[:]), reads=[pb], writes=[mqTg])
                    yield

            def epilogue(g):
                load_wB("out")
                mixg, mqTg = mixTg[g % 2], mqT[g % 3]
                h = ht[1]
                for tt in range(4):
                    t = g * 4 + tt
                    k.dma(h[:], hin[0][t * 128:(t + 1) * 128, :], reads=[hin[1][t]], writes=[h])
                    yield from self.mem_attend_g(MW, mqTg, tt * 128, memkT, memv, mmB, col0=0)
                    yield
                    pb = nb()
                    for j in range(2):
                        k.op("pe", lambda e: e.transpose(pb[:, j * 128:(j + 1) * 128], mmB[:, j * 128:(j + 1) * 128], self.ident),
                             reads=[mmB, self.cst], writes=[pb], inc=(j == 1))
                    k.op("dve", lambda e: e.tensor_copy(mixg[:, 6:8, tt * 128:(tt + 1) * 128], pb[:, 0:256].rearrange("p (c t) -> p c t", c=2)),
                         reads=[pb], writes=[mixg])
                    yield
                    for half in range(2):
                        pb = nb()
                        for fc in range(8):
                            k.op("pe", lambda e: e.matmul(pb[:], mixg[:, fc, tt * 128:(tt + 1) * 128], wB[:, fc, half * 512:(half + 1) * 512],
                                                          start=(fc == 0), stop=(fc == 7)),
                                 reads=[mixg, wB], writes=[pb], inc=(fc == 7))
                        k.op("dve", lambda e: e.tensor_tensor(h[:, half * 512:(half + 1) * 512], h[:, half * 512:(half + 1) * 512], pb[:], ALU.add),
                             reads=[h, pb], writes=[h])
                        yield
                    k.dma(hout[0][t * 128:(t + 1) * 128, :], h[:], reads=[h], writes=[hout[1][t]])

            def side_thread(g):
                if g >= 1:
                    yield from epilogue(g - 1)
                if g + 1 < NGB:
                    yield from prologue(g + 1)

            for _ in prologue(0):
                pass
            for g in range(NGB):
                qTg, mixg = qT[g % 2], mixTg[g % 2]
                side = side_thread(g)
                items = [(2 * p + s, kb) for p in range(NHB // 2) for kb in range(4 * g + 3, -1, -1) for s in range(2)]

                def geom(i):
                    hh, kb = items[i]
                    r = max(kb - 4 * g, 0)
                    return hh, kb, hh // 2, hh % 2, r * 128, kb >= 4 * g

                def s1_pe(i):
                    hh, kb, fc, s, c0, diag = geom(i)
                    ps_ = slice(s * 64, (s + 1) * 64)
                    cs = slice(c0, 512)
                    pz = PZ[i % 2]
                    k.op("pe", lambda e: e.matmul(pz[:, cs], KT[ps_, fc, kb * 128:(kb + 1) * 128], qTg[ps_, fc, cs], start=True, stop=True),
                         reads=[KT, qTg], writes=[pz])

                def s1_act(i):
                    hh, kb, fc, s, c0, diag = geom(i)
                    cs = slice(c0, 512)
                    pz, E, sp = PZ[i % 2], Eb[i % 3], spb[i % 3]
                    k.op("act", lambda e: e.activation(E[:, cs], pz[:, cs], AF.Exp), reads=[pz], writes=[E])
                    k.op("act", lambda e: e.activation(sp[:, cs], E[:, cs], AF.Ln, bias=1.0), reads=[E], writes=[sp])
                    if diag:
                        k.op("dve", lambda e: e.tensor_tensor(sp[:, c0:c0 + 128], sp[:, c0:c0 + 128], strictTb, ALU.mult),
                             reads=[sp, cstb], writes=[sp])

                def s2_peA(i):
                    hh, kb, fc, s, c0, diag = geom(i)
                    cs = slice(c0, 512)
                    C, sp = PC[s], spb[i % 3]
                    if kb == 4 * g + 3:
                        k.op("dve", lambda e: e.memset(C[:], 0.0), writes=[C])
                    k.op("pe", lambda e: e.matmul(C[:, cs], NGEb, sp[:, cs], start=False, stop=False, skip_group_check=True),
                         reads=[cstb, sp], writes=[C])

                def s2_act(i):
                    hh, kb, fc, s, c0, diag = geom(i)
                    cs = slice(c0, 512)
                    C, ea_ = PC[s], eab[i % 2]
                    k.op("act", lambda e: e.activation(ea_[:, cs], C[:, cs], AF.Exp), reads=[C], writes=[ea_])

                def s2_peB(i):
                    hh, kb, fc, s, c0, diag = geom(i)
                    cs = slice(c0, 512)
                    C, sp = PC[s], spb[i % 3]
                    if kb > 0:
                        k.op("pe", lambda e: e.matmul(C[:, cs], NLTb, sp[:, cs], start=False, stop=False, skip_group_check=True),
                             reads=[cstb, sp], writes=[C])

                def s3_pool(i):
                    hh, kb, fc, s, c0, diag = geom(i)
                    cs = slice(c0, 512)
                    E, ea_, a_ = Eb[i % 3], eab[i % 2], ab[i % 2]
                    k.op("dve", lambda e: e.tensor_tensor(a_[:, cs], E[:, cs], ea_[:, cs], ALU.mult), reads=[E, ea_], writes=[a_])
                    if diag:
                        k.op("dve", lambda e: e.tensor_tensor(a_[:, c0:c0 + 128], a_[:, c0:c0 + 128], strictTb, ALU.mult),
                             reads=[a_, cstb], writes=[a_])

                def s3_pe(i):
                    hh, kb, fc, s, c0, diag = geom(i)
                    cs = slice(c0, 512)
                    a_ = ab[i % 2]
                    po = PO[fc % 2]
                    if kb == 4 * g + 3 and s == 0:
                        k.op("dve", lambda e: e.memset(po[:], 0.0), writes=[po])
                    vblk = Vt[:, kb, hh * 64:(hh + 1) * 64]
                    if s == 0:
                        k.op("pe", lambda e: e.matmul(po[0:64, cs], vblk, a_[:, cs], start=False, stop=False, skip_group_check=True),
                             reads=[Vt, a_], writes=[po])
                    else:
                        k.op("pe", lambda e: e.matmul(po[64:128, cs], vblk, a_[:, cs], start=False, stop=False, skip_group_check=True,
                                                      tile_position=(0, 64)), reads=[Vt, a_], writes=[po])
                    if kb == 0 and s == 1:
                        k.op("act", lambda e: e.copy(mixg[:, fc, :], po[:]), reads=[po], writes=[mixg])

                n_it = len(items)
                ok = lambda i: 0 <= i < n_it
                dumrhs = cstb[:, 0:4, :].rearrange("p c t -> p (c t)")
                stride = max(1, n_it // 72)
                for step in range(-3, n_it + 1):
                    i0, i1, i2_, i3, i4 = step + 3, step + 2, step + 1, step, step - 1
                    if ok(i3):
                        s3_pool(i3)
                    if ok(i0):
                        for _d in range(NDUM):
                            k.op("pe", lambda e: e.matmul(PZ[i0 % 2][:], NGEb, dumrhs, start=True, stop=True),
                                 reads=[cstb], writes=[PZ[i0 % 2]], inc=False)
                        s1_pe(i0)
                    if ok(i2_):
                        s2_peA(i2_)
                    if ok(i4):
                        s3_pe(i4)
                    if ok(i1):
                        s1_act(i1)
                    if ok(i2_):
                        s2_act(i2_)
                    if ok(i3):
                        s2_peB(i3)
                    if side is not None and step >= 0 and step % stride == 0:
                        try:
                            next(side)
                        except StopIteration:
                            side = None
                if side is not None:
                    for _ in side:
                        pass
            for _ in epilogue(NGB - 1):
                pass
            k.barrier()
            del self.nextbank

    def build(self, phases):
        nc, k = self.nc, self.k
        P = self.P = {}
        shapes = dict(
            x=[S, D], mem=[256, D], a_norm=[1, D], a_w_in=[1, D, 3340], a_conv=[1, 4, 2304],
            a_log=[1, 6], a_dt_bias=[1, 6], a_out_gain=[1, 128], a_w_out=[1, D, D],
            kv_norm=[D], w_kv=[D, 1536], b_norm=[1, D], b_w_in=[1, D, D], b_w_out=[1, D, D],
            mem_norm=[2, D], w_mem_kv=[2, D, 512], ffn_norm=[2, D], w_group=[2, D, 4], b_group=[2, 4],
            w_router=[2, D, 16], b_router=[2, 16], w1=[2, 16, D, 256], w3=[2, 16, D, 256],
            w2=[2, 16, 256, D], final_norm=[D], wgr=[2, 128, 8, 20], rbias=[2, 20], convw=[128, 18, 4])
        for n, s in shapes.items():
            P[n] = self.din(n, s)
        out = nc.dram_tensor("out", [S, D], F32, kind="ExternalOutput").ap()
        mkbufs = lambda nm: [Buf(None, "%s%d" % (nm, i)) for i in range(NT)]
        hx = (P["x"], mkbufs("x"))
        hA = (self.dscratch("hA", [S, D]), mkbufs("hA"))
        hB = (self.dscratch("hB", [S, D]), mkbufs("hB"))
        ho = (out, mkbufs("out"))
        with ExitStack() as gst:
            self.load_consts(gst)
            self.epsbuf = self.sbuf(gst, "epsb", [128, 1], F32)
            self.epsb = self.epsbuf
            k.op("dve", lambda e: e.memset(self.epsbuf[:], EPS), writes=[self.epsbuf])
            cur = hx
            seq = {"A": hA, "M0": hB, "B": hA, "M1": ho}
            for ph in phases:
                dst = seq[ph] if ph != phases[-1] else ho
                if ph == "M0":
                    self.moe_phase(0, cur, dst, final=False)
                elif ph == "M1":
                    self.moe_phase(1, cur, dst, final=True)
                elif ph == "A":
                    self.mixer_a_phase(cur, dst)
                elif ph == "B":
                    self.mixer_b_phase(cur, dst)
                cur = dst
            for b in ho[1]:
                if b.lw is not None:
                    k._wait("sp", b.lw)
        return nc


def make_consts():
    c = np.zeros((128, 8, 128), np.float32)
    i = np.arange(128)
    c[:, 0, :] = np.eye(128)
    c[:, 1, :] = (i[:, None] <= i[None, :])
    c[:, 2, :] = (i[:, None] > i[None, :])
    c[:, 3, :] = (i[:, None] < i[None, :])
    c[:, 4, :] = 1.0
    c[:, 5, :] = -(i[:, None] >= i[None, :]).astype(np.float32)
    c[:, 6, :] = -(i[:, None] < i[None, :]).astype(np.float32)
    c[:, 7, :] = -30000.0 * (i[:, None] >= i[None, :])
    return c


_CACHE = {}


def run(inputs, phases=("A", "M0", "B", "M1"), ncores=NCORES, trace=False):
    key = tuple(phases)
    if key not in _CACHE:
        mk = MK(phases)
        _CACHE[key] = mk.build(list(phases))
    nc = _CACHE[key]
    consts = make_consts()
    inputs = dict(inputs)
    wg = np.concatenate([np.asarray(inputs["w_group"]), np.asarray(inputs["w_router"])], axis=2)
    inputs["wgr"] = np.ascontiguousarray(wg.reshape(2, 8, 128, 20).transpose(0, 2, 1, 3))
    inputs["rbias"] = np.concatenate([np.asarray(inputs["b_group"]), np.asarray(inputs["b_router"])], axis=1)
    cw = np.asarray(inputs["a_conv"])[0]
    inputs["convw"] = np.ascontiguousarray(cw.reshape(4, 18, 128).transpose(2, 1, 0))
    in_maps = []
    for c in range(ncores):
        m = {"consts": consts}
        for n, v in inputs.items():
            v = np.asarray(v)
            if n in ("x", "mem"):
                m[n] = np.ascontiguousarray(v[c])
            else:
                m[n] = np.ascontiguousarray(v, dtype=np.float32)
        in_maps.append(m)
    res = run_bass_kernel_spmd(nc, in_maps, core_ids=list(range(ncores)), trace=trace)
    outs = np.stack([r["out"] for r in res.results], axis=0)
    return outs, res


def kernel(**inputs):
    outs, _ = run(inputs)
    return outs.astype(np.float32)
```

```python
from contextlib import ExitStack
import os
import numpy as np
import concourse.bass as bass
import concourse.mybir as mybir
from concourse.bass_utils import run_bass_kernel_spmd

F32 = mybir.dt.float32
BF16 = mybir.dt.bfloat16
AF = mybir.ActivationFunctionType
ALU = mybir.AluOpType
AX = mybir.AxisListType

S = 4096
D = 1024
NT = S // 128
EPS = 1e-6
NCORES = 8


class Buf:
    __slots__ = ("ap", "name", "_lw", "_rd", "_excl")
    lw = property(lambda self: self._lw, lambda self, v: setattr(self, "_lw", v))
    rd = property(lambda self: self._rd, lambda self, v: setattr(self, "_rd", v))
    excl = property(lambda self: self._excl, lambda self, v: setattr(self, "_excl", v))

    def __init__(self, ap, name="", excl=False):
        self.ap = ap
        self.name = name
        self.excl = excl
        self.lw = None
        self.rd = []

    def __getitem__(self, idx):
        return self.ap[idx]


class View(Buf):
    __slots__ = ("parent",)

    def __init__(self, parent, ap):
        self.parent = parent
        self.ap = ap
        self.name = parent.name

    lw = property(lambda self: self.parent.lw, lambda self, v: setattr(self.parent, "lw", v))
    rd = property(lambda self: self.parent.rd, lambda self, v: setattr(self.parent, "rd", v))
    excl = property(lambda self: self.parent.excl, lambda self, v: None)


class K:
    NDMA = 48

    def __init__(self, nc):
        self.nc = nc
        self.eng = {"pe": nc.tensor, "act": nc.scalar, "dve": nc.vector,
                    "pool": nc.gpsimd, "sp": nc.sync}
        self.sem = {e: nc.alloc_semaphore("s_" + e) for e in ("pe", "act", "dve", "pool")}
        self.cnt = {e: 0 for e in self.sem}
        self.waited = {}
        self.dsem = [nc.alloc_semaphore("d%d" % i) for i in range(self.NDMA)]
        self.dcnt = [0] * self.NDMA
        self.dnext = 0
        self.dnext_sw = 0
        self.nins = 0

    def _semh(self, key):
        return self.sem[key] if isinstance(key, str) else self.dsem[key]

    def _wait(self, e, dep):
        key, val = dep
        if key == e and e == "pe":
            return
        w = self.waited.get((e, key), 0)
        if w >= val:
            return
        self.eng[e].wait_ge(self._semh(key), val)
        self.nins += 1
        self.waited[(e, key)] = val

    def _deps(self, e, reads, writes):
        best = {}
        for r in reads:
            if r.lw is not None:
                if best.get(r.lw[0], 0) < r.lw[1]:
                    best[r.lw[0]] = r.lw[1]
            if r.excl:
                for key, val in r.rd:
                    if key != e and best.get(key, 0) < val:
                        best[key] = val
        for w in writes:
            if w.lw is not None:
                if best.get(w.lw[0], 0) < w.lw[1]:
                    best[w.lw[0]] = w.lw[1]
            for key, val in w.rd:
                if best.get(key, 0) < val:
                    best[key] = val
        for key, val in best.items():
            self._wait(e, (key, val))

    def _mark(self, tag, reads, writes):
        for r in reads:
            r.rd.append(tag)
            if len(r.rd) > 64:
                best = {}
                for key, val in r.rd:
                    if best.get(key, 0) < val:
                        best[key] = val
                r.rd = list(best.items())
        for w in writes:
            w.lw = tag
            w.rd = []

    def op(self, e, fn, reads=(), writes=(), inc=True):
        self._deps(e, reads, writes)
        ins = fn(self.eng[e])
        self.nins += 1
        if inc:
            ins.then_inc(self.sem[e], 1)
            self.cnt[e] += 1
            tag = (e, self.cnt[e])
        else:
            tag = (e, self.cnt[e] + 1)
        self._mark(tag, reads, writes)
        return ins

    def dma(self, out, in_, reads=(), writes=(), q="sp", **kw):
        if q == "pool":
            slot = 32 + self.dnext_sw
            self.dnext_sw = (self.dnext_sw + 1) % (self.NDMA - 32)
        else:
            slot = self.dnext
            self.dnext = (self.dnext + 1) % 32
        if self.dcnt[slot] > 0:
            self._wait(q, (slot, 16 * self.dcnt[slot]))
        self._deps(q, reads, writes)
        ins = self.eng[q].dma_start(out=out, in_=in_, **kw)
        self.nins += 1
        ins.then_inc(self.dsem[slot], 16)
        self.dcnt[slot] += 1
        tag = (slot, 16 * self.dcnt[slot])
        self._mark(tag, reads, writes)
        return tag

    def barrier(self):
        for e in ("pe", "act", "dve", "pool", "sp"):
            for e2 in ("pe", "act", "dve", "pool"):
                if e2 != e and self.cnt[e2] > 0:
                    self._wait(e, (e2, self.cnt[e2]))
            for slot in range(self.NDMA):
                if self.dcnt[slot] > 0:
                    self._wait(e, (slot, 16 * self.dcnt[slot]))


class MK:
    def __init__(self, phases, h0_from_input=True):
        self.nc = nc = bass.Bass("TRN2", target_bir_lowering=False)
        self.k = K(nc)
        self.uid = 0
        self.ins = {}
        self.ps = [Buf(nc.alloc_psum_tensor("psb%d" % i, [128, 512], F32).ap(), "ps%d" % i, excl=True)
                   for i in range(8)]

    def din(self, name, shape):
        ap = self.nc.dram_tensor(name, list(shape), F32, kind="ExternalInput").ap()
        self.ins[name] = ap
        return ap

    def dscratch(self, name, shape, dt=F32):
        return self.nc.dram_tensor(name, list(shape), dt, kind="Internal").ap()

    def sbuf(self, st, name, shape, dt):
        self.uid += 1
        h = st.enter_context(self.nc.sbuf_tensor("%s_%d" % (name, self.uid), list(shape), dt))
        return Buf(h.ap(), name)

    def load_consts(self, st):
        k = self.k
        c = self.din("consts", [128, 8, 128])
        self.cst = self.sbuf(st, "cst", [128, 8, 128], F32)
        k.dma(self.cst[:], c, writes=[self.cst])
        self.ident = self.cst[:, 0, :]
        self.U = self.cst[:, 1, :]
        self.SL = self.cst[:, 2, :]
        self.strictT = self.cst[:, 3, :]
        self.ones = self.cst[:, 4, :]
        self.cstb = self.sbuf(st, "cstb", [128, 8, 128], BF16)
        k.dma(self.cstb[:], c, writes=[self.cstb], q="pool")
        self.identb = self.cstb[:, 0, :]
        self.onesb = self.cstb[:, 4, :]

    def rmsnorm(self, h, gainb, xn, junk, st2):
        k = self.k
        k.op("act", lambda e: e.activation(junk[:], h[:], AF.Square, accum_out=st2[:, 0:1]),
             reads=[h], writes=[junk, st2])
        k.op("act", lambda e: e.activation(st2[:, 1:2], st2[:, 0:1], AF.Ln, bias=self.epsb[:, 0:1], scale=1.0 / D),
             reads=[st2, self.epsbuf], writes=[st2])
        k.op("act", lambda e: e.activation(st2[:, 2:3], st2[:, 1:2], AF.Exp, scale=-0.5), reads=[st2], writes=[st2])
        k.op("dve", lambda e: e.scalar_tensor_tensor(xn[:], h[:], st2[:, 2:3], gainb[:], ALU.mult, ALU.mult),
             reads=[h, st2, gainb], writes=[xn])

    def transpose8(self, src, dsts, psa, psb, evac=("act", "dve"), second="pool"):
        k = self.k
        for half, ps in enumerate((psa, psb)):
            for j in range(4):
                c = half * 4 + j
                k.op("pe", lambda e: e.transpose(ps[:, j * 128:(j + 1) * 128], src[:, c * 128:(c + 1) * 128], self.ident),
                     reads=[src, self.cst], writes=[ps], inc=(j == 3))
            dbuf, fn = dsts[0]
            eng = evac[half % len(evac)]
            pv = ps[:].rearrange("p (c t) -> p c t", c=4)
            if eng == "act":
                k.op("act", lambda e: e.copy(fn(half), pv), reads=[ps], writes=[dbuf])
            else:
                k.op(eng, lambda e: e.tensor_copy(fn(half), pv), reads=[ps], writes=[dbuf])
            for dbuf2, fn2 in dsts[1:]:
                k.op(second, lambda e: e.tensor_copy(fn2(half), fn(half)), reads=[dbuf], writes=[dbuf2])

    def moe_phase(self, l, hin, hout, final=False, out_ap=None):
        nc, k = self.nc, self.k
        G = 1024
        NTG = G // 128
        NG = S // G
        NTB = G // 512
        NEX = 16
        P = self.P
        with ExitStack() as st:
            sb = lambda n, s, d: self.sbuf(st, n, s, d)
            gain = sb("gain", [128, D], F32)
            k.dma(gain[:], P["ffn_norm"][l].partition_broadcast(128), writes=[gain])
            if final:
                fgain = sb("fgain", [128, D], F32)
                k.dma(fgain[:], P["final_norm"].partition_broadcast(128), writes=[fgain])
            wgr = sb("wgr", [128, 8, 20], F32)
            k.dma(wgr[:], P["wgr"][l], writes=[wgr])
            rb = sb("rbias", [128, 20], F32)
            k.dma(rb[:], P["rbias"][l].partition_broadcast(128), writes=[rb])
            xnT = [sb("xnT%d" % i, [128, 8, G], BF16) for i in range(2)]
            yacc = [[sb("yacc%d_%d" % (j, i), [128, D], F32) for i in range(NTG)] for j in range(2)]
            comb = [[sb("comb%d_%d" % (j, i), [128, 16], F32) for i in range(NTG)] for j in range(2)]
            w1b = [sb("w1b%d" % i, [128, 8, 256], BF16) for i in range(2)]
            w3b = [sb("w3b%d" % i, [128, 8, 256], BF16) for i in range(2)]
            w2b = [sb("w2b%d" % i, [128, 2, D], BF16) for i in range(2)]
            ht = [sb("ht%d" % i, [128, D], F32) for i in range(2)]
            htc = [sb("htc%d" % i, [128, D], F32) for i in range(2)]
            xn = [sb("xn%d" % i, [128, D], F32) for i in range(2)]
            xnT32 = [sb("xnT32_%d" % i, [128, 8, 128], F32) for i in range(2)]
            stt = [sb("stt%d" % i, [128, 4], F32) for i in range(2)]
            sttc = [sb("sttc%d" % i, [128, 4], F32) for i in range(2)]
            rt = [sb("rt%d" % i, [128, 96], F32) for i in range(2)]
            hid = [sb("hid%d" % i, [128, 2, 512], BF16) for i in range(2)]
            sil = [sb("sil%d" % i, [128, 512], F32) for i in range(2)]
            ps = self.ps
            w1d, w3d, w2d = P["w1"], P["w3"], P["w2"]

            def load_w(e, slot):
                k.dma(w1b[slot][:], w1d[l, e].rearrange("(c p) f -> p c f", p=128), writes=[w1b[slot]], q="pool")
                k.dma(w3b[slot][:], w3d[l, e].rearrange("(c p) f -> p c f", p=128), writes=[w3b[slot]], q="pool")
                k.dma(w2b[slot][:], w2d[l, e].rearrange("(c p) n -> p c n", p=128), writes=[w2b[slot]], q="pool")

            def stage_a(g, tis=None, banks=None):
                par = g % 2
                xT = xnT[par]
                for ti in (range(NTG) if tis is None else tis):
                    t = g * NTG + ti
                    b = ti % 2
                    h = ht[b]
                    k.dma(h[:], hin[0][t * 128:(t + 1) * 128, :], reads=[hin[1][t]], writes=[h])
                    self.rmsnorm(h, gain, xn[b], xn[b], stt[b])
                    yield
                    x32 = xnT32[b]
                    self.transpose8(
                        xn[b],
                        [(x32, lambda half: x32[:, half * 4:(half + 1) * 4, :]),
                         (xT, lambda half: xT[:, half * 4:(half + 1) * 4, ti * 128:(ti + 1) * 128])],
                        ps[6] if banks is None else banks[0], ps[7] if banks is None else banks[1])
                    yield
                    pr = ps[6 + (ti % 2)] if banks is None else banks[2]
                    for dc in range(8):
                        k.op("pe", lambda e: e.matmul(pr[:, 0:20], x32[:, dc, :], wgr[:, dc, :], start=(dc == 0), stop=(dc == 7)),
                             reads=[x32, wgr], writes=[pr], inc=(dc == 7))
                    yield
                    r = rt[b]
                    R = lambda a, n: r[:, a:a + n]
                    lg, gmax, ngmax, oh, ge, gsum, pg = R(0, 20), R(20, 1), R(21, 1), R(22, 4), R(26, 4), R(30, 1), R(31, 1)
                    tmp, elsel, m1, nm1, ee, mask1, ee2 = R(32, 16), R(48, 4), R(52, 1), R(53, 1), R(54, 4), R(58, 4), R(62, 4)
                    v2, mask2, den, rden, wl, scl = R(66, 1), R(67, 4), R(71, 1), R(72, 1), R(73, 4), R(77, 1)
                    dv = lambda fn, rd=(), wr=(): k.op("dve", fn, reads=[r] + list(rd), writes=[r] + list(wr))
                    dv(lambda e: e.tensor_tensor(lg, pr[:, 0:20], rb[:], ALU.add), rd=[pr, rb])
                    dv(lambda e: e.tensor_reduce(gmax, lg[:, 0:4], AX.X, ALU.max))
                    dv(lambda e: e.tensor_single_scalar(ngmax, gmax, -1.0, ALU.mult))
                    dv(lambda e: e.tensor_scalar(oh, lg[:, 0:4], gmax, None, ALU.is_equal))
                    yield
                    k.op("act", lambda e: e.activation(ge, lg[:, 0:4], AF.Exp, bias=ngmax, accum_out=gsum), reads=[r], writes=[r])
                    dv(lambda e: e.reciprocal(pg, gsum))
                    dv(lambda e: e.tensor_tensor(tmp.rearrange("p (g j) -> p g j", g=4),
                                                 lg[:, 4:20].rearrange("p (g j) -> p g j", g=4),
                                                 oh.unsqueeze(2).to_broadcast([128, 4, 4]), ALU.mult))
                    dv(lambda e: e.tensor_reduce(elsel, tmp.rearrange("p (g j) -> p j g", g=4), AX.X, ALU.add))
                    yield
                    dv(lambda e: e.tensor_reduce(m1, elsel, AX.X, ALU.max))
                    dv(lambda e: e.tensor_single_scalar(nm1, m1, -1.0, ALU.mult))
                    k.op("act", lambda e: e.activation(ee, elsel, AF.Exp, bias=nm1), reads=[r], writes=[r])
                    dv(lambda e: e.tensor_scalar(mask1, elsel, m1, None, ALU.is_equal))
                    yield
                    dv(lambda e: e.scalar_tensor_tensor(ee2, mask1, -2.0, ee, ALU.mult, ALU.add))
                    dv(lambda e: e.tensor_reduce(v2, ee2, AX.X, ALU.max))
                    dv(lambda e: e.tensor_scalar(mask2, ee2, v2, None, ALU.is_equal))
                    dv(lambda e: e.tensor_single_scalar(den, v2, 1.0, ALU.add))
                    yield
                    dv(lambda e: e.reciprocal(rden, den))
                    dv(lambda e: e.scalar_tensor_tensor(wl, mask2, v2, mask1, ALU.mult, ALU.add))
                    dv(lambda e: e.tensor_tensor(scl, pg, rden, ALU.mult))
                    dv(lambda e: e.tensor_scalar(wl, wl, scl, None, ALU.mult))
                    cb = comb[par][ti]
                    dv(lambda e: e.tensor_tensor(cb[:].rearrange("p (g j) -> p g j", g=4),
                                                 oh.unsqueeze(2).to_broadcast([128, 4, 4]),
                                                 wl.unsqueeze(1).to_broadcast([128, 4, 4]), ALU.mult), wr=[cb])
                    yield

            def stage_b(g):
                par = g % 2
                xT = xnT[par]
                work = [(ex, tb) for ex in range(NEX) for tb in range(NTB)]

                def up(idx):
                    ex, tb = work[idx]
                    slot = ex % 2
                    w1, w3 = w1b[slot], w3b[slot]
                    hd = hid[idx % 2]
                    for fc in range(2):
                        p1 = ps[fc]
                        p3 = ps[2 + fc]
                        for wsrc, pdst in ((w1, p1), (w3, p3)):
                            for dc in range(8):
                                k.op("pe", lambda e: e.matmul(pdst[:], wsrc[:, dc, fc * 128:(fc + 1) * 128], xT[:, dc, tb * 512:(tb + 1) * 512],
                                                              start=(dc == 0), stop=(dc == 7)),
                                     reads=[wsrc, xT], writes=[pdst], inc=(dc == 7))
                                if dc % 4 == 3:
                                    yield
                        sl = sil[fc]
                        k.op("act", lambda e: e.activation(sl[:], p1[:], AF.Silu), reads=[p1], writes=[sl])
                        k.op("dve", lambda e: e.tensor_tensor(hd[:, fc, :], sl[:], p3[:], ALU.mult), reads=[sl, p3], writes=[hd])

                def down(idx):
                    ex, tb = work[idx]
                    slot = ex % 2
                    w2 = w2b[slot]
                    hd = hid[idx % 2]
                    for tt in range(4):
                        ti = tb * 4 + tt
                        for half in range(2):
                            py = ps[4 + half]
                            for fc in range(2):
                                k.op("pe", lambda e: e.matmul(py[:], hd[:, fc, tt * 128:(tt + 1) * 128], w2[:, fc, half * 512:(half + 1) * 512],
                                                              start=(fc == 0), stop=(fc == 1)),
                                     reads=[hd, w2], writes=[py], inc=(fc == 1))
                            ya = yacc[par][ti]
                            cs = comb[par][ti][:, ex:ex + 1]
                            if ex == 0:
                                k.op("dve", lambda e: e.tensor_scalar(ya[:, half * 512:(half + 1) * 512], py[:], cs, None, ALU.mult),
                                     reads=[py, comb[par][ti]], writes=[ya])
                            else:
                                k.op("dve", lambda e: e.scalar_tensor_tensor(ya[:, half * 512:(half + 1) * 512], py[:], cs,
                                                                             ya[:, half * 512:(half + 1) * 512], ALU.mult, ALU.add),
                                     reads=[py, comb[par][ti], ya], writes=[ya])
                            yield
                    if tb == NTB - 1:
                        if ex + 2 < NEX:
                            load_w(ex + 2, slot)
                        elif g + 1 < NG:
                            load_w(ex + 2 - NEX, slot)

                def rr(*gens):
                    gens = [g_ for g_ in gens if g_ is not None]
                    while gens:
                        for g_ in list(gens):
                            try:
                                next(g_)
                                yield
                            except StopIteration:
                                gens.remove(g_)

                yield from up(0)
                for idx in range(len(work)):
                    nu = up(idx + 1) if idx + 1 < len(work) else None
                    if nu is not None:
                        next(nu)
                        yield
                    yield from rr(nu, down(idx))

            def stage_c(g, tis=None):
                par = g % 2
                for ti in (range(NTG) if tis is None else tis):
                    t = g * NTG + ti
                    b = ti % 2
                    h = htc[b]
                    ya = yacc[par][ti]
                    k.dma(h[:], hin[0][t * 128:(t + 1) * 128, :], reads=[hin[1][t]], writes=[h])
                    k.op("pool", lambda e: e.tensor_tensor(ya[:], h[:], ya[:], ALU.add), reads=[h, ya], writes=[ya])
                    yield
                    if final:
                        self.rmsnorm(ya, fgain, h, h, sttc[b])
                        k.dma(hout[0][t * 128:(t + 1) * 128, :], h[:], reads=[h], writes=[hout[1][t]])
                    else:
                        k.dma(hout[0][t * 128:(t + 1) * 128, :], ya[:], reads=[ya], writes=[hout[1][t]])
                    yield

            def run_group(bg, ag, cg):
                others = [[g_, per] for g_, per in ((ag, 4), (cg, 24)) if g_ is not None]
                step = 0
                b_alive = bg is not None
                while b_alive or others:
                    if b_alive:
                        try:
                            next(bg)
                        except StopIteration:
                            b_alive = False
                    for item in list(others):
                        if (not b_alive) or step % item[1] == 0:
                            try:
                                next(item[0])
                            except StopIteration:
                                others.remove(item)
                    step += 1

            load_w(0, 0)
            load_w(1, 1)
            ga = [stage_a(0, range(0, NTG, 2), (ps[0], ps[1], ps[2])), stage_a(0, range(1, NTG, 2), (ps[3], ps[4], ps[5]))]
            while ga:
                for g_ in list(ga):
                    try:
                        next(g_)
                    except StopIteration:
                        ga.remove(g_)
            for g in range(NG):
                run_group(stage_b(g), stage_a(g + 1) if g + 1 < NG else None, stage_c(g - 1) if g >= 1 else None)
            gc = [stage_c(NG - 1, range(0, NTG, 2)), stage_c(NG - 1, range(1, NTG, 2))]
            while gc:
                for g_ in list(gc):
                    try:
                        next(g_)
                    except StopIteration:
                        gc.remove(g_)
            k.barrier()

    def nextbank(self):
        self.pbi = (getattr(self, "pbi", -1) + 1) % 8
        return self.ps[self.pbi]

    def mem_kv(self, st, l):
        k, P = self.k, self.P
        sb = lambda n, s, d: self.sbuf(st, n, s, d)
        memkT = sb("memkT", [128, 2, 256], BF16)
        memv = sb("memv", [128, 2, 256], BF16)
        with ExitStack() as st2:
            sb2 = lambda n, s, d: self.sbuf(st2, n, s, d)
            g = sb2("mg", [128, D], F32)
            k.dma(g[:], P["mem_norm"][l].partition_broadcast(128), writes=[g])
            w = sb2("wmkv", [128, 8, 512], BF16)
            k.dma(w[:], P["w_mem_kv"][l].rearrange("(c p) n -> p c n", p=128), writes=[w], q="pool")
            mT = sb2("memnT", [128, 8, 256], BF16)
            junk = sb2("mjunk", [128, D], F32)
            for mt in range(2):
                h = sb2("mh%d" % mt, [128, D], F32)
                xn = sb2("mxn%d" % mt, [128, D], F32)
                stt = sb2("mst%d" % mt, [128, 4], F32)
                k.dma(h[:], P["mem"][mt * 128:(mt + 1) * 128, :], writes=[h])
                self.rmsnorm(h, g, xn, junk, stt)
                self.transpose8(xn, [(mT, lambda half: mT[:, half * 4:(half + 1) * 4, mt * 128:(mt + 1) * 128])],
                                self.nextbank(), self.nextbank())
            for j in range(2):
                pb = self.nextbank()
                for dc in range(8):
                    k.op("pe", lambda e: e.matmul(pb[:, 0:256], w[:, dc, j * 128:(j + 1) * 128], mT[:, dc, :],
                                                  start=(dc == 0), stop=(dc == 7)),
                         reads=[w, mT], writes=[pb], inc=(dc == 7))
                k.op("act", lambda e: e.copy(memkT[:, j, :], pb[:, 0:256]), reads=[pb], writes=[memkT])
            for mt in range(2):
                pb = self.nextbank()
                for dc in range(8):
                    k.op("pe", lambda e: e.matmul(pb[:, 0:256], mT[:, dc, mt * 128:(mt + 1) * 128], w[:, dc, 256:512],
                                                  start=(dc == 0), stop=(dc == 7)),
                         reads=[w, mT], writes=[pb], inc=(dc == 7))
                k.op("dve", lambda e: e.tensor_copy(memv[:, mt, :], pb[:, 0:256]), reads=[pb], writes=[memv])
            k.barrier()
        return memkT, memv

    def mem_attend(self, W, mqT, qoff, memkT, memv, mix, col0=768):
        for _ in self.mem_attend_g(W, mqT, qoff, memkT, memv, mix, col0):
            pass

    def mem_attend_g(self, W, mqT, qoff, memkT, memv, mix, col0=768, banks=None):
        k = self.k
        pe_ = W["pexp"]; ms = W["mstat"]; pT = W["pT"]
        if banks is None:
            banks = [self.nextbank(), self.nextbank()]
        for hh in range(4):
            pair, s = hh // 2, hh % 2
            pb = banks[s]
            k.op("pe", lambda e: e.matmul(pb[:, pair * 256:(pair + 1) * 256], mqT[s * 64:(s + 1) * 64, pair, qoff:qoff + 128],
                                          memkT[s * 64:(s + 1) * 64, pair, :], start=True, stop=True),
                 reads=[mqT, memkT], writes=[pb])
        for s in range(2):
            pb = banks[s]
            k.op("dve", lambda e: e.tensor_reduce(ms[:, s:s + 3:2], pb[:].rearrange("p (h m) -> p h m", h=2), AX.X, ALU.max),
                 reads=[pb], writes=[ms])
        k.op("dve", lambda e: e.tensor_single_scalar(ms[:, 4:8], ms[:, 0:4], -0.125, ALU.mult), reads=[ms], writes=[ms])
        yield
        for hh in range(4):
            pair, s = hh // 2, hh % 2
            pb = banks[s]
            k.op("act", lambda e: e.activation(pe_[:, hh, :], pb[:, pair * 256:(pair + 1) * 256], AF.Exp, bias=ms[:, 4 + hh:5 + hh],
                                               scale=0.125, accum_out=ms[:, 8 + hh:9 + hh]),
                 reads=[pb, ms], writes=[pe_, ms])
        k.op("dve", lambda e: e.reciprocal(ms[:, 12:16], ms[:, 8:12]), reads=[ms], writes=[ms])
        yield
        for half in range(2):
            pb = self.nextbank()
            for j in range(4):
                idx = half * 4 + j
                hh, mc = idx // 2, idx % 2
                k.op("pe", lambda e: e.transpose(pb[:, j * 128:(j + 1) * 128], pe_[:, hh, mc * 128:(mc + 1) * 128], self.ident),
                     reads=[pe_, self.cst], writes=[pb], inc=(j == 3))
            if half == 0:
                k.op("act", lambda e: e.copy(pT[:, 0:4, :], pb[:].rearrange("p (c t) -> p c t", c=4)), reads=[pb], writes=[pT])
            else:
                k.op("dve", lambda e: e.tensor_copy(pT[:, 4:8, :], pb[:].rearrange("p (c t) -> p c t", c=4)), reads=[pb], writes=[pT])
        yield
        pb = self.nextbank()
        for hh in range(4):
            for mc in range(2):
                k.op("pe", lambda e: e.matmul(pb[:, hh * 64:(hh + 1) * 64], pT[:, hh * 2 + mc, :], memv[:, mc, hh * 64:(hh + 1) * 64],
                                              start=(mc == 0), stop=(mc == 1)),
                     reads=[pT, memv], writes=[pb], inc=(mc == 1))
        k.op("dve", lambda e: e.tensor_tensor(mix[:, col0:col0 + 256].rearrange("p (h d) -> p h d", h=4),
                                              pb[:, 0:256].rearrange("p (h d) -> p h d", h=4),
                                              ms[:, 12:16].unsqueeze(2).to_broadcast([128, 4, 64]), ALU.mult),
             reads=[pb, ms], writes=[mix])

    def mem_work(self, st):
        sb = lambda n, s, d: self.sbuf(st, n, s, d)
        return {"pexp": sb("pexp", [128, 4, 256], F32), "mstat": sb("mstat", [128, 16], F32),
                "pT": sb("pT", [128, 8, 128], BF16)}

    def out_proj(self, mix, mixT, w_out, h, hn, dst_ap, dst_buf):
        k = self.k
        self.transpose8(mix, [(mixT, lambda half: mixT[:, half * 4:(half + 1) * 4, :])], self.nextbank(), self.nextbank())
        for half in range(2):
            pb = self.nextbank()
            for fc in range(8):
                k.op("pe", lambda e: e.matmul(pb[:], mixT[:, fc, :], w_out[:, fc, half * 512:(half + 1) * 512],
                                              start=(fc == 0), stop=(fc == 7)),
                     reads=[mixT, w_out], writes=[pb], inc=(fc == 7))
            k.op("dve", lambda e: e.tensor_tensor(hn[:, half * 512:(half + 1) * 512], h[:, half * 512:(half + 1) * 512], pb[:], ALU.add),
                 reads=[h, pb], writes=[hn])
        k.dma(dst_ap, hn[:], reads=[hn], writes=[dst_buf])

    def mixer_a_phase(self, hin, hout):
        nc, k, P = self.nc, self.k, self.P
        NTA = int(os.environ.get("MK_NTA", str(NT)))
        with ExitStack() as st:
            sb = lambda n, s, d: self.sbuf(st, n, s, d)
            memkT, memv = self.mem_kv(st, 0)
            gain = sb("gainA", [128, D], F32)
            k.dma(gain[:], P["a_norm"][0].partition_broadcast(128), writes=[gain])
            w_in = sb("w_inA", [128, 8, 3340], BF16)
            for c in range(8):
                k.dma(w_in[:, c, :], P["a_w_in"][0, c * 128:(c + 1) * 128, :], writes=[w_in], q="pool")
            w_out = sb("w_outA", [128, 8, D], BF16)
            k.dma(w_out[:], P["a_w_out"][0].rearrange("(c p) n -> p c n", p=128), writes=[w_out], q="pool")
            convw = sb("convw", [128, 18, 4], F32)
            k.dma(convw[:], P["convw"], writes=[convw])
            sc6 = sb("sc6", [128, 32], F32)
            k.dma(sc6[:, 0:6], P["a_log"][0].partition_broadcast(128), writes=[sc6])
            k.dma(sc6[:, 6:12], P["a_dt_bias"][0].partition_broadcast(128), writes=[sc6])
            k.op("act", lambda e: e.activation(sc6[:, 12:18], sc6[:, 0:6], AF.Exp), reads=[sc6], writes=[sc6])
            k.op("dve", lambda e: e.tensor_single_scalar(sc6[:, 12:18], sc6[:, 12:18], -1.0, ALU.mult), reads=[sc6], writes=[sc6])
            ogain = sb("ogain", [128, 128], F32)
            k.dma(ogain[:], P["a_out_gain"][0].partition_broadcast(128), writes=[ogain])
            MW = self.mem_work(st)
            pc = sb("pc", [128, 18, 131], F32)
            k.op("dve", lambda e: e.memset(pc[:], 0.0), writes=[pc])
            Sf = [sb("Sf%d" % h, [128, 128], F32) for h in range(6)]
            Sb = [sb("Sb%d" % h, [128, 128], BF16) for h in range(6)]
            for h in range(6):
                k.op("dve", lambda e: e.memset(Sf[h][:], 0.0), writes=[Sf[h]])
                k.op("pool", lambda e: e.memset(Sb[h][:], 0.0), writes=[Sb[h]])
            hA = sb("htA", [128, D], F32)
            xT = sb("xnTA", [128, 8, 128], BF16)
            cv = sb("cv", [128, 12, 128], F32)
            cvv = sb("cvv", [128, 6, 128], F32)
            ctmp = sb("ctmp", [128, 128], F32)
            cvjunk = View(cv, cv[:, 0:8, :].rearrange("p c t -> p (c t)"))
            sq = sb("sq", [128, 12, 128], BF16)
            rs = sb("rs", [128, 12, 128], F32)
            kn32 = sb("kn32", [128, 6, 128], F32)
            sttA = sb("sttA", [128, 4], F32)
            qkT = [sb("qkT%d" % i, [128, 6, 2, 128], BF16) for i in range(3)]
            sc = [sb("scA%d" % i, [128, 96], F32) for i in range(3)]
            gg = [sb("gg%d" % i, [128, 768], F32) for i in range(3)]
            mqT = [sb("mqTA%d" % i, [128, 2, 128], BF16) for i in range(3)]
            SLg = [sb("SLg%d" % i, [128, 128], F32) for i in range(6)]
            dm = sb("dm", [128, 6, 128], F32)
            dmi = sb("dmi", [128, 6, 128], F32)
            Wq = [[sb("W%d_%d" % (h, i), [128, 3, 128], BF16) for i in range(2)] for h in range(6)]
            Q0f = [sb("Q0f%d" % i, [128, 128], F32) for i in range(6)]
            kt = [sb("kt%d" % i, [128, 6, 128], BF16) for i in range(2)]
            vtok = [sb("vtok%d" % i, [128, 6, 128], F32) for i in range(2)]
            attnT = [sb("attnT%d" % i, [128, 6, 128], BF16) for i in range(2)]
            TT = [sb("TT%d" % i, [128, 6, 128], BF16) for i in range(2)]
            Rall = sb("Rall", [128, 6, 128], BF16)
            vnew = sb("vnewA", [128, 6, 128], BF16)
            oall = sb("oall", [128, 6, 128], F32)
            ost = sb("ost", [128, 24], F32)
            ojunk = sb("ojunk", [128, 128], F32)
            mix = sb("mixA", [128, D], F32)
            mixT = sb("mixTA", [128, 8, 128], BF16)
            hC = sb("htC", [128, D], F32)
            ident, U, SL, ones = self.ident, self.U, self.SL, self.ones
            NEGs = self.cst[:, 7, :]
            identb = self.identb
            cst, cstb = self.cst, self.cstb
            QS = float(128 ** -0.5)
            ctm = View(rs, rs[:, 0:9, :])
            rsjunk = View(rs, rs[:, 0:8, :].rearrange("p c t -> p (c t)"))
            rotA = [0]

            def nbA():
                rotA[0] = (rotA[0] + 1) % 6
                return self.ps[rotA[0]]
            self.nextbank = nbA
            membanks = [self.ps[6], self.ps[7]]

            def frontA(t):
                i3 = t % 3
                h = hA
                k.dma(h[:], hin[0][t * 128:(t + 1) * 128, :], reads=[hin[1][t]], writes=[h])
                self.rmsnorm(h, gain, h, rsjunk, sttA)
                yield
                self.transpose8(h, [(xT, lambda half: xT[:, half * 4:(half + 1) * 4, :])], self.nextbank(), self.nextbank())
                yield
                for g4 in range(5):
                    nf = 4 if g4 < 4 else 2
                    pb = self.nextbank()
                    for j in range(nf):
                        fc = g4 * 4 + j
                        for dc in range(8):
                            k.op("pe", lambda e: e.matmul(pb[:, j * 128:(j + 1) * 128], w_in[:, dc, fc * 128:(fc + 1) * 128], xT[:, dc, :],
                                                          start=(dc == 0), stop=(dc == 7)),
                                 reads=[w_in, xT], writes=[pb], inc=(dc == 7 and j == nf - 1))
                    dstv = pc[:, g4 * 4:g4 * 4 + nf, 3:131]
                    srcv = pb[:, 0:nf * 128].rearrange("p (c t) -> p c t", c=nf)
                    k.op("act", lambda e: e.copy(dstv, srcv), reads=[pb], writes=[pc])
                    yield
                pb = self.nextbank()
                for j in range(2):
                    for dc in range(8):
                        k.op("pe", lambda e: e.matmul(pb[:, j * 128:(j + 1) * 128], w_in[:, dc, 3084 + j * 128:3084 + (j + 1) * 128], xT[:, dc, :],
                                                      start=(dc == 0), stop=(dc == 7)),
                             reads=[w_in, xT], writes=[pb], inc=(dc == 7 and j == 1))
                k.op("act", lambda e: e.copy(mqT[i3][:], pb[:, 0:256].rearrange("p (c t) -> p c t", c=2)), reads=[pb], writes=[mqT[i3]])
                yield
                pg1 = self.nextbank()
                for dc in range(8):
                    k.op("pe", lambda e: e.matmul(pg1[:], xT[:, dc, :], w_in[:, dc, 2304:2816], start=(dc == 0), stop=(dc == 7)),
                         reads=[w_in, xT], writes=[pg1], inc=(dc == 7))
                pg2 = self.nextbank()
                for dc in range(8):
                    k.op("pe", lambda e: e.matmul(pg2[:, 0:268], xT[:, dc, :], w_in[:, dc, 2816:3084], start=(dc == 0), stop=(dc == 7)),
                         reads=[w_in, xT], writes=[pg2], inc=(dc == 7))
                g_g = gg[i3]
                k.op("act", lambda e: e.activation(g_g[:, 0:512], pg1[:], AF.Silu), reads=[pg1], writes=[g_g])
                k.op("act", lambda e: e.activation(g_g[:, 512:768], pg2[:, 0:256], AF.Silu), reads=[pg2], writes=[g_g])
                s_ = sc[i3]
                C = lambda a_, n=6: s_[:, a_:a_ + n]
                beta, tt_, ex_, sp_, g_, gcl, egc, negc, etl, egl, dd, eb_ = (C(0), C(6), C(12), C(18), C(24), C(32, 16), C(48), C(54), C(60), C(66), C(72), C(78))
                k.op("act", lambda e: e.activation(eb_, pg2[:, 256:262], AF.Exp, scale=-1.0), reads=[pg2], writes=[s_])
                k.op("dve", lambda e: e.tensor_tensor(tt_, pg2[:, 262:268], sc6[:, 6:12], ALU.add), reads=[pg2, sc6], writes=[s_])
                k.op("dve", lambda e: e.tensor_single_scalar(eb_, eb_, 1.0, ALU.add), reads=[s_], writes=[s_])
                k.op("dve", lambda e: e.reciprocal(beta, eb_), reads=[s_], writes=[s_])
                k.op("pool", lambda e: e.tensor_tensor(g_g[:].rearrange("p (h d) -> p h d", h=6), g_g[:].rearrange("p (h d) -> p h d", h=6),
                                                       ogain[:].unsqueeze(1).to_broadcast([128, 6, 128]), ALU.mult),
                     reads=[g_g, ogain], writes=[g_g])
                yield
                k.op("act", lambda e: e.activation(ex_, tt_, AF.Exp), reads=[s_], writes=[s_])
                k.op("act", lambda e: e.activation(sp_, ex_, AF.Ln, bias=1.0), reads=[s_], writes=[s_])
                k.op("dve", lambda e: e.tensor_tensor(g_, sp_, sc6[:, 12:18], ALU.mult), reads=[s_, sc6], writes=[s_])
                yield
                pgc = self.nextbank()
                k.op("pe", lambda e: e.matmul(pgc[:, 0:6], U, g_, start=True, stop=True), reads=[cst, s_], writes=[pgc])
                k.op("pe", lambda e: e.matmul(pgc[:, 8:14], ones, g_, start=True, stop=True), reads=[cst, s_], writes=[pgc])
                k.op("dve", lambda e: e.tensor_copy(gcl, pgc[:, 0:16]), reads=[pgc], writes=[s_])
                k.op("dve", lambda e: e.tensor_tensor(dd, s_[:, 40:46], s_[:, 32:38], ALU.subtract), reads=[s_], writes=[s_])
                yield
                k.op("act", lambda e: e.activation(egc, s_[:, 32:38], AF.Exp), reads=[s_], writes=[s_])
                k.op("act", lambda e: e.activation(etl, dd, AF.Exp), reads=[s_], writes=[s_])
                k.op("act", lambda e: e.activation(egl, s_[:, 40:46], AF.Exp), reads=[s_], writes=[s_])
                k.op("dve", lambda e: e.tensor_single_scalar(negc, egc, -1.0, ALU.mult), reads=[s_], writes=[s_])
                yield
                for (c0, nchk, dstb, dst) in ((0, 9, cv, cv[:, 0:9, :]), (9, 3, cv, cv[:, 9:12, :]), (12, 6, cvv, cvv[:, 0:6, :])):
                    wv = lambda j: convw[:, c0:c0 + nchk, j:j + 1].to_broadcast([128, nchk, 128])
                    k.op("dve", lambda e: e.tensor_tensor(dst, pc[:, c0:c0 + nchk, 0:128], wv(0), ALU.mult), reads=[pc, convw], writes=[dstb])
                    for j in range(1, 4):
                        tv = ctm[:, 0:nchk, :]
                        k.op("dve", lambda e: e.tensor_tensor(tv, pc[:, c0:c0 + nchk, j:j + 128], wv(j), ALU.mult), reads=[pc, convw], writes=[ctm])
                        k.op("dve", lambda e: e.tensor_tensor(dst, dst, tv, ALU.add), reads=[ctm, dstb], writes=[dstb])
                        yield
                k.op("pool", lambda e: e.tensor_copy(pc[:, :, 0:3], pc[:, :, 128:131]), reads=[pc], writes=[pc])
                k.op("act", lambda e: e.activation(cv[:], cv[:], AF.Silu), reads=[cv], writes=[cv])
                k.op("act", lambda e: e.activation(cvv[:], cvv[:], AF.Silu), reads=[cvv], writes=[cvv])
                yield

            def frontB(t):
                i3 = t % 3
                b = t % 2
                s_ = sc[i3]
                C = lambda a_, n=6: s_[:, a_:a_ + n]
                beta, g_ = C(0), C(24)
                qk = qkT[i3]
                qkv = cv
                k.op("act", lambda e: e.activation(sq[:], qkv[:, 0:12, :], AF.Square), reads=[qkv], writes=[sq])
                yield
                for g3 in range(3):
                    pb = self.nextbank()
                    k.op("pe", lambda e: e.matmul(pb[:], self.onesb, sq[:, g3 * 4:(g3 + 1) * 4, :], start=True, stop=True),
                         reads=[cstb, sq], writes=[pb])
                    k.op("act", lambda e: e.activation(rs[:, g3 * 4:(g3 + 1) * 4, :], pb[:].rearrange("p (c t) -> p c t", c=4), AF.Ln,
                                                       bias=self.epsb[:, 0:1]), reads=[pb, self.epsbuf], writes=[rs])
                    k.op("act", lambda e: e.activation(rs[:, g3 * 4:(g3 + 1) * 4, :], rs[:, g3 * 4:(g3 + 1) * 4, :], AF.Exp, scale=-0.5),
                         reads=[rs], writes=[rs])
                yield
                k.op("dve", lambda e: e.scalar_tensor_tensor(qk[:, :, 1, :], qkv[:, 0:6, :], QS, rs[:, 0:6, :], ALU.mult, ALU.mult),
                     reads=[qkv, rs], writes=[qk])
                k.op("dve", lambda e: e.tensor_tensor(kn32[:], qkv[:, 6:12, :], rs[:, 6:12, :], ALU.mult), reads=[qkv, rs], writes=[kn32])
                k.op("pool", lambda e: e.tensor_copy(qk[:, :, 0, :], kn32[:]), reads=[kn32], writes=[qk])
                yield
                s_etl = C(60)
                for grp in range(3):
                    pb = self.nextbank()
                    for j in range(4):
                        idx = grp * 4 + j
                        src = cvv[:, idx, :] if idx < 6 else kn32[:, idx - 6, :]
                        srcb = cvv if idx < 6 else kn32
                        k.op("pe", lambda e: e.transpose(pb[:, j * 128:(j + 1) * 128], src, ident), reads=[srcb, cst], writes=[pb], inc=(j == 3))
                    for j in range(4):
                        idx = grp * 4 + j
                        if idx < 6:
                            k.op("act", lambda e: e.copy(vtok[b][:, idx, :], pb[:, j * 128:(j + 1) * 128]), reads=[pb], writes=[vtok[b]])
                        else:
                            hh = idx - 6
                            k.op("dve", lambda e: e.tensor_scalar(kt[b][:, hh, :], pb[:, j * 128:(j + 1) * 128], s_etl[:, hh:hh + 1], None, ALU.mult),
                                 reads=[pb, s_], writes=[kt[b]])
                    yield
                for hh in range(6):
                    sg = SLg[hh]
                    k.op("dve", lambda e: e.tensor_scalar(sg[:], SL, g_[:, hh:hh + 1], None, ALU.mult), reads=[cst, s_], writes=[sg])
                yield
                for hp in range(3):
                    pb = self.nextbank()
                    for j in range(2):
                        hh = hp * 2 + j
                        sg = SLg[hh]
                        k.op("pe", lambda e: e.matmul(pb[:, j * 128:(j + 1) * 128], sg[:], U, start=True, stop=False),
                             reads=[sg, cst], writes=[pb], inc=False)
                        k.op("pe", lambda e: e.matmul(pb[:, j * 128:(j + 1) * 128], ident, NEGs, start=False, stop=True),
                             reads=[cst], writes=[pb])
                    k.op("act", lambda e: e.activation(dm[:, hp * 2:hp * 2 + 2, :], pb[:, 0:256].rearrange("p (c t) -> p c t", c=2), AF.Exp),
                         reads=[pb], writes=[dm])
                    yield
                k.op("pool", lambda e: e.tensor_tensor(dmi[:], dm[:], ident.unsqueeze(1).to_broadcast([128, 6, 128]), ALU.add),
                     reads=[dm, cst], writes=[dmi])
                yield
                for hh in range(6):
                    pb = self.nextbank()
                    W0 = Wq[hh][0]
                    qf = Q0f[hh]
                    k.op("pe", lambda e: e.matmul(pb[:, 0:256], qk[:, hh, 0, :], qk[:, hh, :, :].rearrange("p a t -> p (a t)"),
                                                  start=True, stop=True), reads=[qk], writes=[pb])
                    k.op("dve", lambda e: e.scalar_tensor_tensor(qf[:], pb[:, 0:128], beta[:, hh:hh + 1], dm[:, hh, :], ALU.mult, ALU.mult),
                         reads=[pb, s_, dm], writes=[qf])
                    k.op("dve", lambda e: e.tensor_tensor(attnT[b][:, hh, :], pb[:, 128:256], dmi[:, hh, :], ALU.mult),
                         reads=[pb, dmi], writes=[attnT[b]])
                    if hh % 3 == 2:
                        yield
                for hh in range(6):
                    W0 = Wq[hh][0]
                    qf = Q0f[hh]
                    k.op("pool", lambda e: e.tensor_copy(W0[:, 0, :], qf[:]), reads=[qf], writes=[W0])
                    k.op("pool", lambda e: e.tensor_tensor(Wq[hh][1][:, 1, :], ident, qf[:], ALU.subtract), reads=[cst, qf], writes=[Wq[hh][1]])
                    pb2 = self.nextbank()
                    k.op("pe", lambda e: e.transpose(pb2[:, 0:128], qf[:], ident), reads=[qf, cst], writes=[pb2])
                    k.op("act", lambda e: e.copy(W0[:, 2, :], pb2[:, 0:128]), reads=[pb2], writes=[W0])
                    if hh % 3 == 2:
                        yield
                for lvl in range(7):
                    for hh in range(6):
                        Wc = Wq[hh][lvl % 2]
                        Wn = Wq[hh][(lvl + 1) % 2]
                        pb = self.nextbank()
                        Qk, Xk, Pk = Wc[:, 0, :], Wc[:, 1, :], Wc[:, 2, :]
                        mm = lambda out, l_, r_, st_, sp_2, inc_: k.op(
                            "pe", lambda e: e.matmul(out, l_, r_, start=st_, stop=sp_2), reads=[Wc, cstb], writes=[pb], inc=inc_)
                        if lvl == 0:
                            mm(pb[:, 0:128], Pk, Qk, True, True, False)
                            mm(pb[:, 256:384], Qk, Pk, True, True, True)
                            k.op("act", lambda e: e.copy(Wn[:, 0, :], pb[:, 0:128]), reads=[pb], writes=[Wn])
                            k.op("act", lambda e: e.copy(Wn[:, 2, :], pb[:, 256:384]), reads=[pb], writes=[Wn])
                        elif lvl < 6:
                            mm(pb[:, 0:128], Pk, Qk, True, True, False)
                            mm(pb[:, 128:256], Pk, Xk, True, False, False)
                            mm(pb[:, 128:256], identb, Xk, False, True, False)
                            mm(pb[:, 256:384], Qk, Pk, True, True, True)
                            k.op("act", lambda e: e.copy(Wn[:], pb[:, 0:384].rearrange("p (c t) -> p c t", c=3)), reads=[pb], writes=[Wn])
                        else:
                            mm(pb[:, 128:256], Pk, Xk, True, False, False)
                            mm(pb[:, 128:256], identb, Xk, False, True, True)
                            k.op("act", lambda e: e.copy(TT[b][:, hh, :], pb[:, 128:256]), reads=[pb], writes=[TT[b]])
                    yield

            def back(t):
                i3 = t % 3
                b = t % 2
                s_ = sc[i3]
                C = lambda a_, n=6: s_[:, a_:a_ + n]
                beta, egc, negc, egl = C(0), C(48), C(54), C(66)
                qk, g_g = qkT[i3], gg[i3]
                k.dma(hC[:], hin[0][t * 128:(t + 1) * 128, :], reads=[hin[1][t]], writes=[hC])
                for hp in range(3):
                    pb = self.nextbank()
                    for j in range(2):
                        hh = hp * 2 + j
                        o = j * 256
                        k.op("pe", lambda e: e.matmul(pb[:, o:o + 128], qk[:, hh, 0, :], Sb[hh][:], start=True, stop=True),
                             reads=[qk, Sb[hh]], writes=[pb], inc=False)
                        k.op("pe", lambda e: e.matmul(pb[:, o + 128:o + 256], qk[:, hh, 1, :], Sb[hh][:], start=True, stop=True),
                             reads=[qk, Sb[hh]], writes=[pb], inc=(j == 1))
                    for j in range(2):
                        hh = hp * 2 + j
                        o = j * 256
                        k.op("dve", lambda e: e.scalar_tensor_tensor(Rall[:, hh, :], pb[:, o:o + 128], negc[:, hh:hh + 1], vtok[b][:, hh, :], ALU.mult, ALU.add),
                             reads=[pb, s_, vtok[b]], writes=[Rall])
                        k.op("dve", lambda e: e.tensor_scalar(oall[:, hh, :], pb[:, o + 128:o + 256], egc[:, hh:hh + 1], None, ALU.mult),
                             reads=[pb, s_], writes=[oall])
                yield
                for (h0, nh) in ((0, 4), (4, 2)):
                    pb = self.nextbank()
                    for j in range(nh):
                        hh = h0 + j
                        k.op("pe", lambda e: e.matmul(pb[:, j * 128:(j + 1) * 128], TT[b][:, hh, :], Rall[:, hh, :], start=True, stop=True),
                             reads=[TT[b], Rall], writes=[pb], inc=(j == nh - 1))
                    k.op("dve", lambda e: e.tensor_tensor(vnew[:, h0:h0 + nh, :], pb[:, 0:nh * 128].rearrange("p (c t) -> p c t", c=nh),
                                                          beta[:, h0:h0 + nh].unsqueeze(2).to_broadcast([128, nh, 128]), ALU.mult),
                         reads=[pb, s_], writes=[vnew])
                yield
                for hp in range(3):
                    pb = self.nextbank()
                    for j in range(2):
                        hh = hp * 2 + j
                        o = j * 256
                        k.op("pe", lambda e: e.matmul(pb[:, o:o + 128], attnT[b][:, hh, :], vnew[:, hh, :], start=True, stop=True),
                             reads=[attnT[b], vnew], writes=[pb], inc=False)
                        k.op("pe", lambda e: e.matmul(pb[:, o + 128:o + 256], kt[b][:, hh, :], vnew[:, hh, :], start=True, stop=True),
                             reads=[kt[b], vnew], writes=[pb], inc=(j == 1))
                    for j in range(2):
                        hh = hp * 2 + j
                        o = j * 256
                        k.op("dve", lambda e: e.scalar_tensor_tensor(Sb[hh][:], Sf[hh][:], egl[:, hh:hh + 1], pb[:, o + 128:o + 256], ALU.mult, ALU.add),
                             reads=[Sf[hh], s_, pb], writes=[Sb[hh]])
                        k.op("dve", lambda e: e.scalar_tensor_tensor(Sf[hh][:], Sf[hh][:], egl[:, hh:hh + 1], pb[:, o + 128:o + 256], ALU.mult, ALU.add),
                             reads=[Sf[hh], s_, pb], writes=[Sf[hh]])
                        k.op("dve", lambda e: e.tensor_tensor(oall[:, hh, :], oall[:, hh, :], pb[:, o:o + 128], ALU.add),
                             reads=[oall, pb], writes=[oall])
                yield
                for hh in range(6):
                    k.op("act", lambda e: e.activation(ojunk[:], oall[:, hh, :], AF.Square, accum_out=ost[:, hh:hh + 1]),
                         reads=[oall], writes=[ojunk, ost])
                k.op("act", lambda e: e.activation(ost[:, 8:14], ost[:, 0:6], AF.Ln, bias=self.epsb[:, 0:1], scale=1.0 / 128),
                     reads=[ost, self.epsbuf], writes=[ost])
                k.op("act", lambda e: e.activation(ost[:, 16:22], ost[:, 8:14], AF.Exp, scale=-0.5), reads=[ost], writes=[ost])
                yield
                for hh in range(6):
                    k.op("dve", lambda e: e.scalar_tensor_tensor(mix[:, hh * 128:(hh + 1) * 128], oall[:, hh, :], ost[:, 16 + hh:17 + hh],
                                                                 g_g[:, hh * 128:(hh + 1) * 128], ALU.mult, ALU.mult),
                         reads=[oall, ost, g_g], writes=[mix])
                yield
                yield from self.mem_attend_g(MW, mqT[i3], 0, memkT, memv, mix, banks=membanks)
                yield
                self.transpose8(mix, [(mixT, lambda half: mixT[:, half * 4:(half + 1) * 4, :])], self.nextbank(), self.nextbank())
                yield
                for half in range(2):
                    pb = self.nextbank()
                    for fc in range(8):
                        k.op("pe", lambda e: e.matmul(pb[:], mixT[:, fc, :], w_out[:, fc, half * 512:(half + 1) * 512],
                                                      start=(fc == 0), stop=(fc == 7)),
                             reads=[mixT, w_out], writes=[pb], inc=(fc == 7))
                    k.op("dve", lambda e: e.tensor_tensor(hC[:, half * 512:(half + 1) * 512], hC[:, half * 512:(half + 1) * 512], pb[:], ALU.add),
                         reads=[hC, pb], writes=[hC])
                k.dma(hout[0][t * 128:(t + 1) * 128, :], hC[:], reads=[hC], writes=[hout[1][t]])

            AMULT = [int(v) for v in os.environ.get("MK_AMULT", "1,1,1").split(",")]

            def run(*gens):
                gens = [[g, m] for g, m in zip(gens, AMULT) if g is not None]
                while gens:
                    for item in list(gens):
                        for _ in range(item[1]):
                            try:
                                next(item[0])
                            except StopIteration:
                                gens.remove(item)
                                break

            mk = lambda fn, t: fn(t) if 0 <= t < NTA else None
            for step in range(-2, NTA):
                run(mk(back, step), mk(frontB, step + 1), mk(frontA, step + 2))
            k.barrier()
            del self.nextbank

    def mixer_b_phase(self, hin, hout):
        nc, k, P = self.nc, self.k, self.P
        NGB = int(os.environ.get("MK_NGB", "8"))
        NHB = int(os.environ.get("MK_NHB", "12"))
        NDUM = int(os.environ.get("MK_NDUM", "1"))
        with ExitStack() as st:
            sb = lambda n, s, d: self.sbuf(st, n, s, d)
            memkT, memv = self.mem_kv(st, 1)
            win_d = self.dscratch("b_w_in_bf", [D, D], BF16)
            wout_d = self.dscratch("b_w_out_bf", [D, D], BF16)
            wdb = [Buf(None, "win_d"), Buf(None, "wout_d")]
            k.dma(win_d, P["b_w_in"][0], writes=[wdb[0]], q="pool")
            k.dma(wout_d, P["b_w_out"][0], writes=[wdb[1]], q="pool")
            KT = sb("KT", [128, 6, S], BF16)
            Vt = sb("Vt", [128, NT, 768], BF16)
            ht = [sb("htB%d" % i, [128, D], F32) for i in range(2)]
            xn = sb("xnB", [128, D], F32)
            stt = [sb("sttB%d" % i, [128, 4], F32) for i in range(2)]
            with ExitStack() as st1:
                sb1 = lambda n, s, d: self.sbuf(st1, n, s, d)
                kvg = sb1("kvg", [128, D], F32)
                k.dma(kvg[:], P["kv_norm"].partition_broadcast(128), writes=[kvg])
                w_kv = sb1("w_kv", [128, 8, 1536], BF16)
                for c in range(8):
                    k.dma(w_kv[:, c, :], P["w_kv"][c * 128:(c + 1) * 128, :], writes=[w_kv], q="pool")
                xTg1 = [sb1("xTg1_%d" % i, [128, 8, 512], BF16) for i in range(2)]

                def p1_x(g):
                    xTg_ = xTg1[g % 2]
                    for tt in range(4):
                        t = g * 4 + tt
                        b = tt % 2
                        h = ht[b]
                        k.dma(h[:], hin[0][t * 128:(t + 1) * 128, :], reads=[hin[1][t]], writes=[h])
                        self.rmsnorm(h, kvg, xn, xn, stt[b])
                        yield
                        self.transpose8(xn, [(xTg_, lambda half: xTg_[:, half * 4:(half + 1) * 4, tt * 128:(tt + 1) * 128])],
                                        self.nextbank(), self.nextbank())
                        yield

                def p1_y(g):
                    xTg_ = xTg1[g % 2]
                    for fc in range(6):
                        pb = self.nextbank()
                        for dc in range(8):
                            k.op("pe", lambda e: e.matmul(pb[:], w_kv[:, dc, fc * 128:(fc + 1) * 128], xTg_[:, dc, :],
                                                          start=(dc == 0), stop=(dc == 7)),
                                 reads=[w_kv, xTg_], writes=[pb], inc=(dc == 7))
                        if fc % 2 == 0:
                            k.op("act", lambda e: e.copy(KT[:, fc, g * 512:(g + 1) * 512], pb[:]), reads=[pb], writes=[KT])
                        else:
                            k.op("dve", lambda e: e.tensor_copy(KT[:, fc, g * 512:(g + 1) * 512], pb[:]), reads=[pb], writes=[KT])
                        yield
                    for tt in range(4):
                        t = g * 4 + tt
                        for half, (c0, c1) in enumerate(((0, 512), (512, 768))):
                            pb = self.nextbank()
                            for dc in range(8):
                                k.op("pe", lambda e: e.matmul(pb[:, 0:c1 - c0], xTg_[:, dc, tt * 128:(tt + 1) * 128], w_kv[:, dc, 768 + c0:768 + c1],
                                                              start=(dc == 0), stop=(dc == 7)),
                                     reads=[w_kv, xTg_], writes=[pb], inc=(dc == 7))
                            if half == 0:
                                k.op("dve", lambda e: e.tensor_copy(Vt[:, t, c0:c1], pb[:, 0:c1 - c0]), reads=[pb], writes=[Vt])
                            else:
                                k.op("act", lambda e: e.copy(Vt[:, t, c0:c1], pb[:, 0:c1 - c0]), reads=[pb], writes=[Vt])
                        yield

                def rr1(*gens):
                    gens = [g_ for g_ in gens if g_ is not None]
                    while gens:
                        for g_ in list(gens):
                            try:
                                next(g_)
                            except StopIteration:
                                gens.remove(g_)

                rr1(p1_x(0))
                for g in range(NT // 4):
                    rr1(p1_y(g), p1_x(g + 1) if g + 1 < NT // 4 else None)
                k.barrier()
            bg = sb("bgain", [128, D], F32)
            k.dma(bg[:], P["b_norm"][0].partition_broadcast(128), writes=[bg])
            wB = sb("wB", [128, 8, D], BF16)
            xTg = sb("xTg", [128, 8, 512], BF16)
            qT = [sb("qT%d" % i, [128, 6, 512], BF16) for i in range(2)]
            mqT = [sb("mqTB%d" % i, [128, 2, 512], BF16) for i in range(3)]
            mixTg = [sb("mixTg%d" % i, [128, 8, 512], BF16) for i in range(2)]
            Eb = [sb("Eb%d" % i, [128, 512], F32) for i in range(3)]
            spb = [sb("spb%d" % i, [128, 512], BF16) for i in range(3)]
            eab = [sb("eab%d" % i, [128, 512], F32) for i in range(2)]
            ab = [sb("ab%d" % i, [128, 512], BF16) for i in range(2)]
            MW = self.mem_work(st)
            mmB = sb("mmB", [128, 256], F32)
            NGEb = self.cstb[:, 5, :]
            NLTb = self.cstb[:, 6, :]
            strictTb = self.cstb[:, 3, :]
            cstb = self.cstb
            PZ = [self.ps[0], self.ps[1]]
            PC = [self.ps[2], self.ps[3]]
            PO = [self.ps[4], self.ps[5]]
            rot = [0]

            def nb():
                rot[0] = (rot[0] + 1) % 2
                return self.ps[6 + rot[0]]
            self.nextbank = nb
            wcur = [None]

            def load_wB(which):
                if wcur[0] != which:
                    srcd = win_d if which == "in" else wout_d
                    k.dma(wB[:], srcd.rearrange("(c p) n -> p c n", p=128), reads=[wdb[0 if which == "in" else 1]], writes=[wB])
                    wcur[0] = which

            def prologue(g):
                load_wB("in")
                qTg, mqTg = qT[g % 2], mqT[g % 3]
                h = ht[0]
                for tt in range(4):
                    t = g * 4 + tt
                    k.dma(h[:], hin[0][t * 128:(t + 1) * 128, :], reads=[hin[1][t]], writes=[h])
                    self.rmsnorm(h, bg, xn, xn, stt[0])
                    yield
                    self.transpose8(xn, [(xTg, lambda half: xTg[:, half * 4:(half + 1) * 4, tt * 128:(tt + 1) * 128])], nb(), nb(),
                                    evac=("dve", "dve"))
                    yield
                for fc in range(8):
                    pb = nb()
                    for dc in range(8):
                        k.op("pe", lambda e: e.matmul(pb[:], wB[:, dc, fc * 128:(fc + 1) * 128], xTg[:, dc, :], start=(dc == 0), stop=(dc == 7)),
                             reads=[wB, xTg], writes=[pb], inc=(dc == 7))
                    if fc < 6:
                        k.op("dve", lambda e: e.tensor_single_scalar(qTg[:, fc, :], pb[:], 0.125, ALU.mult), reads=[pb], writes=[qTg])
                    else:
                        k.op("dve", lambda e: e.tensor_copy(mqTg[:, fc - 6, :], pb[:]), reads=[pb], writes=[mqTg])
                    yield

            def epilogue(g):
                load_wB("out")
                mixg, mqTg = mixTg[g % 2], mqT[g % 3]
                h = ht[1]
                for tt in range(4):
                    t = g * 4 + tt
                    k.dma(h[:], hin[0][t * 128:(t + 1) * 128, :], reads=[hin[1][t]], writes=[h])
                    yield from self.mem_attend_g(MW, mqTg, tt * 128, memkT, memv, mmB, col0=0)
                    yield
                    pb = nb()
                    for j in range(2):
                        k.op("pe", lambda e: e.transpose(pb[:, j * 128:(j + 1) * 128], mmB[:, j * 128:(j + 1) * 128], self.ident),
                             reads=[mmB, self.cst], writes=[pb], inc=(j == 1))
                    k.op("dve", lambda e: e.tensor_copy(mixg[:, 6:8, tt * 128:(tt + 1) * 128], pb[:, 0:256].rearrange("p (c t) -> p c t", c=2)),
                         reads=[pb], writes=[mixg])
                    yield
                    for half in range(2):
                        pb = nb()
                        for fc in range(8):
                            k.op("pe", lambda e: e.matmul(pb[:], mixg[:, fc, tt * 128:(tt + 1) * 128], wB[:, fc, half * 512:(half + 1) * 512],
                                                          start=(fc == 0), stop=(fc == 7)),
                                 reads=[mixg, wB], writes=[pb], inc=(fc == 7))
                        k.op("dve", lambda e: e.tensor_tensor(h[:, half * 512:(half + 1) * 512], h[:, half * 512:(half + 1) * 512], pb[:], ALU.add),
                             reads=[h, pb], writes=[h])
                        yield
                    k.dma(hout[0][t * 128:(t + 1) * 128, :], h[:], reads=[h], writes=[hout[1][t]])

            def side_thread(g):
                if g >= 1:
                    yield from epilogue(g - 1)
                if g + 1 < NGB:
                    yield from prologue(g + 1)

            for _ in prologue(0):
                pass
            for g in range(NGB):
                qTg, mixg = qT[g % 2], mixTg[g % 2]
                side = side_thread(g)
                items = [(2 * p + s, kb) for p in range(NHB // 2) for kb in range(4 * g + 3, -1, -1) for s in range(2)]

                def geom(i):
                    hh, kb = items[i]
                    r = max(kb - 4 * g, 0)
                    return hh, kb, hh // 2, hh % 2, r * 128, kb >= 4 * g

                def s1_pe(i):
                    hh, kb, fc, s, c0, diag = geom(i)
                    ps_ = slice(s * 64, (s + 1) * 64)
                    cs = slice(c0, 512)
                    pz = PZ[i % 2]
                    k.op("pe", lambda e: e.matmul(pz[:, cs], KT[ps_, fc, kb * 128:(kb + 1) * 128], qTg[ps_, fc, cs], start=True, stop=True),
                         reads=[KT, qTg], writes=[pz])

                def s1_act(i):
                    hh, kb, fc, s, c0, diag = geom(i)
                    cs = slice(c0, 512)
                    pz, E, sp = PZ[i % 2], Eb[i % 3], spb[i % 3]
                    k.op("act", lambda e: e.activation(E[:, cs], pz[:, cs], AF.Exp), reads=[pz], writes=[E])
                    k.op("act", lambda e: e.activation(sp[:, cs], E[:, cs], AF.Ln, bias=1.0), reads=[E], writes=[sp])
                    if diag:
                        k.op("dve", lambda e: e.tensor_tensor(sp[:, c0:c0 + 128], sp[:, c0:c0 + 128], strictTb, ALU.mult),
                             reads=[sp, cstb], writes=[sp])

                def s2_peA(i):
                    hh, kb, fc, s, c0, diag = geom(i)
                    cs = slice(c0, 512)
                    C, sp = PC[s], spb[i % 3]
                    if kb == 4 * g + 3:
                        k.op("dve", lambda e: e.memset(C[:], 0.0), writes=[C])
                    k.op("pe", lambda e: e.matmul(C[:, cs], NGEb, sp[:, cs], start=False, stop=False, skip_group_check=True),
                         reads=[cstb, sp], writes=[C])

                def s2_act(i):
                    hh, kb, fc, s, c0, diag = geom(i)
                    cs = slice(c0, 512)
                    C, ea_ = PC[s], eab[i % 2]
                    k.op("act", lambda e: e.activation(ea_[:, cs], C[:, cs], AF.Exp), reads=[C], writes=[ea_])

                def s2_peB(i):
                    hh, kb, fc, s, c0, diag = geom(i)
                    cs = slice(c0, 512)
                    C, sp = PC[s], spb[i % 3]
                    if kb > 0:
                        k.op("pe", lambda e: e.matmul(C[:, cs], NLTb, sp[:, cs], start=False, stop=False, skip_group_check=True),
                             reads=[cstb, sp], writes=[C])

                def s3_pool(i):
                    hh, kb, fc, s, c0, diag = geom(i)
                    cs = slice(c0, 512)
                    E, ea_, a_ = Eb[i % 3], eab[i % 2], ab[i % 2]
                    k.op("dve", lambda e: e.tensor_tensor(a_[:, cs], E[:, cs], ea_[:, cs], ALU.mult), reads=[E, ea_], writes=[a_])
                    if diag:
                        k.op("dve", lambda e: e.tensor_tensor(a_[:, c0:c0 + 128], a_[:, c0:c0 + 128], strictTb, ALU.mult),
                             reads=[a_, cstb], writes=[a_])

                def s3_pe(i):
                    hh, kb, fc, s, c0, diag = geom(i)
                    cs = slice(c0, 512)
                    a_ = ab[i % 2]
                    po = PO[fc % 2]
                    if kb == 4 * g + 3 and s == 0:
                        k.op("dve", lambda e: e.memset(po[:], 0.0), writes=[po])
                    vblk = Vt[:, kb, hh * 64:(hh + 1) * 64]
                    if s == 0:
                        k.op("pe", lambda e: e.matmul(po[0:64, cs], vblk, a_[:, cs], start=False, stop=False, skip_group_check=True),
                             reads=[Vt, a_], writes=[po])
                    else:
                        k.op("pe", lambda e: e.matmul(po[64:128, cs], vblk, a_[:, cs], start=False, stop=False, skip_group_check=True,
                                                      tile_position=(0, 64)), reads=[Vt, a_], writes=[po])
                    if kb == 0 and s == 1:
                        k.op("act", lambda e: e.copy(mixg[:, fc, :], po[:]), reads=[po], writes=[mixg])

                n_it = len(items)
                ok = lambda i: 0 <= i < n_it
                dumrhs = cstb[:, 0:4, :].rearrange("p c t -> p (c t)")
                stride = max(1, n_it // 72)
                for step in range(-3, n_it + 1):
                    i0, i1, i2_, i3, i4 = step + 3, step + 2, step + 1, step, step - 1
                    if ok(i3):
                        s3_pool(i3)
                    if ok(i0):
                        for _d in range(NDUM):
                            k.op("pe", lambda e: e.matmul(PZ[i0 % 2][:], NGEb, dumrhs, start=True, stop=True),
                                 reads=[cstb], writes=[PZ[i0 % 2]], inc=False)
                        s1_pe(i0)
                    if ok(i2_):
                        s2_peA(i2_)
                    if ok(i4):
                        s3_pe(i4)
                    if ok(i1):
                        s1_act(i1)
                    if ok(i2_):
                        s2_act(i2_)
                    if ok(i3):
                        s2_peB(i3)
                    if side is not None and step >= 0 and step % stride == 0:
                        try:
                            next(side)
                        except StopIteration:
                            side = None
                if side is not None:
                    for _ in side:
                        pass
            for _ in epilogue(NGB - 1):
                pass
            k.barrier()
            del self.nextbank

    def build(self, phases):
        nc, k = self.nc, self.k
        P = self.P = {}
        shapes = dict(
            x=[S, D], mem=[256, D], a_norm=[1, D], a_w_in=[1, D, 3340], a_conv=[1, 4, 2304],
            a_log=[1, 6], a_dt_bias=[1, 6], a_out_gain=[1, 128], a_w_out=[1, D, D],
            kv_norm=[D], w_kv=[D, 1536], b_norm=[1, D], b_w_in=[1, D, D], b_w_out=[1, D, D],
            mem_norm=[2, D], w_mem_kv=[2, D, 512], ffn_norm=[2, D], w_group=[2, D, 4], b_group=[2, 4],
            w_router=[2, D, 16], b_router=[2, 16], w1=[2, 16, D, 256], w3=[2, 16, D, 256],
            w2=[2, 16, 256, D], final_norm=[D], wgr=[2, 128, 8, 20], rbias=[2, 20], convw=[128, 18, 4])
        for n, s in shapes.items():
            P[n] = self.din(n, s)
        out = nc.dram_tensor("out", [S, D], F32, kind="ExternalOutput").ap()
        mkbufs = lambda nm: [Buf(None, "%s%d" % (nm, i)) for i in range(NT)]
        hx = (P["x"], mkbufs("x"))
        hA = (self.dscratch("hA", [S, D]), mkbufs("hA"))
        hB = (self.dscratch("hB", [S, D]), mkbufs("hB"))
        ho = (out, mkbufs("out"))
        with ExitStack() as gst:
            self.load_consts(gst)
            self.epsbuf = self.sbuf(gst, "epsb", [128, 1], F32)
            self.epsb = self.epsbuf
            k.op("dve", lambda e: e.memset(self.epsbuf[:], EPS), writes=[self.epsbuf])
            cur = hx
            seq = {"A": hA, "M0": hB, "B": hA, "M1": ho}
            for ph in phases:
                dst = seq[ph] if ph != phases[-1] else ho
                if ph == "M0":
                    self.moe_phase(0, cur, dst, final=False)
                elif ph == "M1":
                    self.moe_phase(1, cur, dst, final=True)
                elif ph == "A":
                    self.mixer_a_phase(cur, dst)
                elif ph == "B":
                    self.mixer_b_phase(cur, dst)
                cur = dst
            for b in ho[1]:
                if b.lw is not None:
                    k._wait("sp", b.lw)
        return nc


def make_consts():
    c = np.zeros((128, 8, 128), np.float32)
    i = np.arange(128)
    c[:, 0, :] = np.eye(128)
    c[:, 1, :] = (i[:, None] <= i[None, :])
    c[:, 2, :] = (i[:, None] > i[None, :])
    c[:, 3, :] = (i[:, None] < i[None, :])
    c[:, 4, :] = 1.0
    c[:, 5, :] = -(i[:, None] >= i[None, :]).astype(np.float32)
    c[:, 6, :] = -(i[:, None] < i[None, :]).astype(np.float32)
    c[:, 7, :] = -30000.0 * (i[:, None] >= i[None, :])
    return c


_CACHE = {}


def run(inputs, phases=("A", "M0", "B", "M1"), ncores=NCORES, trace=False):
    key = tuple(phases)
    if key not in _CACHE:
        mk = MK(phases)
        _CACHE[key] = mk.build(list(phases))
    nc = _CACHE[key]
    consts = make_consts()
    inputs = dict(inputs)
    wg = np.concatenate([np.asarray(inputs["w_group"]), np.asarray(inputs["w_router"])], axis=2)
    inputs["wgr"] = np.ascontiguousarray(wg.reshape(2, 8, 128, 20).transpose(0, 2, 1, 3))
    inputs["rbias"] = np.concatenate([np.asarray(inputs["b_group"]), np.asarray(inputs["b_router"])], axis=1)
    cw = np.asarray(inputs["a_conv"])[0]
    inputs["convw"] = np.ascontiguousarray(cw.reshape(4, 18, 128).transpose(2, 1, 0))
    in_maps = []
    for c in range(ncores):
        m = {"consts": consts}
        for n, v in inputs.items():
            v = np.asarray(v)
            if n in ("x", "mem"):
                m[n] = np.ascontiguousarray(v[c])
            else:
                m[n] = np.ascontiguousarray(v, dtype=np.float32)
        in_maps.append(m)
    res = run_bass_kernel_spmd(nc, in_maps, core_ids=list(range(ncores)), trace=trace)
    outs = np.stack([r["out"] for r in res.results], axis=0)
    return outs, res


def kernel(**inputs):
    outs, _ = run(inputs)
    return outs.astype(np.float32)
```

```python
from contextlib import ExitStack
import os
import numpy as np
import concourse.bass as bass
import concourse.mybir as mybir
from concourse.bass_utils import run_bass_kernel_spmd

F32 = mybir.dt.float32
BF16 = mybir.dt.bfloat16
AF = mybir.ActivationFunctionType
ALU = mybir.AluOpType
AX = mybir.AxisListType

S = 4096
D = 1024
NT = S // 128
EPS = 1e-6
NCORES = 8


class Buf:
    __slots__ = ("ap", "name", "_lw", "_rd", "_excl")
    lw = property(lambda self: self._lw, lambda self, v: setattr(self, "_lw", v))
    rd = property(lambda self: self._rd, lambda self, v: setattr(self, "_rd", v))
    excl = property(lambda self: self._excl, lambda self, v: setattr(self, "_excl", v))

    def __init__(self, ap, name="", excl=False):
        self.ap = ap
        self.name = name
        self.excl = excl
        self.lw = None
        self.rd = []

    def __getitem__(self, idx):
        return self.ap[idx]


class View(Buf):
    __slots__ = ("parent",)

    def __init__(self, parent, ap):
        self.parent = parent
        self.ap = ap
        self.name = parent.name

    lw = property(lambda self: self.parent.lw, lambda self, v: setattr(self.parent, "lw", v))
    rd = property(lambda self: self.parent.rd, lambda self, v: setattr(self.parent, "rd", v))
    excl = property(lambda self: self.parent.excl, lambda self, v: None)


class K:
    NDMA = 48

    def __init__(self, nc):
        self.nc = nc
        self.eng = {"pe": nc.tensor, "act": nc.scalar, "dve": nc.vector,
                    "pool": nc.gpsimd, "sp": nc.sync}
        self.sem = {e: nc.alloc_semaphore("s_" + e) for e in ("pe", "act", "dve", "pool")}
        self.cnt = {e: 0 for e in self.sem}
        self.waited = {}
        self.dsem = [nc.alloc_semaphore("d%d" % i) for i in range(self.NDMA)]
        self.dcnt = [0] * self.NDMA
        self.dnext = 0
        self.dnext_sw = 0
        self.nins = 0

    def _semh(self, key):
        return self.sem[key] if isinstance(key, str) else self.dsem[key]

    def _wait(self, e, dep):
        key, val = dep
        if key == e and e == "pe":
            return
        w = self.waited.get((e, key), 0)
        if w >= val:
            return
        self.eng[e].wait_ge(self._semh(key), val)
        self.nins += 1
        self.waited[(e, key)] = val

    def _deps(self, e, reads, writes):
        best = {}
        for r in reads:
            if r.lw is not None:
                if best.get(r.lw[0], 0) < r.lw[1]:
                    best[r.lw[0]] = r.lw[1]
            if r.excl:
                for key, val in r.rd:
                    if key != e and best.get(key, 0) < val:
                        best[key] = val
        for w in writes:
            if w.lw is not None:
                if best.get(w.lw[0], 0) < w.lw[1]:
                    best[w.lw[0]] = w.lw[1]
            for key, val in w.rd:
                if best.get(key, 0) < val:
                    best[key] = val
        for key, val in best.items():
            self._wait(e, (key, val))

    def _mark(self, tag, reads, writes):
        for r in reads:
            r.rd.append(tag)
            if len(r.rd) > 64:
                best = {}
                for key, val in r.rd:
                    if best.get(key, 0) < val:
                        best[key] = val
                r.rd = list(best.items())
        for w in writes:
            w.lw = tag
            w.rd = []

    def op(self, e, fn, reads=(), writes=(), inc=True):
        self._deps(e, reads, writes)
        ins = fn(self.eng[e])
        self.nins += 1
        if inc:
            ins.then_inc(self.sem[e], 1)
            self.cnt[e] += 1
            tag = (e, self.cnt[e])
        else:
            tag = (e, self.cnt[e] + 1)
        self._mark(tag, reads, writes)
        return ins

    def dma(self, out, in_, reads=(), writes=(), q="sp", **kw):
        if q == "pool":
            slot = 32 + self.dnext_sw
            self.dnext_sw = (self.dnext_sw + 1) % (self.NDMA - 32)
        else:
            slot = self.dnext
            self.dnext = (self.dnext + 1) % 32
        if self.dcnt[slot] > 0:
            self._wait(q, (slot, 16 * self.dcnt[slot]))
        self._deps(q, reads, writes)
        ins = self.eng[q].dma_start(out=out, in_=in_, **kw)
        self.nins += 1
        ins.then_inc(self.dsem[slot], 16)
        self.dcnt[slot] += 1
        tag = (slot, 16 * self.dcnt[slot])
        self._mark(tag, reads, writes)
        return tag

    def barrier(self):
        for e in ("pe", "act", "dve", "pool", "sp"):
            for e2 in ("pe", "act", "dve", "pool"):
                if e2 != e and self.cnt[e2] > 0:
                    self._wait(e, (e2, self.cnt[e2]))
            for slot in range(self.NDMA):
                if self.dcnt[slot] > 0:
                    self._wait(e, (slot, 16 * self.dcnt[slot]))


class MK:
    def __init__(self, phases, h0_from_input=True):
        self.nc = nc = bass.Bass("TRN2", target_bir_lowering=False)
        self.k = K(nc)
        self.uid = 0
        self.ins = {}
        self.ps = [Buf(nc.alloc_psum_tensor("psb%d" % i, [128, 512], F32).ap(), "ps%d" % i, excl=True)
                   for i in range(8)]

    def din(self, name, shape):
        ap = self.nc.dram_tensor(name, list(shape), F32, kind="ExternalInput").ap()
        self.ins[name] = ap
        return ap

    def dscratch(self, name, shape, dt=F32):
        return self.nc.dram_tensor(name, list(shape), dt, kind="Internal").ap()

    def sbuf(self, st, name, shape, dt):
        self.uid += 1
        h = st.enter_context(self.nc.sbuf_tensor("%s_%d" % (name, self.uid), list(shape), dt))
        return Buf(h.ap(), name)

    def load_consts(self, st):
        k = self.k
        c = self.din("consts", [128, 8, 128])
        self.cst = self.sbuf(st, "cst", [128, 8, 128], F32)
        k.dma(self.cst[:], c, writes=[self.cst])
        self.ident = self.cst[:, 0, :]
        self.U = self.cst[:, 1, :]
        self.SL = self.cst[:, 2, :]
        self.strictT = self.cst[:, 3, :]
        self.ones = self.cst[:, 4, :]
        self.cstb = self.sbuf(st, "cstb", [128, 8, 128], BF16)
        k.dma(self.cstb[:], c, writes=[self.cstb], q="pool")
        self.identb = self.cstb[:, 0, :]
        self.onesb = self.cstb[:, 4, :]

    def rmsnorm(self, h, gainb, xn, junk, st2):
        k = self.k
        k.op("act", lambda e: e.activation(junk[:], h[:], AF.Square, accum_out=st2[:, 0:1]),
             reads=[h], writes=[junk, st2])
        k.op("act", lambda e: e.activation(st2[:, 1:2], st2[:, 0:1], AF.Ln, bias=self.epsb[:, 0:1], scale=1.0 / D),
             reads=[st2, self.epsbuf], writes=[st2])
        k.op("act", lambda e: e.activation(st2[:, 2:3], st2[:, 1:2], AF.Exp, scale=-0.5), reads=[st2], writes=[st2])
        k.op("dve", lambda e: e.scalar_tensor_tensor(xn[:], h[:], st2[:, 2:3], gainb[:], ALU.mult, ALU.mult),
             reads=[h, st2, gainb], writes=[xn])

    def transpose8(self, src, dsts, psa, psb, evac=("act", "dve"), second="pool"):
        k = self.k
        for half, ps in enumerate((psa, psb)):
            for j in range(4):
                c = half * 4 + j
                k.op("pe", lambda e: e.transpose(ps[:, j * 128:(j + 1) * 128], src[:, c * 128:(c + 1) * 128], self.ident),
                     reads=[src, self.cst], writes=[ps], inc=(j == 3))
            dbuf, fn = dsts[0]
            eng = evac[half % len(evac)]
            pv = ps[:].rearrange("p (c t) -> p c t", c=4)
            if eng == "act":
                k.op("act", lambda e: e.copy(fn(half), pv), reads=[ps], writes=[dbuf])
            else:
                k.op(eng, lambda e: e.tensor_copy(fn(half), pv), reads=[ps], writes=[dbuf])
            for dbuf2, fn2 in dsts[1:]:
                k.op(second, lambda e: e.tensor_copy(fn2(half), fn(half)), reads=[dbuf], writes=[dbuf2])

    def moe_phase(self, l, hin, hout, final=False, out_ap=None):
        nc, k = self.nc, self.k
        G = 1024
        NTG = G // 128
        NG = S // G
        NTB = G // 512
        NEX = 16
        P = self.P
        with ExitStack() as st:
            sb = lambda n, s, d: self.sbuf(st, n, s, d)
            gain = sb("gain", [128, D], F32)
            k.dma(gain[:], P["ffn_norm"][l].partition_broadcast(128), writes=[gain])
            if final:
                fgain = sb("fgain", [128, D], F32)
                k.dma(fgain[:], P["final_norm"].partition_broadcast(128), writes=[fgain])
            wgr = sb("wgr", [128, 8, 20], F32)
            k.dma(wgr[:], P["wgr"][l], writes=[wgr])
            rb = sb("rbias", [128, 20], F32)
            k.dma(rb[:], P["rbias"][l].partition_broadcast(128), writes=[rb])
            xnT = [sb("xnT%d" % i, [128, 8, G], BF16) for i in range(2)]
            yacc = [[sb("yacc%d_%d" % (j, i), [128, D], F32) for i in range(NTG)] for j in range(2)]
            comb = [[sb("comb%d_%d" % (j, i), [128, 16], F32) for i in range(NTG)] for j in range(2)]
            w1b = [sb("w1b%d" % i, [128, 8, 256], BF16) for i in range(2)]
            w3b = [sb("w3b%d" % i, [128, 8, 256], BF16) for i in range(2)]
            w2b = [sb("w2b%d" % i, [128, 2, D], BF16) for i in range(2)]
            ht = [sb("ht%d" % i, [128, D], F32) for i in range(2)]
            htc = [sb("htc%d" % i, [128, D], F32) for i in range(2)]
            xn = [sb("xn%d" % i, [128, D], F32) for i in range(2)]
            xnT32 = [sb("xnT32_%d" % i, [128, 8, 128], F32) for i in range(2)]
            stt = [sb("stt%d" % i, [128, 4], F32) for i in range(2)]
            sttc = [sb("sttc%d" % i, [128, 4], F32) for i in range(2)]
            rt = [sb("rt%d" % i, [128, 96], F32) for i in range(2)]
            hid = [sb("hid%d" % i, [128, 2, 512], BF16) for i in range(2)]
            sil = [sb("sil%d" % i, [128, 512], F32) for i in range(2)]
            ps = self.ps
            w1d, w3d, w2d = P["w1"], P["w3"], P["w2"]

            def load_w(e, slot):
                k.dma(w1b[slot][:], w1d[l, e].rearrange("(c p) f -> p c f", p=128), writes=[w1b[slot]], q="pool")
                k.dma(w3b[slot][:], w3d[l, e].rearrange("(c p) f -> p c f", p=128), writes=[w3b[slot]], q="pool")
                k.dma(w2b[slot][:], w2d[l, e].rearrange("(c p) n -> p c n", p=128), writes=[w2b[slot]], q="pool")

            def stage_a(g, tis=None, banks=None):
                par = g % 2
                xT = xnT[par]
                for ti in (range(NTG) if tis is None else tis):
                    t = g * NTG + ti
                    b = ti % 2
                    h = ht[b]
                    k.dma(h[:], hin[0][t * 128:(t + 1) * 128, :], reads=[hin[1][t]], writes=[h])
                    self.rmsnorm(h, gain, xn[b], xn[b], stt[b])
                    yield
                    x32 = xnT32[b]
                    self.transpose8(
                        xn[b],
                        [(x32, lambda half: x32[:, half * 4:(half + 1) * 4, :]),
                         (xT, lambda half: xT[:, half * 4:(half + 1) * 4, ti * 128:(ti + 1) * 128])],
                        ps[6] if banks is None else banks[0], ps[7] if banks is None else banks[1])
                    yield
                    pr = ps[6 + (ti % 2)] if banks is None else banks[2]
                    for dc in range(8):
                        k.op("pe", lambda e: e.matmul(pr[:, 0:20], x32[:, dc, :], wgr[:, dc, :], start=(dc == 0), stop=(dc == 7)),
                             reads=[x32, wgr], writes=[pr], inc=(dc == 7))
                    yield
                    r = rt[b]
                    R = lambda a, n: r[:, a:a + n]
                    lg, gmax, ngmax, oh, ge, gsum, pg = R(0, 20), R(20, 1), R(21, 1), R(22, 4), R(26, 4), R(30, 1), R(31, 1)
                    tmp, elsel, m1, nm1, ee, mask1, ee2 = R(32, 16), R(48, 4), R(52, 1), R(53, 1), R(54, 4), R(58, 4), R(62, 4)
                    v2, mask2, den, rden, wl, scl = R(66, 1), R(67, 4), R(71, 1), R(72, 1), R(73, 4), R(77, 1)
                    dv = lambda fn, rd=(), wr=(): k.op("dve", fn, reads=[r] + list(rd), writes=[r] + list(wr))
                    dv(lambda e: e.tensor_tensor(lg, pr[:, 0:20], rb[:], ALU.add), rd=[pr, rb])
                    dv(lambda e: e.tensor_reduce(gmax, lg[:, 0:4], AX.X, ALU.max))
                    dv(lambda e: e.tensor_single_scalar(ngmax, gmax, -1.0, ALU.mult))
                    dv(lambda e: e.tensor_scalar(oh, lg[:, 0:4], gmax, None, ALU.is_equal))
                    yield
                    k.op("act", lambda e: e.activation(ge, lg[:, 0:4], AF.Exp, bias=ngmax, accum_out=gsum), reads=[r], writes=[r])
                    dv(lambda e: e.reciprocal(pg, gsum))
                    dv(lambda e: e.tensor_tensor(tmp.rearrange("p (g j) -> p g j", g=4),
                                                 lg[:, 4:20].rearrange("p (g j) -> p g j", g=4),
                                                 oh.unsqueeze(2).to_broadcast([128, 4, 4]), ALU.mult))
                    dv(lambda e: e.tensor_reduce(elsel, tmp.rearrange("p (g j) -> p j g", g=4), AX.X, ALU.add))
                    yield
                    dv(lambda e: e.tensor_reduce(m1, elsel, AX.X, ALU.max))
                    dv(lambda e: e.tensor_single_scalar(nm1, m1, -1.0, ALU.mult))
                    k.op("act", lambda e: e.activation(ee, elsel, AF.Exp, bias=nm1), reads=[r], writes=[r])
                    dv(lambda e: e.tensor_scalar(mask1, elsel, m1, None, ALU.is_equal))
                    yield
                    dv(lambda e: e.scalar_tensor_tensor(ee2, mask1, -2.0, ee, ALU.mult, ALU.add))
                    dv(lambda e: e.tensor_reduce(v2, ee2, AX.X, ALU.max))
                    dv(lambda e: e.tensor_scalar(mask2, ee2, v2, None, ALU.is_equal))
                    dv(lambda e: e.tensor_single_scalar(den, v2, 1.0, ALU.add))
                    yield
                    dv(lambda e: e.reciprocal(rden, den))
                    dv(lambda e: e.scalar_tensor_tensor(wl, mask2, v2, mask1, ALU.mult, ALU.add))
                    dv(lambda e: e.tensor_tensor(scl, pg, rden, ALU.mult))
                    dv(lambda e: e.tensor_scalar(wl, wl, scl, None, ALU.mult))
                    cb = comb[par][ti]
                    dv(lambda e: e.tensor_tensor(cb[:].rearrange("p (g j) -> p g j", g=4),
                                                 oh.unsqueeze(2).to_broadcast([128, 4, 4]),
                                                 wl.unsqueeze(1).to_broadcast([128, 4, 4]), ALU.mult), wr=[cb])
                    yield

            def stage_b(g):
                par = g % 2
                xT = xnT[par]
                work = [(ex, tb) for ex in range(NEX) for tb in range(NTB)]

                def up(idx):
                    ex, tb = work[idx]
                    slot = ex % 2
                    w1, w3 = w1b[slot], w3b[slot]
                    hd = hid[idx % 2]
                    for fc in range(2):
                        p1 = ps[fc]
                        p3 = ps[2 + fc]
                        for wsrc, pdst in ((w1, p1), (w3, p3)):
                            for dc in range(8):
                                k.op("pe", lambda e: e.matmul(pdst[:], wsrc[:, dc, fc * 128:(fc + 1) * 128], xT[:, dc, tb * 512:(tb + 1) * 512],
                                                              start=(dc == 0), stop=(dc == 7)),
                                     reads=[wsrc, xT], writes=[pdst], inc=(dc == 7))
                                if dc % 4 == 3:
                                    yield
                        sl = sil[fc]
                        k.op("act", lambda e: e.activation(sl[:], p1[:], AF.Silu), reads=[p1], writes=[sl])
                        k.op("dve", lambda e: e.tensor_tensor(hd[:, fc, :], sl[:], p3[:], ALU.mult), reads=[sl, p3], writes=[hd])

                def down(idx):
                    ex, tb = work[idx]
                    slot = ex % 2
                    w2 = w2b[slot]
                    hd = hid[idx % 2]
                    for tt in range(4):
                        ti = tb * 4 + tt
                        for half in range(2):
                            py = ps[4 + half]
                            for fc in range(2):
                                k.op("pe", lambda e: e.matmul(py[:], hd[:, fc, tt * 128:(tt + 1) * 128], w2[:, fc, half * 512:(half + 1) * 512],
                                                              start=(fc == 0), stop=(fc == 1)),
                                     reads=[hd, w2], writes=[py], inc=(fc == 1))
                            ya = yacc[par][ti]
                            cs = comb[par][ti][:, ex:ex + 1]
                            if ex == 0:
                                k.op("dve", lambda e: e.tensor_scalar(ya[:, half * 512:(half + 1) * 512], py[:], cs, None, ALU.mult),
                                     reads=[py, comb[par][ti]], writes=[ya])
                            else:
                                k.op("dve", lambda e: e.scalar_tensor_tensor(ya[:, half * 512:(half + 1) * 512], py[:], cs,
                                                                             ya[:, half * 512:(half + 1) * 512], ALU.mult, ALU.add),
                                     reads=[py, comb[par][ti], ya], writes=[ya])
                            yield
                    if tb == NTB - 1:
                        if ex + 2 < NEX:
                            load_w(ex + 2, slot)
                        elif g + 1 < NG:
                            load_w(ex + 2 - NEX, slot)

                def rr(*gens):
                    gens = [g_ for g_ in gens if g_ is not None]
                    while gens:
                        for g_ in list(gens):
                            try:
                                next(g_)
                                yield
                            except StopIteration:
                                gens.remove(g_)

                yield from up(0)
                for idx in range(len(work)):
                    nu = up(idx + 1) if idx + 1 < len(work) else None
                    if nu is not None:
                        next(nu)
                        yield
                    yield from rr(nu, down(idx))

            def stage_c(g, tis=None):
                par = g % 2
                for ti in (range(NTG) if tis is None else tis):
                    t = g * NTG + ti
                    b = ti % 2
                    h = htc[b]
                    ya = yacc[par][ti]
                    k.dma(h[:], hin[0][t * 128:(t + 1) * 128, :], reads=[hin[1][t]], writes=[h])
                    k.op("pool", lambda e: e.tensor_tensor(ya[:], h[:], ya[:], ALU.add), reads=[h, ya], writes=[ya])
                    yield
                    if final:
                        self.rmsnorm(ya, fgain, h, h, sttc[b])
                        k.dma(hout[0][t * 128:(t + 1) * 128, :], h[:], reads=[h], writes=[hout[1][t]])
                    else:
                        k.dma(hout[0][t * 128:(t + 1) * 128, :], ya[:], reads=[ya], writes=[hout[1][t]])
                    yield

            def run_group(bg, ag, cg):
                others = [[g_, per] for g_, per in ((ag, 4), (cg, 24)) if g_ is not None]
                step = 0
                b_alive = bg is not None
                while b_alive or others:
                    if b_alive:
                        try:
                            next(bg)
                        except StopIteration:
                            b_alive = False
                    for item in list(others):
                        if (not b_alive) or step % item[1] == 0:
                            try:
                                next(item[0])
                            except StopIteration:
                                others.remove(item)
                    step += 1

            load_w(0, 0)
            load_w(1, 1)
            ga = [stage_a(0, range(0, NTG, 2), (ps[0], ps[1], ps[2])), stage_a(0, range(1, NTG, 2), (ps[3], ps[4], ps[5]))]
            while ga:
                for g_ in list(ga):
                    try:
                        next(g_)
                    except StopIteration:
                        ga.remove(g_)
            for g in range(NG):
                run_group(stage_b(g), stage_a(g + 1) if g + 1 < NG else None, stage_c(g - 1) if g >= 1 else None)
            gc = [stage_c(NG - 1, range(0, NTG, 2)), stage_c(NG - 1, range(1, NTG, 2))]
            while gc:
                for g_ in list(gc):
                    try:
                        next(g_)
                    except StopIteration:
                        gc.remove(g_)
            k.barrier()

    def nextbank(self):
        self.pbi = (getattr(self, "pbi", -1) + 1) % 8
        return self.ps[self.pbi]

    def mem_kv(self, st, l):
        k, P = self.k, self.P
        sb = lambda n, s, d: self.sbuf(st, n, s, d)
        memkT = sb("memkT", [128, 2, 256], BF16)
        memv = sb("memv", [128, 2, 256], BF16)
        with ExitStack() as st2:
            sb2 = lambda n, s, d: self.sbuf(st2, n, s, d)
            g = sb2("mg", [128, D], F32)
            k.dma(g[:], P["mem_norm"][l].partition_broadcast(128), writes=[g])
            w = sb2("wmkv", [128, 8, 512], BF16)
            k.dma(w[:], P["w_mem_kv"][l].rearrange("(c p) n -> p c n", p=128), writes=[w], q="pool")
            mT = sb2("memnT", [128, 8, 256], BF16)
            junk = sb2("mjunk", [128, D], F32)
            for mt in range(2):
                h = sb2("mh%d" % mt, [128, D], F32)
                xn = sb2("mxn%d" % mt, [128, D], F32)
                stt = sb2("mst%d" % mt, [128, 4], F32)
                k.dma(h[:], P["mem"][mt * 128:(mt + 1) * 128, :], writes=[h])
                self.rmsnorm(h, g, xn, junk, stt)
                self.transpose8(xn, [(mT, lambda half: mT[:, half * 4:(half + 1) * 4, mt * 128:(mt + 1) * 128])],
                                self.nextbank(), self.nextbank())
            for j in range(2):
                pb = self.nextbank()
                for dc in range(8):
                    k.op("pe", lambda e: e.matmul(pb[:, 0:256], w[:, dc, j * 128:(j + 1) * 128], mT[:, dc, :],
                                                  start=(dc == 0), stop=(dc == 7)),
                         reads=[w, mT], writes=[pb], inc=(dc == 7))
                k.op("act", lambda e: e.copy(memkT[:, j, :], pb[:, 0:256]), reads=[pb], writes=[memkT])
            for mt in range(2):
                pb = self.nextbank()
                for dc in range(8):
                    k.op("pe", lambda e: e.matmul(pb[:, 0:256], mT[:, dc, mt * 128:(mt + 1) * 128], w[:, dc, 256:512],
                                                  start=(dc == 0), stop=(dc == 7)),
                         reads=[w, mT], writes=[pb], inc=(dc == 7))
                k.op("dve", lambda e: e.tensor_copy(memv[:, mt, :], pb[:, 0:256]), reads=[pb], writes=[memv])
            k.barrier()
        return memkT, memv

    def mem_attend(self, W, mqT, qoff, memkT, memv, mix, col0=768):
        for _ in self.mem_attend_g(W, mqT, qoff, memkT, memv, mix, col0):
            pass

    def mem_attend_g(self, W, mqT, qoff, memkT, memv, mix, col0=768, banks=None):
        k = self.k
        pe_ = W["pexp"]; ms = W["mstat"]; pT = W["pT"]
        if banks is None:
            banks = [self.nextbank(), self.nextbank()]
        for hh in range(4):
            pair, s = hh // 2, hh % 2
            pb = banks[s]
            k.op("pe", lambda e: e.matmul(pb[:, pair * 256:(pair + 1) * 256], mqT[s * 64:(s + 1) * 64, pair, qoff:qoff + 128],
                                          memkT[s * 64:(s + 1) * 64, pair, :], start=True, stop=True),
                 reads=[mqT, memkT], writes=[pb])
        for s in range(2):
            pb = banks[s]
            k.op("dve", lambda e: e.tensor_reduce(ms[:, s:s + 3:2], pb[:].rearrange("p (h m) -> p h m", h=2), AX.X, ALU.max),
                 reads=[pb], writes=[ms])
        k.op("dve", lambda e: e.tensor_single_scalar(ms[:, 4:8], ms[:, 0:4], -0.125, ALU.mult), reads=[ms], writes=[ms])
        yield
        for hh in range(4):
            pair, s = hh // 2, hh % 2
            pb = banks[s]
            k.op("act", lambda e: e.activation(pe_[:, hh, :], pb[:, pair * 256:(pair + 1) * 256], AF.Exp, bias=ms[:, 4 + hh:5 + hh],
                                               scale=0.125, accum_out=ms[:, 8 + hh:9 + hh]),
                 reads=[pb, ms], writes=[pe_, ms])
        k.op("dve", lambda e: e.reciprocal(ms[:, 12:16], ms[:, 8:12]), reads=[ms], writes=[ms])
        yield
        for half in range(2):
            pb = self.nextbank()
            for j in range(4):
                idx = half * 4 + j
                hh, mc = idx // 2, idx % 2
                k.op("pe", lambda e: e.transpose(pb[:, j * 128:(j + 1) * 128], pe_[:, hh, mc * 128:(mc + 1) * 128], self.ident),
                     reads=[pe_, self.cst], writes=[pb], inc=(j == 3))
            if half == 0:
                k.op("act", lambda e: e.copy(pT[:, 0:4, :], pb[:].rearrange("p (c t) -> p c t", c=4)), reads=[pb], writes=[pT])
            else:
                k.op("dve", lambda e: e.tensor_copy(pT[:, 4:8, :], pb[:].rearrange("p (c t) -> p c t", c=4)), reads=[pb], writes=[pT])
        yield
        pb = self.nextbank()
        for hh in range(4):
            for mc in range(2):
                k.op("pe", lambda e: e.matmul(pb[:, hh * 64:(hh + 1) * 64], pT[:, hh * 2 + mc, :], memv[:, mc, hh * 64:(hh + 1) * 64],
                                              start=(mc == 0), stop=(mc == 1)),
                     reads=[pT, memv], writes=[pb], inc=(mc == 1))
        k.op("dve", lambda e: e.tensor_tensor(mix[:, col0:col0 + 256].rearrange("p (h d) -> p h d", h=4),
                                              pb[:, 0:256].rearrange("p (h d) -> p h d", h=4),
                                              ms[:, 12:16].unsqueeze(2).to_broadcast([128, 4, 64]), ALU.mult),
             reads=[pb, ms], writes=[mix])

    def mem_work(self, st):
        sb = lambda n, s, d: self.sbuf(st, n, s, d)
        return {"pexp": sb("pexp", [128, 4, 256], F32), "mstat": sb("mstat", [128, 16], F32),
                "pT": sb("pT", [128, 8, 128], BF16)}

    def out_proj(self, mix, mixT, w_out, h, hn, dst_ap, dst_buf):
        k = self.k
        self.transpose8(mix, [(mixT, lambda half: mixT[:, half * 4:(half + 1) * 4, :])], self.nextbank(), self.nextbank())
        for half in range(2):
            pb = self.nextbank()
            for fc in range(8):
                k.op("pe", lambda e: e.matmul(pb[:], mixT[:, fc, :], w_out[:, fc, half * 512:(half + 1) * 512],
                                              start=(fc == 0), stop=(fc == 7)),
                     reads=[mixT, w_out], writes=[pb], inc=(fc == 7))
            k.op("dve", lambda e: e.tensor_tensor(hn[:, half * 512:(half + 1) * 512], h[:, half * 512:(half + 1) * 512], pb[:], ALU.add),
                 reads=[h, pb], writes=[hn])
        k.dma(dst_ap, hn[:], reads=[hn], writes=[dst_buf])

    def mixer_a_phase(self, hin, hout):
        nc, k, P = self.nc, self.k, self.P
        NTA = int(os.environ.get("MK_NTA", str(NT)))
        with ExitStack() as st:
            sb = lambda n, s, d: self.sbuf(st, n, s, d)
            memkT, memv = self.mem_kv(st, 0)
            gain = sb("gainA", [128, D], F32)
            k.dma(gain[:], P["a_norm"][0].partition_broadcast(128), writes=[gain])
            w_in = sb("w_inA", [128, 8, 3340], BF16)
            for c in range(8):
                k.dma(w_in[:, c, :], P["a_w_in"][0, c * 128:(c + 1) * 128, :], writes=[w_in], q="pool")
            w_out = sb("w_outA", [128, 8, D], BF16)
            k.dma(w_out[:], P["a_w_out"][0].rearrange("(c p) n -> p c n", p=128), writes=[w_out], q="pool")
            convw = sb("convw", [128, 18, 4], F32)
            k.dma(convw[:], P["convw"], writes=[convw])
            sc6 = sb("sc6", [128, 32], F32)
            k.dma(sc6[:, 0:6], P["a_log"][0].partition_broadcast(128), writes=[sc6])
            k.dma(sc6[:, 6:12], P["a_dt_bias"][0].partition_broadcast(128), writes=[sc6])
            k.op("act", lambda e: e.activation(sc6[:, 12:18], sc6[:, 0:6], AF.Exp), reads=[sc6], writes=[sc6])
            k.op("dve", lambda e: e.tensor_single_scalar(sc6[:, 12:18], sc6[:, 12:18], -1.0, ALU.mult), reads=[sc6], writes=[sc6])
            ogain = sb("ogain", [128, 128], F32)
            k.dma(ogain[:], P["a_out_gain"][0].partition_broadcast(128), writes=[ogain])
            MW = self.mem_work(st)
            pc = sb("pc", [128, 18, 131], F32)
            k.op("dve", lambda e: e.memset(pc[:], 0.0), writes=[pc])
            Sf = [sb("Sf%d" % h, [128, 128], F32) for h in range(6)]
            Sb = [sb("Sb%d" % h, [128, 128], BF16) for h in range(6)]
            for h in range(6):
                k.op("dve", lambda e: e.memset(Sf[h][:], 0.0), writes=[Sf[h]])
                k.op("pool", lambda e: e.memset(Sb[h][:], 0.0), writes=[Sb[h]])
            hA = sb("htA", [128, D], F32)
            xT = sb("xnTA", [128, 8, 128], BF16)
            cv = sb("cv", [128, 12, 128], F32)
            cvv = sb("cvv", [128, 6, 128], F32)
            ctmp = sb("ctmp", [128, 128], F32)
            cvjunk = View(cv, cv[:, 0:8, :].rearrange("p c t -> p (c t)"))
            sq = sb("sq", [128, 12, 128], BF16)
            rs = sb("rs", [128, 12, 128], F32)
            kn32 = sb("kn32", [128, 6, 128], F32)
            sttA = sb("sttA", [128, 4], F32)
            qkT = [sb("qkT%d" % i, [128, 6, 2, 128], BF16) for i in range(3)]
            sc = [sb("scA%d" % i, [128, 96], F32) for i in range(3)]
            gg = [sb("gg%d" % i, [128, 768], F32) for i in range(3)]
            mqT = [sb("mqTA%d" % i, [128, 2, 128], BF16) for i in range(3)]
            SLg = [sb("SLg%d" % i, [128, 128], F32) for i in range(6)]
            dm = sb("dm", [128, 6, 128], F32)
            dmi = sb("dmi", [128, 6, 128], F32)
            Wq = [[sb("W%d_%d" % (h, i), [128, 3, 128], BF16) for i in range(2)] for h in range(6)]
            Q0f = [sb("Q0f%d" % i, [128, 128], F32) for i in range(6)]
            kt = [sb("kt%d" % i, [128, 6, 128], BF16) for i in range(2)]
            vtok = [sb("vtok%d" % i, [128, 6, 128], F32) for i in range(2)]
            attnT = [sb("attnT%d" % i, [128, 6, 128], BF16) for i in range(2)]
            TT = [sb("TT%d" % i, [128, 6, 128], BF16) for i in range(2)]
            Rall = sb("Rall", [128, 6, 128], BF16)
            vnew = sb("vnewA", [128, 6, 128], BF16)
            oall = sb("oall", [128, 6, 128], F32)
            ost = sb("ost", [128, 24], F32)
            ojunk = sb("ojunk", [128, 128], F32)
            mix = sb("mixA", [128, D], F32)
            mixT = sb("mixTA", [128, 8, 128], BF16)
            hC = sb("htC", [128, D], F32)
            ident, U, SL, ones = self.ident, self.U, self.SL, self.ones
            NEGs = self.cst[:, 7, :]
            identb = self.identb
            cst, cstb = self.cst, self.cstb
            QS = float(128 ** -0.5)
            ctm = View(rs, rs[:, 0:9, :])
            rsjunk = View(rs, rs[:, 0:8, :].rearrange("p c t -> p (c t)"))
            rotA = [0]

            def nbA():
                rotA[0] = (rotA[0] + 1) % 6
                return self.ps[rotA[0]]
            self.nextbank = nbA
            membanks = [self.ps[6], self.ps[7]]

            def frontA(t):
                i3 = t % 3
                h = hA
                k.dma(h[:], hin[0][t * 128:(t + 1) * 128, :], reads=[hin[1][t]], writes=[h])
                self.rmsnorm(h, gain, h, rsjunk, sttA)
                yield
                self.transpose8(h, [(xT, lambda half: xT[:, half * 4:(half + 1) * 4, :])], self.nextbank(), self.nextbank())
                yield
                for g4 in range(5):
                    nf = 4 if g4 < 4 else 2
                    pb = self.nextbank()
                    for j in range(nf):
                        fc = g4 * 4 + j
                        for dc in range(8):
                            k.op("pe", lambda e: e.matmul(pb[:, j * 128:(j + 1) * 128], w_in[:, dc, fc * 128:(fc + 1) * 128], xT[:, dc, :],
                                                          start=(dc == 0), stop=(dc == 7)),
                                 reads=[w_in, xT], writes=[pb], inc=(dc == 7 and j == nf - 1))
                    dstv = pc[:, g4 * 4:g4 * 4 + nf, 3:131]
                    srcv = pb[:, 0:nf * 128].rearrange("p (c t) -> p c t", c=nf)
                    k.op("act", lambda e: e.copy(dstv, srcv), reads=[pb], writes=[pc])
                    yield
                pb = self.nextbank()
                for j in range(2):
                    for dc in range(8):
                        k.op("pe", lambda e: e.matmul(pb[:, j * 128:(j + 1) * 128], w_in[:, dc, 3084 + j * 128:3084 + (j + 1) * 128], xT[:, dc, :],
                                                      start=(dc == 0), stop=(dc == 7)),
                             reads=[w_in, xT], writes=[pb], inc=(dc == 7 and j == 1))
                k.op("act", lambda e: e.copy(mqT[i3][:], pb[:, 0:256].rearrange("p (c t) -> p c t", c=2)), reads=[pb], writes=[mqT[i3]])
                yield
                pg1 = self.nextbank()
                for dc in range(8):
                    k.op("pe", lambda e: e.matmul(pg1[:], xT[:, dc, :], w_in[:, dc, 2304:2816], start=(dc == 0), stop=(dc == 7)),
                         reads=[w_in, xT], writes=[pg1], inc=(dc == 7))
                pg2 = self.nextbank()
                for dc in range(8):
                    k.op("pe", lambda e: e.matmul(pg2[:, 0:268], xT[:, dc, :], w_in[:, dc, 2816:3084], start=(dc == 0), stop=(dc == 7)),
                         reads=[w_in, xT], writes=[pg2], inc=(dc == 7))
                g_g = gg[i3]
                k.op("act", lambda e: e.activation(g_g[:, 0:512], pg1[:], AF.Silu), reads=[pg1], writes=[g_g])
                k.op("act", lambda e: e.activation(g_g[:, 512:768], pg2[:, 0:256], AF.Silu), reads=[pg2], writes=[g_g])
                s_ = sc[i3]
                C = lambda a_, n=6: s_[:, a_:a_ + n]
                beta, tt_, ex_, sp_, g_, gcl, egc, negc, etl, egl, dd, eb_ = (C(0), C(6), C(12), C(18), C(24), C(32, 16), C(48), C(54), C(60), C(66), C(72), C(78))
                k.op("act", lambda e: e.activation(eb_, pg2[:, 256:262], AF.Exp, scale=-1.0), reads=[pg2], writes=[s_])
                k.op("dve", lambda e: e.tensor_tensor(tt_, pg2[:, 262:268], sc6[:, 6:12], ALU.add), reads=[pg2, sc6], writes=[s_])
                k.op("dve", lambda e: e.tensor_single_scalar(eb_, eb_, 1.0, ALU.add), reads=[s_], writes=[s_])
                k.op("dve", lambda e: e.reciprocal(beta, eb_), reads=[s_], writes=[s_])
                k.op("pool", lambda e: e.tensor_tensor(g_g[:].rearrange("p (h d) -> p h d", h=6), g_g[:].rearrange("p (h d) -> p h d", h=6),
                                                       ogain[:].unsqueeze(1).to_broadcast([128, 6, 128]), ALU.mult),
                     reads=[g_g, ogain], writes=[g_g])
                yield
                k.op("act", lambda e: e.activation(ex_, tt_, AF.Exp), reads=[s_], writes=[s_])
                k.op("act", lambda e: e.activation(sp_, ex_, AF.Ln, bias=1.0), reads=[s_], writes=[s_])
                k.op("dve", lambda e: e.tensor_tensor(g_, sp_, sc6[:, 12:18], ALU.mult), reads=[s_, sc6], writes=[s_])
                yield
                pgc = self.nextbank()
                k.op("pe", lambda e: e.matmul(pgc[:, 0:6], U, g_, start=True, stop=True), reads=[cst, s_], writes=[pgc])
                k.op("pe", lambda e: e.matmul(pgc[:, 8:14], ones, g_, start=True, stop=True), reads=[cst, s_], writes=[pgc])
                k.op("dve", lambda e: e.tensor_copy(gcl, pgc[:, 0:16]), reads=[pgc], writes=[s_])
                k.op("dve", lambda e: e.tensor_tensor(dd, s_[:, 40:46], s_[:, 32:38], ALU.subtract), reads=[s_], writes=[s_])
                yield
                k.op("act", lambda e: e.activation(egc, s_[:, 32:38], AF.Exp), reads=[s_], writes=[s_])
                k.op("act", lambda e: e.activation(etl, dd, AF.Exp), reads=[s_], writes=[s_])
                k.op("act", lambda e: e.activation(egl, s_[:, 40:46], AF.Exp), reads=[s_], writes=[s_])
                k.op("dve", lambda e: e.tensor_single_scalar(negc, egc, -1.0, ALU.mult), reads=[s_], writes=[s_])
                yield
                for (c0, nchk, dstb, dst) in ((0, 9, cv, cv[:, 0:9, :]), (9, 3, cv, cv[:, 9:12, :]), (12, 6, cvv, cvv[:, 0:6, :])):
                    wv = lambda j: convw[:, c0:c0 + nchk, j:j + 1].to_broadcast([128, nchk, 128])
                    k.op("dve", lambda e: e.tensor_tensor(dst, pc[:, c0:c0 + nchk, 0:128], wv(0), ALU.mult), reads=[pc, convw], writes=[dstb])
                    for j in range(1, 4):
                        tv = ctm[:, 0:nchk, :]
                        k.op("dve", lambda e: e.tensor_tensor(tv, pc[:, c0:c0 + nchk, j:j + 128], wv(j), ALU.mult), reads=[pc, convw], writes=[ctm])
                        k.op("dve", lambda e: e.tensor_tensor(dst, dst, tv, ALU.add), reads=[ctm, dstb], writes=[dstb])
                        yield
                k.op("pool", lambda e: e.tensor_copy(pc[:, :, 0:3], pc[:, :, 128:131]), reads=[pc], writes=[pc])
                k.op("act", lambda e: e.activation(cv[:], cv[:], AF.Silu), reads=[cv], writes=[cv])
                k.op("act", lambda e: e.activation(cvv[:], cvv[:], AF.Silu), reads=[cvv], writes=[cvv])
                yield

            def frontB(t):
                i3 = t % 3
                b = t % 2
                s_ = sc[i3]
                C = lambda a_, n=6: s_[:, a_:a_ + n]
                beta, g_ = C(0), C(24)
                qk = qkT[i3]
                qkv = cv
                k.op("act", lambda e: e.activation(sq[:], qkv[:, 0:12, :], AF.Square), reads=[qkv], writes=[sq])
                yield
                for g3 in range(3):
                    pb = self.nextbank()
                    k.op("pe", lambda e: e.matmul(pb[:], self.onesb, sq[:, g3 * 4:(g3 + 1) * 4, :], start=True, stop=True),
                         reads=[cstb, sq], writes=[pb])
                    k.op("act", lambda e: e.activation(rs[:, g3 * 4:(g3 + 1) * 4, :], pb[:].rearrange("p (c t) -> p c t", c=4), AF.Ln,
                                                       bias=self.epsb[:, 0:1]), reads=[pb, self.epsbuf], writes=[rs])
                    k.op("act", lambda e: e.activation(rs[:, g3 * 4:(g3 + 1) * 4, :], rs[:, g3 * 4:(g3 + 1) * 4, :], AF.Exp, scale=-0.5),
                         reads=[rs], writes=[rs])
                yield
                k.op("dve", lambda e: e.scalar_tensor_tensor(qk[:, :, 1, :], qkv[:, 0:6, :], QS, rs[:, 0:6, :], ALU.mult, ALU.mult),
                     reads=[qkv, rs], writes=[qk])
                k.op("dve", lambda e: e.tensor_tensor(kn32[:], qkv[:, 6:12, :], rs[:, 6:12, :], ALU.mult), reads=[qkv, rs], writes=[kn32])
                k.op("act", lambda e: e.copy(qk[:, :, 0, :], kn32[:]), reads=[kn32], writes=[qk])
                yield
                s_etl = C(60)
                for grp in range(3):
                    pb = self.nextbank()
                    for j in range(4):
                        idx = grp * 4 + j
                        src = cvv[:, idx, :] if idx < 6 else kn32[:, idx - 6, :]
                        srcb = cvv if idx < 6 else kn32
                        k.op("pe", lambda e: e.transpose(pb[:, j * 128:(j + 1) * 128], src, ident), reads=[srcb, cst], writes=[pb], inc=(j == 3))
                    for j in range(4):
                        idx = grp * 4 + j
                        if idx < 6:
                            k.op("act", lambda e: e.copy(vtok[b][:, idx, :], pb[:, j * 128:(j + 1) * 128]), reads=[pb], writes=[vtok[b]])
                        else:
                            hh = idx - 6
                            k.op("dve", lambda e: e.tensor_scalar(kt[b][:, hh, :], pb[:, j * 128:(j + 1) * 128], s_etl[:, hh:hh + 1], None, ALU.mult),
                                 reads=[pb, s_], writes=[kt[b]])
                    yield
                for hh in range(6):
                    sg = SLg[hh]
                    k.op("dve", lambda e: e.tensor_scalar(sg[:], SL, g_[:, hh:hh + 1], None, ALU.mult), reads=[cst, s_], writes=[sg])
                yield
                for hp in range(3):
                    pb = self.nextbank()
                    for j in range(2):
                        hh = hp * 2 + j
                        sg = SLg[hh]
                        k.op("pe", lambda e: e.matmul(pb[:, j * 128:(j + 1) * 128], sg[:], U, start=True, stop=False),
                             reads=[sg, cst], writes=[pb], inc=False)
                        k.op("pe", lambda e: e.matmul(pb[:, j * 128:(j + 1) * 128], ident, NEGs, start=False, stop=True),
                             reads=[cst], writes=[pb])
                    k.op("act", lambda e: e.activation(dm[:, hp * 2:hp * 2 + 2, :], pb[:, 0:256].rearrange("p (c t) -> p c t", c=2), AF.Exp),
                         reads=[pb], writes=[dm])
                    yield
                k.op("pool", lambda e: e.tensor_tensor(dmi[:], dm[:], ident.unsqueeze(1).to_broadcast([128, 6, 128]), ALU.add),
                     reads=[dm, cst], writes=[dmi])
                yield
                for hh in range(6):
                    pb = self.nextbank()
                    W0 = Wq[hh][0]
                    qf = Q0f[hh]
                    k.op("pe", lambda e: e.matmul(pb[:, 0:256], qk[:, hh, 0, :], qk[:, hh, :, :].rearrange("p a t -> p (a t)"),
                                                  start=True, stop=True), reads=[qk], writes=[pb])
                    k.op("dve", lambda e: e.scalar_tensor_tensor(qf[:], pb[:, 0:128], beta[:, hh:hh + 1], dm[:, hh, :], ALU.mult, ALU.mult),
                         reads=[pb, s_, dm], writes=[qf])
                    k.op("dve", lambda e: e.tensor_tensor(attnT[b][:, hh, :], pb[:, 128:256], dmi[:, hh, :], ALU.mult),
                         reads=[pb, dmi], writes=[attnT[b]])
                    k.op("dve", lambda e: e.tensor_copy(W0[:, 0, :], qf[:]), reads=[qf], writes=[W0])
                    k.op("dve", lambda e: e.tensor_tensor(Wq[hh][1][:, 1, :], ident, qf[:], ALU.subtract), reads=[cst, qf], writes=[Wq[hh][1]])
                    if hh % 3 == 2:
                        yield
                for hh in range(6):
                    W0 = Wq[hh][0]
                    qf = Q0f[hh]
                    pb2 = self.nextbank()
                    k.op("pe", lambda e: e.transpose(pb2[:, 0:128], qf[:], ident), reads=[qf, cst], writes=[pb2])
                    k.op("act", lambda e: e.copy(W0[:, 2, :], pb2[:, 0:128]), reads=[pb2], writes=[W0])
                    if hh % 3 == 2:
                        yield
                for lvl in range(7):
                    for hh in range(6):
                        Wc = Wq[hh][lvl % 2]
                        Wn = Wq[hh][(lvl + 1) % 2]
                        pb = self.nextbank()
                        Qk, Xk, Pk = Wc[:, 0, :], Wc[:, 1, :], Wc[:, 2, :]
                        mm = lambda out, l_, r_, st_, sp_2, inc_: k.op(
                            "pe", lambda e: e.matmul(out, l_, r_, start=st_, stop=sp_2), reads=[Wc, cstb], writes=[pb], inc=inc_)
                        if lvl == 0:
                            mm(pb[:, 0:128], Pk, Qk, True, True, False)
                            mm(pb[:, 256:384], Qk, Pk, True, True, True)
                            k.op("act", lambda e: e.copy(Wn[:, 0, :], pb[:, 0:128]), reads=[pb], writes=[Wn])
                            k.op("act", lambda e: e.copy(Wn[:, 2, :], pb[:, 256:384]), reads=[pb], writes=[Wn])
                        elif lvl < 6:
                            mm(pb[:, 0:128], Pk, Qk, True, True, False)
                            mm(pb[:, 128:256], Pk, Xk, True, False, False)
                            mm(pb[:, 128:256], identb, Xk, False, True, False)
                            mm(pb[:, 256:384], Qk, Pk, True, True, True)
                            k.op("act", lambda e: e.copy(Wn[:], pb[:, 0:384].rearrange("p (c t) -> p c t", c=3)), reads=[pb], writes=[Wn])
                        else:
                            mm(pb[:, 128:256], Pk, Xk, True, False, False)
                            mm(pb[:, 128:256], identb, Xk, False, True, True)
                            k.op("act", lambda e: e.copy(TT[b][:, hh, :], pb[:, 128:256]), reads=[pb], writes=[TT[b]])
                    yield

            def back(t):
                i3 = t % 3
                b = t % 2
                s_ = sc[i3]
                C = lambda a_, n=6: s_[:, a_:a_ + n]
                beta, egc, negc, egl = C(0), C(48), C(54), C(66)
                qk, g_g = qkT[i3], gg[i3]
                k.dma(hC[:], hin[0][t * 128:(t + 1) * 128, :], reads=[hin[1][t]], writes=[hC])
                for hp in range(3):
                    pb = self.nextbank()
                    for j in range(2):
                        hh = hp * 2 + j
                        o = j * 256
                        k.op("pe", lambda e: e.matmul(pb[:, o:o + 128], qk[:, hh, 0, :], Sb[hh][:], start=True, stop=True),
                             reads=[qk, Sb[hh]], writes=[pb], inc=False)
                        k.op("pe", lambda e: e.matmul(pb[:, o + 128:o + 256], qk[:, hh, 1, :], Sb[hh][:], start=True, stop=True),
                             reads=[qk, Sb[hh]], writes=[pb], inc=(j == 1))
                    for j in range(2):
                        hh = hp * 2 + j
                        o = j * 256
                        k.op("dve", lambda e: e.scalar_tensor_tensor(Rall[:, hh, :], pb[:, o:o + 128], negc[:, hh:hh + 1], vtok[b][:, hh, :], ALU.mult, ALU.add),
                             reads=[pb, s_, vtok[b]], writes=[Rall])
                        k.op("dve", lambda e: e.tensor_scalar(oall[:, hh, :], pb[:, o + 128:o + 256], egc[:, hh:hh + 1], None, ALU.mult),
                             reads=[pb, s_], writes=[oall])
                yield
                for (h0, nh) in ((0, 4), (4, 2)):
                    pb = self.nextbank()
                    for j in range(nh):
                        hh = h0 + j
                        k.op("pe", lambda e: e.matmul(pb[:, j * 128:(j + 1) * 128], TT[b][:, hh, :], Rall[:, hh, :], start=True, stop=True),
                             reads=[TT[b], Rall], writes=[pb], inc=(j == nh - 1))
                    k.op("dve", lambda e: e.tensor_tensor(vnew[:, h0:h0 + nh, :], pb[:, 0:nh * 128].rearrange("p (c t) -> p c t", c=nh),
                                                          beta[:, h0:h0 + nh].unsqueeze(2).to_broadcast([128, nh, 128]), ALU.mult),
                         reads=[pb, s_], writes=[vnew])
                yield
                for hp in range(3):
                    pb = self.nextbank()
                    for j in range(2):
                        hh = hp * 2 + j
                        o = j * 256
                        k.op("pe", lambda e: e.matmul(pb[:, o:o + 128], attnT[b][:, hh, :], vnew[:, hh, :], start=True, stop=True),
                             reads=[attnT[b], vnew], writes=[pb], inc=False)
                        k.op("pe", lambda e: e.matmul(pb[:, o + 128:o + 256], kt[b][:, hh, :], vnew[:, hh, :], start=True, stop=True),
                             reads=[kt[b], vnew], writes=[pb], inc=(j == 1))
                    for j in range(2):
                        hh = hp * 2 + j
                        o = j * 256
                        k.op("dve", lambda e: e.scalar_tensor_tensor(Sb[hh][:], Sf[hh][:], egl[:, hh:hh + 1], pb[:, o + 128:o + 256], ALU.mult, ALU.add),
                             reads=[Sf[hh], s_, pb], writes=[Sb[hh]])
                        k.op("dve", lambda e: e.scalar_tensor_tensor(Sf[hh][:], Sf[hh][:], egl[:, hh:hh + 1], pb[:, o + 128:o + 256], ALU.mult, ALU.add),
                             reads=[Sf[hh], s_, pb], writes=[Sf[hh]])
                        k.op("dve", lambda e: e.tensor_tensor(oall[:, hh, :], oall[:, hh, :], pb[:, o:o + 128], ALU.add),
                             reads=[oall, pb], writes=[oall])
                yield
                for hh in range(6):
                    k.op("act", lambda e: e.activation(ojunk[:], oall[:, hh, :], AF.Square, accum_out=ost[:, hh:hh + 1]),
                         reads=[oall], writes=[ojunk, ost])
                k.op("act", lambda e: e.activation(ost[:, 8:14], ost[:, 0:6], AF.Ln, bias=self.epsb[:, 0:1], scale=1.0 / 128),
                     reads=[ost, self.epsbuf], writes=[ost])
                k.op("act", lambda e: e.activation(ost[:, 16:22], ost[:, 8:14], AF.Exp, scale=-0.5), reads=[ost], writes=[ost])
                yield
                for hh in range(6):
                    k.op("dve", lambda e: e.scalar_tensor_tensor(mix[:, hh * 128:(hh + 1) * 128], oall[:, hh, :], ost[:, 16 + hh:17 + hh],
                                                                 g_g[:, hh * 128:(hh + 1) * 128], ALU.mult, ALU.mult),
                         reads=[oall, ost, g_g], writes=[mix])
                yield
                yield from self.mem_attend_g(MW, mqT[i3], 0, memkT, memv, mix, banks=membanks)
                yield
                self.transpose8(mix, [(mixT, lambda half: mixT[:, half * 4:(half + 1) * 4, :])], self.nextbank(), self.nextbank())
                yield
                for half in range(2):
                    pb = self.nextbank()
                    for fc in range(8):
                        k.op("pe", lambda e: e.matmul(pb[:], mixT[:, fc, :], w_out[:, fc, half * 512:(half + 1) * 512],
                                                      start=(fc == 0), stop=(fc == 7)),
                             reads=[mixT, w_out], writes=[pb], inc=(fc == 7))
                    k.op("dve", lambda e: e.tensor_tensor(hC[:, half * 512:(half + 1) * 512], hC[:, half * 512:(half + 1) * 512], pb[:], ALU.add),
                         reads=[hC, pb], writes=[hC])
                k.dma(hout[0][t * 128:(t + 1) * 128, :], hC[:], reads=[hC], writes=[hout[1][t]])

            AMULT = [int(v) for v in os.environ.get("MK_AMULT", "1,1,1").split(",")]

            def run(*gens):
                gens = [[g, m] for g, m in zip(gens, AMULT) if g is not None]
                while gens:
                    for item in list(gens):
                        for _ in range(item[1]):
                            try:
                                next(item[0])
                            except StopIteration:
                                gens.remove(item)
                                break

            mk = lambda fn, t: fn(t) if 0 <= t < NTA else None
            for step in range(-2, NTA):
                run(mk(back, step), mk(frontB, step + 1), mk(frontA, step + 2))
            k.barrier()
            del self.nextbank

    def mixer_b_phase(self, hin, hout):
        nc, k, P = self.nc, self.k, self.P
        NGB = int(os.environ.get("MK_NGB", "8"))
        NHB = int(os.environ.get("MK_NHB", "12"))
        NDUM = int(os.environ.get("MK_NDUM", "1"))
        with ExitStack() as st:
            sb = lambda n, s, d: self.sbuf(st, n, s, d)
            memkT, memv = self.mem_kv(st, 1)
            win_d = self.dscratch("b_w_in_bf", [D, D], BF16)
            wout_d = self.dscratch("b_w_out_bf", [D, D], BF16)
            wdb = [Buf(None, "win_d"), Buf(None, "wout_d")]
            k.dma(win_d, P["b_w_in"][0], writes=[wdb[0]], q="pool")
            k.dma(wout_d, P["b_w_out"][0], writes=[wdb[1]], q="pool")
            KT = sb("KT", [128, 6, S], BF16)
            Vt = sb("Vt", [128, NT, 768], BF16)
            ht = [sb("htB%d" % i, [128, D], F32) for i in range(2)]
            xn = sb("xnB", [128, D], F32)
            stt = [sb("sttB%d" % i, [128, 4], F32) for i in range(2)]
            with ExitStack() as st1:
                sb1 = lambda n, s, d: self.sbuf(st1, n, s, d)
                kvg = sb1("kvg", [128, D], F32)
                k.dma(kvg[:], P["kv_norm"].partition_broadcast(128), writes=[kvg])
                w_kv = sb1("w_kv", [128, 8, 1536], BF16)
                for c in range(8):
                    k.dma(w_kv[:, c, :], P["w_kv"][c * 128:(c + 1) * 128, :], writes=[w_kv], q="pool")
                xTg1 = [sb1("xTg1_%d" % i, [128, 8, 512], BF16) for i in range(2)]

                def p1_x(g):
                    xTg_ = xTg1[g % 2]
                    for tt in range(4):
                        t = g * 4 + tt
                        b = tt % 2
                        h = ht[b]
                        k.dma(h[:], hin[0][t * 128:(t + 1) * 128, :], reads=[hin[1][t]], writes=[h])
                        self.rmsnorm(h, kvg, xn, xn, stt[b])
                        yield
                        self.transpose8(xn, [(xTg_, lambda half: xTg_[:, half * 4:(half + 1) * 4, tt * 128:(tt + 1) * 128])],
                                        self.nextbank(), self.nextbank())
                        yield

                def p1_y(g):
                    xTg_ = xTg1[g % 2]
                    for fc in range(6):
                        pb = self.nextbank()
                        for dc in range(8):
                            k.op("pe", lambda e: e.matmul(pb[:], w_kv[:, dc, fc * 128:(fc + 1) * 128], xTg_[:, dc, :],
                                                          start=(dc == 0), stop=(dc == 7)),
                                 reads=[w_kv, xTg_], writes=[pb], inc=(dc == 7))
                        if fc % 2 == 0:
                            k.op("act", lambda e: e.copy(KT[:, fc, g * 512:(g + 1) * 512], pb[:]), reads=[pb], writes=[KT])
                        else:
                            k.op("dve", lambda e: e.tensor_copy(KT[:, fc, g * 512:(g + 1) * 512], pb[:]), reads=[pb], writes=[KT])
                        yield
                    for tt in range(4):
                        t = g * 4 + tt
                        for half, (c0, c1) in enumerate(((0, 512), (512, 768))):
                            pb = self.nextbank()
                            for dc in range(8):
                                k.op("pe", lambda e: e.matmul(pb[:, 0:c1 - c0], xTg_[:, dc, tt * 128:(tt + 1) * 128], w_kv[:, dc, 768 + c0:768 + c1],
                                                              start=(dc == 0), stop=(dc == 7)),
                                     reads=[w_kv, xTg_], writes=[pb], inc=(dc == 7))
                            if half == 0:
                                k.op("dve", lambda e: e.tensor_copy(Vt[:, t, c0:c1], pb[:, 0:c1 - c0]), reads=[pb], writes=[Vt])
                            else:
                                k.op("act", lambda e: e.copy(Vt[:, t, c0:c1], pb[:, 0:c1 - c0]), reads=[pb], writes=[Vt])
                        yield

                def rr1(*gens):
                    gens = [g_ for g_ in gens if g_ is not None]
                    while gens:
                        for g_ in list(gens):
                            try:
                                next(g_)
                            except StopIteration:
                                gens.remove(g_)

                rr1(p1_x(0))
                for g in range(NT // 4):
                    rr1(p1_y(g), p1_x(g + 1) if g + 1 < NT // 4 else None)
                k.barrier()
            bg = sb("bgain", [128, D], F32)
            k.dma(bg[:], P["b_norm"][0].partition_broadcast(128), writes=[bg])
            wB = sb("wB", [128, 8, D], BF16)
            xTg = sb("xTg", [128, 8, 512], BF16)
            qT = [sb("qT%d" % i, [128, 6, 512], BF16) for i in range(2)]
            mqT = [sb("mqTB%d" % i, [128, 2, 512], BF16) for i in range(3)]
            mixTg = [sb("mixTg%d" % i, [128, 8, 512], BF16) for i in range(2)]
            Eb = [sb("Eb%d" % i, [128, 512], F32) for i in range(3)]
            spb = [sb("spb%d" % i, [128, 512], BF16) for i in range(3)]
            eab = [sb("eab%d" % i, [128, 512], F32) for i in range(2)]
            ab = [sb("ab%d" % i, [128, 512], BF16) for i in range(2)]
            MW = self.mem_work(st)
            mmB = sb("mmB", [128, 256], F32)
            NGEb = self.cstb[:, 5, :]
            NLTb = self.cstb[:, 6, :]
            strictTb = self.cstb[:, 3, :]
            cstb = self.cstb
            PZ = [self.ps[0], self.ps[1]]
            PC = [self.ps[2], self.ps[3]]
            PO = [self.ps[4], self.ps[5]]
            rot = [0]

            def nb():
                rot[0] = (rot[0] + 1) % 2
                return self.ps[6 + rot[0]]
            self.nextbank = nb
            wcur = [None]

            def load_wB(which):
                if wcur[0] != which:
                    srcd = win_d if which == "in" else wout_d
                    k.dma(wB[:], srcd.rearrange("(c p) n -> p c n", p=128), reads=[wdb[0 if which == "in" else 1]], writes=[wB])
                    wcur[0] = which

            def prologue(g):
                load_wB("in")
                qTg, mqTg = qT[g % 2], mqT[g % 3]
                h = ht[0]
                for tt in range(4):
                    t = g * 4 + tt
                    k.dma(h[:], hin[0][t * 128:(t + 1) * 128, :], reads=[hin[1][t]], writes=[h])
                    self.rmsnorm(h, bg, xn, xn, stt[0])
                    yield
                    self.transpose8(xn, [(xTg, lambda half: xTg[:, half * 4:(half + 1) * 4, tt * 128:(tt + 1) * 128])], nb(), nb(),
                                    evac=("dve", "dve"))
                    yield
                for fc in range(8):
                    pb = nb()
                    for dc in range(8):
                        k.op("pe", lambda e: e.matmul(pb[:], wB[:, dc, fc * 128:(fc + 1) * 128], xTg[:, dc, :], start=(dc == 0), stop=(dc == 7)),
                             reads=[wB, xTg], writes=[pb], inc=(dc == 7))
                    if fc < 6:
                        k.op("dve", lambda e: e.tensor_single_scalar(qTg[:, fc, :], pb[:], 0.125, ALU.mult), reads=[pb], writes=[qTg])
                    else:
                        k.op("dve", lambda e: e.tensor_copy(mqTg[:, fc - 6, :], pb[:]), reads=[pb], writes=[mqTg])
                    yield

            def epilogue(g):
                load_wB("out")
                mixg, mqTg = mixTg[g % 2], mqT[g % 3]
                h = ht[1]
                for tt in range(4):
                    t = g * 4 + tt
                    k.dma(h[:], hin[0][t * 128:(t + 1) * 128, :], reads=[hin[1][t]], writes=[h])
                    yield from self.mem_attend_g(MW, mqTg, tt * 128, memkT, memv, mmB, col0=0)
                    yield
                    pb = nb()
                    for j in range(2):
                        k.op("pe", lambda e: e.transpose(pb[:, j * 128:(j + 1) * 128], mmB[:, j * 128:(j + 1) * 128], self.ident),
                             reads=[mmB, self.cst], writes=[pb], inc=(j == 1))
                    k.op("dve", lambda e: e.tensor_copy(mixg[:, 6:8, tt * 128:(tt + 1) * 128], pb[:, 0:256].rearrange("p (c t) -> p c t", c=2)),
                         reads=[pb], writes=[mixg])
                    yield
                    for half in range(2):
                        pb = nb()
                        for fc in range(8):
                            k.op("pe", lambda e: e.matmul(pb[:], mixg[:, fc, tt * 128:(tt + 1) * 128], wB[:, fc, half * 512:(half + 1) * 512],
                                                          start=(fc == 0), stop=(fc == 7)),
                                 reads=[mixg, wB], writes=[pb], inc=(fc == 7))
                        k.op("dve", lambda e: e.tensor_tensor(h[:, half * 512:(half + 1) * 512], h[:, half * 512:(half + 1) * 512], pb[:], ALU.add),
                             reads=[h, pb], writes=[h])
                        yield
                    k.dma(hout[0][t * 128:(t + 1) * 128, :], h[:], reads=[h], writes=[hout[1][t]])

            def side_thread(g):
                if g >= 1:
                    yield from epilogue(g - 1)
                if g + 1 < NGB:
                    yield from prologue(g + 1)

            for _ in prologue(0):
                pass
            for g in range(NGB):
                qTg, mixg = qT[g % 2], mixTg[g % 2]
                side = side_thread(g)
                items = [(2 * p + s, kb) for p in range(NHB // 2) for kb in range(4 * g + 3, -1, -1) for s in range(2)]

                def geom(i):
                    hh, kb = items[i]
                    r = max(kb - 4 * g, 0)
                    return hh, kb, hh // 2, hh % 2, r * 128, kb >= 4 * g

                def s1_pe(i):
                    hh, kb, fc, s, c0, diag = geom(i)
                    ps_ = slice(s * 64, (s + 1) * 64)
                    cs = slice(c0, 512)
                    pz = PZ[i % 2]
                    k.op("pe", lambda e: e.matmul(pz[:, cs], KT[ps_, fc, kb * 128:(kb + 1) * 128], qTg[ps_, fc, cs], start=True, stop=True),
                         reads=[KT, qTg], writes=[pz])

                def s1_act(i):
                    hh, kb, fc, s, c0, diag = geom(i)
                    cs = slice(c0, 512)
                    pz, E, sp = PZ[i % 2], Eb[i % 3], spb[i % 3]
                    k.op("act", lambda e: e.activation(E[:, cs], pz[:, cs], AF.Exp), reads=[pz], writes=[E])
                    k.op("act", lambda e: e.activation(sp[:, cs], E[:, cs], AF.Ln, bias=1.0), reads=[E], writes=[sp])
                    if diag:
                        k.op("dve", lambda e: e.tensor_tensor(sp[:, c0:c0 + 128], sp[:, c0:c0 + 128], strictTb, ALU.mult),
                             reads=[sp, cstb], writes=[sp])

                def s2_peA(i):
                    hh, kb, fc, s, c0, diag = geom(i)
                    cs = slice(c0, 512)
                    C, sp = PC[s], spb[i % 3]
                    if kb == 4 * g + 3:
                        k.op("dve", lambda e: e.memset(C[:], 0.0), writes=[C])
                    k.op("pe", lambda e: e.matmul(C[:, cs], NGEb, sp[:, cs], start=False, stop=False, skip_group_check=True),
                         reads=[cstb, sp], writes=[C])

                def s2_act(i):
                    hh, kb, fc, s, c0, diag = geom(i)
                    cs = slice(c0, 512)
                    C, ea_ = PC[s], eab[i % 2]
                    k.op("act", lambda e: e.activation(ea_[:, cs], C[:, cs], AF.Exp), reads=[C], writes=[ea_])

                def s2_peB(i):
                    hh, kb, fc, s, c0, diag = geom(i)
                    cs = slice(c0, 512)
                    C, sp = PC[s], spb[i % 3]
                    if kb > 0:
                        k.op("pe", lambda e: e.matmul(C[:, cs], NLTb, sp[:, cs], start=False, stop=False, skip_group_check=True),
                             reads=[cstb, sp], writes=[C])

                def s3_pool(i):
                    hh, kb, fc, s, c0, diag = geom(i)
                    cs = slice(c0, 512)
                    E, ea_, a_ = Eb[i % 3], eab[i % 2], ab[i % 2]
                    k.op("dve", lambda e: e.tensor_tensor(a_[:, cs], E[:, cs], ea_[:, cs], ALU.mult), reads=[E, ea_], writes=[a_])
                    if diag:
                        k.op("dve", lambda e: e.tensor_tensor(a_[:, c0:c0 + 128], a_[:, c0:c0 + 128], strictTb, ALU.mult),
                             reads=[a_, cstb], writes=[a_])

                def s3_pe(i):
                    hh, kb, fc, s, c0, diag = geom(i)
                    cs = slice(c0, 512)
                    a_ = ab[i % 2]
                    po = PO[fc % 2]
                    if kb == 4 * g + 3 and s == 0:
                        k.op("dve", lambda e: e.memset(po[:], 0.0), writes=[po])
                    vblk = Vt[:, kb, hh * 64:(hh + 1) * 64]
                    if s == 0:
                        k.op("pe", lambda e: e.matmul(po[0:64, cs], vblk, a_[:, cs], start=False, stop=False, skip_group_check=True),
                             reads=[Vt, a_], writes=[po])
                    else:
                        k.op("pe", lambda e: e.matmul(po[64:128, cs], vblk, a_[:, cs], start=False, stop=False, skip_group_check=True,
                                                      tile_position=(0, 64)), reads=[Vt, a_], writes=[po])
                    if kb == 0 and s == 1:
                        k.op("act", lambda e: e.copy(mixg[:, fc, :], po[:]), reads=[po], writes=[mixg])

                n_it = len(items)
                ok = lambda i: 0 <= i < n_it
                dumrhs = cstb[:, 0:4, :].rearrange("p c t -> p (c t)")
                stride = max(1, n_it // 72)
                for step in range(-3, n_it + 1):
                    i0, i1, i2_, i3, i4 = step + 3, step + 2, step + 1, step, step - 1
                    if ok(i3):
                        s3_pool(i3)
                    if ok(i0):
                        for _d in range(NDUM):
                            k.op("pe", lambda e: e.matmul(PZ[i0 % 2][:], NGEb, dumrhs, start=True, stop=True),
                                 reads=[cstb], writes=[PZ[i0 % 2]], inc=False)
                        s1_pe(i0)
                    if ok(i2_):
                        s2_peA(i2_)
                    if ok(i4):
                        s3_pe(i4)
                    if ok(i1):
                        s1_act(i1)
                    if ok(i2_):
                        s2_act(i2_)
                    if ok(i3):
                        s2_peB(i3)
                    if side is not None and step >= 0 and step % stride == 0:
                        try:
                            next(side)
                        except StopIteration:
                            side = None
                if side is not None:
                    for _ in side:
                        pass
            for _ in epilogue(NGB - 1):
                pass
            k.barrier()
            del self.nextbank

    def build(self, phases):
        nc, k = self.nc, self.k
        P = self.P = {}
        shapes = dict(
            x=[S, D], mem=[256, D], a_norm=[1, D], a_w_in=[1, D, 3340], a_conv=[1, 4, 2304],
            a_log=[1, 6], a_dt_bias=[1, 6], a_out_gain=[1, 128], a_w_out=[1, D, D],
            kv_norm=[D], w_kv=[D, 1536], b_norm=[1, D], b_w_in=[1, D, D], b_w_out=[1, D, D],
            mem_norm=[2, D], w_mem_kv=[2, D, 512], ffn_norm=[2, D], w_group=[2, D, 4], b_group=[2, 4],
            w_router=[2, D, 16], b_router=[2, 16], w1=[2, 16, D, 256], w3=[2, 16, D, 256],
            w2=[2, 16, 256, D], final_norm=[D], wgr=[2, 128, 8, 20], rbias=[2, 20], convw=[128, 18, 4])
        for n, s in shapes.items():
            P[n] = self.din(n, s)
        out = nc.dram_tensor("out", [S, D], F32, kind="ExternalOutput").ap()
        mkbufs = lambda nm: [Buf(None, "%s%d" % (nm, i)) for i in range(NT)]
        hx = (P["x"], mkbufs("x"))
        hA = (self.dscratch("hA", [S, D]), mkbufs("hA"))
        hB = (self.dscratch("hB", [S, D]), mkbufs("hB"))
        ho = (out, mkbufs("out"))
        with ExitStack() as gst:
            self.load_consts(gst)
            self.epsbuf = self.sbuf(gst, "epsb", [128, 1], F32)
            self.epsb = self.epsbuf
            k.op("dve", lambda e: e.memset(self.epsbuf[:], EPS), writes=[self.epsbuf])
            cur = hx
            seq = {"A": hA, "M0": hB, "B": hA, "M1": ho}
            for ph in phases:
                dst = seq[ph] if ph != phases[-1] else ho
                if ph == "M0":
                    self.moe_phase(0, cur, dst, final=False)
                elif ph == "M1":
                    self.moe_phase(1, cur, dst, final=True)
                elif ph == "A":
                    self.mixer_a_phase(cur, dst)
                elif ph == "B":
                    self.mixer_b_phase(cur, dst)
                cur = dst
            for b in ho[1]:
                if b.lw is not None:
                    k._wait("sp", b.lw)
        return nc


def make_consts():
    c = np.zeros((128, 8, 128), np.float32)
    i = np.arange(128)
    c[:, 0, :] = np.eye(128)
    c[:, 1, :] = (i[:, None] <= i[None, :])
    c[:, 2, :] = (i[:, None] > i[None, :])
    c[:, 3, :] = (i[:, None] < i[None, :])
    c[:, 4, :] = 1.0
    c[:, 5, :] = -(i[:, None] >= i[None, :]).astype(np.float32)
    c[:, 6, :] = -(i[:, None] < i[None, :]).astype(np.float32)
    c[:, 7, :] = -30000.0 * (i[:, None] >= i[None, :])
    return c


_CACHE = {}


def run(inputs, phases=("A", "M0", "B", "M1"), ncores=NCORES, trace=False):
    key = tuple(phases)
    if key not in _CACHE:
        mk = MK(phases)
        _CACHE[key] = mk.build(list(phases))
    nc = _CACHE[key]
    consts = make_consts()
    inputs = dict(inputs)
    wg = np.concatenate([np.asarray(inputs["w_group"]), np.asarray(inputs["w_router"])], axis=2)
    inputs["wgr"] = np.ascontiguousarray(wg.reshape(2, 8, 128, 20).transpose(0, 2, 1, 3))
    inputs["rbias"] = np.concatenate([np.asarray(inputs["b_group"]), np.asarray(inputs["b_router"])], axis=1)
    cw = np.asarray(inputs["a_conv"])[0]
    inputs["convw"] = np.ascontiguousarray(cw.reshape(4, 18, 128).transpose(2, 1, 0))
    in_maps = []
    for c in range(ncores):
        m = {"consts": consts}
        for n, v in inputs.items():
            v = np.asarray(v)
            if n in ("x", "mem"):
                m[n] = np.ascontiguousarray(v[c])
            else:
                m[n] = np.ascontiguousarray(v, dtype=np.float32)
        in_maps.append(m)
    res = run_bass_kernel_spmd(nc, in_maps, core_ids=list(range(ncores)), trace=trace)
    outs = np.stack([r["out"] for r in res.results], axis=0)
    return outs, res


def kernel(**inputs):
    outs, _ = run(inputs)
    return outs.astype(np.float32)
```
